# Optimizing a Trainium2 kernel written in Bass

```python
import jax, jax.numpy as jnp
from jax import lax
import numpy as np

D_MODEL = 2048
BATCH = 4
SEQ = 4096
DEPTH = 1

CHUNK = 64
HEAD_DIM = 64
D_MIX = D_MODEL
N_HEADS_A = (D_MIX // 2) // HEAD_DIM
N_KV_A = N_HEADS_A // 4
WINDOW = 128
N_PREV_A = WINDOW // CHUNK
N_HEADS_B = (D_MIX // 2) // HEAD_DIM
N_PREV_B = 8
REL_CLIP = 128
N_REL = 2 * REL_CLIP + 1
DA_Q = N_HEADS_A * HEAD_DIM
DA_KV = N_KV_A * HEAD_DIM
DB = N_HEADS_B * HEAD_DIM
D_IN = DA_Q + 2 * DA_KV + 3 * DB
N_GROUPS = 4
EXPERTS_PER_GROUP = 8
N_EXPERTS = N_GROUPS * EXPERTS_PER_GROUP
TOP_K_INNER = 2
D_EXPERT = D_MODEL // 4
EPS = 1e-6
NEG_INF = -1e30

kernel_name = "hybrid_chunk_swa_relbias_hiermoe"


def rms_norm(x, g):
    xf = x.astype(jnp.float32)
    y = xf * lax.rsqrt(jnp.mean(xf * xf, axis=-1, keepdims=True) + EPS)
    return (y * g.astype(jnp.float32)).astype(x.dtype)


def modulate(h, shift, scale):
    return h * (1 + scale[:, None, :]) + shift[:, None, :]


def alibi_slopes(n_heads):
    return jnp.exp2(-8.0 * jnp.arange(1, n_heads + 1, dtype=jnp.float32) / n_heads)


def chunk_band_attention(q, k, v, n_prev, bias_fn, sinks):
    b, s, hq, dh = q.shape
    hkv = k.shape[2]
    rep = hq // hkv
    n_chunks = s // CHUNK
    band = (n_prev + 1) * CHUNK
    pad = n_prev * CHUNK
    kp = jnp.pad(k, ((0, 0), (pad, 0), (0, 0), (0, 0)))
    vp = jnp.pad(v, ((0, 0), (pad, 0), (0, 0), (0, 0)))
    qc = q.reshape(b, n_chunks, CHUNK, hkv, rep, dh).transpose(1, 0, 2, 3, 4, 5)
    scale = dh ** -0.5

    def one_chunk(args):
        q_i, ci = args
        kb = lax.dynamic_slice_in_dim(kp, ci * CHUNK, band, axis=1)
        vb = lax.dynamic_slice_in_dim(vp, ci * CHUNK, band, axis=1)
        logits = jnp.einsum('bqgrd,bkgd->bgrqk', q_i, kb).astype(jnp.float32) * scale
        q_pos = ci * CHUNK + jnp.arange(CHUNK)
        k_pos = (ci - n_prev) * CHUNK + jnp.arange(band)
        rel = q_pos[:, None] - k_pos[None, :]
        logits = logits + bias_fn(rel).reshape(hkv, rep, CHUNK, band)[None]
        logits = jnp.where((k_pos >= 0)[None, None, None, None, :], logits, NEG_INF)
        m = jnp.max(logits, axis=-1, keepdims=True)
        if sinks is not None:
            sink = sinks.astype(jnp.float32).reshape(1, hkv, rep, 1, 1)
            m = jnp.maximum(m, sink)
        p = jnp.exp(logits - m)
        denom = jnp.sum(p, axis=-1, keepdims=True)
        if sinks is not None:
            denom = denom + jnp.exp(sink - m)
        probs = (p / denom).astype(v.dtype)
        return jnp.einsum('bgrqk,bkgd->bqgrd', probs, vb)

    out = lax.map(one_chunk, (qc, jnp.arange(n_chunks)))
    return out.transpose(1, 0, 2, 3, 4, 5).reshape(b, s, hq * dh)


def hier_moe(h, w_rg, b_rg, w_re, b_re, w_gate, w_up, w_down):
    t = h.reshape(-1, D_MODEL)
    n_tok = t.shape[0]
    g_logits = (t @ w_rg + b_rg).astype(jnp.float32)
    g_prob = jax.nn.softmax(g_logits, axis=-1)
    g_idx = jnp.argmax(g_logits, axis=-1)
    p_g = jnp.take_along_axis(g_prob, g_idx[:, None], axis=-1)
    e_logits = (t @ w_re + b_re).astype(jnp.float32).reshape(n_tok, N_GROUPS, EXPERTS_PER_GROUP)
    sel = jnp.take_along_axis(e_logits, g_idx[:, None, None], axis=1)[:, 0]
    top_v, top_i = lax.top_k(sel, TOP_K_INNER)
    w_sel = jax.nn.softmax(top_v, axis=-1) * p_g
    ids = g_idx[:, None] * EXPERTS_PER_GROUP + top_i
    combine = jnp.sum(jax.nn.one_hot(ids, N_EXPERTS, dtype=jnp.float32) * w_sel[..., None], axis=1)
    y = jnp.zeros((n_tok, D_MODEL), jnp.float32)
    for e in range(N_EXPERTS):
        a = jax.nn.silu(t @ w_gate[e]) * (t @ w_up[e])
        y = y + combine[:, e:e + 1] * (a @ w_down[e]).astype(jnp.float32)
    return y.astype(h.dtype).reshape(h.shape)


def setup_inputs(seed: int = 0) -> dict:
    key = jax.random.key(seed)
    ks = jax.random.split(key, 24)
    f32 = jnp.float32
    L = DEPTH

    def nrm(k, shape, s):
        return jax.random.normal(k, shape, f32) * s

    return {
        "x": nrm(ks[0], (BATCH, SEQ, D_MODEL), 1.0),
        "c": nrm(ks[1], (BATCH, D_MODEL), 1.0),
        "w_ada": nrm(ks[2], (L, D_MODEL, 6 * D_MODEL), 0.5 * D_MODEL ** -0.5),
        "b_ada": nrm(ks[3], (L, 6 * D_MODEL), 0.02),
        "g_mix": 1.0 + nrm(ks[4], (L, D_MODEL), 0.02),
        "w_in": nrm(ks[5], (L, D_MODEL, D_IN), D_MODEL ** -0.5),
        "sinks_a": nrm(ks[6], (L, N_HEADS_A), 0.5),
        "rel_bias_b": nrm(ks[7], (L, N_HEADS_B, N_REL), 0.2),
        "g_out_a": 1.0 + nrm(ks[8], (L, DA_Q), 0.02),
        "g_out_b": 1.0 + nrm(ks[9], (L, DB), 0.02),
        "w_out": nrm(ks[10], (L, D_MIX, D_MODEL), D_MIX ** -0.5),
        "g_ffn": 1.0 + nrm(ks[11], (L, D_MODEL), 0.02),
        "w_router_group": nrm(ks[12], (L, D_MODEL, N_GROUPS), D_MODEL ** -0.5),
        "b_router_group": nrm(ks[13], (L, N_GROUPS), 0.01),
        "w_router_expert": nrm(ks[14], (L, D_MODEL, N_EXPERTS), D_MODEL ** -0.5),
        "b_router_expert": nrm(ks[15], (L, N_EXPERTS), 0.01),
        "w_gate": nrm(ks[16], (L, N_EXPERTS, D_MODEL, D_EXPERT), D_MODEL ** -0.5),
        "w_up": nrm(ks[17], (L, N_EXPERTS, D_MODEL, D_EXPERT), D_MODEL ** -0.5),
        "w_down": nrm(ks[18], (L, N_EXPERTS, D_EXPERT, D_MODEL), D_EXPERT ** -0.5),
        "w_ada_final": nrm(ks[19], (D_MODEL, 2 * D_MODEL), 0.5 * D_MODEL ** -0.5),
        "b_ada_final": nrm(ks[20], (2 * D_MODEL,), 0.02),
        "g_final": 1.0 + nrm(ks[21], (D_MODEL,), 0.02),
    }


def reference(x, c, w_ada, b_ada, g_mix, w_in, sinks_a, rel_bias_b, g_out_a, g_out_b, w_out,
              g_ffn, w_router_group, b_router_group, w_router_expert, b_router_expert,
              w_gate, w_up, w_down, w_ada_final, b_ada_final, g_final):
    b, s, _ = x.shape
    c_act = jax.nn.silu(c)
    slopes = alibi_slopes(N_HEADS_A)
    for l in range(DEPTH):
        mod = c_act @ w_ada[l] + b_ada[l]
        sh1, sc1, gt1, sh2, sc2, gt2 = jnp.split(mod, 6, axis=-1)

        h = modulate(rms_norm(x, g_mix[l]), sh1, sc1)
        proj = h @ w_in[l]
        qa, ka, va, qb, kb, vb = jnp.split(
            proj, np.cumsum([DA_Q, DA_KV, DA_KV, DB, DB]).tolist(), axis=-1)
        qa = qa.reshape(b, s, N_HEADS_A, HEAD_DIM)
        ka = ka.reshape(b, s, N_KV_A, HEAD_DIM)
        va = va.reshape(b, s, N_KV_A, HEAD_DIM)
        qb = qb.reshape(b, s, N_HEADS_B, HEAD_DIM)
        kb = kb.reshape(b, s, N_HEADS_B, HEAD_DIM)
        vb = vb.reshape(b, s, N_HEADS_B, HEAD_DIM)

        def alibi_bias(rel):
            return -slopes[:, None, None] * jnp.abs(rel).astype(jnp.float32)[None]

        rb = rel_bias_b[l]

        def rel_bias(rel):
            idx = jnp.clip(rel, -REL_CLIP, REL_CLIP) + REL_CLIP
            return rb[:, idx].astype(jnp.float32)

        o_a = chunk_band_attention(qa, ka, va, N_PREV_A, alibi_bias, sinks_a[l])
        o_b = chunk_band_attention(qb, kb, vb, N_PREV_B, rel_bias, None)
        o = jnp.concatenate([rms_norm(o_a, g_out_a[l]), rms_norm(o_b, g_out_b[l])], axis=-1)
        x = x + gt1[:, None, :] * (o @ w_out[l])

        h = modulate(rms_norm(x, g_ffn[l]), sh2, sc2)
        y = hier_moe(h, w_router_group[l], b_router_group[l], w_router_expert[l],
                     b_router_expert[l], w_gate[l], w_up[l], w_down[l])
        x = x + gt2[:, None, :] * y

    modf = c_act @ w_ada_final + b_ada_final
    shf, scf = jnp.split(modf, 2, axis=-1)
    return modulate(rms_norm(x, g_final), shf, scf)
```

```python
import types
import numpy as np
from contextlib import ExitStack
import concourse.bass as bass
import concourse.mybir as mybir
from concourse.bass_utils import run_bass_kernel_spmd

F32 = mybir.dt.float32
BF16 = mybir.dt.bfloat16
AF = mybir.ActivationFunctionType
ALU = mybir.AluOpType
AX = mybir.AxisListType

ENGS = ("pe", "act", "dve", "pool", "sp")
NEG = -30000.0
EPS = 1e-6


def _freeze(fn):
    if fn.__closure__ is None:
        return fn
    cells = []
    for c in fn.__closure__:
        try:
            cells.append(types.CellType(c.cell_contents))
        except ValueError:
            cells.append(c)
    return types.FunctionType(fn.__code__, fn.__globals__, fn.__name__, fn.__defaults__, tuple(cells))


class Sched:
    def __init__(self):
        self.ops = []
        self.last_w = {}
        self.readers = {}
        self.dma_cnt = {}
        self.dma_last = {}
        self.eng_ops = {e: [] for e in ENGS}
        self.fence_set = set()
        self.fence_passed = {e: True for e in ENGS}

    def fence(self):
        s = set()
        for e in ENGS:
            if self.eng_ops[e]:
                s.add(self.eng_ops[e][-1])
        for k, i in self.dma_last.items():
            s.add(i)
        self.fence_set = s
        self.fence_passed = {e: False for e in ENGS}

    def add(self, eng, fn, reads=(), writes=(), dma=None):
        i = len(self.ops)
        deps = set()
        for r in reads:
            w = self.last_w.get(r)
            if w is not None:
                deps.add(w)
        for r in writes:
            w = self.last_w.get(r)
            if w is not None:
                deps.add(w)
            for rd in self.readers.get(r, ()):
                deps.add(rd)
        if not self.fence_passed[eng]:
            deps |= self.fence_set
            self.fence_passed[eng] = True
        op = dict(eng=eng, fn=_freeze(fn), deps=deps, dma=dma, idx=i)
        if dma is not None:
            self.dma_cnt[dma] = self.dma_cnt.get(dma, 0) + 16
            op["cnt"] = self.dma_cnt[dma]
            self.dma_last[dma] = i
        self.eng_ops[eng].append(i)
        op["ord"] = len(self.eng_ops[eng])
        self.ops.append(op)
        for r in reads:
            self.readers.setdefault(r, []).append(i)
        for r in writes:
            self.last_w[r] = i
            self.readers[r] = []
        return i

    def plan(self):
        ops = self.ops
        clock = {e: {} for e in ENGS}
        opclock = [None] * len(ops)
        waits = [None] * len(ops)
        signal = [False] * len(ops)

        def key_of(j):
            o = ops[j]
            if o["dma"] is not None:
                return ("dma", o["dma"]), o["cnt"]
            return o["eng"], o["ord"]

        for i, o in enumerate(ops):
            e = o["eng"]
            ck = clock[e]
            w = []
            for j in sorted(o["deps"]):
                pj = ops[j]
                if pj["dma"] is None and pj["eng"] == e == "pe":
                    continue
                k, v = key_of(j)
                if ck.get(k, 0) >= v:
                    continue
                w.append(j)
                signal[j] = True
                ck[k] = v
                for kk, vv in opclock[j].items():
                    if ck.get(kk, 0) < vv:
                        ck[kk] = vv
            waits[i] = w
            oc = dict(ck)
            if o["dma"] is not None:
                oc[("dma", o["dma"])] = o["cnt"]
            else:
                oc[e] = max(oc.get(e, 0), o["ord"])
            opclock[i] = oc
        semval = [0] * len(ops)
        for e in ENGS:
            c = 0
            for i in self.eng_ops[e]:
                if ops[i]["dma"] is None and signal[i]:
                    c += 1
                    semval[i] = c
        self.waits, self.signal, self.semval = waits, signal, semval

    def emit(self, nc, es, final_waits=()):
        self.plan()
        ops, waits, signal, semval = self.ops, self.waits, self.signal, self.semval
        eng_sems = {e: es.enter_context(nc.semaphore(f"s_{e}")) for e in ENGS}
        dma_sems = {k: es.enter_context(nc.semaphore(f"d_{k}")) for k in self.dma_cnt}
        block = es.enter_context(nc.Block())
        handles = {"pe": "tensor", "act": "scalar", "dve": "vector",
                   "pool": "gpsimd", "sp": "sync"}

        def run_engine(e, h):
            for i in self.eng_ops[e]:
                o = ops[i]
                for j in waits[i]:
                    pj = ops[j]
                    if pj["dma"] is not None:
                        h.wait_ge(dma_sems[pj["dma"]], pj["cnt"])
                    else:
                        h.wait_ge(eng_sems[pj["eng"]], semval[j])
                ins = o["fn"](h)
                if o["dma"] is not None:
                    ins.then_inc(dma_sems[o["dma"]], 16)
                elif signal[i]:
                    ins.then_inc(eng_sems[e], 1)
            if e == "sp":
                for k in final_waits:
                    h.wait_ge(dma_sems[k], self.dma_cnt[k])

        for e in ENGS:
            if not self.eng_ops[e] and not (e == "sp" and final_waits):
                continue
            getattr(block, handles[e])(lambda h, e=e: run_engine(e, h))


D = 2048
KC = 16
NB_TOK_CORE = 2048
HALF = 1024
HALO = 512
WIN = HALF + HALO
NWT = WIN // 128
NOT = HALF // 128
N_EXP = 32
DE = 512
NMODBLK = 32
SLOPES = [2.0 ** (-8.0 * (h + 1) / 16.0) for h in range(16)]
V_SH1, V_SC1, V_GT1, V_SH2, V_SC2, V_GT2, V_SHF, V_SCF = range(8)


def build_program(n_experts=N_EXP, dbg=None):
    nc = bass.Bass("TRN2", target_bir_lowering=False)
    S = Sched()

    def dram_in(name, shape, dt=F32):
        return nc.dram_tensor(name, list(shape), dt, kind="ExternalInput").ap()

    xh = dram_in("xh", [HALO + NB_TOK_CORE, D])
    c_col = dram_in("c_col", [128, KC])
    w_mod = dram_in("w_mod", [D, NMODBLK * 512])
    b_mod = dram_in("b_mod", [1, NMODBLK * 512])
    gcols = dram_in("gcols", [128, 3, KC])
    g_fin = dram_in("g_fin", [1, D])
    w_in_u = dram_in("w_in_u", [16, 128, KC, 384])
    w_out = dram_in("w_out", [D, D])
    w_rt = dram_in("w_rt", [D, 36])
    b_rt = dram_in("b_rt", [1, 36])
    w_gate = dram_in("w_gate", [N_EXP, D, DE])
    w_up = dram_in("w_up", [N_EXP, D, DE])
    w_down = dram_in("w_down", [N_EXP, DE, D])
    tabB_d = dram_in("tabB", [128, 16, 2, 128])
    constB_d = dram_in("constB", [128, 16])
    sinks_d = dram_in("sinks", [128, 16])
    hm_d = dram_in("hm", [128, 1])
    cst_d = dram_in("cst", [128, 6, 128])
    y_out = nc.dram_tensor("y", [NB_TOK_CORE, D], F32, kind="ExternalOutput").ap()
    modrow = nc.dram_tensor("modrow", [1, NMODBLK * 512], F32, kind="Internal").ap()
    dbg_out = {}
    if dbg:
        for k, shp in dbg.items():
            if k.startswith('_'):
                continue
            dbg_out[k] = nc.dram_tensor("dbg_" + k, list(shp), F32, kind="ExternalOutput").ap()

    es = ExitStack()
    ARENA_F32 = 51200
    arena = es.enter_context(nc.sbuf_tensor("arena", [128, ARENA_F32], F32))
    psum = es.enter_context(nc.psum_tensor("psum", [128, 4096], F32))

    def sview(off, dt, shape):
        n = int(np.prod(shape))
        nbytes = n * (4 if dt == F32 else 2)
        assert off % 4 == 0 and nbytes % 4 == 0
        assert off + nbytes <= ARENA_F32 * 4, (off, nbytes)
        a = arena[:, off // 4:(off + nbytes) // 4]
        if dt != F32:
            a = a.bitcast(dt)
        if len(shape) >= 2:
            names = [f"d{i}" for i in range(len(shape))]
            pat = "p (" + " ".join(names) + ") -> p " + " ".join(names)
            a = a.rearrange(pat, **{n: int(v) for n, v in zip(names[:-1], shape[:-1])})
        return a

    class Alloc:
        def __init__(self, base, limit):
            self.base, self.off, self.limit = base, base, limit

        def reset(self):
            self.off = self.base

        def get(self, dt, shape):
            n = int(np.prod(shape)) * (4 if dt == F32 else 2)
            n = (n + 63) // 64 * 64
            v = sview(self.off, dt, shape)
            self.off += n
            assert self.off <= self.limit, (self.off, self.limit)
            return v

    P_BYTES = 20480
    FT_BYTES = KC * WIN * 2
    OB_BYTES = 65536
    PA = Alloc(0, P_BYTES)
    FT = sview(P_BYTES, BF16, [KC, WIN])
    OB_OFF = P_BYTES + FT_BYTES
    O_sb = sview(OB_OFF, BF16, [NOT, D])
    X1 = sview(OB_OFF, F32, [NOT, D])
    SC = Alloc(OB_OFF + OB_BYTES, ARENA_F32 * 4)

    def bank(b, lo=0, hi=512):
        return psum[:, b * 512 + lo:b * 512 + hi]

    def bank_bf(b):
        return psum[:, b * 512:(b + 1) * 512].bitcast(BF16)

    colall = PA.get(F32, [NMODBLK * 4])
    gc = PA.get(F32, [3, KC])
    gm1 = PA.get(F32, [KC])
    gm2 = PA.get(F32, [KC])
    ident_f = PA.get(F32, [128])
    cst_b = PA.get(BF16, [3, 128])
    relA = PA.get(F32, [2, 128])
    maskA = PA.get(F32, [2, 128])
    tabB = PA.get(BF16, [16, 2, 128])
    constB = PA.get(F32, [16])
    esink = PA.get(F32, [16])
    hm = PA.get(F32, [1])
    brow = PA.get(F32, [36])
    wr = PA.get(BF16, [KC, 36])
    comb = PA.get(F32, [NOT, 32])
    stat = PA.get(F32, [64])
    ident_b = cst_b[:, 0, :]
    maskF_b = cst_b[:, 1, :]

    def bcast(ap_row):
        return ap_row.to_broadcast([128, ap_row.shape[1]])

    def dma(eng, out, in_, key, reads=(), writes=()):
        return S.add(eng, lambda h: h.dma_start(out=out, in_=in_), reads=reads, writes=writes, dma=key)

    SC.reset()
    cst_f = SC.get(F32, [6, 128])
    ccol = SC.get(F32, [KC])
    cact = SC.get(F32, [KC])
    carep = SC.get(BF16, [KC, 128])
    sink_f = SC.get(F32, [16])
    dma("sp", cst_f, cst_d, "c0", writes=["cst_f"])
    dma("pool", tabB, tabB_d, "c1", writes=["tabB"])
    dma("sp", ccol, c_col, "c2", writes=["ccol"])
    dma("sp", gc, gcols, "c3", writes=["gc"])
    dma("sp", constB, constB_d, "c4", writes=["constB"])
    dma("sp", sink_f, sinks_d, "c5", writes=["sink_f"])
    dma("sp", hm, hm_d, "c6", writes=["hm"])
    dma("sp", brow, bcast(b_rt), "c7", writes=["brow"])
    dma("pool", wr, w_rt.rearrange("(kc p) n -> p kc n", p=128), "c8", writes=["wr"])

    S.add("dve", lambda h: h.tensor_copy(out=ident_f, in_=cst_f[:, 0, :]), reads=["cst_f"], writes=["ident_f"])
    S.add("dve", lambda h: h.tensor_copy(out=cst_b, in_=cst_f[:, 0:3, :]), reads=["cst_f"], writes=["cst_b"])
    S.add("dve", lambda h: h.tensor_copy(out=relA, in_=cst_f[:, 3:5, :]), reads=["cst_f"], writes=["relA"])
    S.add("dve", lambda h: h.tensor_copy(out=maskA, in_=cst_f[:, 1:3, :]), reads=["cst_f"], writes=["maskA"])
    S.add("dve", lambda h: h.tensor_tensor(out=tabB[:, :, 1, :], in0=tabB[:, :, 1, :],
                                            in1=cst_b[:, 2, :].unsqueeze(1).to_broadcast([128, 16, 128]),
                                            op=ALU.add), reads=["cst_b", "tabB"], writes=["tabB"])
    S.add("act", lambda h: h.activation(out=esink, in_=sink_f, func=AF.Exp), reads=["sink_f"], writes=["esink"])
    S.add("act", lambda h: h.activation(out=cact, in_=ccol, func=AF.Silu), reads=["ccol"], writes=["cact"])
    S.add("dve", lambda h: h.tensor_copy(out=carep, in_=cact.unsqueeze(2).to_broadcast([128, KC, 128])),
          reads=["cact"], writes=["carep"])

    wblk = [SC.get(BF16, [KC, 512]) for _ in range(2)]
    rowt = [SC.get(F32, [512]) for _ in range(2)]
    biasb = [SC.get(F32, [512]) for _ in range(2)]
    diag = [SC.get(F32, [4, 128]) for _ in range(2)]
    w_mod_v = w_mod.rearrange("(kc p) n -> p kc n", p=128)
    for j in range(NMODBLK):
        wb = wblk[j % 2]
        rb_, bb, dg = rowt[j % 2], biasb[j % 2], diag[j % 2]
        acc = bank(4 + j % 2)
        dma("pool", wb, w_mod_v[:, :, j * 512:(j + 1) * 512], f"wm{j % 2}", writes=[f"wblk{j % 2}"])
        dma("sp", bb, bcast(b_mod[:, j * 512:(j + 1) * 512]), f"bm{j % 2}",
            writes=[f"biasb{j % 2}"])
        for kc in range(KC):
            S.add("pe", lambda h, kc=kc, wb=wb, acc=acc: h.matmul(acc, lhsT=carep[:, kc, :], rhs=wb[:, kc, :],
                                                                   start=(kc == 0), stop=(kc == KC - 1)),
                  reads=["carep", f"wblk{j % 2}"], writes=[f"acc{j % 2}"])
        S.add("dve", lambda h, rb_=rb_, bb=bb, acc=acc: h.tensor_tensor(out=rb_, in0=acc, in1=bb, op=ALU.add),
              reads=[f"acc{j % 2}", f"biasb{j % 2}"], writes=[f"rowt{j % 2}"])
        dma("sp", modrow[:, j * 512:(j + 1) * 512], rb_[0:1, :], f"mrow{j % 2}", reads=[f"rowt{j % 2}"], writes=[f"modrow{j % 2}"])
        S.add("dve", lambda h, rb_=rb_, dg=dg: h.tensor_tensor(
            out=dg, in0=rb_.rearrange("p (a b) -> p a b", a=4),
            in1=ident_f.unsqueeze(1).to_broadcast([128, 4, 128]), op=ALU.mult),
            reads=[f"rowt{j % 2}", "ident_f"], writes=[f"diag{j % 2}"])
        S.add("dve", lambda h, dg=dg, j=j: h.tensor_reduce(out=colall[:, j * 4:(j + 1) * 4], in_=dg,
                                                            axis=AX.X, op=ALU.add),
              reads=[f"diag{j % 2}"], writes=["colall"])

    def colv(v):
        return colall[:, v * KC:(v + 1) * KC]

    for (gm, gi, vs) in ((gm1, 0, V_SC1), (gm2, 1, V_SC2)):
        S.add("dve", lambda h, gm=gm, gi=gi, vs=vs: h.scalar_tensor_tensor(
            out=gm, in0=colv(vs), scalar=1.0, in1=gc[:, gi, :], op0=ALU.add, op1=ALU.mult),
            reads=["colall", "gc"], writes=["gm"])

    def dbg_dump(name, src_ap, dst_ap, reads):
        if dbg and name in dbg:
            dma("sp", dst_ap, src_ap, "dbg", reads=reads, writes=["dbgout"])

    if dbg and "colall" in dbg:
        dbg_dump("colall", colall, dbg_out["colall"], ["colall"])

    def rms_rstd(eng_h, ss, rstd, n, reads, writes):
        S.add("dve", lambda h: h.tensor_scalar(out=rstd, in0=ss, scalar1=1.0 / n, scalar2=EPS,
                                                op0=ALU.mult, op1=ALU.add), reads=reads, writes=writes)
        S.add("act", lambda h: h.activation(out=rstd, in_=rstd, func=AF.Ln), reads=writes, writes=writes)
        S.add("act", lambda h: h.activation(out=rstd, in_=rstd, func=AF.Exp, scale=-0.5), reads=writes, writes=writes)

    def transposes_to_FT(src_bf, src_res, lt, scale_col, bias_col, tag):
        for hb in range(2):
            pb = bank_bf(6 + hb)
            for k8 in range(8):
                kc = hb * 8 + k8
                S.add("pe", lambda h, pb=pb, k8=k8, kc=kc: h.transpose(
                    out=pb[:, k8 * 128:(k8 + 1) * 128], in_=src_bf[:, kc * 128:(kc + 1) * 128], identity=ident_b),
                    reads=[src_res, "cst_b"], writes=[f"tp{hb}"])
            for k8 in range(8):
                kc = hb * 8 + k8
                dst = FT[:, kc, lt * 128:(lt + 1) * 128]
                src = pb[:, k8 * 128:(k8 + 1) * 128]
                if k8 % 2 == 0:
                    if bias_col is not None:
                        S.add("act", lambda h, dst=dst, src=src, kc=kc: h.activation(
                            out=dst, in_=src, func=AF.Identity, scale=scale_col[:, kc:kc + 1],
                            bias=bias_col[:, kc:kc + 1]),
                            reads=[f"tp{hb}", "gm", "colall", "gc"], writes=[("FT", lt)])
                    else:
                        S.add("act", lambda h, dst=dst, src=src, kc=kc: h.activation(
                            out=dst, in_=src, func=AF.Copy, scale=scale_col[:, kc:kc + 1]),
                            reads=[f"tp{hb}", "gm", "colall", "gc"], writes=[("FT", lt)])
                else:
                    if bias_col is not None:
                        S.add("dve", lambda h, dst=dst, src=src, kc=kc: h.tensor_scalar(
                            out=dst, in0=src, scalar1=scale_col[:, kc:kc + 1], scalar2=bias_col[:, kc:kc + 1],
                            op0=ALU.mult, op1=ALU.add),
                            reads=[f"tp{hb}", "gm", "colall", "gc"], writes=[("FT", lt)])
                    else:
                        S.add("dve", lambda h, dst=dst, src=src, kc=kc: h.tensor_scalar(
                            out=dst, in0=src, scalar1=scale_col[:, kc:kc + 1], scalar2=None, op0=ALU.mult),
                            reads=[f"tp{hb}", "gm", "colall", "gc"], writes=[("FT", lt)])

    for H in range(2):
        row0 = H * HALF
        S.fence()
        SC.reset()
        xt = [SC.get(F32, [D]) for _ in range(2)]
        xnb = [SC.get(BF16, [D]) for _ in range(2)]
        junk = SC.get(BF16, [D])
        for lt in range(NWT):
            b2 = lt % 2
            dma("sp", xt[b2], xh[row0 + lt * 128: row0 + (lt + 1) * 128, :], f"xt{b2}", writes=[f"xt{b2}"])
            ss = stat[:, b2:b2 + 1]
            rs = stat[:, 2 + b2:3 + b2]
            S.add("act", lambda h, b2=b2, ss=ss: h.activation(out=junk, in_=xt[b2], func=AF.Square, accum_out=ss),
                  reads=[f"xt{b2}"], writes=["junk", f"ss{b2}"])
            rms_rstd(None, ss, rs, D, [f"ss{b2}"], [f"rs{b2}"])
            S.add("act", lambda h, b2=b2, rs=rs: h.activation(out=xnb[b2], in_=xt[b2], func=AF.Copy, scale=rs),
                  reads=[f"xt{b2}", f"rs{b2}"], writes=[f"xnb{b2}"])
            transposes_to_FT(xnb[b2], f"xnb{b2}", lt, gm1, colv(V_SH1), "h")
        if dbg and "hT" in dbg and H == dbg.get("_H", 1):
            tmpf = SC.get(F32, [KC, 128])
            for lt in range(NWT):
                S.add("dve", lambda h, lt=lt: h.tensor_copy(out=tmpf, in_=FT[:, :, lt * 128:(lt + 1) * 128]),
                      reads=[("FT", lt)], writes=["tmpf"])
                dma("sp", dbg_out["hT"][lt], tmpf, "dbg", reads=["tmpf"], writes=["dbgout"])

        S.fence()
        SC.reset()
        Wu = [SC.get(BF16, [KC, 384]) for _ in range(2)]
        QT = SC.get(BF16, [HALF])
        KT = SC.get(BF16, [WIN])
        VA = SC.get(BF16, [NWT, 2, 65])
        tabA = [SC.get(BF16, [2, 2, 2, 128]) for _ in range(2)]
        tA_f = SC.get(F32, [2, 128])
        tA_h = SC.get(F32, [2, 128])
        PT = [SC.get(BF16, [5, 128]) for _ in range(2)]
        rec = [SC.get(F32, [2]) for _ in range(2)]
        S.add("pool", lambda h: h.memset(VA[:, :, :, 64:65], 1.0), writes=["VA"])
        if H == 0:
            S.add("pool", lambda h: h.tensor_copy(out=VA[:, 0:4, :, 64:65],
                                                  in_=hm.unsqueeze(1).unsqueeze(1).to_broadcast([128, 4, 2, 1])),
                  reads=["hm"], writes=["VA"])
        for b2 in range(2):
            S.add("pool", lambda h, b2=b2: h.memset(PT[b2], 0.0), writes=[f"PT{b2}"])

        acc_i = [0]

        def next_acc():
            a = acc_i[0] % 2
            acc_i[0] += 1
            return a

        for u in range(16):
            grpA = u < 8
            wu = Wu[u % 2]
            dma("pool", wu, w_in_u[u], f"wu{u % 2}", writes=[f"wu{u % 2}"])
            wres = f"wu{u % 2}"
            for tb in range(HALF // 512):
                a = next_acc()
                for kc in range(KC):
                    S.add("pe", lambda h, a=a, kc=kc, tb=tb, wu=wu: h.matmul(
                        bank(4 + a), lhsT=wu[:, kc, 0:128], rhs=FT[:, kc, HALO + tb * 512: HALO + (tb + 1) * 512],
                        start=(kc == 0), stop=(kc == KC - 1)),
                        reads=[wres] + [("FT", 4 + tb * 4 + i) for i in range(4)], writes=[f"acc{a}"])
                S.add("dve", lambda h, a=a, tb=tb: h.tensor_scalar(
                    out=QT[:, tb * 512:(tb + 1) * 512], in0=bank(4 + a), scalar1=0.125, scalar2=None, op0=ALU.mult),
                    reads=[f"acc{a}"], writes=["QT"])
            do_kv = (not grpA) or (u % 2 == 0)
            if do_kv:
                for tb in range(WIN // 512):
                    a = next_acc()
                    for kc in range(KC):
                        S.add("pe", lambda h, a=a, kc=kc, tb=tb, wu=wu: h.matmul(
                            bank(4 + a), lhsT=wu[:, kc, 128:256], rhs=FT[:, kc, tb * 512:(tb + 1) * 512],
                            start=(kc == 0), stop=(kc == KC - 1)),
                            reads=[wres] + [("FT", tb * 4 + i) for i in range(4)], writes=[f"acc{a}"])
                    S.add("dve", lambda h, a=a, tb=tb: h.tensor_copy(out=KT[:, tb * 512:(tb + 1) * 512], in_=bank(4 + a)),
                          reads=[f"acc{a}"], writes=["KT"])
                for lt in range(NWT):
                    a = next_acc()
                    for kc in range(KC):
                        S.add("pe", lambda h, a=a, kc=kc, lt=lt, wu=wu: h.matmul(
                            bank(4 + a, 0, 128), lhsT=FT[:, kc, lt * 128:(lt + 1) * 128], rhs=wu[:, kc, 256:384],
                            start=(kc == 0), stop=(kc == KC - 1)),
                            reads=[wres, ("FT", lt)], writes=[f"acc{a}"])
                    src = bank(4 + a, 0, 128).rearrange("p (a b) -> p a b", a=2)
                    if H == 0 and lt < 4:
                        S.add("dve", lambda h, lt=lt, src=src: h.tensor_scalar(
                            out=VA[:, lt, :, 0:64], in0=src, scalar1=hm[:, 0:1], scalar2=None, op0=ALU.mult),
                            reads=[f"acc{a}", "hm"], writes=["VA"])
                    else:
                        S.add("dve", lambda h, lt=lt, src=src: h.tensor_copy(out=VA[:, lt, :, 0:64], in_=src),
                              reads=[f"acc{a}"], writes=["VA"])
            if grpA:
                tb_ = tabA[u % 2]
                for j in range(2):
                    hd = 2 * u + j
                    S.add("pool", lambda h, hd=hd: h.tensor_scalar(
                        out=tA_f, in0=relA, scalar1=-SLOPES[hd], scalar2=None, op0=ALU.mult),
                        reads=["relA", "tA_h"], writes=["tA_f"])
                    S.add("pool", lambda h: h.tensor_tensor(out=tA_f, in0=tA_f, in1=maskA, op=ALU.add),
                          reads=["tA_f", "maskA"], writes=["tA_f"])
                    S.add("pool", lambda h, j=j, tb_=tb_: h.tensor_copy(out=tb_[:, j, 0, :, :], in_=tA_f),
                          reads=["tA_f"], writes=[f"tabA{u % 2}"])
                    S.add("pool", lambda h, j=j, tb_=tb_: h.tensor_copy(out=tA_h, in_=tb_[:, j, 0, :, :]),
                          reads=[f"tabA{u % 2}"], writes=["tA_h"])
                    S.add("pool", lambda h: h.tensor_tensor(out=tA_h, in0=tA_f, in1=tA_h, op=ALU.subtract),
                          reads=["tA_f", "tA_h"], writes=["tA_h"])
                    S.add("pool", lambda h, j=j, tb_=tb_: h.tensor_copy(out=tb_[:, j, 1, :, :], in_=tA_h),
                          reads=["tA_h"], writes=[f"tabA{u % 2}"])
            items = [(qt, j) for qt in range(NOT) for j in range(2)]

            def emit_qk(n):
                qt, j = items[n]
                sb = n % 2
                hd = (2 * u + j) if grpA else (2 * (u - 8) + j)
                pl, ph = j * 64, (j + 1) * 64
                q_ap = QT[pl:ph, qt * 128:(qt + 1) * 128]
                s_main = bank(sb, 0, 384)
                s_last = bank(2 + sb, 0, 256)
                blocks = range(3, 5) if grpA else range(5)
                for i in blocks:
                    lt = qt + i
                    dst = s_main[:, i * 128:(i + 1) * 128] if i < 3 else s_last[:, (i - 3) * 128:(i - 2) * 128]
                    sres = f"S{sb}" if i < 3 else f"S2{sb}"
                    has_tab = grpA or i in (0, 3, 4)
                    S.add("pe", lambda h, dst=dst, lt=lt, pl=pl, ph=ph, q_ap=q_ap, has_tab=has_tab: h.matmul(
                        dst, lhsT=KT[pl:ph, lt * 128:(lt + 1) * 128], rhs=q_ap, start=True, stop=not has_tab),
                        reads=["KT", "QT"], writes=[sres])
                    if grpA:
                        tb_ = tabA[u % 2]
                        for hl in range(2):
                            S.add("pe", lambda h, dst=dst, j=j, hl=hl, i=i, tb_=tb_: h.matmul(
                                dst, lhsT=ident_b, rhs=tb_[:, j, hl, i - 3, :], start=False, stop=(hl == 1)),
                                reads=["cst_b", f"tabA{u % 2}"], writes=[sres])
                    elif i == 0:
                        S.add("pe", lambda h, dst=dst: h.matmul(dst, lhsT=ident_b, rhs=maskF_b, start=False, stop=True),
                              reads=["cst_b"], writes=[sres])
                    elif i >= 3:
                        S.add("pe", lambda h, dst=dst, hd=hd, i=i: h.matmul(
                            dst, lhsT=ident_b, rhs=tabB[:, hd, i - 3, :], start=False, stop=True),
                            reads=["cst_b", "tabB"], writes=[sres])
                pt = PT[sb]
                if not grpA:
                    S.add("act", lambda h, pt=pt, s_main=s_main, hd=hd: h.activation(
                        out=pt[:, 0:3, :], in_=s_main.rearrange("p (a b) -> p a b", a=3), func=AF.Exp,
                        bias=constB[:, hd:hd + 1]),
                        reads=[f"S{sb}", "constB"], writes=[f"PT{sb}"])
                S.add("act", lambda h, pt=pt, s_last=s_last: h.activation(
                    out=pt[:, 3:5, :], in_=s_last.rearrange("p (a b) -> p a b", a=2), func=AF.Exp),
                    reads=[f"S2{sb}"], writes=[f"PT{sb}"])

            def emit_pv(n):
                qt, j = items[n]
                sb = n % 2
                ob = qt % 2
                o_ps = bank(6 + ob, 0, 130).rearrange("p (a b) -> p a b", a=2)
                blocks = list(range(3, 5)) if grpA else list(range(5))
                for bi, i in enumerate(blocks):
                    lt = qt + i
                    S.add("pe", lambda h, i=i, lt=lt, j=j, sb=sb, o_ps=o_ps, bi=bi, nb_=len(blocks): h.matmul(
                        o_ps[:, j, :], lhsT=PT[sb][:, i, :], rhs=VA[:, lt, 0 if grpA else j, :],
                        start=(bi == 0), stop=(bi == nb_ - 1)),
                        reads=[f"PT{sb}", "VA"], writes=[f"OP{ob}"])
                if j == 1:
                    rc = rec[ob]
                    den = o_ps[:, :, 64:65]
                    if grpA:
                        S.add("dve", lambda h, rc=rc, den=den: h.tensor_tensor(
                            out=rc.unsqueeze(2), in0=den, in1=esink[:, 2 * u:2 * u + 2].unsqueeze(2), op=ALU.add),
                            reads=[f"OP{ob}", "esink"], writes=[f"rec{ob}"])
                        S.add("dve", lambda h, rc=rc: h.reciprocal(out=rc, in_=rc),
                              reads=[f"rec{ob}"], writes=[f"rec{ob}"])
                    else:
                        S.add("dve", lambda h, rc=rc, den=den: h.reciprocal(out=rc.unsqueeze(2), in_=den),
                              reads=[f"OP{ob}"], writes=[f"rec{ob}"])
                    S.add("dve", lambda h, rc=rc, o_ps=o_ps, qt=qt: h.tensor_tensor(
                        out=O_sb[:, qt, u * 128:(u + 1) * 128].rearrange("p (a b) -> p a b", a=2),
                        in0=o_ps[:, :, 0:64], in1=rc.unsqueeze(2).to_broadcast([128, 2, 64]), op=ALU.mult),
                        reads=[f"OP{ob}", f"rec{ob}"], writes=[("O", qt, u)])

            for n in range(len(items) + 1):
                if n < len(items):
                    emit_qk(n)
                if n >= 1:
                    emit_pv(n - 1)

        if dbg and "O" in dbg and H == dbg.get("_H", 1):
            tmpo = SC.get(F32, [D])
            for t in range(NOT):
                S.add("dve", lambda h, t=t: h.tensor_copy(out=tmpo, in_=O_sb[:, t, :]),
                      reads=[("O", t, u) for u in range(16)], writes=["tmpo"])
                dma("sp", dbg_out["O"][t], tmpo, "dbg", reads=["tmpo"], writes=["dbgout"])

        S.fence()
        SC.reset()
        junk = SC.get(BF16, [D])
        onb = [SC.get(BF16, [D]) for _ in range(2)]
        for t in range(NOT):
            b2 = t % 2
            ss2 = stat[:, 4 + 2 * b2: 6 + 2 * b2]
            rs2 = stat[:, 8 + 2 * b2: 10 + 2 * b2]
            ores = [("O", t, u) for u in range(16)]
            for g in range(2):
                S.add("act", lambda h, t=t, g=g, ss2=ss2: h.activation(
                    out=junk[:, 0:1024], in_=O_sb[:, t, g * 1024:(g + 1) * 1024], func=AF.Square,
                    accum_out=ss2[:, g:g + 1]), reads=ores, writes=["junk", f"ss2{b2}"])
            rms_rstd(None, ss2, rs2, 1024, [f"ss2{b2}"], [f"rs2{b2}"])
            for g in range(2):
                S.add("act", lambda h, t=t, g=g, rs2=rs2, b2=b2: h.activation(
                    out=onb[b2][:, g * 1024:(g + 1) * 1024], in_=O_sb[:, t, g * 1024:(g + 1) * 1024],
                    func=AF.Copy, scale=rs2[:, g:g + 1]), reads=ores + [f"rs2{b2}"], writes=[f"onb{b2}"])
            transposes_to_FT(onb[b2], f"onb{b2}", 4 + t, gc[:, 2, :], None, "o")

        S.fence()
        SC.reset()
        Wo = [SC.get(BF16, [KC, 512]) for _ in range(2)]
        gt1row = SC.get(F32, [D])
        tmpx = [SC.get(F32, [512]) for _ in range(2)]
        dma("sp", gt1row, bcast(modrow[:, V_GT1 * D:(V_GT1 + 1) * D]), "gt1r",
            reads=["modrow0", "modrow1"], writes=["gt1row"])
        for t in range(NOT):
            r = HALO + row0 + t * 128
            dma("sp", X1[:, t, :], xh[r:r + 128, :], f"x1_{t}", writes=[("X1", t, nb) for nb in range(4)])
        w_out_v = w_out.rearrange("(kc p) n -> p kc n", p=128)
        for nb in range(4):
            wo = Wo[nb % 2]
            dma("pool", wo, w_out_v[:, :, nb * 512:(nb + 1) * 512], f"wo{nb % 2}", writes=[f"wo{nb % 2}"])
            for t in range(NOT):
                a = next_acc()
                for kc in range(KC):
                    S.add("pe", lambda h, a=a, kc=kc, t=t, wo=wo: h.matmul(
                        bank(4 + a), lhsT=FT[:, kc, (4 + t) * 128:(5 + t) * 128], rhs=wo[:, kc, :],
                        start=(kc == 0), stop=(kc == KC - 1)),
                        reads=[f"wo{nb % 2}", ("FT", 4 + t)], writes=[f"acc{a}"])
                tx = tmpx[a]
                S.add("dve", lambda h, a=a, tx=tx, nb=nb: h.tensor_tensor(
                    out=tx, in0=bank(4 + a), in1=gt1row[:, nb * 512:(nb + 1) * 512], op=ALU.mult),
                    reads=[f"acc{a}", "gt1row"], writes=[f"tmpx{a}"])
                S.add("dve", lambda h, tx=tx, t=t, nb=nb: h.tensor_tensor(
                    out=X1[:, t, nb * 512:(nb + 1) * 512], in0=X1[:, t, nb * 512:(nb + 1) * 512], in1=tx, op=ALU.add),
                    reads=[f"tmpx{a}", ("X1", t, nb)], writes=[("X1", t, nb)])
        if dbg and "X1" in dbg and H == dbg.get("_H", 1):
            for t in range(NOT):
                dma("sp", dbg_out["X1"][t], X1[:, t, :], "dbg", reads=[("X1", t, nb) for nb in range(4)],
                    writes=["dbgout"])

        S.fence()
        SC.reset()
        junk = SC.get(BF16, [D])
        xnb = [SC.get(BF16, [D]) for _ in range(2)]
        L = SC.get(F32, [NOT, 36])
        for t in range(NOT):
            b2 = t % 2
            xres = [("X1", t, nb) for nb in range(4)]
            ss = stat[:, b2:b2 + 1]
            rs = stat[:, 2 + b2:3 + b2]
            S.add("act", lambda h, t=t, ss=ss: h.activation(out=junk, in_=X1[:, t, :], func=AF.Square, accum_out=ss),
                  reads=xres, writes=["junk", f"ss{b2}"])
            rms_rstd(None, ss, rs, D, [f"ss{b2}"], [f"rs{b2}"])
            S.add("act", lambda h, t=t, rs=rs, b2=b2: h.activation(out=xnb[b2], in_=X1[:, t, :], func=AF.Copy, scale=rs),
                  reads=xres + [f"rs{b2}"], writes=[f"xnb{b2}"])
            transposes_to_FT(xnb[b2], f"xnb{b2}", 4 + t, gm2, colv(V_SH2), "h2")
            a = next_acc()
            for kc in range(KC):
                S.add("pe", lambda h, a=a, kc=kc, t=t: h.matmul(
                    bank(4 + a, 0, 36), lhsT=FT[:, kc, (4 + t) * 128:(5 + t) * 128], rhs=wr[:, kc, :],
                    start=(kc == 0), stop=(kc == KC - 1)),
                    reads=["wr", ("FT", 4 + t)], writes=[f"acc{a}"])
            S.add("dve", lambda h, a=a, t=t: h.tensor_tensor(out=L[:, t, :], in0=bank(4 + a, 0, 36), in1=brow, op=ALU.add),
                  reads=[f"acc{a}", "brow"], writes=["L"])
        gl = L[:, :, 0:4]
        el = L[:, :, 4:36]
        gmax = SC.get(F32, [NOT]); gsum = SC.get(F32, [NOT]); pg = SC.get(F32, [NOT])
        gt_ = SC.get(F32, [NOT, 4]); ohg = SC.get(F32, [NOT, 4]); pen = SC.get(F32, [NOT, 4])
        elm = SC.get(F32, [NOT, 32]); oh1 = SC.get(F32, [NOT, 32]); oh2 = SC.get(F32, [NOT, 32])
        elm2 = SC.get(F32, [NOT, 32])
        m1 = SC.get(F32, [NOT]); m2 = SC.get(F32, [NOT]); e2 = SC.get(F32, [NOT]); w1 = SC.get(F32, [NOT])
        w2 = SC.get(F32, [NOT])
        RR = ["L", "rt"]

        def dv(fn):
            S.add("dve", fn, reads=RR, writes=["rt"])

        dv(lambda h: h.tensor_reduce(out=gmax, in_=gl, axis=AX.X, op=ALU.max))
        dv(lambda h: h.tensor_tensor(out=gt_, in0=gl, in1=gmax.unsqueeze(2).to_broadcast([128, NOT, 4]), op=ALU.subtract))
        S.add("act", lambda h: h.activation(out=gt_, in_=gt_, func=AF.Exp), reads=RR, writes=["rt"])
        dv(lambda h: h.tensor_reduce(out=gsum, in_=gt_, axis=AX.X, op=ALU.add))
        dv(lambda h: h.reciprocal(out=pg, in_=gsum))
        dv(lambda h: h.tensor_tensor(out=ohg, in0=gl, in1=gmax.unsqueeze(2).to_broadcast([128, NOT, 4]), op=ALU.is_ge))
        dv(lambda h: h.tensor_scalar(out=pen, in0=ohg, scalar1=-1.0, scalar2=1e9, op0=ALU.add, op1=ALU.mult))
        dv(lambda h: h.tensor_tensor(
            out=elm.rearrange("p t (g j) -> p t g j", g=4), in0=el.rearrange("p t (g j) -> p t g j", g=4),
            in1=pen.unsqueeze(3).to_broadcast([128, NOT, 4, 8]), op=ALU.add))
        dv(lambda h: h.tensor_reduce(out=m1, in_=elm, axis=AX.X, op=ALU.max))
        dv(lambda h: h.tensor_tensor(out=oh1, in0=elm, in1=m1.unsqueeze(2).to_broadcast([128, NOT, 32]), op=ALU.is_ge))
        dv(lambda h: h.scalar_tensor_tensor(out=elm2, in0=oh1, scalar=-1e9, in1=elm, op0=ALU.mult, op1=ALU.add))
        dv(lambda h: h.tensor_reduce(out=m2, in_=elm2, axis=AX.X, op=ALU.max))
        dv(lambda h: h.tensor_tensor(out=oh2, in0=elm2, in1=m2.unsqueeze(2).to_broadcast([128, NOT, 32]), op=ALU.is_ge))
        dv(lambda h: h.tensor_tensor(out=e2, in0=m2, in1=m1, op=ALU.subtract))
        S.add("act", lambda h: h.activation(out=e2, in_=e2, func=AF.Exp), reads=RR, writes=["rt"])
        dv(lambda h: h.tensor_scalar(out=w1, in0=e2, scalar1=1.0, scalar2=None, op0=ALU.add))
        dv(lambda h: h.reciprocal(out=w1, in_=w1))
        dv(lambda h: h.tensor_tensor(out=w1, in0=w1, in1=pg, op=ALU.mult))
        dv(lambda h: h.tensor_tensor(out=w2, in0=w1, in1=e2, op=ALU.mult))
        dv(lambda h: h.tensor_tensor(out=oh1, in0=oh1, in1=w1.unsqueeze(2).to_broadcast([128, NOT, 32]), op=ALU.mult))
        dv(lambda h: h.tensor_tensor(out=oh2, in0=oh2, in1=w2.unsqueeze(2).to_broadcast([128, NOT, 32]), op=ALU.mult))
        S.add("dve", lambda h: h.tensor_tensor(out=comb, in0=oh1, in1=oh2, op=ALU.add), reads=RR, writes=["comb"])
        if dbg and "comb" in dbg and H == dbg.get("_H", 1):
            dma("sp", dbg_out["comb"], comb, "dbg", reads=["comb"], writes=["dbgout"])

        S.fence()
        SC.reset()
        Wring = [SC.get(BF16, [KC * 512]) for _ in range(3)]
        aT = SC.get(BF16, [4, HALF])
        sg = [SC.get(F32, [512]) for _ in range(2)]
        gt2row = SC.get(BF16, [D])
        dma("pool", gt2row, bcast(modrow[:, V_GT2 * D:(V_GT2 + 1) * D]), "gt2r",
            reads=["modrow0", "modrow1"], writes=["gt2row"])
        ring_i = [0]

        def ring_next():
            r = ring_i[0] % 3
            ring_i[0] += 1
            return r

        gu_i = [0]
        y_i = [0]
        for e in range(n_experts):
            rg, ru, rd = ring_next(), ring_next(), ring_next()
            Wg = Wring[rg].rearrange("p (a b) -> p a b", a=KC)
            Wup = Wring[ru].rearrange("p (a b) -> p a b", a=KC)
            Wd = Wring[rd].rearrange("p (a b) -> p a b", a=4)
            dma("pool", Wg, w_gate[e].rearrange("(kc p) n -> p kc n", p=128), f"ring{rg}", writes=[f"ring{rg}"])
            dma("pool", Wup, w_up[e].rearrange("(kc p) n -> p kc n", p=128), f"ring{ru}", writes=[f"ring{ru}"])
            dma("pool", Wd, w_down[e].rearrange("(kc p) n -> p kc n", p=128), f"ring{rd}", writes=[f"ring{rd}"])
            S.add("pool", lambda h, Wd=Wd: h.tensor_tensor(
                out=Wd, in0=Wd, in1=gt2row.unsqueeze(1).to_broadcast([128, 4, D]), op=ALU.mult),
                reads=[f"ring{rd}", "gt2row"], writes=[f"ring{rd}"])
            for mb in range(4):
                for tb in range(HALF // 512):
                    gi = gu_i[0] % 2
                    gu_i[0] += 1
                    G, U = bank(gi), bank(2 + gi)
                    toks = [("FT", 4 + tb * 4 + i) for i in range(4)]
                    for kc in range(KC):
                        S.add("pe", lambda h, kc=kc, G=G, Wg=Wg, mb=mb, tb=tb: h.matmul(
                            G, lhsT=Wg[:, kc, mb * 128:(mb + 1) * 128],
                            rhs=FT[:, kc, HALO + tb * 512:HALO + (tb + 1) * 512],
                            start=(kc == 0), stop=(kc == KC - 1)),
                            reads=[f"ring{rg}"] + toks, writes=[f"G{gi}"])
                    for kc in range(KC):
                        S.add("pe", lambda h, kc=kc, U=U, Wup=Wup, mb=mb, tb=tb: h.matmul(
                            U, lhsT=Wup[:, kc, mb * 128:(mb + 1) * 128],
                            rhs=FT[:, kc, HALO + tb * 512:HALO + (tb + 1) * 512],
                            start=(kc == 0), stop=(kc == KC - 1)),
                            reads=[f"ring{ru}"] + toks, writes=[f"U{gi}"])
                    S.add("act", lambda h, gi=gi, G=G: h.activation(out=sg[gi], in_=G, func=AF.Silu),
                          reads=[f"G{gi}"], writes=[f"sg{gi}"])
                    S.add("dve", lambda h, gi=gi, U=U, mb=mb, tb=tb: h.tensor_tensor(
                        out=aT[:, mb, tb * 512:(tb + 1) * 512], in0=sg[gi], in1=U, op=ALU.mult),
                        reads=[f"sg{gi}", f"U{gi}"], writes=[("aT", tb)])
            for t in range(NOT):
                for nb in range(4):
                    yi = y_i[0] % 2
                    y_i[0] += 1
                    Y = bank(4 + yi)
                    for kc in range(4):
                        S.add("pe", lambda h, kc=kc, Y=Y, Wd=Wd, t=t, nb=nb: h.matmul(
                            Y, lhsT=aT[:, kc, t * 128:(t + 1) * 128], rhs=Wd[:, kc, nb * 512:(nb + 1) * 512],
                            start=(kc == 0), stop=(kc == 3)),
                            reads=[f"ring{rd}", ("aT", t // 4)], writes=[f"acc{yi}"])
                    S.add("dve", lambda h, Y=Y, t=t, nb=nb, e=e: h.scalar_tensor_tensor(
                        out=X1[:, t, nb * 512:(nb + 1) * 512], in0=Y, scalar=comb[:, t, e:e + 1],
                        in1=X1[:, t, nb * 512:(nb + 1) * 512], op0=ALU.mult, op1=ALU.add),
                        reads=[f"acc{yi}", "comb", ("X1", t, nb)], writes=[("X1", t, nb)])

        S.fence()
        SC.reset()
        junk = SC.get(BF16, [D])
        gfrow = SC.get(F32, [D])
        shfrow = SC.get(F32, [D])
        scfrow = SC.get(F32, [D])
        ot = [SC.get(F32, [D]) for _ in range(2)]
        dma("sp", gfrow, bcast(g_fin), "gfr", writes=["gfrow"])
        dma("sp", shfrow, bcast(modrow[:, V_SHF * D:(V_SHF + 1) * D]), "shfr",
            reads=["modrow0", "modrow1"], writes=["shfrow"])
        dma("sp", scfrow, bcast(modrow[:, V_SCF * D:(V_SCF + 1) * D]), "scfr",
            reads=["modrow0", "modrow1"], writes=["scfrow"])
        S.add("dve", lambda h: h.scalar_tensor_tensor(out=gfrow, in0=scfrow, scalar=1.0, in1=gfrow,
                                                       op0=ALU.add, op1=ALU.mult),
              reads=["gfrow", "scfrow"], writes=["gfrow"])
        for t in range(NOT):
            b2 = t % 2
            xres = [("X1", t, nb) for nb in range(4)]
            ss = stat[:, b2:b2 + 1]
            rs = stat[:, 2 + b2:3 + b2]
            S.add("act", lambda h, t=t, ss=ss: h.activation(out=junk, in_=X1[:, t, :], func=AF.Square, accum_out=ss),
                  reads=xres, writes=["junk", f"ss{b2}"])
            rms_rstd(None, ss, rs, D, [f"ss{b2}"], [f"rs{b2}"])
            S.add("dve", lambda h, t=t, rs=rs, b2=b2: h.scalar_tensor_tensor(
                out=ot[b2], in0=X1[:, t, :], scalar=rs, in1=gfrow, op0=ALU.mult, op1=ALU.mult),
                reads=xres + [f"rs{b2}", "gfrow"], writes=[f"ot{b2}"])
            S.add("pool", lambda h, b2=b2: h.tensor_tensor(out=ot[b2], in0=ot[b2], in1=shfrow, op=ALU.add),
                  reads=[f"ot{b2}", "shfrow"], writes=[f"ot{b2}"])
            r = H * HALF + t * 128
            dma("sp", y_out[r:r + 128, :], ot[b2], f"yo{b2}", reads=[f"ot{b2}"], writes=["yout"])

    finals = ["yo0", "yo1"] + (["dbg"] if dbg and "dbg" in S.dma_cnt else [])
    S.emit(nc, es, final_waits=finals)
    es.close()
    return nc


def _col(v):
    return np.ascontiguousarray(np.asarray(v, np.float32).reshape(KC, 128).T)


def prepare_shared(inp):
    f = lambda a: np.asarray(a, dtype=np.float32)
    sh = {}
    sh["w_mod"] = np.ascontiguousarray(np.concatenate([f(inp["w_ada"])[0], f(inp["w_ada_final"])], axis=1))
    sh["b_mod"] = np.ascontiguousarray(np.concatenate([f(inp["b_ada"])[0], f(inp["b_ada_final"])])[None, :])
    g_out = np.concatenate([f(inp["g_out_a"])[0], f(inp["g_out_b"])[0]])
    sh["gcols"] = np.ascontiguousarray(np.stack([_col(f(inp["g_mix"])[0]), _col(f(inp["g_ffn"])[0]), _col(g_out)], axis=1))
    sh["g_fin"] = f(inp["g_final"])[None, :].copy()
    w_in = f(inp["w_in"])[0]
    units = []
    for a in range(8):
        g = a // 2
        q = w_in[:, a * 128:(a + 1) * 128]
        k = w_in[:, 1024 + g * 64:1024 + (g + 1) * 64]
        v = w_in[:, 1280 + g * 64:1280 + (g + 1) * 64]
        units.append(np.concatenate([q, k, k, v, v], axis=1))
    for bp in range(8):
        units.append(np.concatenate([w_in[:, 1536 + bp * 128:1536 + (bp + 1) * 128],
                                     w_in[:, 2560 + bp * 128:2560 + (bp + 1) * 128],
                                     w_in[:, 3584 + bp * 128:3584 + (bp + 1) * 128]], axis=1))
    wu = np.stack(units)
    sh["w_in_u"] = np.ascontiguousarray(wu.reshape(16, KC, 128, 384).transpose(0, 2, 1, 3))
    sh["w_out"] = np.ascontiguousarray(f(inp["w_out"])[0])
    sh["w_rt"] = np.ascontiguousarray(np.concatenate([f(inp["w_router_group"])[0], f(inp["w_router_expert"])[0]], axis=1))
    sh["b_rt"] = np.concatenate([f(inp["b_router_group"])[0], f(inp["b_router_expert"])[0]])[None, :].copy()
    sh["w_gate"] = np.ascontiguousarray(f(inp["w_gate"])[0])
    sh["w_up"] = np.ascontiguousarray(f(inp["w_up"])[0])
    sh["w_down"] = np.ascontiguousarray(f(inp["w_down"])[0])
    rb = f(inp["rel_bias_b"])[0]
    ki = np.arange(128)[:, None]
    qi = np.arange(128)[None, :]
    idx0 = np.clip(128 + qi - ki, -128, 128) + 128
    idx1 = np.clip(qi - ki, -128, 128) + 128
    tab = np.stack([rb[:, idx0], rb[:, idx1]], axis=1)
    sh["tabB"] = np.ascontiguousarray(tab.transpose(2, 0, 1, 3))
    sh["constB"] = np.ascontiguousarray(np.broadcast_to(rb[:, 256][None, :], (128, 16)))
    sh["sinks"] = np.ascontiguousarray(np.broadcast_to(f(inp["sinks_a"])[0][None, :], (128, 16)))
    cst = np.zeros((128, 6, 128), np.float32)
    cst[:, 0, :] = np.eye(128, dtype=np.float32)
    cst[:64, 1, 64:] = NEG
    cst[64:, 2, :64] = NEG
    cst[:, 3, :] = 128 + qi - ki
    cst[:, 4, :] = np.abs(qi - ki)
    sh["cst"] = cst
    return sh


def prepare_core(inp, core):
    b, hf = core // 2, core % 2
    x = np.asarray(inp["x"], dtype=np.float32)
    xh = np.zeros((HALO + NB_TOK_CORE, D), np.float32)
    lo = hf * NB_TOK_CORE
    xh[HALO:] = x[b, lo:lo + NB_TOK_CORE]
    if hf > 0:
        xh[:HALO] = x[b, lo - HALO:lo]
    d = {"xh": xh, "c_col": _col(np.asarray(inp["c"], np.float32)[b]),
         "hm": np.full((128, 1), 1.0 if hf > 0 else 0.0, np.float32)}
    return d


_NC_CACHE = {}


def kernel(**inputs):
    if "nc" not in _NC_CACHE:
        _NC_CACHE["nc"] = build_program()
    nc = _NC_CACHE["nc"]
    sh = prepare_shared(inputs)
    in_maps = []
    for core in range(8):
        d = dict(sh)
        d.update(prepare_core(inputs, core))
        in_maps.append(d)
    res = run_bass_kernel_spmd(nc, in_maps, core_ids=list(range(8)))
    B, S_, _ = np.asarray(inputs["x"]).shape
    out = np.empty((B, S_, D), np.float32)
    for core in range(8):
        b, hf = core // 2, core % 2
        out[b, hf * NB_TOK_CORE:(hf + 1) * NB_TOK_CORE] = res.results[core]["y"]
    return out
```

```python
import types
import numpy as np
from contextlib import ExitStack
import concourse.bass as bass
import concourse.mybir as mybir
from concourse.bass_utils import run_bass_kernel_spmd

F32 = mybir.dt.float32
BF16 = mybir.dt.bfloat16
AF = mybir.ActivationFunctionType
ALU = mybir.AluOpType
AX = mybir.AxisListType

ENGS = ("pe", "act", "dve", "pool", "sp")
NEG = -30000.0
EPS = 1e-6


def _freeze(fn):
    if fn.__closure__ is None:
        return fn
    cells = []
    for c in fn.__closure__:
        try:
            cells.append(types.CellType(c.cell_contents))
        except ValueError:
            cells.append(c)
    return types.FunctionType(fn.__code__, fn.__globals__, fn.__name__, fn.__defaults__, tuple(cells))


class Sched:
    def __init__(self):
        self.ops = []
        self.last_w = {}
        self.readers = {}
        self.dma_cnt = {}
        self.dma_last = {}
        self.eng_ops = {e: [] for e in ENGS}
        self.fence_set = set()
        self.fence_passed = {e: True for e in ENGS}

    def fence(self):
        s = set()
        for e in ENGS:
            if self.eng_ops[e]:
                s.add(self.eng_ops[e][-1])
        for k, i in self.dma_last.items():
            s.add(i)
        self.fence_set = s
        self.fence_passed = {e: False for e in ENGS}

    def add(self, eng, fn, reads=(), writes=(), dma=None):
        i = len(self.ops)
        deps = set()
        for r in reads:
            w = self.last_w.get(r)
            if w is not None:
                deps.add(w)
        for r in writes:
            w = self.last_w.get(r)
            if w is not None:
                deps.add(w)
            for rd in self.readers.get(r, ()):
                deps.add(rd)
        if not self.fence_passed[eng]:
            deps |= self.fence_set
            self.fence_passed[eng] = True
        if dma is not None and eng == "pool" and dma in self.dma_last:
            deps.add(self.dma_last[dma])
        op = dict(eng=eng, fn=_freeze(fn), deps=deps, dma=dma, idx=i)
        if dma is not None:
            self.dma_cnt[dma] = self.dma_cnt.get(dma, 0) + 16
            op["cnt"] = self.dma_cnt[dma]
            self.dma_last[dma] = i
        self.eng_ops[eng].append(i)
        op["ord"] = len(self.eng_ops[eng])
        self.ops.append(op)
        for r in reads:
            self.readers.setdefault(r, []).append(i)
        for r in writes:
            self.last_w[r] = i
            self.readers[r] = []
        return i

    def plan(self):
        ops = self.ops
        clock = {e: {} for e in ENGS}
        opclock = [None] * len(ops)
        waits = [None] * len(ops)
        signal = [False] * len(ops)

        def key_of(j):
            o = ops[j]
            if o["dma"] is not None:
                return ("dma", o["dma"]), o["cnt"]
            return o["eng"], o["ord"]

        for i, o in enumerate(ops):
            e = o["eng"]
            ck = clock[e]
            w = []
            for j in sorted(o["deps"]):
                pj = ops[j]
                if pj["dma"] is None and pj["eng"] == e == "pe":
                    continue
                k, v = key_of(j)
                if ck.get(k, 0) >= v:
                    continue
                w.append(j)
                signal[j] = True
                ck[k] = v
                for kk, vv in opclock[j].items():
                    if ck.get(kk, 0) < vv:
                        ck[kk] = vv
            waits[i] = w
            oc = dict(ck)
            if o["dma"] is not None:
                oc[("dma", o["dma"])] = o["cnt"]
            else:
                oc[e] = max(oc.get(e, 0), o["ord"])
            opclock[i] = oc
        semval = [0] * len(ops)
        for e in ENGS:
            c = 0
            for i in self.eng_ops[e]:
                if ops[i]["dma"] is None and signal[i]:
                    c += 1
                    semval[i] = c
        self.waits, self.signal, self.semval = waits, signal, semval

    def emit(self, nc, es, final_waits=()):
        self.plan()
        ops, waits, signal, semval = self.ops, self.waits, self.signal, self.semval
        eng_sems = {e: es.enter_context(nc.semaphore(f"s_{e}")) for e in ENGS}
        dma_sems = {k: es.enter_context(nc.semaphore(f"d_{k}")) for k in self.dma_cnt}
        block = es.enter_context(nc.Block())
        handles = {"pe": "tensor", "act": "scalar", "dve": "vector",
                   "pool": "gpsimd", "sp": "sync"}

        def run_engine(e, h):
            for i in self.eng_ops[e]:
                o = ops[i]
                for j in waits[i]:
                    pj = ops[j]
                    if pj["dma"] is not None:
                        h.wait_ge(dma_sems[pj["dma"]], pj["cnt"])
                    else:
                        h.wait_ge(eng_sems[pj["eng"]], semval[j])
                ins = o["fn"](h)
                if o["dma"] is not None:
                    ins.then_inc(dma_sems[o["dma"]], 16)
                elif signal[i]:
                    ins.then_inc(eng_sems[e], 1)
            if e == "sp":
                for k in final_waits:
                    h.wait_ge(dma_sems[k], self.dma_cnt[k])

        for e in ENGS:
            if not self.eng_ops[e] and not (e == "sp" and final_waits):
                continue
            getattr(block, handles[e])(lambda h, e=e: run_engine(e, h))


D = 2048
KC = 16
NB_TOK_CORE = 2048
HALF = 1024
HALO = 512
WIN = HALF + HALO
NWT = WIN // 128
NOT = HALF // 128
N_EXP = 32
DE = 512
NMODBLK = 32
NST = 47
I32 = mybir.dt.int32
SLOPES = [2.0 ** (-8.0 * (h + 1) / 16.0) for h in range(16)]
V_SH1, V_SC1, V_GT1, V_SH2, V_SC2, V_GT2, V_SHF, V_SCF = range(8)


def build_program(n_experts=N_EXP, dbg=None):
    nc = bass.Bass("TRN2", target_bir_lowering=False)
    S = Sched()

    def dram_in(name, shape, dt=F32):
        return nc.dram_tensor(name, list(shape), dt, kind="ExternalInput").ap()

    xh = dram_in("xh", [HALO + NB_TOK_CORE, D])
    c_col = dram_in("c_col", [128, KC])
    w_mod = dram_in("w_mod", [D, NMODBLK * 512])
    b_mod = dram_in("b_mod", [1, NMODBLK * 512])
    gcols = dram_in("gcols", [128, 3, KC])
    g_fin = dram_in("g_fin", [1, D])
    w_in_u = dram_in("w_in_u", [16, 128, KC, 384])
    w_out = dram_in("w_out", [D, D])
    w_rt = dram_in("w_rt", [D, 36])
    b_rt = dram_in("b_rt", [1, 36])
    w_gate = dram_in("w_gate", [N_EXP * 128, KC * DE])
    w_up = dram_in("w_up", [N_EXP * 128, KC * DE])
    w_down = dram_in("w_down", [N_EXP * 128, 4 * D])
    g_ffn_row = dram_in("g_ffn_row", [1, D])
    cst2_d = dram_in("cst2", [128, 192])
    tabB_d = dram_in("tabB", [128, 16, 2, 128])
    constB_d = dram_in("constB", [128, 16])
    sinks_d = dram_in("sinks", [128, 16])
    hm_d = dram_in("hm", [128, 1])
    cst_d = dram_in("cst", [128, 6, 128])
    y_out = nc.dram_tensor("y", [NB_TOK_CORE, D], F32, kind="ExternalOutput").ap()
    modrow = nc.dram_tensor("modrow", [1, NMODBLK * 512], F32, kind="Internal").ap()
    HS = nc.dram_tensor("HS", [NST * 128, D], BF16, kind="Internal").ap()
    YS = nc.dram_tensor("YS", [NST * 128, D], F32, kind="Internal").ap()
    dbg_out = {}
    if dbg:
        for k, shp in dbg.items():
            if k.startswith('_'):
                continue
            dbg_out[k] = nc.dram_tensor("dbg_" + k, list(shp), F32, kind="ExternalOutput").ap()

    es = ExitStack()
    ARENA_F32 = 51200
    arena = es.enter_context(nc.sbuf_tensor("arena", [128, ARENA_F32], F32))
    psum = es.enter_context(nc.psum_tensor("psum", [128, 4096], F32))

    def sview(off, dt, shape):
        n = int(np.prod(shape))
        nbytes = n * (2 if dt == BF16 else 4)
        assert off % 4 == 0 and nbytes % 4 == 0
        assert off + nbytes <= ARENA_F32 * 4, (off, nbytes)
        a = arena[:, off // 4:(off + nbytes) // 4]
        if dt != F32:
            a = a.bitcast(dt)
        if len(shape) >= 2:
            names = [f"d{i}" for i in range(len(shape))]
            pat = "p (" + " ".join(names) + ") -> p " + " ".join(names)
            a = a.rearrange(pat, **{n: int(v) for n, v in zip(names[:-1], shape[:-1])})
        return a

    class Alloc:
        def __init__(self, base, limit):
            self.base, self.off, self.limit = base, base, limit

        def reset(self):
            self.off = self.base

        def get(self, dt, shape):
            n = int(np.prod(shape)) * (2 if dt == BF16 else 4)
            n = (n + 63) // 64 * 64
            v = sview(self.off, dt, shape)
            self.off += n
            assert self.off <= self.limit, (self.off, self.limit)
            return v

    P_BYTES = 20480
    FT_BYTES = KC * WIN * 2
    OB_BYTES = 65536
    PA = Alloc(0, P_BYTES)
    FT = sview(P_BYTES, BF16, [KC, WIN])
    OB_OFF = P_BYTES + FT_BYTES
    O_sb = sview(OB_OFF, BF16, [NOT, D])
    X1 = sview(OB_OFF, F32, [NOT, D])
    SC = Alloc(OB_OFF + OB_BYTES, ARENA_F32 * 4)

    def bank(b, lo=0, hi=512):
        return psum[:, b * 512 + lo:b * 512 + hi]

    def bank_bf(b):
        return psum[:, b * 512:(b + 1) * 512].bitcast(BF16)

    colall = PA.get(F32, [NMODBLK * 4])
    gc = PA.get(F32, [3, KC])
    gm1 = PA.get(F32, [KC])
    gm2 = PA.get(F32, [KC])
    ident_f = PA.get(F32, [128])
    cst_b = PA.get(BF16, [3, 128])
    relA = PA.get(F32, [2, 128])
    maskA = PA.get(F32, [2, 128])
    tabB = PA.get(BF16, [16, 2, 128])
    constB = PA.get(F32, [16])
    esink = PA.get(F32, [16])
    hm = PA.get(F32, [1])
    brow = PA.get(F32, [36])
    wr = PA.get(BF16, [KC, 36])
    comb = PA.get(F32, [NOT, 32])
    rsP = PA.get(F32, [NOT])
    w1p = PA.get(F32, [NOT])
    w2p = PA.get(F32, [NOT])
    slot1_i = PA.get(I32, [NOT])
    slot2_i = PA.get(I32, [NOT])
    idxW = PA.get(I32, [NST])
    cst2 = PA.get(F32, [64])
    ones_b = PA.get(BF16, [128])
    tri_b = PA.get(BF16, [128])
    piota = cst2[:, 0:1]
    thr8 = cst2[:, 1:9]
    s128 = cst2[:, 9:9 + NST]
    stat = PA.get(F32, [64])
    ident_b = cst_b[:, 0, :]
    maskF_b = cst_b[:, 1, :]

    _bc_regs = {}

    def bc_reg(h, val):
        if val not in _bc_regs:
            r = h.alloc_register(f"bc{val}")
            h.reg_mov(r, val)
            _bc_regs[val] = r
        return _bc_regs[val]

    def bcast(ap_row):
        return ap_row.to_broadcast([128, ap_row.shape[1]])

    def dma(eng, out, in_, key, reads=(), writes=()):
        return S.add(eng, lambda h: h.dma_start(out=out, in_=in_), reads=reads, writes=writes, dma=key)

    SC.reset()
    cst_f = SC.get(F32, [6, 128])
    ccol = SC.get(F32, [KC])
    cact = SC.get(F32, [KC])
    carep = SC.get(BF16, [KC, 128])
    sink_f = SC.get(F32, [16])
    dma("sp", cst_f, cst_d, "c0", writes=["cst_f"])
    dma("pool", tabB, tabB_d, "c1", writes=["tabB"])
    dma("sp", ccol, c_col, "c2", writes=["ccol"])
    dma("sp", gc, gcols, "c3", writes=["gc"])
    dma("sp", constB, constB_d, "c4", writes=["constB"])
    dma("sp", sink_f, sinks_d, "c5", writes=["sink_f"])
    dma("sp", hm, hm_d, "c6", writes=["hm"])
    dma("sp", brow, bcast(b_rt), "c7", writes=["brow"])
    dma("pool", wr, w_rt.rearrange("(kc p) n -> p kc n", p=128), "c8", writes=["wr"])
    dma("pool", tri_b, cst2_d[:, 0:128], "c9", writes=["tri_b"])
    dma("sp", cst2[:, 0:56], cst2_d[:, 128:184], "c10", writes=["cst2"])
    S.add("pool", lambda h: h.memset(ones_b, 1.0), writes=["ones_b"])

    S.add("dve", lambda h: h.tensor_copy(out=ident_f, in_=cst_f[:, 0, :]), reads=["cst_f"], writes=["ident_f"])
    S.add("dve", lambda h: h.tensor_copy(out=cst_b, in_=cst_f[:, 0:3, :]), reads=["cst_f"], writes=["cst_b"])
    S.add("dve", lambda h: h.tensor_copy(out=relA, in_=cst_f[:, 3:5, :]), reads=["cst_f"], writes=["relA"])
    S.add("dve", lambda h: h.tensor_copy(out=maskA, in_=cst_f[:, 1:3, :]), reads=["cst_f"], writes=["maskA"])
    S.add("dve", lambda h: h.tensor_tensor(out=tabB[:, :, 1, :], in0=tabB[:, :, 1, :],
                                            in1=cst_b[:, 2, :].unsqueeze(1).to_broadcast([128, 16, 128]),
                                            op=ALU.add), reads=["cst_b", "tabB"], writes=["tabB"])
    S.add("act", lambda h: h.activation(out=esink, in_=sink_f, func=AF.Exp), reads=["sink_f"], writes=["esink"])
    S.add("act", lambda h: h.activation(out=cact, in_=ccol, func=AF.Silu), reads=["ccol"], writes=["cact"])
    S.add("dve", lambda h: h.tensor_copy(out=carep, in_=cact.unsqueeze(2).to_broadcast([128, KC, 128])),
          reads=["cact"], writes=["carep"])

    wblk = [SC.get(BF16, [KC, 512]) for _ in range(2)]
    rowt = [SC.get(F32, [512]) for _ in range(2)]
    biasb = [SC.get(F32, [512]) for _ in range(2)]
    diag = [SC.get(F32, [4, 128]) for _ in range(2)]
    w_mod_v = w_mod.rearrange("(kc p) n -> p kc n", p=128)
    for j in range(NMODBLK):
        wb = wblk[j % 2]
        rb_, bb, dg = rowt[j % 2], biasb[j % 2], diag[j % 2]
        acc = bank(4 + j % 2)
        dma("pool", wb, w_mod_v[:, :, j * 512:(j + 1) * 512], f"wm{j % 2}", writes=[f"wblk{j % 2}"])
        dma("sp", bb, bcast(b_mod[:, j * 512:(j + 1) * 512]), f"bm{j % 2}",
            writes=[f"biasb{j % 2}"])
        for kc in range(KC):
            S.add("pe", lambda h, kc=kc, wb=wb, acc=acc: h.matmul(acc, lhsT=carep[:, kc, :], rhs=wb[:, kc, :],
                                                                   start=(kc == 0), stop=(kc == KC - 1)),
                  reads=["carep", f"wblk{j % 2}"], writes=[f"acc{j % 2}"])
        S.add("dve", lambda h, rb_=rb_, bb=bb, acc=acc: h.tensor_tensor(out=rb_, in0=acc, in1=bb, op=ALU.add),
              reads=[f"acc{j % 2}", f"biasb{j % 2}"], writes=[f"rowt{j % 2}"])
        dma("sp", modrow[:, j * 512:(j + 1) * 512], rb_[0:1, :], f"mrow{j % 2}", reads=[f"rowt{j % 2}"], writes=[f"modrow{j % 2}"])
        S.add("dve", lambda h, rb_=rb_, dg=dg: h.tensor_tensor(
            out=dg, in0=rb_.rearrange("p (a b) -> p a b", a=4),
            in1=ident_f.unsqueeze(1).to_broadcast([128, 4, 128]), op=ALU.mult),
            reads=[f"rowt{j % 2}", "ident_f"], writes=[f"diag{j % 2}"])
        S.add("dve", lambda h, dg=dg, j=j: h.tensor_reduce(out=colall[:, j * 4:(j + 1) * 4], in_=dg,
                                                            axis=AX.X, op=ALU.add),
              reads=[f"diag{j % 2}"], writes=["colall"])

    def colv(v):
        return colall[:, v * KC:(v + 1) * KC]

    for (gm, gi, vs) in ((gm1, 0, V_SC1), (gm2, 1, V_SC2)):
        S.add("dve", lambda h, gm=gm, gi=gi, vs=vs: h.scalar_tensor_tensor(
            out=gm, in0=colv(vs), scalar=1.0, in1=gc[:, gi, :], op0=ALU.add, op1=ALU.mult),
            reads=["colall", "gc"], writes=["gm"])

    def dbg_dump(name, src_ap, dst_ap, reads):
        if dbg and name in dbg:
            dma("sp", dst_ap, src_ap, "dbg", reads=reads, writes=["dbgout"])

    if dbg and "colall" in dbg:
        dbg_dump("colall", colall, dbg_out["colall"], ["colall"])

    def rms_rstd(eng_h, ss, rstd, n, reads, writes):
        S.add("dve", lambda h: h.tensor_scalar(out=rstd, in0=ss, scalar1=1.0 / n, scalar2=EPS,
                                                op0=ALU.mult, op1=ALU.add), reads=reads, writes=writes)
        S.add("act", lambda h: h.activation(out=rstd, in_=rstd, func=AF.Ln), reads=writes, writes=writes)
        S.add("act", lambda h: h.activation(out=rstd, in_=rstd, func=AF.Exp, scale=-0.5), reads=writes, writes=writes)

    def transposes_to_FT(src_bf, src_res, lt, scale_col, bias_col, tag):
        for hb in range(2):
            pb = bank_bf(6 + hb)
            for k8 in range(8):
                kc = hb * 8 + k8
                S.add("pe", lambda h, pb=pb, k8=k8, kc=kc: h.transpose(
                    out=pb[:, k8 * 128:(k8 + 1) * 128], in_=src_bf[:, kc * 128:(kc + 1) * 128], identity=ident_b),
                    reads=[src_res, "cst_b"], writes=[f"tp{hb}"])
            for k8 in range(8):
                kc = hb * 8 + k8
                dst = FT[:, kc, lt * 128:(lt + 1) * 128]
                src = pb[:, k8 * 128:(k8 + 1) * 128]
                if k8 % 2 == 0:
                    if bias_col is not None:
                        S.add("act", lambda h, dst=dst, src=src, kc=kc: h.activation(
                            out=dst, in_=src, func=AF.Identity, scale=scale_col[:, kc:kc + 1],
                            bias=bias_col[:, kc:kc + 1]),
                            reads=[f"tp{hb}", "gm", "colall", "gc"], writes=[("FT", lt)])
                    else:
                        S.add("act", lambda h, dst=dst, src=src, kc=kc: h.activation(
                            out=dst, in_=src, func=AF.Copy, scale=scale_col[:, kc:kc + 1]),
                            reads=[f"tp{hb}", "gm", "colall", "gc"], writes=[("FT", lt)])
                else:
                    if bias_col is not None:
                        S.add("dve", lambda h, dst=dst, src=src, kc=kc: h.tensor_scalar(
                            out=dst, in0=src, scalar1=scale_col[:, kc:kc + 1], scalar2=bias_col[:, kc:kc + 1],
                            op0=ALU.mult, op1=ALU.add),
                            reads=[f"tp{hb}", "gm", "colall", "gc"], writes=[("FT", lt)])
                    else:
                        S.add("dve", lambda h, dst=dst, src=src, kc=kc: h.tensor_scalar(
                            out=dst, in0=src, scalar1=scale_col[:, kc:kc + 1], scalar2=None, op0=ALU.mult),
                            reads=[f"tp{hb}", "gm", "colall", "gc"], writes=[("FT", lt)])

    for H in range(2):
        row0 = H * HALF
        S.fence()
        SC.reset()
        xt = [SC.get(F32, [D]) for _ in range(2)]
        xnb = [SC.get(BF16, [D]) for _ in range(2)]
        junk = SC.get(BF16, [D])
        for lt in range(NWT):
            b2 = lt % 2
            dma("sp", xt[b2], xh[row0 + lt * 128: row0 + (lt + 1) * 128, :], f"xt{b2}", writes=[f"xt{b2}"])
            ss = stat[:, b2:b2 + 1]
            rs = stat[:, 2 + b2:3 + b2]
            S.add("act", lambda h, b2=b2, ss=ss: h.activation(out=junk, in_=xt[b2], func=AF.Square, accum_out=ss),
                  reads=[f"xt{b2}"], writes=["junk", f"ss{b2}"])
            rms_rstd(None, ss, rs, D, [f"ss{b2}"], [f"rs{b2}"])
            S.add("act", lambda h, b2=b2, rs=rs: h.activation(out=xnb[b2], in_=xt[b2], func=AF.Copy, scale=rs),
                  reads=[f"xt{b2}", f"rs{b2}"], writes=[f"xnb{b2}"])
            transposes_to_FT(xnb[b2], f"xnb{b2}", lt, gm1, colv(V_SH1), "h")
        if dbg and "hT" in dbg and H == dbg.get("_H", 1):
            tmpf = SC.get(F32, [KC, 128])
            for lt in range(NWT):
                S.add("dve", lambda h, lt=lt: h.tensor_copy(out=tmpf, in_=FT[:, :, lt * 128:(lt + 1) * 128]),
                      reads=[("FT", lt)], writes=["tmpf"])
                dma("sp", dbg_out["hT"][lt], tmpf, "dbg", reads=["tmpf"], writes=["dbgout"])

        S.fence()
        SC.reset()
        Wu = [SC.get(BF16, [KC, 384]) for _ in range(2)]
        QT = SC.get(BF16, [HALF])
        KT = SC.get(BF16, [WIN])
        VA = SC.get(BF16, [NWT, 2, 65])
        tabA = [SC.get(BF16, [2, 2, 2, 128]) for _ in range(2)]
        tA_f = SC.get(F32, [2, 128])
        tA_h = SC.get(F32, [2, 128])
        PT = [SC.get(BF16, [5, 128]) for _ in range(2)]
        rec = [SC.get(F32, [2]) for _ in range(2)]
        S.add("pool", lambda h: h.memset(VA[:, :, :, 64:65], 1.0), writes=["VA"])
        if H == 0:
            S.add("pool", lambda h: h.tensor_copy(out=VA[:, 0:4, :, 64:65],
                                                  in_=hm.unsqueeze(1).unsqueeze(1).to_broadcast([128, 4, 2, 1])),
                  reads=["hm"], writes=["VA"])
        for b2 in range(2):
            S.add("pool", lambda h, b2=b2: h.memset(PT[b2], 0.0), writes=[f"PT{b2}"])

        acc_i = [0]

        def next_acc():
            a = acc_i[0] % 2
            acc_i[0] += 1
            return a

        for u in range(16):
            grpA = u < 8
            wu = Wu[u % 2]
            dma("pool", wu, w_in_u[u], f"wu{u % 2}", writes=[f"wu{u % 2}"])
            wres = f"wu{u % 2}"
            for tb in range(HALF // 512):
                a = next_acc()
                for kc in range(KC):
                    S.add("pe", lambda h, a=a, kc=kc, tb=tb, wu=wu: h.matmul(
                        bank(4 + a), lhsT=wu[:, kc, 0:128], rhs=FT[:, kc, HALO + tb * 512: HALO + (tb + 1) * 512],
                        start=(kc == 0), stop=(kc == KC - 1)),
                        reads=[wres] + [("FT", 4 + tb * 4 + i) for i in range(4)], writes=[f"acc{a}"])
                S.add("dve", lambda h, a=a, tb=tb: h.tensor_scalar(
                    out=QT[:, tb * 512:(tb + 1) * 512], in0=bank(4 + a), scalar1=0.125, scalar2=None, op0=ALU.mult),
                    reads=[f"acc{a}"], writes=["QT"])
            do_kv = (not grpA) or (u % 2 == 0)
            if do_kv:
                for tb in range(WIN // 512):
                    a = next_acc()
                    for kc in range(KC):
                        S.add("pe", lambda h, a=a, kc=kc, tb=tb, wu=wu: h.matmul(
                            bank(4 + a), lhsT=wu[:, kc, 128:256], rhs=FT[:, kc, tb * 512:(tb + 1) * 512],
                            start=(kc == 0), stop=(kc == KC - 1)),
                            reads=[wres] + [("FT", tb * 4 + i) for i in range(4)], writes=[f"acc{a}"])
                    S.add("dve", lambda h, a=a, tb=tb: h.tensor_copy(out=KT[:, tb * 512:(tb + 1) * 512], in_=bank(4 + a)),
                          reads=[f"acc{a}"], writes=["KT"])
                for lt in range(NWT):
                    a = next_acc()
                    for kc in range(KC):
                        S.add("pe", lambda h, a=a, kc=kc, lt=lt, wu=wu: h.matmul(
                            bank(4 + a, 0, 128), lhsT=FT[:, kc, lt * 128:(lt + 1) * 128], rhs=wu[:, kc, 256:384],
                            start=(kc == 0), stop=(kc == KC - 1)),
                            reads=[wres, ("FT", lt)], writes=[f"acc{a}"])
                    src = bank(4 + a, 0, 128).rearrange("p (a b) -> p a b", a=2)
                    if H == 0 and lt < 4:
                        S.add("dve", lambda h, lt=lt, src=src: h.tensor_scalar(
                            out=VA[:, lt, :, 0:64], in0=src, scalar1=hm[:, 0:1], scalar2=None, op0=ALU.mult),
                            reads=[f"acc{a}", "hm"], writes=["VA"])
                    else:
                        S.add("dve", lambda h, lt=lt, src=src: h.tensor_copy(out=VA[:, lt, :, 0:64], in_=src),
                              reads=[f"acc{a}"], writes=["VA"])
            if grpA:
                tb_ = tabA[u % 2]
                for j in range(2):
                    hd = 2 * u + j
                    S.add("pool", lambda h, hd=hd: h.tensor_scalar(
                        out=tA_f, in0=relA, scalar1=-SLOPES[hd], scalar2=None, op0=ALU.mult),
                        reads=["relA", "tA_h"], writes=["tA_f"])
                    S.add("pool", lambda h: h.tensor_tensor(out=tA_f, in0=tA_f, in1=maskA, op=ALU.add),
                          reads=["tA_f", "maskA"], writes=["tA_f"])
                    S.add("pool", lambda h, j=j, tb_=tb_: h.tensor_copy(out=tb_[:, j, 0, :, :], in_=tA_f),
                          reads=["tA_f"], writes=[f"tabA{u % 2}"])
                    S.add("pool", lambda h, j=j, tb_=tb_: h.tensor_copy(out=tA_h, in_=tb_[:, j, 0, :, :]),
                          reads=[f"tabA{u % 2}"], writes=["tA_h"])
                    S.add("pool", lambda h: h.tensor_tensor(out=tA_h, in0=tA_f, in1=tA_h, op=ALU.subtract),
                          reads=["tA_f", "tA_h"], writes=["tA_h"])
                    S.add("pool", lambda h, j=j, tb_=tb_: h.tensor_copy(out=tb_[:, j, 1, :, :], in_=tA_h),
                          reads=["tA_h"], writes=[f"tabA{u % 2}"])
            items = [(qt, j) for qt in range(NOT) for j in range(2)]

            def emit_qk(n):
                qt, j = items[n]
                sb = n % 2
                hd = (2 * u + j) if grpA else (2 * (u - 8) + j)
                pl, ph = j * 64, (j + 1) * 64
                q_ap = QT[pl:ph, qt * 128:(qt + 1) * 128]
                s_main = bank(sb, 0, 384)
                s_last = bank(2 + sb, 0, 256)
                blocks = range(3, 5) if grpA else range(5)
                for i in blocks:
                    lt = qt + i
                    dst = s_main[:, i * 128:(i + 1) * 128] if i < 3 else s_last[:, (i - 3) * 128:(i - 2) * 128]
                    sres = f"S{sb}" if i < 3 else f"S2{sb}"
                    has_tab = grpA or i in (0, 3, 4)
                    S.add("pe", lambda h, dst=dst, lt=lt, pl=pl, ph=ph, q_ap=q_ap, has_tab=has_tab: h.matmul(
                        dst, lhsT=KT[pl:ph, lt * 128:(lt + 1) * 128], rhs=q_ap, start=True, stop=not has_tab),
                        reads=["KT", "QT"], writes=[sres])
                    if grpA:
                        tb_ = tabA[u % 2]
                        for hl in range(2):
                            S.add("pe", lambda h, dst=dst, j=j, hl=hl, i=i, tb_=tb_: h.matmul(
                                dst, lhsT=ident_b, rhs=tb_[:, j, hl, i - 3, :], start=False, stop=(hl == 1)),
                                reads=["cst_b", f"tabA{u % 2}"], writes=[sres])
                    elif i == 0:
                        S.add("pe", lambda h, dst=dst: h.matmul(dst, lhsT=ident_b, rhs=maskF_b, start=False, stop=True),
                              reads=["cst_b"], writes=[sres])
                    elif i >= 3:
                        S.add("pe", lambda h, dst=dst, hd=hd, i=i: h.matmul(
                            dst, lhsT=ident_b, rhs=tabB[:, hd, i - 3, :], start=False, stop=True),
                            reads=["cst_b", "tabB"], writes=[sres])
                pt = PT[sb]
                if not grpA:
                    S.add("act", lambda h, pt=pt, s_main=s_main, hd=hd: h.activation(
                        out=pt[:, 0:3, :], in_=s_main.rearrange("p (a b) -> p a b", a=3), func=AF.Exp,
                        bias=constB[:, hd:hd + 1]),
                        reads=[f"S{sb}", "constB"], writes=[f"PT{sb}"])
                S.add("act", lambda h, pt=pt, s_last=s_last: h.activation(
                    out=pt[:, 3:5, :], in_=s_last.rearrange("p (a b) -> p a b", a=2), func=AF.Exp),
                    reads=[f"S2{sb}"], writes=[f"PT{sb}"])

            def emit_pv(n):
                qt, j = items[n]
                sb = n % 2
                ob = qt % 2
                o_ps = bank(6 + ob, 0, 130).rearrange("p (a b) -> p a b", a=2)
                blocks = list(range(3, 5)) if grpA else list(range(5))
                for bi, i in enumerate(blocks):
                    lt = qt + i
                    S.add("pe", lambda h, i=i, lt=lt, j=j, sb=sb, o_ps=o_ps, bi=bi, nb_=len(blocks): h.matmul(
                        o_ps[:, j, :], lhsT=PT[sb][:, i, :], rhs=VA[:, lt, 0 if grpA else j, :],
                        start=(bi == 0), stop=(bi == nb_ - 1)),
                        reads=[f"PT{sb}", "VA"], writes=[f"OP{ob}"])
                if j == 1:
                    rc = rec[ob]
                    den = o_ps[:, :, 64:65]
                    if grpA:
                        S.add("dve", lambda h, rc=rc, den=den: h.tensor_tensor(
                            out=rc.unsqueeze(2), in0=den, in1=esink[:, 2 * u:2 * u + 2].unsqueeze(2), op=ALU.add),
                            reads=[f"OP{ob}", "esink"], writes=[f"rec{ob}"])
                        S.add("dve", lambda h, rc=rc: h.reciprocal(out=rc, in_=rc),
                              reads=[f"rec{ob}"], writes=[f"rec{ob}"])
                    else:
                        S.add("dve", lambda h, rc=rc, den=den: h.reciprocal(out=rc.unsqueeze(2), in_=den),
                              reads=[f"OP{ob}"], writes=[f"rec{ob}"])
                    S.add("dve", lambda h, rc=rc, o_ps=o_ps, qt=qt: h.tensor_tensor(
                        out=O_sb[:, qt, u * 128:(u + 1) * 128].rearrange("p (a b) -> p a b", a=2),
                        in0=o_ps[:, :, 0:64], in1=rc.unsqueeze(2).to_broadcast([128, 2, 64]), op=ALU.mult),
                        reads=[f"OP{ob}", f"rec{ob}"], writes=[("O", qt, u)])

            for n in range(len(items) + 1):
                if n < len(items):
                    emit_qk(n)
                if n >= 1:
                    emit_pv(n - 1)

        if dbg and "O" in dbg and H == dbg.get("_H", 1):
            tmpo = SC.get(F32, [D])
            for t in range(NOT):
                S.add("dve", lambda h, t=t: h.tensor_copy(out=tmpo, in_=O_sb[:, t, :]),
                      reads=[("O", t, u) for u in range(16)], writes=["tmpo"])
                dma("sp", dbg_out["O"][t], tmpo, "dbg", reads=["tmpo"], writes=["dbgout"])

        S.fence()
        SC.reset()
        junk = SC.get(BF16, [D])
        onb = [SC.get(BF16, [D]) for _ in range(2)]
        for t in range(NOT):
            b2 = t % 2
            ss2 = stat[:, 4 + 2 * b2: 6 + 2 * b2]
            rs2 = stat[:, 8 + 2 * b2: 10 + 2 * b2]
            ores = [("O", t, u) for u in range(16)]
            for g in range(2):
                S.add("act", lambda h, t=t, g=g, ss2=ss2: h.activation(
                    out=junk[:, 0:1024], in_=O_sb[:, t, g * 1024:(g + 1) * 1024], func=AF.Square,
                    accum_out=ss2[:, g:g + 1]), reads=ores, writes=["junk", f"ss2{b2}"])
            rms_rstd(None, ss2, rs2, 1024, [f"ss2{b2}"], [f"rs2{b2}"])
            for g in range(2):
                S.add("act", lambda h, t=t, g=g, rs2=rs2, b2=b2: h.activation(
                    out=onb[b2][:, g * 1024:(g + 1) * 1024], in_=O_sb[:, t, g * 1024:(g + 1) * 1024],
                    func=AF.Copy, scale=rs2[:, g:g + 1]), reads=ores + [f"rs2{b2}"], writes=[f"onb{b2}"])
            transposes_to_FT(onb[b2], f"onb{b2}", 4 + t, gc[:, 2, :], None, "o")

        S.fence()
        SC.reset()
        Wo = [SC.get(BF16, [KC, 512]) for _ in range(2)]
        gt1row = SC.get(F32, [D])
        tmpx = [SC.get(F32, [512]) for _ in range(2)]
        dma("sp", gt1row, bcast(modrow[:, V_GT1 * D:(V_GT1 + 1) * D]), "gt1r",
            reads=["modrow0", "modrow1"], writes=["gt1row"])
        for t in range(NOT):
            r = HALO + row0 + t * 128
            dma("sp", X1[:, t, :], xh[r:r + 128, :], f"x1_{t}", writes=[("X1", t, nb) for nb in range(4)])
        w_out_v = w_out.rearrange("(kc p) n -> p kc n", p=128)
        for nb in range(4):
            wo = Wo[nb % 2]
            dma("pool", wo, w_out_v[:, :, nb * 512:(nb + 1) * 512], f"wo{nb % 2}", writes=[f"wo{nb % 2}"])
            for t in range(NOT):
                a = next_acc()
                for kc in range(KC):
                    S.add("pe", lambda h, a=a, kc=kc, t=t, wo=wo: h.matmul(
                        bank(4 + a), lhsT=FT[:, kc, (4 + t) * 128:(5 + t) * 128], rhs=wo[:, kc, :],
                        start=(kc == 0), stop=(kc == KC - 1)),
                        reads=[f"wo{nb % 2}", ("FT", 4 + t)], writes=[f"acc{a}"])
                tx = tmpx[a]
                S.add("dve", lambda h, a=a, tx=tx, nb=nb: h.tensor_tensor(
                    out=tx, in0=bank(4 + a), in1=gt1row[:, nb * 512:(nb + 1) * 512], op=ALU.mult),
                    reads=[f"acc{a}", "gt1row"], writes=[f"tmpx{a}"])
                S.add("dve", lambda h, tx=tx, t=t, nb=nb: h.tensor_tensor(
                    out=X1[:, t, nb * 512:(nb + 1) * 512], in0=X1[:, t, nb * 512:(nb + 1) * 512], in1=tx, op=ALU.add),
                    reads=[f"tmpx{a}", ("X1", t, nb)], writes=[("X1", t, nb)])
        if dbg and "X1" in dbg and H == dbg.get("_H", 1):
            for t in range(NOT):
                dma("sp", dbg_out["X1"][t], X1[:, t, :], "dbg", reads=[("X1", t, nb) for nb in range(4)],
                    writes=["dbgout"])

        S.fence()
        SC.reset()
        junk = SC.get(BF16, [D])
        xnb = [SC.get(BF16, [D]) for _ in range(2)]
        L = SC.get(F32, [NOT, 36])
        for t in range(NOT):
            b2 = t % 2
            xres = [("X1", t, nb) for nb in range(4)]
            ss = stat[:, b2:b2 + 1]
            rs = rsP[:, t:t + 1]
            S.add("act", lambda h, t=t, ss=ss: h.activation(out=junk, in_=X1[:, t, :], func=AF.Square, accum_out=ss),
                  reads=xres, writes=["junk", f"ss{b2}"])
            rms_rstd(None, ss, rs, D, [f"ss{b2}"], [f"rsP{t}"])
            S.add("act", lambda h, t=t, rs=rs, b2=b2: h.activation(out=xnb[b2], in_=X1[:, t, :], func=AF.Copy, scale=rs),
                  reads=xres + [f"rsP{t}"], writes=[f"xnb{b2}"])
            transposes_to_FT(xnb[b2], f"xnb{b2}", 4 + t, gm2, colv(V_SH2), "h2")
            a = next_acc()
            for kc in range(KC):
                S.add("pe", lambda h, a=a, kc=kc, t=t: h.matmul(
                    bank(4 + a, 0, 36), lhsT=FT[:, kc, (4 + t) * 128:(5 + t) * 128], rhs=wr[:, kc, :],
                    start=(kc == 0), stop=(kc == KC - 1)),
                    reads=["wr", ("FT", 4 + t)], writes=[f"acc{a}"])
            S.add("dve", lambda h, a=a, t=t: h.tensor_tensor(out=L[:, t, :], in0=bank(4 + a, 0, 36), in1=brow, op=ALU.add),
                  reads=[f"acc{a}", "brow"], writes=["L"])
        gl = L[:, :, 0:4]
        el = L[:, :, 4:36]
        gmax = SC.get(F32, [NOT]); gsum = SC.get(F32, [NOT]); pg = SC.get(F32, [NOT])
        gt_ = SC.get(F32, [NOT, 4]); ohg = SC.get(F32, [NOT, 4]); pen = SC.get(F32, [NOT, 4])
        elm = SC.get(F32, [NOT, 32]); oh1 = SC.get(F32, [NOT, 32]); oh2 = SC.get(F32, [NOT, 32])
        elm2 = SC.get(F32, [NOT, 32])
        m1 = SC.get(F32, [NOT]); m2 = SC.get(F32, [NOT]); e2 = SC.get(F32, [NOT]); w1 = SC.get(F32, [NOT])
        w2 = SC.get(F32, [NOT])
        RR = ["L", "rt"]

        def dv(fn):
            S.add("dve", fn, reads=RR, writes=["rt"])

        dv(lambda h: h.tensor_reduce(out=gmax, in_=gl, axis=AX.X, op=ALU.max))
        dv(lambda h: h.tensor_tensor(out=gt_, in0=gl, in1=gmax.unsqueeze(2).to_broadcast([128, NOT, 4]), op=ALU.subtract))
        S.add("act", lambda h: h.activation(out=gt_, in_=gt_, func=AF.Exp), reads=RR, writes=["rt"])
        dv(lambda h: h.tensor_reduce(out=gsum, in_=gt_, axis=AX.X, op=ALU.add))
        dv(lambda h: h.reciprocal(out=pg, in_=gsum))
        dv(lambda h: h.tensor_tensor(out=ohg, in0=gl, in1=gmax.unsqueeze(2).to_broadcast([128, NOT, 4]), op=ALU.is_ge))
        dv(lambda h: h.tensor_scalar(out=pen, in0=ohg, scalar1=-1.0, scalar2=1e9, op0=ALU.add, op1=ALU.mult))
        dv(lambda h: h.tensor_tensor(
            out=elm.rearrange("p t (g j) -> p t g j", g=4), in0=el.rearrange("p t (g j) -> p t g j", g=4),
            in1=pen.unsqueeze(3).to_broadcast([128, NOT, 4, 8]), op=ALU.add))
        dv(lambda h: h.tensor_reduce(out=m1, in_=elm, axis=AX.X, op=ALU.max))
        dv(lambda h: h.tensor_tensor(out=oh1, in0=elm, in1=m1.unsqueeze(2).to_broadcast([128, NOT, 32]), op=ALU.is_ge))
        dv(lambda h: h.scalar_tensor_tensor(out=elm2, in0=oh1, scalar=-1e9, in1=elm, op0=ALU.mult, op1=ALU.add))
        dv(lambda h: h.tensor_reduce(out=m2, in_=elm2, axis=AX.X, op=ALU.max))
        dv(lambda h: h.tensor_tensor(out=oh2, in0=elm2, in1=m2.unsqueeze(2).to_broadcast([128, NOT, 32]), op=ALU.is_ge))
        dv(lambda h: h.tensor_tensor(out=e2, in0=m2, in1=m1, op=ALU.subtract))
        S.add("act", lambda h: h.activation(out=e2, in_=e2, func=AF.Exp), reads=RR, writes=["rt"])
        dv(lambda h: h.tensor_scalar(out=w1, in0=e2, scalar1=1.0, scalar2=None, op0=ALU.add))
        dv(lambda h: h.reciprocal(out=w1, in_=w1))
        dv(lambda h: h.tensor_tensor(out=w1, in0=w1, in1=pg, op=ALU.mult))
        dv(lambda h: h.tensor_tensor(out=w2, in0=w1, in1=e2, op=ALU.mult))
        dv(lambda h: h.tensor_tensor(out=elm, in0=oh1, in1=w1.unsqueeze(2).to_broadcast([128, NOT, 32]), op=ALU.mult))
        dv(lambda h: h.tensor_tensor(out=elm2, in0=oh2, in1=w2.unsqueeze(2).to_broadcast([128, NOT, 32]), op=ALU.mult))
        S.add("dve", lambda h: h.tensor_tensor(out=comb, in0=elm, in1=elm2, op=ALU.add), reads=RR, writes=["comb"])
        if dbg and "comb" in dbg and H == dbg.get("_H", 1):
            dma("sp", dbg_out["comb"], comb, "dbg", reads=["comb"], writes=["dbgout"])
        S.add("dve", lambda h: h.tensor_copy(out=w1p, in_=w1), reads=RR, writes=["w1p"])
        S.add("dve", lambda h: h.tensor_copy(out=w2p, in_=w2), reads=RR, writes=["w2p"])

        ohA = SC.get(F32, [NOT, 32]); ohAb = SC.get(BF16, [NOT, 32])
        cntS = SC.get(F32, [32]); rk = SC.get(F32, [NOT, 32])
        cmp8 = SC.get(F32, [32, 8]); nt_ = SC.get(F32, [32]); padded = SC.get(F32, [32])
        cs = [SC.get(F32, [32]) for _ in range(2)]
        off = SC.get(F32, [32]); sbt = SC.get(F32, [NOT, 32]); tmp3 = SC.get(F32, [NOT, 32])
        s1f = SC.get(F32, [NOT]); s2f = SC.get(F32, [NOT])
        cmpS = SC.get(F32, [NST, 32]); eS = SC.get(F32, [NST]); idxf = SC.get(F32, [NST])
        dv(lambda h: h.tensor_tensor(out=ohA, in0=oh1, in1=oh2, op=ALU.add))
        dv(lambda h: h.tensor_copy(out=ohAb, in_=ohA))
        CNT = bank(4, 0, 32)
        RK = bank(5, 0, NOT * 32).rearrange("p (t e) -> p t e", t=NOT)
        for t in range(NOT):
            S.add("pe", lambda h, t=t: h.matmul(CNT, lhsT=ones_b, rhs=ohAb[:, t, :], start=(t == 0), stop=(t == NOT - 1)),
                  reads=["rt", "ones_b"], writes=["acc0"])
        for t in range(NOT):
            for tp in range(t):
                S.add("pe", lambda h, t=t, tp=tp: h.matmul(RK[:, t, :], lhsT=ones_b, rhs=ohAb[:, tp, :],
                                                           start=(tp == 0), stop=False),
                      reads=["rt", "ones_b"], writes=["acc1"])
            S.add("pe", lambda h, t=t: h.matmul(RK[:, t, :], lhsT=tri_b, rhs=ohAb[:, t, :], start=(t == 0), stop=True),
                  reads=["rt", "tri_b"], writes=["acc1"])
        S.add("dve", lambda h: h.tensor_copy(out=cntS, in_=CNT), reads=["acc0"], writes=["rt"])
        S.add("dve", lambda h: h.tensor_copy(out=rk, in_=RK), reads=["acc1"], writes=["rt"])
        RR2 = ["rt", "cst2"]

        def dv2(fn):
            S.add("dve", fn, reads=RR2, writes=["rt"])

        dv2(lambda h: h.tensor_tensor(out=cmp8, in0=cntS.unsqueeze(2).to_broadcast([128, 32, 8]),
                                      in1=thr8.unsqueeze(1).to_broadcast([128, 32, 8]), op=ALU.is_gt))
        dv2(lambda h: h.tensor_reduce(out=nt_, in_=cmp8, axis=AX.X, op=ALU.add))
        dv2(lambda h: h.tensor_scalar(out=padded, in0=nt_, scalar1=128.0, scalar2=None, op0=ALU.mult))
        dv2(lambda h: h.tensor_copy(out=cs[0], in_=padded))
        cur = 0
        for sh in (1, 2, 4, 8, 16):
            a_, b_ = cs[cur], cs[1 - cur]
            dv2(lambda h, a_=a_, b_=b_: h.tensor_copy(out=b_, in_=a_))
            dv2(lambda h, a_=a_, b_=b_, sh=sh: h.tensor_tensor(out=b_[:, sh:32], in0=a_[:, sh:32], in1=a_[:, 0:32 - sh], op=ALU.add))
            cur = 1 - cur
        incl = cs[cur]
        dv2(lambda h: h.tensor_tensor(out=off, in0=incl, in1=padded, op=ALU.subtract))
        dv2(lambda h: h.tensor_tensor(out=sbt, in0=rk, in1=off.unsqueeze(1).to_broadcast([128, NOT, 32]), op=ALU.add))
        dv2(lambda h: h.tensor_tensor(out=tmp3, in0=sbt, in1=oh1, op=ALU.mult))
        dv2(lambda h: h.tensor_reduce(out=s1f, in_=tmp3, axis=AX.X, op=ALU.add))
        dv2(lambda h: h.tensor_tensor(out=tmp3, in0=sbt, in1=oh2, op=ALU.mult))
        dv2(lambda h: h.tensor_reduce(out=s2f, in_=tmp3, axis=AX.X, op=ALU.add))
        S.add("dve", lambda h: h.tensor_copy(out=slot1_i, in_=s1f), reads=RR2, writes=["slot1"])
        S.add("dve", lambda h: h.tensor_copy(out=slot2_i, in_=s2f), reads=RR2, writes=["slot2"])
        dv2(lambda h: h.tensor_tensor(out=cmpS, in0=incl.unsqueeze(1).to_broadcast([128, NST, 32]),
                                      in1=s128.unsqueeze(2).to_broadcast([128, NST, 32]), op=ALU.is_le))
        dv2(lambda h: h.tensor_reduce(out=eS, in_=cmpS, axis=AX.X, op=ALU.add))
        dv2(lambda h: h.tensor_scalar(out=idxf, in0=eS, scalar1=128.0, scalar2=piota, op0=ALU.mult, op1=ALU.add))
        S.add("dve", lambda h: h.tensor_copy(out=idxW, in_=idxf), reads=RR2, writes=["idxW"])
        if dbg and "slots" in dbg and H == dbg.get("_H", 1):
            dma("sp", dbg_out["slots"][:, 0:NOT], s1f, "dbg", reads=["rt"], writes=["dbgout"])
            dma("sp", dbg_out["slots"][:, NOT:2 * NOT], s2f, "dbg", reads=["rt"], writes=["dbgout"])
            dma("sp", dbg_out["slots"][:, 2 * NOT:2 * NOT + NST], idxf, "dbg", reads=["rt"], writes=["dbgout"])

        gm2row = SC.get(F32, [D]); sh2row = SC.get(F32, [D])
        h2f = SC.get(F32, [D])
        gfr = h2f
        h2tok = xnb
        zt = junk
        S.add("pool", lambda h: h.memset(zt, 0.0), reads=["junk"], writes=["junk"])
        for st in range(NST):
            dma("sp", HS[st * 128:(st + 1) * 128, :], zt, "hsz", reads=["junk"], writes=["HS"])
        dma("sp", gm2row, bcast(modrow[:, V_SC2 * D:(V_SC2 + 1) * D]), "r0", reads=["modrow0", "modrow1"], writes=["gm2row"])
        dma("sp", sh2row, bcast(modrow[:, V_SH2 * D:(V_SH2 + 1) * D]), "r1", reads=["modrow0", "modrow1"], writes=["sh2row"])
        dma("sp", gfr, bcast(g_ffn_row), "r2", writes=["h2f"])
        S.add("dve", lambda h: h.scalar_tensor_tensor(out=gm2row, in0=gm2row, scalar=1.0, in1=gfr, op0=ALU.add, op1=ALU.mult),
              reads=["gm2row", "h2f"], writes=["gm2row"])
        for t in range(NOT):
            b2 = t % 2
            xres = [("X1", t, nb) for nb in range(4)]
            S.add("dve", lambda h, t=t: h.scalar_tensor_tensor(out=h2f, in0=X1[:, t, :], scalar=rsP[:, t:t + 1], in1=gm2row,
                                                               op0=ALU.mult, op1=ALU.mult),
                  reads=xres + [f"rsP{t}", "gm2row"], writes=["h2f"])
            S.add("dve", lambda h, b2=b2: h.tensor_tensor(out=h2tok[b2], in0=h2f, in1=sh2row, op=ALU.add),
                  reads=["h2f", "sh2row"], writes=[f"xnb{b2}"])
            for k_, sl in enumerate((slot1_i, slot2_i)):
                S.add("pool", lambda h, sl=sl, t=t, b2=b2: h.indirect_dma_start(
                    out=HS, out_offset=bass.IndirectOffsetOnAxis(ap=sl[:, t:t + 1], axis=0),
                    in_=h2tok[b2], in_offset=None, bounds_check=bc_reg(h, NST * 128 - 1), oob_is_err=False),
                    reads=[f"xnb{b2}", "slot1", "slot2", "HS"], writes=[("HSs", t, k_)], dma=f"hss{b2}{k_}")

        S.fence()
        SC.reset()
        ring = [sview(P_BYTES + r * 16384, BF16, [8192]) for r in range(3)] + [SC.get(BF16, [8192]) for _ in range(2)]
        NR_ = len(ring)
        hsb = [SC.get(BF16, [D]) for _ in range(2)]
        hsT = [SC.get(BF16, [KC, 128]) for _ in range(2)]
        aT = [SC.get(BF16, [4, 128]) for _ in range(2)]
        sg = [SC.get(F32, [128]) for _ in range(2)]
        ysb = [SC.get(F32, [D]) for _ in range(2)]
        ring_i = [0]

        def ring_next():
            r = ring_i[0] % NR_
            ring_i[0] += 1
            return r

        gu_i = [0]
        y_i = [0]
        n_tiles = dbg.get("_ntiles", NST) if dbg else NST
        for st in range(n_tiles):
            b2 = st % 2
            rg, ru, rd = ring_next(), ring_next(), ring_next()
            for (r_, wsrc) in ((rg, w_gate), (ru, w_up), (rd, w_down)):
                S.add("pool", lambda h, r_=r_, wsrc=wsrc, st=st: h.indirect_dma_start(
                    out=ring[r_], out_offset=None, in_=wsrc,
                    in_offset=bass.IndirectOffsetOnAxis(ap=idxW[:, st:st + 1], axis=0),
                    bounds_check=bc_reg(h, N_EXP * 128 - 1), oob_is_err=False),
                    reads=["idxW"], writes=[f"ring{r_}"], dma=f"ring{r_}")
            Wg = ring[rg].rearrange("p (a b) -> p a b", a=KC)
            Wup = ring[ru].rearrange("p (a b) -> p a b", a=KC)
            Wd = ring[rd].rearrange("p (a b) -> p a b", a=4)
            dma("sp", hsb[b2], HS[st * 128:(st + 1) * 128, :], f"hsl{b2}",
                reads=["HS"] + [("HSs", t, k_) for t in range(NOT) for k_ in range(2)], writes=[f"hsb{b2}"])
            for hb in range(2):
                pb = bank_bf(6 + hb)
                for k8 in range(8):
                    kc = hb * 8 + k8
                    S.add("pe", lambda h, pb=pb, k8=k8, kc=kc, b2=b2: h.transpose(
                        out=pb[:, k8 * 128:(k8 + 1) * 128], in_=hsb[b2][:, kc * 128:(kc + 1) * 128], identity=ident_b),
                        reads=[f"hsb{b2}", "cst_b"], writes=[f"tp{hb}"])
                dst = hsT[b2][:, hb * 8:(hb + 1) * 8, :]
                src = pb.rearrange("p (a b) -> p a b", a=8)
                if hb == 0:
                    S.add("act", lambda h, dst=dst, src=src: h.activation(out=dst, in_=src, func=AF.Copy),
                          reads=[f"tp{hb}"], writes=[f"hsT{b2}"])
                else:
                    S.add("dve", lambda h, dst=dst, src=src: h.tensor_copy(out=dst, in_=src),
                          reads=[f"tp{hb}"], writes=[f"hsT{b2}"])
            for mb in range(4):
                gi = gu_i[0] % 2
                gu_i[0] += 1
                G, U = bank(gi, 0, 128), bank(2 + gi, 0, 128)
                for kc in range(KC):
                    S.add("pe", lambda h, kc=kc, G=G, Wg=Wg, mb=mb, b2=b2: h.matmul(
                        G, lhsT=Wg[:, kc, mb * 128:(mb + 1) * 128], rhs=hsT[b2][:, kc, :],
                        start=(kc == 0), stop=(kc == KC - 1)),
                        reads=[f"ring{rg}", f"hsT{b2}"], writes=[f"G{gi}"])
                for kc in range(KC):
                    S.add("pe", lambda h, kc=kc, U=U, Wup=Wup, mb=mb, b2=b2: h.matmul(
                        U, lhsT=Wup[:, kc, mb * 128:(mb + 1) * 128], rhs=hsT[b2][:, kc, :],
                        start=(kc == 0), stop=(kc == KC - 1)),
                        reads=[f"ring{ru}", f"hsT{b2}"], writes=[f"U{gi}"])
                S.add("act", lambda h, gi=gi, G=G: h.activation(out=sg[gi], in_=G, func=AF.Silu),
                      reads=[f"G{gi}"], writes=[f"sg{gi}"])
                S.add("dve", lambda h, gi=gi, U=U, mb=mb, b2=b2: h.tensor_tensor(
                    out=aT[b2][:, mb, :], in0=sg[gi], in1=U, op=ALU.mult),
                    reads=[f"sg{gi}", f"U{gi}"], writes=[f"aT{b2}"])
            for nb in range(4):
                yi = y_i[0] % 2
                y_i[0] += 1
                Y = bank(4 + yi)
                for kc in range(4):
                    S.add("pe", lambda h, kc=kc, Y=Y, Wd=Wd, nb=nb, b2=b2: h.matmul(
                        Y, lhsT=aT[b2][:, kc, :], rhs=Wd[:, kc, nb * 512:(nb + 1) * 512],
                        start=(kc == 0), stop=(kc == 3)),
                        reads=[f"ring{rd}", f"aT{b2}"], writes=[f"acc{yi}"])
                if nb % 2 == 0:
                    S.add("act", lambda h, Y=Y, nb=nb, b2=b2: h.activation(
                        out=ysb[b2][:, nb * 512:(nb + 1) * 512], in_=Y, func=AF.Copy),
                        reads=[f"acc{yi}"], writes=[f"ysb{b2}"])
                else:
                    S.add("dve", lambda h, Y=Y, nb=nb, b2=b2: h.tensor_copy(out=ysb[b2][:, nb * 512:(nb + 1) * 512], in_=Y),
                          reads=[f"acc{yi}"], writes=[f"ysb{b2}"])
            dma("sp", YS[st * 128:(st + 1) * 128, :], ysb[b2], f"yst{b2}", reads=[f"ysb{b2}"], writes=[("YS", st)])

        S.fence()
        SC.reset()
        junk = SC.get(BF16, [D])
        gfrow = SC.get(F32, [D])
        shfrow = SC.get(F32, [D])
        gt2row = SC.get(F32, [D])
        G1 = SC.get(F32, [D])
        G2 = SC.get(F32, [D])
        ot = [SC.get(F32, [D]) for _ in range(2)]
        scfrow = ot[1]
        dma("sp", gfrow, bcast(g_fin), "gfr", writes=["gfrow"])
        dma("sp", shfrow, bcast(modrow[:, V_SHF * D:(V_SHF + 1) * D]), "shfr",
            reads=["modrow0", "modrow1"], writes=["shfrow"])
        dma("sp", scfrow, bcast(modrow[:, V_SCF * D:(V_SCF + 1) * D]), "scfr",
            reads=["modrow0", "modrow1"], writes=["ot1"])
        dma("sp", gt2row, bcast(modrow[:, V_GT2 * D:(V_GT2 + 1) * D]), "gt2r",
            reads=["modrow0", "modrow1"], writes=["gt2row"])
        S.add("dve", lambda h: h.scalar_tensor_tensor(out=gfrow, in0=scfrow, scalar=1.0, in1=gfrow,
                                                       op0=ALU.add, op1=ALU.mult),
              reads=["gfrow", "ot1"], writes=["gfrow"])
        ys_all = [("YS", st) for st in range(n_tiles)]
        for t in range(NOT):
            b2 = t % 2
            xres = [("X1", t, nb) for nb in range(4)]
            for (Gb, sl, nm) in ((G1, slot1_i, "G1"), (G2, slot2_i, "G2")):
                S.add("pool", lambda h, Gb=Gb, sl=sl, t=t: h.indirect_dma_start(
                    out=Gb, out_offset=None, in_=YS,
                    in_offset=bass.IndirectOffsetOnAxis(ap=sl[:, t:t + 1], axis=0),
                    bounds_check=bc_reg(h, NST * 128 - 1), oob_is_err=False),
                    reads=ys_all + ["slot1", "slot2"], writes=[nm], dma=nm)
            S.add("dve", lambda h, t=t: h.tensor_scalar(out=G1, in0=G1, scalar1=w1p[:, t:t + 1], scalar2=None, op0=ALU.mult),
                  reads=["G1", "w1p"], writes=["G1"])
            S.add("dve", lambda h, t=t: h.scalar_tensor_tensor(out=G1, in0=G2, scalar=w2p[:, t:t + 1], in1=G1,
                                                               op0=ALU.mult, op1=ALU.add),
                  reads=["G1", "G2", "w2p"], writes=["G1"])
            S.add("pool", lambda h: h.tensor_tensor(out=G1, in0=G1, in1=gt2row, op=ALU.mult),
                  reads=["G1", "gt2row"], writes=["G1"])
            S.add("dve", lambda h, t=t: h.tensor_tensor(out=X1[:, t, :], in0=X1[:, t, :], in1=G1, op=ALU.add),
                  reads=["G1"] + xres, writes=xres)
            ss = stat[:, b2:b2 + 1]
            rs = stat[:, 2 + b2:3 + b2]
            S.add("act", lambda h, t=t, ss=ss: h.activation(out=junk, in_=X1[:, t, :], func=AF.Square, accum_out=ss),
                  reads=xres, writes=["junk", f"ss{b2}"])
            rms_rstd(None, ss, rs, D, [f"ss{b2}"], [f"rs{b2}"])
            S.add("dve", lambda h, t=t, rs=rs, b2=b2: h.scalar_tensor_tensor(
                out=ot[b2], in0=X1[:, t, :], scalar=rs, in1=gfrow, op0=ALU.mult, op1=ALU.mult),
                reads=xres + [f"rs{b2}", "gfrow"], writes=[f"ot{b2}"])
            S.add("pool", lambda h, b2=b2: h.tensor_tensor(out=ot[b2], in0=ot[b2], in1=shfrow, op=ALU.add),
                  reads=[f"ot{b2}", "shfrow"], writes=[f"ot{b2}"])
            r = H * HALF + t * 128
            dma("sp", y_out[r:r + 128, :], ot[b2], f"yo{b2}", reads=[f"ot{b2}"], writes=["yout"])

    finals = ["yo0", "yo1"] + (["dbg"] if dbg and "dbg" in S.dma_cnt else [])
    S.emit(nc, es, final_waits=finals)
    es.close()
    return nc


def _col(v):
    return np.ascontiguousarray(np.asarray(v, np.float32).reshape(KC, 128).T)


def prepare_shared(inp):
    f = lambda a: np.asarray(a, dtype=np.float32)
    sh = {}
    sh["w_mod"] = np.ascontiguousarray(np.concatenate([f(inp["w_ada"])[0], f(inp["w_ada_final"])], axis=1))
    sh["b_mod"] = np.ascontiguousarray(np.concatenate([f(inp["b_ada"])[0], f(inp["b_ada_final"])])[None, :])
    g_out = np.concatenate([f(inp["g_out_a"])[0], f(inp["g_out_b"])[0]])
    sh["gcols"] = np.ascontiguousarray(np.stack([_col(f(inp["g_mix"])[0]), _col(f(inp["g_ffn"])[0]), _col(g_out)], axis=1))
    sh["g_fin"] = f(inp["g_final"])[None, :].copy()
    w_in = f(inp["w_in"])[0]
    units = []
    for a in range(8):
        g = a // 2
        q = w_in[:, a * 128:(a + 1) * 128]
        k = w_in[:, 1024 + g * 64:1024 + (g + 1) * 64]
        v = w_in[:, 1280 + g * 64:1280 + (g + 1) * 64]
        units.append(np.concatenate([q, k, k, v, v], axis=1))
    for bp in range(8):
        units.append(np.concatenate([w_in[:, 1536 + bp * 128:1536 + (bp + 1) * 128],
                                     w_in[:, 2560 + bp * 128:2560 + (bp + 1) * 128],
                                     w_in[:, 3584 + bp * 128:3584 + (bp + 1) * 128]], axis=1))
    wu = np.stack(units)
    sh["w_in_u"] = np.ascontiguousarray(wu.reshape(16, KC, 128, 384).transpose(0, 2, 1, 3))
    sh["w_out"] = np.ascontiguousarray(f(inp["w_out"])[0])
    sh["w_rt"] = np.ascontiguousarray(np.concatenate([f(inp["w_router_group"])[0], f(inp["w_router_expert"])[0]], axis=1))
    sh["b_rt"] = np.concatenate([f(inp["b_router_group"])[0], f(inp["b_router_expert"])[0]])[None, :].copy()
    sh["w_gate"] = np.ascontiguousarray(f(inp["w_gate"])[0].reshape(N_EXP, KC, 128, DE).transpose(0, 2, 1, 3)).reshape(N_EXP * 128, KC * DE)
    sh["w_up"] = np.ascontiguousarray(f(inp["w_up"])[0].reshape(N_EXP, KC, 128, DE).transpose(0, 2, 1, 3)).reshape(N_EXP * 128, KC * DE)
    sh["w_down"] = np.ascontiguousarray(f(inp["w_down"])[0].reshape(N_EXP, 4, 128, D).transpose(0, 2, 1, 3)).reshape(N_EXP * 128, 4 * D)
    sh["g_ffn_row"] = f(inp["g_ffn"])[0][None, :].copy()
    c2 = np.zeros((128, 192), np.float32)
    c2[:, 0:128] = (np.arange(128)[:, None] < np.arange(128)[None, :]).astype(np.float32)
    c2[:, 128] = np.arange(128)
    c2[:, 129:137] = 128.0 * np.arange(8)[None, :]
    c2[:, 137:137 + NST] = 128.0 * np.arange(NST)[None, :]
    sh["cst2"] = c2
    rb = f(inp["rel_bias_b"])[0]
    ki = np.arange(128)[:, None]
    qi = np.arange(128)[None, :]
    idx0 = np.clip(128 + qi - ki, -128, 128) + 128
    idx1 = np.clip(qi - ki, -128, 128) + 128
    tab = np.stack([rb[:, idx0], rb[:, idx1]], axis=1)
    sh["tabB"] = np.ascontiguousarray(tab.transpose(2, 0, 1, 3))
    sh["constB"] = np.ascontiguousarray(np.broadcast_to(rb[:, 256][None, :], (128, 16)))
    sh["sinks"] = np.ascontiguousarray(np.broadcast_to(f(inp["sinks_a"])[0][None, :], (128, 16)))
    cst = np.zeros((128, 6, 128), np.float32)
    cst[:, 0, :] = np.eye(128, dtype=np.float32)
    cst[:64, 1, 64:] = NEG
    cst[64:, 2, :64] = NEG
    cst[:, 3, :] = 128 + qi - ki
    cst[:, 4, :] = np.abs(qi - ki)
    sh["cst"] = cst
    return sh


def prepare_core(inp, core):
    b, hf = core // 2, core % 2
    x = np.asarray(inp["x"], dtype=np.float32)
    xh = np.zeros((HALO + NB_TOK_CORE, D), np.float32)
    lo = hf * NB_TOK_CORE
    xh[HALO:] = x[b, lo:lo + NB_TOK_CORE]
    if hf > 0:
        xh[:HALO] = x[b, lo - HALO:lo]
    d = {"xh": xh, "c_col": _col(np.asarray(inp["c"], np.float32)[b]),
         "hm": np.full((128, 1), 1.0 if hf > 0 else 0.0, np.float32)}
    return d


_NC_CACHE = {}


def kernel(**inputs):
    if "nc" not in _NC_CACHE:
        _NC_CACHE["nc"] = build_program()
    nc = _NC_CACHE["nc"]
    sh = prepare_shared(inputs)
    in_maps = []
    for core in range(8):
        d = dict(sh)
        d.update(prepare_core(inputs, core))
        in_maps.append(d)
    res = run_bass_kernel_spmd(nc, in_maps, core_ids=list(range(8)))
    B, S_, _ = np.asarray(inputs["x"]).shape
    out = np.empty((B, S_, D), np.float32)
    for core in range(8):
        b, hf = core // 2, core % 2
        out[b, hf * NB_TOK_CORE:(hf + 1) * NB_TOK_CORE] = res.results[core]["y"]
    return out
```

```python
import types
import numpy as np
from contextlib import ExitStack
import concourse.bass as bass
import concourse.mybir as mybir
from concourse.bass_utils import run_bass_kernel_spmd

F32 = mybir.dt.float32
BF16 = mybir.dt.bfloat16
AF = mybir.ActivationFunctionType
ALU = mybir.AluOpType
AX = mybir.AxisListType

ENGS = ("pe", "act", "dve", "pool", "sp")
NEG = -30000.0
EPS = 1e-6


def _freeze(fn):
    if fn.__closure__ is None:
        return fn
    cells = []
    for c in fn.__closure__:
        try:
            cells.append(types.CellType(c.cell_contents))
        except ValueError:
            cells.append(c)
    return types.FunctionType(fn.__code__, fn.__globals__, fn.__name__, fn.__defaults__, tuple(cells))


class Sched:
    def __init__(self):
        self.ops = []
        self.last_w = {}
        self.readers = {}
        self.dma_cnt = {}
        self.dma_last = {}
        self.eng_ops = {e: [] for e in ENGS}
        self.fence_set = set()
        self.fence_passed = {e: True for e in ENGS}
        self.profile_scopes = False
        self.profile_engs = ('pe',)
        self.phase = 'setup'

    def fence(self):
        s = set()
        for e in ENGS:
            if self.eng_ops[e]:
                s.add(self.eng_ops[e][-1])
        for k, i in self.dma_last.items():
            s.add(i)
        self.fence_set = s
        self.fence_passed = {e: False for e in ENGS}

    def add(self, eng, fn, reads=(), writes=(), dma=None):
        i = len(self.ops)
        deps = set()
        for r in reads:
            w = self.last_w.get(r)
            if w is not None:
                deps.add(w)
        for r in writes:
            w = self.last_w.get(r)
            if w is not None:
                deps.add(w)
            for rd in self.readers.get(r, ()):
                deps.add(rd)
        if not self.fence_passed[eng]:
            deps |= self.fence_set
            self.fence_passed[eng] = True
        if dma is not None and eng == "pool" and dma in self.dma_last:
            deps.add(self.dma_last[dma])
        op = dict(eng=eng, fn=_freeze(fn), deps=deps, dma=dma, idx=i, phase=getattr(self, 'phase', None))
        if dma is not None:
            self.dma_cnt[dma] = self.dma_cnt.get(dma, 0) + 16
            op["cnt"] = self.dma_cnt[dma]
            self.dma_last[dma] = i
        self.eng_ops[eng].append(i)
        op["ord"] = len(self.eng_ops[eng])
        self.ops.append(op)
        for r in reads:
            self.readers.setdefault(r, []).append(i)
        for r in writes:
            self.last_w[r] = i
            self.readers[r] = []
        return i

    def plan(self):
        ops = self.ops
        clock = {e: {} for e in ENGS}
        opclock = [None] * len(ops)
        waits = [None] * len(ops)
        signal = [False] * len(ops)

        def key_of(j):
            o = ops[j]
            if o["dma"] is not None:
                return ("dma", o["dma"]), o["cnt"]
            return o["eng"], o["ord"]

        for i, o in enumerate(ops):
            e = o["eng"]
            ck = clock[e]
            w = []
            for j in sorted(o["deps"]):
                pj = ops[j]
                if pj["dma"] is None and pj["eng"] == e == "pe":
                    continue
                k, v = key_of(j)
                if ck.get(k, 0) >= v:
                    continue
                w.append(j)
                signal[j] = True
                ck[k] = v
                for kk, vv in opclock[j].items():
                    if ck.get(kk, 0) < vv:
                        ck[kk] = vv
            waits[i] = w
            oc = dict(ck)
            if o["dma"] is not None:
                oc[("dma", o["dma"])] = o["cnt"]
            else:
                oc[e] = max(oc.get(e, 0), o["ord"])
            opclock[i] = oc
        semval = [0] * len(ops)
        for e in ENGS:
            c = 0
            for i in self.eng_ops[e]:
                if ops[i]["dma"] is None and signal[i]:
                    c += 1
                    semval[i] = c
        self.waits, self.signal, self.semval = waits, signal, semval

    def emit(self, nc, es, final_waits=()):
        self.plan()
        ops, waits, signal, semval = self.ops, self.waits, self.signal, self.semval
        eng_sems = {e: es.enter_context(nc.semaphore(f"s_{e}")) for e in ENGS}
        dma_sems = {k: es.enter_context(nc.semaphore(f"d_{k}")) for k in self.dma_cnt}
        block = es.enter_context(nc.Block())
        handles = {"pe": "tensor", "act": "scalar", "dve": "vector",
                   "pool": "gpsimd", "sp": "sync"}

        def run_engine(e, h):
            cur_scope = [None, None]
            for i in self.eng_ops[e]:
                o = ops[i]
                if self.profile_scopes and e in self.profile_engs and o["phase"] != cur_scope[0]:
                    if cur_scope[1] is not None:
                        cur_scope[1].__exit__(None, None, None)
                    cur_scope[0] = o["phase"]
                    cur_scope[1] = nc.named_scope(str(o["phase"]))
                    cur_scope[1].__enter__()
                for j in waits[i]:
                    pj = ops[j]
                    if pj["dma"] is not None:
                        h.wait_ge(dma_sems[pj["dma"]], pj["cnt"])
                    else:
                        h.wait_ge(eng_sems[pj["eng"]], semval[j])
                ins = o["fn"](h)
                if o["dma"] is not None:
                    ins.then_inc(dma_sems[o["dma"]], 16)
                elif signal[i]:
                    ins.then_inc(eng_sems[e], 1)
            if cur_scope[1] is not None:
                cur_scope[1].__exit__(None, None, None)
            if e == "sp":
                for k in final_waits:
                    h.wait_ge(dma_sems[k], self.dma_cnt[k])

        for e in ENGS:
            if not self.eng_ops[e] and not (e == "sp" and final_waits):
                continue
            getattr(block, handles[e])(lambda h, e=e: run_engine(e, h))


D = 2048
KC = 16
NB_TOK_CORE = 2048
HALF = 1024
HALO = 512
WIN = HALF + HALO
NWT = WIN // 128
NOT = HALF // 128
N_EXP = 32
DE = 512
NMODBLK = 32
SUB = 2
TS = SUB * 128
NTT = 16
NST = (2 * NB_TOK_CORE + 32 * (TS - 1)) // TS
I32 = mybir.dt.int32
SLOPES = [2.0 ** (-8.0 * (h + 1) / 16.0) for h in range(16)]
V_SH1, V_SC1, V_GT1, V_SH2, V_SC2, V_GT2, V_SHF, V_SCF = range(8)


def build_program(n_experts=N_EXP, dbg=None, profile_scopes=False):
    nc = bass.Bass("TRN2", target_bir_lowering=False)
    S = Sched()
    S.profile_scopes = profile_scopes

    def dram_in(name, shape, dt=F32):
        return nc.dram_tensor(name, list(shape), dt, kind="ExternalInput").ap()

    xh = dram_in("xh", [HALO + NB_TOK_CORE, D])
    c_col = dram_in("c_col", [128, KC])
    w_mod = dram_in("w_mod", [D, NMODBLK * 512])
    b_mod = dram_in("b_mod", [1, NMODBLK * 512])
    gcols = dram_in("gcols", [128, 3, KC])
    g_fin = dram_in("g_fin", [1, D])
    w_in_u = dram_in("w_in_u", [16, 128, KC, 384])
    w_out = dram_in("w_out", [D, D])
    w_rt = dram_in("w_rt", [D, 36])
    b_rt = dram_in("b_rt", [1, 36])
    w_gate = dram_in("w_gate", [N_EXP * 128, KC * DE])
    w_up = dram_in("w_up", [N_EXP * 128, KC * DE])
    w_down = dram_in("w_down", [N_EXP * 128, 4 * D])
    g_ffn_row = dram_in("g_ffn_row", [1, D])
    cst2_d = dram_in("cst2", [128, 192])
    tabB_d = dram_in("tabB", [128, 16, 2, 128])
    constB_d = dram_in("constB", [128, 16])
    sinks_d = dram_in("sinks", [128, 16])
    hm_d = dram_in("hm", [128, 1])
    cst_d = dram_in("cst", [128, 6, 128])
    y_out = nc.dram_tensor("y", [NB_TOK_CORE, D], F32, kind="ExternalOutput").ap()
    modrow = nc.dram_tensor("modrow", [1, NMODBLK * 512], F32, kind="Internal").ap()
    HS = nc.dram_tensor("HS", [NST * TS, D], BF16, kind="Internal").ap()
    YS = nc.dram_tensor("YS", [NST * TS, D], F32, kind="Internal").ap()
    H2S = nc.dram_tensor("H2S", [NB_TOK_CORE, D], BF16, kind="Internal").ap()
    X1S = nc.dram_tensor("X1S", [NB_TOK_CORE, D], F32, kind="Internal").ap()
    dbg_out = {}
    if dbg:
        for k, shp in dbg.items():
            if k.startswith('_'):
                continue
            dbg_out[k] = nc.dram_tensor("dbg_" + k, list(shp), F32, kind="ExternalOutput").ap()

    es = ExitStack()
    ARENA_F32 = 51200
    arena = es.enter_context(nc.sbuf_tensor("arena", [128, ARENA_F32], F32))
    psum = es.enter_context(nc.psum_tensor("psum", [128, 4096], F32))

    def sview(off, dt, shape):
        n = int(np.prod(shape))
        nbytes = n * (2 if dt == BF16 else 4)
        assert off % 4 == 0 and nbytes % 4 == 0
        assert off + nbytes <= ARENA_F32 * 4, (off, nbytes)
        a = arena[:, off // 4:(off + nbytes) // 4]
        if dt != F32:
            a = a.bitcast(dt)
        if len(shape) >= 2:
            names = [f"d{i}" for i in range(len(shape))]
            pat = "p (" + " ".join(names) + ") -> p " + " ".join(names)
            a = a.rearrange(pat, **{n: int(v) for n, v in zip(names[:-1], shape[:-1])})
        return a

    class Alloc:
        def __init__(self, base, limit):
            self.base, self.off, self.limit = base, base, limit

        def reset(self):
            self.off = self.base

        def get(self, dt, shape):
            n = int(np.prod(shape)) * (2 if dt == BF16 else 4)
            n = (n + 63) // 64 * 64
            v = sview(self.off, dt, shape)
            self.off += n
            assert self.off <= self.limit, (self.off, self.limit)
            return v

    P_BYTES = 24576
    FT_BYTES = KC * WIN * 2
    OB_BYTES = 65536
    PA = Alloc(0, P_BYTES)
    FT = sview(P_BYTES, BF16, [KC, WIN])
    OB_OFF = P_BYTES + FT_BYTES
    O_sb = sview(OB_OFF, BF16, [NOT, D])
    X1 = sview(OB_OFF, F32, [NOT, D])
    SC = Alloc(OB_OFF + OB_BYTES, ARENA_F32 * 4)

    def bank(b, lo=0, hi=512):
        return psum[:, b * 512 + lo:b * 512 + hi]

    def bank_bf(b):
        return psum[:, b * 512:(b + 1) * 512].bitcast(BF16)

    colall = PA.get(F32, [NMODBLK * 4])
    gc = PA.get(F32, [3, KC])
    gm1 = PA.get(F32, [KC])
    gm2 = PA.get(F32, [KC])
    ident_f = PA.get(F32, [128])
    cst_b = PA.get(BF16, [3, 128])
    relA = PA.get(F32, [2, 128])
    maskA = PA.get(F32, [2, 128])
    tabB = PA.get(BF16, [16, 2, 128])
    constB = PA.get(F32, [16])
    esink = PA.get(F32, [16])
    hm = PA.get(F32, [1])
    brow = PA.get(F32, [36])
    wr = PA.get(BF16, [KC, 36])
    comb = PA.get(F32, [NOT, 32])
    rsP = PA.get(F32, [NTT])
    w1p = PA.get(F32, [NTT])
    w2p = PA.get(F32, [NTT])
    slot1_i = PA.get(I32, [NTT])
    slot2_i = PA.get(I32, [NTT])
    oh1_all = PA.get(F32, [NTT, 32])
    oh2_all = PA.get(F32, [NTT, 32])
    idxW = PA.get(I32, [NST])
    cst2 = PA.get(F32, [64])
    ones_b = PA.get(BF16, [128])
    tri_b = PA.get(BF16, [128])
    piota = cst2[:, 0:1]
    thr8 = cst2[:, 1:9]
    s128 = cst2[:, 9:9 + NST]
    stat = PA.get(F32, [64])
    ident_b = cst_b[:, 0, :]
    maskF_b = cst_b[:, 1, :]

    _bc_regs = {}

    def bc_reg(h, val):
        if val not in _bc_regs:
            r = h.alloc_register(f"bc{val}")
            h.reg_mov(r, val)
            _bc_regs[val] = r
        return _bc_regs[val]

    def bcast(ap_row):
        return ap_row.to_broadcast([128, ap_row.shape[1]])

    def dma(eng, out, in_, key, reads=(), writes=()):
        return S.add(eng, lambda h: h.dma_start(out=out, in_=in_), reads=reads, writes=writes, dma=key)

    SC.reset()
    cst_f = SC.get(F32, [6, 128])
    ccol = SC.get(F32, [KC])
    cact = SC.get(F32, [KC])
    carep = SC.get(BF16, [KC, 128])
    sink_f = SC.get(F32, [16])
    dma("sp", cst_f, cst_d, "c0", writes=["cst_f"])
    dma("pool", tabB, tabB_d, "c1", writes=["tabB"])
    dma("sp", ccol, c_col, "c2", writes=["ccol"])
    dma("sp", gc, gcols, "c3", writes=["gc"])
    dma("sp", constB, constB_d, "c4", writes=["constB"])
    dma("sp", sink_f, sinks_d, "c5", writes=["sink_f"])
    dma("sp", hm, hm_d, "c6", writes=["hm"])
    dma("sp", brow, bcast(b_rt), "c7", writes=["brow"])
    dma("pool", wr, w_rt.rearrange("(kc p) n -> p kc n", p=128), "c8", writes=["wr"])
    dma("pool", tri_b, cst2_d[:, 0:128], "c9", writes=["tri_b"])
    dma("sp", cst2[:, 0:56], cst2_d[:, 128:184], "c10", writes=["cst2"])
    S.add("pool", lambda h: h.memset(ones_b, 1.0), writes=["ones_b"])

    S.add("dve", lambda h: h.tensor_copy(out=ident_f, in_=cst_f[:, 0, :]), reads=["cst_f"], writes=["ident_f"])
    S.add("dve", lambda h: h.tensor_copy(out=cst_b, in_=cst_f[:, 0:3, :]), reads=["cst_f"], writes=["cst_b"])
    S.add("dve", lambda h: h.tensor_copy(out=relA, in_=cst_f[:, 3:5, :]), reads=["cst_f"], writes=["relA"])
    S.add("dve", lambda h: h.tensor_copy(out=maskA, in_=cst_f[:, 1:3, :]), reads=["cst_f"], writes=["maskA"])
    S.add("dve", lambda h: h.tensor_tensor(out=tabB[:, :, 1, :], in0=tabB[:, :, 1, :],
                                            in1=cst_b[:, 2, :].unsqueeze(1).to_broadcast([128, 16, 128]),
                                            op=ALU.add), reads=["cst_b", "tabB"], writes=["tabB"])
    S.add("act", lambda h: h.activation(out=esink, in_=sink_f, func=AF.Exp), reads=["sink_f"], writes=["esink"])
    S.add("act", lambda h: h.activation(out=cact, in_=ccol, func=AF.Silu), reads=["ccol"], writes=["cact"])
    S.add("dve", lambda h: h.tensor_copy(out=carep, in_=cact.unsqueeze(2).to_broadcast([128, KC, 128])),
          reads=["cact"], writes=["carep"])

    S.phase = 'p0_mod'
    wblk = [SC.get(BF16, [KC, 512]) for _ in range(2)]
    rowt = [SC.get(F32, [512]) for _ in range(2)]
    biasb = [SC.get(F32, [512]) for _ in range(2)]
    diag = [SC.get(F32, [4, 128]) for _ in range(2)]
    w_mod_v = w_mod.rearrange("(kc p) n -> p kc n", p=128)
    for j in range(NMODBLK):
        wb = wblk[j % 2]
        rb_, bb, dg = rowt[j % 2], biasb[j % 2], diag[j % 2]
        acc = bank(4 + j % 2)
        dma("pool", wb, w_mod_v[:, :, j * 512:(j + 1) * 512], f"wm{j % 2}", writes=[f"wblk{j % 2}"])
        dma("sp", bb, bcast(b_mod[:, j * 512:(j + 1) * 512]), f"bm{j % 2}",
            writes=[f"biasb{j % 2}"])
        for kc in range(KC):
            S.add("pe", lambda h, kc=kc, wb=wb, acc=acc: h.matmul(acc, lhsT=carep[:, kc, :], rhs=wb[:, kc, :],
                                                                   start=(kc == 0), stop=(kc == KC - 1)),
                  reads=["carep", f"wblk{j % 2}"], writes=[f"acc{j % 2}"])
        S.add("dve", lambda h, rb_=rb_, bb=bb, acc=acc: h.tensor_tensor(out=rb_, in0=acc, in1=bb, op=ALU.add),
              reads=[f"acc{j % 2}", f"biasb{j % 2}"], writes=[f"rowt{j % 2}"])
        dma("sp", modrow[:, j * 512:(j + 1) * 512], rb_[0:1, :], f"mrow{j % 2}", reads=[f"rowt{j % 2}"], writes=[f"modrow{j % 2}"])
        S.add("dve", lambda h, rb_=rb_, dg=dg: h.tensor_tensor(
            out=dg, in0=rb_.rearrange("p (a b) -> p a b", a=4),
            in1=ident_f.unsqueeze(1).to_broadcast([128, 4, 128]), op=ALU.mult),
            reads=[f"rowt{j % 2}", "ident_f"], writes=[f"diag{j % 2}"])
        S.add("dve", lambda h, dg=dg, j=j: h.tensor_reduce(out=colall[:, j * 4:(j + 1) * 4], in_=dg,
                                                            axis=AX.X, op=ALU.add),
              reads=[f"diag{j % 2}"], writes=["colall"])

    def colv(v):
        return colall[:, v * KC:(v + 1) * KC]

    for (gm, gi, vs) in ((gm1, 0, V_SC1), (gm2, 1, V_SC2)):
        S.add("dve", lambda h, gm=gm, gi=gi, vs=vs: h.scalar_tensor_tensor(
            out=gm, in0=colv(vs), scalar=1.0, in1=gc[:, gi, :], op0=ALU.add, op1=ALU.mult),
            reads=["colall", "gc"], writes=["gm"])

    def dbg_dump(name, src_ap, dst_ap, reads):
        if dbg and name in dbg:
            dma("sp", dst_ap, src_ap, "dbg", reads=reads, writes=["dbgout"])

    if dbg and "colall" in dbg:
        dbg_dump("colall", colall, dbg_out["colall"], ["colall"])

    def rms_rstd(eng_h, ss, rstd, n, reads, writes):
        S.add("dve", lambda h: h.tensor_scalar(out=rstd, in0=ss, scalar1=1.0 / n, scalar2=EPS,
                                                op0=ALU.mult, op1=ALU.add), reads=reads, writes=writes)
        S.add("act", lambda h: h.activation(out=rstd, in_=rstd, func=AF.Ln), reads=writes, writes=writes)
        S.add("act", lambda h: h.activation(out=rstd, in_=rstd, func=AF.Exp, scale=-0.5), reads=writes, writes=writes)

    def transposes_to_FT(src_bf, src_res, lt, scale_col, bias_col, tag):
        for hb in range(2):
            pb = bank_bf(6 + hb)
            for k8 in range(8):
                kc = hb * 8 + k8
                S.add("pe", lambda h, pb=pb, k8=k8, kc=kc: h.transpose(
                    out=pb[:, k8 * 128:(k8 + 1) * 128], in_=src_bf[:, kc * 128:(kc + 1) * 128], identity=ident_b),
                    reads=[src_res, "cst_b"], writes=[f"tp{hb}"])
            for k8 in range(8):
                kc = hb * 8 + k8
                dst = FT[:, kc, lt * 128:(lt + 1) * 128]
                src = pb[:, k8 * 128:(k8 + 1) * 128]
                if k8 % 2 == 0:
                    if bias_col is not None:
                        S.add("act", lambda h, dst=dst, src=src, kc=kc: h.activation(
                            out=dst, in_=src, func=AF.Identity, scale=scale_col[:, kc:kc + 1],
                            bias=bias_col[:, kc:kc + 1]),
                            reads=[f"tp{hb}", "gm", "colall", "gc"], writes=[("FT", lt)])
                    else:
                        S.add("act", lambda h, dst=dst, src=src, kc=kc: h.activation(
                            out=dst, in_=src, func=AF.Copy, scale=scale_col[:, kc:kc + 1]),
                            reads=[f"tp{hb}", "gm", "colall", "gc"], writes=[("FT", lt)])
                else:
                    if bias_col is not None:
                        S.add("dve", lambda h, dst=dst, src=src, kc=kc: h.tensor_scalar(
                            out=dst, in0=src, scalar1=scale_col[:, kc:kc + 1], scalar2=bias_col[:, kc:kc + 1],
                            op0=ALU.mult, op1=ALU.add),
                            reads=[f"tp{hb}", "gm", "colall", "gc"], writes=[("FT", lt)])
                    else:
                        S.add("dve", lambda h, dst=dst, src=src, kc=kc: h.tensor_scalar(
                            out=dst, in0=src, scalar1=scale_col[:, kc:kc + 1], scalar2=None, op0=ALU.mult),
                            reads=[f"tp{hb}", "gm", "colall", "gc"], writes=[("FT", lt)])

    for H in range(2):
        row0 = H * HALF
        S.fence()
        S.phase = f'H{H}_p1_norm1'
        SC.reset()
        xt = [SC.get(F32, [D]) for _ in range(2)]
        xnb = [SC.get(BF16, [D]) for _ in range(2)]
        junk = SC.get(BF16, [D])
        for lt in range(NWT):
            b2 = lt % 2
            dma("sp", xt[b2], xh[row0 + lt * 128: row0 + (lt + 1) * 128, :], f"xt{b2}", writes=[f"xt{b2}"])
            ss = stat[:, b2:b2 + 1]
            rs = stat[:, 2 + b2:3 + b2]
            S.add("act", lambda h, b2=b2, ss=ss: h.activation(out=junk, in_=xt[b2], func=AF.Square, accum_out=ss),
                  reads=[f"xt{b2}"], writes=["junk", f"ss{b2}"])
            rms_rstd(None, ss, rs, D, [f"ss{b2}"], [f"rs{b2}"])
            S.add("act", lambda h, b2=b2, rs=rs: h.activation(out=xnb[b2], in_=xt[b2], func=AF.Copy, scale=rs),
                  reads=[f"xt{b2}", f"rs{b2}"], writes=[f"xnb{b2}"])
            transposes_to_FT(xnb[b2], f"xnb{b2}", lt, gm1, colv(V_SH1), "h")
        if dbg and "hT" in dbg and H == dbg.get("_H", 1):
            tmpf = SC.get(F32, [KC, 128])
            for lt in range(NWT):
                S.add("dve", lambda h, lt=lt: h.tensor_copy(out=tmpf, in_=FT[:, :, lt * 128:(lt + 1) * 128]),
                      reads=[("FT", lt)], writes=["tmpf"])
                dma("sp", dbg_out["hT"][lt], tmpf, "dbg", reads=["tmpf"], writes=["dbgout"])

        S.fence()
        S.phase = f'H{H}_p2_attn'
        SC.reset()
        Wu = [SC.get(BF16, [KC, 384]) for _ in range(2)]
        QT = SC.get(BF16, [HALF])
        KT = SC.get(BF16, [WIN])
        VA = SC.get(BF16, [NWT, 2, 65])
        tabA = [SC.get(BF16, [2, 2, 2, 128]) for _ in range(2)]
        tA_f = SC.get(F32, [2, 128])
        tA_h = SC.get(F32, [2, 128])
        PT = [SC.get(BF16, [5, 128]) for _ in range(2)]
        rec = [SC.get(F32, [2]) for _ in range(2)]
        S.add("pool", lambda h: h.memset(VA[:, :, :, 64:65], 1.0), writes=["VA"])
        if H == 0:
            S.add("pool", lambda h: h.tensor_copy(out=VA[:, 0:4, :, 64:65],
                                                  in_=hm.unsqueeze(1).unsqueeze(1).to_broadcast([128, 4, 2, 1])),
                  reads=["hm"], writes=["VA"])
        for b2 in range(2):
            S.add("pool", lambda h, b2=b2: h.memset(PT[b2], 0.0), writes=[f"PT{b2}"])

        acc_i = [0]

        def next_acc():
            a = acc_i[0] % 2
            acc_i[0] += 1
            return a

        for u in range(16):
            grpA = u < 8
            wu = Wu[u % 2]
            dma("pool", wu, w_in_u[u], f"wu{u % 2}", writes=[f"wu{u % 2}"])
            wres = f"wu{u % 2}"
            for tb in range(HALF // 512):
                a = next_acc()
                for kc in range(KC):
                    S.add("pe", lambda h, a=a, kc=kc, tb=tb, wu=wu: h.matmul(
                        bank(4 + a), lhsT=wu[:, kc, 0:128], rhs=FT[:, kc, HALO + tb * 512: HALO + (tb + 1) * 512],
                        start=(kc == 0), stop=(kc == KC - 1)),
                        reads=[wres] + [("FT", 4 + tb * 4 + i) for i in range(4)], writes=[f"acc{a}"])
                S.add("dve", lambda h, a=a, tb=tb: h.tensor_scalar(
                    out=QT[:, tb * 512:(tb + 1) * 512], in0=bank(4 + a), scalar1=0.125, scalar2=None, op0=ALU.mult),
                    reads=[f"acc{a}"], writes=["QT"])
            do_kv = (not grpA) or (u % 2 == 0)
            if do_kv:
                for tb in range(WIN // 512):
                    a = next_acc()
                    for kc in range(KC):
                        S.add("pe", lambda h, a=a, kc=kc, tb=tb, wu=wu: h.matmul(
                            bank(4 + a), lhsT=wu[:, kc, 128:256], rhs=FT[:, kc, tb * 512:(tb + 1) * 512],
                            start=(kc == 0), stop=(kc == KC - 1)),
                            reads=[wres] + [("FT", tb * 4 + i) for i in range(4)], writes=[f"acc{a}"])
                    S.add("dve", lambda h, a=a, tb=tb: h.tensor_copy(out=KT[:, tb * 512:(tb + 1) * 512], in_=bank(4 + a)),
                          reads=[f"acc{a}"], writes=["KT"])
                for lt in range(NWT):
                    a = next_acc()
                    for kc in range(KC):
                        S.add("pe", lambda h, a=a, kc=kc, lt=lt, wu=wu: h.matmul(
                            bank(4 + a, 0, 128), lhsT=FT[:, kc, lt * 128:(lt + 1) * 128], rhs=wu[:, kc, 256:384],
                            start=(kc == 0), stop=(kc == KC - 1)),
                            reads=[wres, ("FT", lt)], writes=[f"acc{a}"])
                    src = bank(4 + a, 0, 128).rearrange("p (a b) -> p a b", a=2)
                    if H == 0 and lt < 4:
                        S.add("dve", lambda h, lt=lt, src=src: h.tensor_scalar(
                            out=VA[:, lt, :, 0:64], in0=src, scalar1=hm[:, 0:1], scalar2=None, op0=ALU.mult),
                            reads=[f"acc{a}", "hm"], writes=["VA"])
                    else:
                        S.add("dve", lambda h, lt=lt, src=src: h.tensor_copy(out=VA[:, lt, :, 0:64], in_=src),
                              reads=[f"acc{a}"], writes=["VA"])
            if grpA:
                tb_ = tabA[u % 2]
                for j in range(2):
                    hd = 2 * u + j
                    S.add("pool", lambda h, hd=hd: h.tensor_scalar(
                        out=tA_f, in0=relA, scalar1=-SLOPES[hd], scalar2=None, op0=ALU.mult),
                        reads=["relA", "tA_h"], writes=["tA_f"])
                    S.add("pool", lambda h: h.tensor_tensor(out=tA_f, in0=tA_f, in1=maskA, op=ALU.add),
                          reads=["tA_f", "maskA"], writes=["tA_f"])
                    S.add("pool", lambda h, j=j, tb_=tb_: h.tensor_copy(out=tb_[:, j, 0, :, :], in_=tA_f),
                          reads=["tA_f"], writes=[f"tabA{u % 2}"])
                    S.add("pool", lambda h, j=j, tb_=tb_: h.tensor_copy(out=tA_h, in_=tb_[:, j, 0, :, :]),
                          reads=[f"tabA{u % 2}"], writes=["tA_h"])
                    S.add("pool", lambda h: h.tensor_tensor(out=tA_h, in0=tA_f, in1=tA_h, op=ALU.subtract),
                          reads=["tA_f", "tA_h"], writes=["tA_h"])
                    S.add("pool", lambda h, j=j, tb_=tb_: h.tensor_copy(out=tb_[:, j, 1, :, :], in_=tA_h),
                          reads=["tA_h"], writes=[f"tabA{u % 2}"])
            items = [(qt, j) for qt in range(NOT) for j in range(2)]

            def emit_qk(n):
                qt, j = items[n]
                sb = n % 2
                hd = (2 * u + j) if grpA else (2 * (u - 8) + j)
                pl, ph = j * 64, (j + 1) * 64
                q_ap = QT[pl:ph, qt * 128:(qt + 1) * 128]
                s_main = bank(sb, 0, 384)
                s_last = bank(2 + sb, 0, 256)
                blocks = range(3, 5) if grpA else range(5)
                for i in blocks:
                    lt = qt + i
                    dst = s_main[:, i * 128:(i + 1) * 128] if i < 3 else s_last[:, (i - 3) * 128:(i - 2) * 128]
                    sres = f"S{sb}" if i < 3 else f"S2{sb}"
                    has_tab = grpA or i in (0, 3, 4)
                    S.add("pe", lambda h, dst=dst, lt=lt, pl=pl, ph=ph, q_ap=q_ap, has_tab=has_tab: h.matmul(
                        dst, lhsT=KT[pl:ph, lt * 128:(lt + 1) * 128], rhs=q_ap, start=True, stop=not has_tab),
                        reads=["KT", "QT"], writes=[sres])
                    if grpA:
                        tb_ = tabA[u % 2]
                        for hl in range(2):
                            S.add("pe", lambda h, dst=dst, j=j, hl=hl, i=i, tb_=tb_: h.matmul(
                                dst, lhsT=ident_b, rhs=tb_[:, j, hl, i - 3, :], start=False, stop=(hl == 1)),
                                reads=["cst_b", f"tabA{u % 2}"], writes=[sres])
                    elif i == 0:
                        S.add("pe", lambda h, dst=dst: h.matmul(dst, lhsT=ident_b, rhs=maskF_b, start=False, stop=True),
                              reads=["cst_b"], writes=[sres])
                    elif i >= 3:
                        S.add("pe", lambda h, dst=dst, hd=hd, i=i: h.matmul(
                            dst, lhsT=ident_b, rhs=tabB[:, hd, i - 3, :], start=False, stop=True),
                            reads=["cst_b", "tabB"], writes=[sres])
                pt = PT[sb]
                if not grpA:
                    S.add("act", lambda h, pt=pt, s_main=s_main, hd=hd: h.activation(
                        out=pt[:, 0:3, :], in_=s_main.rearrange("p (a b) -> p a b", a=3), func=AF.Exp,
                        bias=constB[:, hd:hd + 1]),
                        reads=[f"S{sb}", "constB"], writes=[f"PT{sb}"])
                S.add("act", lambda h, pt=pt, s_last=s_last: h.activation(
                    out=pt[:, 3:5, :], in_=s_last.rearrange("p (a b) -> p a b", a=2), func=AF.Exp),
                    reads=[f"S2{sb}"], writes=[f"PT{sb}"])

            def emit_pv(n):
                qt, j = items[n]
                sb = n % 2
                ob = qt % 2
                o_ps = bank(6 + ob, 0, 130).rearrange("p (a b) -> p a b", a=2)
                blocks = list(range(3, 5)) if grpA else list(range(5))
                for bi, i in enumerate(blocks):
                    lt = qt + i
                    S.add("pe", lambda h, i=i, lt=lt, j=j, sb=sb, o_ps=o_ps, bi=bi, nb_=len(blocks): h.matmul(
                        o_ps[:, j, :], lhsT=PT[sb][:, i, :], rhs=VA[:, lt, 0 if grpA else j, :],
                        start=(bi == 0), stop=(bi == nb_ - 1)),
                        reads=[f"PT{sb}", "VA"], writes=[f"OP{ob}"])
                if j == 1:
                    rc = rec[ob]
                    den = o_ps[:, :, 64:65]
                    if grpA:
                        S.add("dve", lambda h, rc=rc, den=den: h.tensor_tensor(
                            out=rc.unsqueeze(2), in0=den, in1=esink[:, 2 * u:2 * u + 2].unsqueeze(2), op=ALU.add),
                            reads=[f"OP{ob}", "esink"], writes=[f"rec{ob}"])
                        S.add("dve", lambda h, rc=rc: h.reciprocal(out=rc, in_=rc),
                              reads=[f"rec{ob}"], writes=[f"rec{ob}"])
                    else:
                        S.add("dve", lambda h, rc=rc, den=den: h.reciprocal(out=rc.unsqueeze(2), in_=den),
                              reads=[f"OP{ob}"], writes=[f"rec{ob}"])
                    S.add("dve", lambda h, rc=rc, o_ps=o_ps, qt=qt: h.tensor_tensor(
                        out=O_sb[:, qt, u * 128:(u + 1) * 128].rearrange("p (a b) -> p a b", a=2),
                        in0=o_ps[:, :, 0:64], in1=rc.unsqueeze(2).to_broadcast([128, 2, 64]), op=ALU.mult),
                        reads=[f"OP{ob}", f"rec{ob}"], writes=[("O", qt, u)])

            for n in range(len(items) + 1):
                if n < len(items):
                    emit_qk(n)
                if n >= 1:
                    emit_pv(n - 1)

        if dbg and "O" in dbg and H == dbg.get("_H", 1):
            tmpo = SC.get(F32, [D])
            for t in range(NOT):
                S.add("dve", lambda h, t=t: h.tensor_copy(out=tmpo, in_=O_sb[:, t, :]),
                      reads=[("O", t, u) for u in range(16)], writes=["tmpo"])
                dma("sp", dbg_out["O"][t], tmpo, "dbg", reads=["tmpo"], writes=["dbgout"])

        S.fence()
        S.phase = f'H{H}_p3a_onorm'
        SC.reset()
        junk = SC.get(BF16, [D])
        onb = [SC.get(BF16, [D]) for _ in range(2)]
        for t in range(NOT):
            b2 = t % 2
            ss2 = stat[:, 4 + 2 * b2: 6 + 2 * b2]
            rs2 = stat[:, 8 + 2 * b2: 10 + 2 * b2]
            ores = [("O", t, u) for u in range(16)]
            for g in range(2):
                S.add("act", lambda h, t=t, g=g, ss2=ss2: h.activation(
                    out=junk[:, 0:1024], in_=O_sb[:, t, g * 1024:(g + 1) * 1024], func=AF.Square,
                    accum_out=ss2[:, g:g + 1]), reads=ores, writes=["junk", f"ss2{b2}"])
            rms_rstd(None, ss2, rs2, 1024, [f"ss2{b2}"], [f"rs2{b2}"])
            for g in range(2):
                S.add("act", lambda h, t=t, g=g, rs2=rs2, b2=b2: h.activation(
                    out=onb[b2][:, g * 1024:(g + 1) * 1024], in_=O_sb[:, t, g * 1024:(g + 1) * 1024],
                    func=AF.Copy, scale=rs2[:, g:g + 1]), reads=ores + [f"rs2{b2}"], writes=[f"onb{b2}"])
            transposes_to_FT(onb[b2], f"onb{b2}", 4 + t, gc[:, 2, :], None, "o")

        S.fence()
        S.phase = f'H{H}_p3b_outproj'
        SC.reset()
        Wo = [SC.get(BF16, [KC, 512]) for _ in range(2)]
        gt1row = SC.get(F32, [D])
        tmpx = [SC.get(F32, [512]) for _ in range(2)]
        dma("sp", gt1row, bcast(modrow[:, V_GT1 * D:(V_GT1 + 1) * D]), "gt1r",
            reads=["modrow0", "modrow1"], writes=["gt1row"])
        for t in range(NOT):
            r = HALO + row0 + t * 128
            dma("sp", X1[:, t, :], xh[r:r + 128, :], f"x1_{t}", writes=[("X1", t, nb) for nb in range(4)])
        w_out_v = w_out.rearrange("(kc p) n -> p kc n", p=128)
        for nb in range(4):
            wo = Wo[nb % 2]
            dma("pool", wo, w_out_v[:, :, nb * 512:(nb + 1) * 512], f"wo{nb % 2}", writes=[f"wo{nb % 2}"])
            for t in range(NOT):
                a = next_acc()
                for kc in range(KC):
                    S.add("pe", lambda h, a=a, kc=kc, t=t, wo=wo: h.matmul(
                        bank(4 + a), lhsT=FT[:, kc, (4 + t) * 128:(5 + t) * 128], rhs=wo[:, kc, :],
                        start=(kc == 0), stop=(kc == KC - 1)),
                        reads=[f"wo{nb % 2}", ("FT", 4 + t)], writes=[f"acc{a}"])
                tx = tmpx[a]
                S.add("dve", lambda h, a=a, tx=tx, nb=nb: h.tensor_tensor(
                    out=tx, in0=bank(4 + a), in1=gt1row[:, nb * 512:(nb + 1) * 512], op=ALU.mult),
                    reads=[f"acc{a}", "gt1row"], writes=[f"tmpx{a}"])
                S.add("dve", lambda h, tx=tx, t=t, nb=nb: h.tensor_tensor(
                    out=X1[:, t, nb * 512:(nb + 1) * 512], in0=X1[:, t, nb * 512:(nb + 1) * 512], in1=tx, op=ALU.add),
                    reads=[f"tmpx{a}", ("X1", t, nb)], writes=[("X1", t, nb)])
        if dbg and "X1" in dbg and H == dbg.get("_H", 1):
            for t in range(NOT):
                dma("sp", dbg_out["X1"][t], X1[:, t, :], "dbg", reads=[("X1", t, nb) for nb in range(4)],
                    writes=["dbgout"])

        S.fence()
        S.phase = f'H{H}_p4_route'
        SC.reset()
        junk = SC.get(BF16, [D])
        xnb = [SC.get(BF16, [D]) for _ in range(2)]
        L = SC.get(F32, [NOT, 36])
        for t in range(NOT):
            b2 = t % 2
            xres = [("X1", t, nb) for nb in range(4)]
            ss = stat[:, b2:b2 + 1]
            rs = rsP[:, H * NOT + t:H * NOT + t + 1]
            S.add("act", lambda h, t=t, ss=ss: h.activation(out=junk, in_=X1[:, t, :], func=AF.Square, accum_out=ss),
                  reads=xres, writes=["junk", f"ss{b2}"])
            rms_rstd(None, ss, rs, D, [f"ss{b2}"], [f"rsP{H * NOT + t}"])
            S.add("act", lambda h, t=t, rs=rs, b2=b2: h.activation(out=xnb[b2], in_=X1[:, t, :], func=AF.Copy, scale=rs),
                  reads=xres + [f"rsP{H * NOT + t}"], writes=[f"xnb{b2}"])
            transposes_to_FT(xnb[b2], f"xnb{b2}", 4 + t, gm2, colv(V_SH2), "h2")
            a = next_acc()
            for kc in range(KC):
                S.add("pe", lambda h, a=a, kc=kc, t=t: h.matmul(
                    bank(4 + a, 0, 36), lhsT=FT[:, kc, (4 + t) * 128:(5 + t) * 128], rhs=wr[:, kc, :],
                    start=(kc == 0), stop=(kc == KC - 1)),
                    reads=["wr", ("FT", 4 + t)], writes=[f"acc{a}"])
            S.add("dve", lambda h, a=a, t=t: h.tensor_tensor(out=L[:, t, :], in0=bank(4 + a, 0, 36), in1=brow, op=ALU.add),
                  reads=[f"acc{a}", "brow"], writes=["L"])
        gl = L[:, :, 0:4]
        el = L[:, :, 4:36]
        gmax = SC.get(F32, [NOT]); gsum = SC.get(F32, [NOT]); pg = SC.get(F32, [NOT])
        gt_ = SC.get(F32, [NOT, 4]); ohg = SC.get(F32, [NOT, 4]); pen = SC.get(F32, [NOT, 4])
        elm = SC.get(F32, [NOT, 32]); oh1 = SC.get(F32, [NOT, 32]); oh2 = SC.get(F32, [NOT, 32])
        elm2 = SC.get(F32, [NOT, 32])
        m1 = SC.get(F32, [NOT]); m2 = SC.get(F32, [NOT]); e2 = SC.get(F32, [NOT]); w1 = SC.get(F32, [NOT])
        w2 = SC.get(F32, [NOT])
        RR = ["L", "rt"]

        def dv(fn):
            S.add("dve", fn, reads=RR, writes=["rt"])

        dv(lambda h: h.tensor_reduce(out=gmax, in_=gl, axis=AX.X, op=ALU.max))
        dv(lambda h: h.tensor_tensor(out=gt_, in0=gl, in1=gmax.unsqueeze(2).to_broadcast([128, NOT, 4]), op=ALU.subtract))
        S.add("act", lambda h: h.activation(out=gt_, in_=gt_, func=AF.Exp), reads=RR, writes=["rt"])
        dv(lambda h: h.tensor_reduce(out=gsum, in_=gt_, axis=AX.X, op=ALU.add))
        dv(lambda h: h.reciprocal(out=pg, in_=gsum))
        dv(lambda h: h.tensor_tensor(out=ohg, in0=gl, in1=gmax.unsqueeze(2).to_broadcast([128, NOT, 4]), op=ALU.is_ge))
        dv(lambda h: h.tensor_scalar(out=pen, in0=ohg, scalar1=-1.0, scalar2=1e9, op0=ALU.add, op1=ALU.mult))
        dv(lambda h: h.tensor_tensor(
            out=elm.rearrange("p t (g j) -> p t g j", g=4), in0=el.rearrange("p t (g j) -> p t g j", g=4),
            in1=pen.unsqueeze(3).to_broadcast([128, NOT, 4, 8]), op=ALU.add))
        dv(lambda h: h.tensor_reduce(out=m1, in_=elm, axis=AX.X, op=ALU.max))
        dv(lambda h: h.tensor_tensor(out=oh1, in0=elm, in1=m1.unsqueeze(2).to_broadcast([128, NOT, 32]), op=ALU.is_ge))
        dv(lambda h: h.scalar_tensor_tensor(out=elm2, in0=oh1, scalar=-1e9, in1=elm, op0=ALU.mult, op1=ALU.add))
        dv(lambda h: h.tensor_reduce(out=m2, in_=elm2, axis=AX.X, op=ALU.max))
        dv(lambda h: h.tensor_tensor(out=oh2, in0=elm2, in1=m2.unsqueeze(2).to_broadcast([128, NOT, 32]), op=ALU.is_ge))
        dv(lambda h: h.tensor_tensor(out=e2, in0=m2, in1=m1, op=ALU.subtract))
        S.add("act", lambda h: h.activation(out=e2, in_=e2, func=AF.Exp), reads=RR, writes=["rt"])
        dv(lambda h: h.tensor_scalar(out=w1, in0=e2, scalar1=1.0, scalar2=None, op0=ALU.add))
        dv(lambda h: h.reciprocal(out=w1, in_=w1))
        dv(lambda h: h.tensor_tensor(out=w1, in0=w1, in1=pg, op=ALU.mult))
        dv(lambda h: h.tensor_tensor(out=w2, in0=w1, in1=e2, op=ALU.mult))
        dv(lambda h: h.tensor_tensor(out=elm, in0=oh1, in1=w1.unsqueeze(2).to_broadcast([128, NOT, 32]), op=ALU.mult))
        dv(lambda h: h.tensor_tensor(out=elm2, in0=oh2, in1=w2.unsqueeze(2).to_broadcast([128, NOT, 32]), op=ALU.mult))
        S.add("dve", lambda h: h.tensor_tensor(out=comb, in0=elm, in1=elm2, op=ALU.add), reads=RR, writes=["comb"])
        if dbg and "comb" in dbg and H == dbg.get("_H", 1):
            dma("sp", dbg_out["comb"], comb, "dbg", reads=["comb"], writes=["dbgout"])
        hs_ = slice(H * NOT, (H + 1) * NOT)
        S.add("dve", lambda h: h.tensor_copy(out=w1p[:, hs_], in_=w1), reads=RR, writes=["w1p"])
        S.add("dve", lambda h: h.tensor_copy(out=w2p[:, hs_], in_=w2), reads=RR, writes=["w2p"])
        S.add("dve", lambda h: h.tensor_copy(out=oh1_all[:, hs_, :], in_=oh1), reads=RR, writes=["ohall"])
        S.add("dve", lambda h: h.tensor_copy(out=oh2_all[:, hs_, :], in_=oh2), reads=RR, writes=["ohall"])

        S.phase = f'H{H}_p4c_spill'
        gm2row = SC.get(F32, [D]); sh2row = SC.get(F32, [D])
        h2f = SC.get(F32, [D])
        gfr = h2f
        h2tok = xnb
        dma("sp", gm2row, bcast(modrow[:, V_SC2 * D:(V_SC2 + 1) * D]), "r0", reads=["modrow0", "modrow1"], writes=["gm2row"])
        dma("sp", sh2row, bcast(modrow[:, V_SH2 * D:(V_SH2 + 1) * D]), "r1", reads=["modrow0", "modrow1"], writes=["sh2row"])
        dma("sp", gfr, bcast(g_ffn_row), "r2", writes=["h2f"])
        S.add("dve", lambda h: h.scalar_tensor_tensor(out=gm2row, in0=gm2row, scalar=1.0, in1=gfr, op0=ALU.add, op1=ALU.mult),
              reads=["gm2row", "h2f"], writes=["gm2row"])
        for t in range(NOT):
            b2 = t % 2
            tt = H * NOT + t
            xres = [("X1", t, nb) for nb in range(4)]
            dma("sp", X1S[tt * 128:(tt + 1) * 128, :], X1[:, t, :], f"x1s{b2}", reads=xres, writes=[("X1S", tt)])
            S.add("dve", lambda h, t=t, tt=tt: h.scalar_tensor_tensor(out=h2f, in0=X1[:, t, :], scalar=rsP[:, tt:tt + 1], in1=gm2row,
                                                                      op0=ALU.mult, op1=ALU.mult),
                  reads=xres + [f"rsP{tt}", "gm2row"], writes=["h2f"])
            S.add("dve", lambda h, b2=b2: h.tensor_tensor(out=h2tok[b2], in0=h2f, in1=sh2row, op=ALU.add),
                  reads=["h2f", "sh2row"], writes=[f"xnb{b2}"])
            dma("sp", H2S[tt * 128:(tt + 1) * 128, :], h2tok[b2], f"h2s{b2}", reads=[f"xnb{b2}"], writes=[("H2S", tt)])

    S.fence()
    S.phase = 'G_p4b_slots'
    SC.reset()
    ohA = SC.get(F32, [NTT, 32]); ohAb = SC.get(BF16, [NTT, 32])
    cntS = SC.get(F32, [32]); rk = SC.get(F32, [NTT, 32])
    cmp8 = SC.get(F32, [32, 8]); nt_ = SC.get(F32, [32]); padded = SC.get(F32, [32])
    cs = [SC.get(F32, [32]) for _ in range(2)]
    off = SC.get(F32, [32]); sbt = SC.get(F32, [NTT, 32]); tmp3 = SC.get(F32, [NTT, 32])
    s1f = SC.get(F32, [NTT]); s2f = SC.get(F32, [NTT])
    cmpS = SC.get(F32, [NST, 32]); eS = SC.get(F32, [NST]); idxf = SC.get(F32, [NST])
    RR2 = ["rt", "cst2", "ohall"]

    def dv2(fn):
        S.add("dve", fn, reads=RR2, writes=["rt"])

    dv2(lambda h: h.tensor_tensor(out=ohA, in0=oh1_all, in1=oh2_all, op=ALU.add))
    dv2(lambda h: h.tensor_copy(out=ohAb, in_=ohA))
    CNT = bank(4, 0, 32)
    RK = bank(5, 0, NTT * 32).rearrange("p (t e) -> p t e", t=NTT)
    for t in range(NTT):
        S.add("pe", lambda h, t=t: h.matmul(CNT, lhsT=ones_b, rhs=ohAb[:, t, :], start=(t == 0), stop=(t == NTT - 1)),
              reads=["rt", "ones_b"], writes=["acc0"])
    for t in range(NTT):
        for tp in range(t):
            S.add("pe", lambda h, t=t, tp=tp: h.matmul(RK[:, t, :], lhsT=ones_b, rhs=ohAb[:, tp, :],
                                                       start=(tp == 0), stop=False),
                  reads=["rt", "ones_b"], writes=["acc1"])
        S.add("pe", lambda h, t=t: h.matmul(RK[:, t, :], lhsT=tri_b, rhs=ohAb[:, t, :], start=(t == 0), stop=True),
              reads=["rt", "tri_b"], writes=["acc1"])
    S.add("dve", lambda h: h.tensor_copy(out=cntS, in_=CNT), reads=["acc0"], writes=["rt"])
    S.add("dve", lambda h: h.tensor_copy(out=rk, in_=RK), reads=["acc1"], writes=["rt"])
    dv2(lambda h: h.tensor_tensor(out=cmp8, in0=cntS.unsqueeze(2).to_broadcast([128, 32, 8]),
                                  in1=thr8.unsqueeze(1).to_broadcast([128, 32, 8]), op=ALU.is_gt))
    dv2(lambda h: h.tensor_reduce(out=nt_, in_=cmp8, axis=AX.X, op=ALU.add))
    dv2(lambda h: h.tensor_scalar(out=padded, in0=nt_, scalar1=float(TS), scalar2=None, op0=ALU.mult))
    dv2(lambda h: h.tensor_copy(out=cs[0], in_=padded))
    cur = 0
    for sh in (1, 2, 4, 8, 16):
        a_, b_ = cs[cur], cs[1 - cur]
        dv2(lambda h, a_=a_, b_=b_: h.tensor_copy(out=b_, in_=a_))
        dv2(lambda h, a_=a_, b_=b_, sh=sh: h.tensor_tensor(out=b_[:, sh:32], in0=a_[:, sh:32], in1=a_[:, 0:32 - sh], op=ALU.add))
        cur = 1 - cur
    incl = cs[cur]
    dv2(lambda h: h.tensor_tensor(out=off, in0=incl, in1=padded, op=ALU.subtract))
    dv2(lambda h: h.tensor_tensor(out=sbt, in0=rk, in1=off.unsqueeze(1).to_broadcast([128, NTT, 32]), op=ALU.add))
    dv2(lambda h: h.tensor_tensor(out=tmp3, in0=sbt, in1=oh1_all, op=ALU.mult))
    dv2(lambda h: h.tensor_reduce(out=s1f, in_=tmp3, axis=AX.X, op=ALU.add))
    dv2(lambda h: h.tensor_tensor(out=tmp3, in0=sbt, in1=oh2_all, op=ALU.mult))
    dv2(lambda h: h.tensor_reduce(out=s2f, in_=tmp3, axis=AX.X, op=ALU.add))
    S.add("dve", lambda h: h.tensor_copy(out=slot1_i, in_=s1f), reads=RR2, writes=["slot1"])
    S.add("dve", lambda h: h.tensor_copy(out=slot2_i, in_=s2f), reads=RR2, writes=["slot2"])
    dv2(lambda h: h.tensor_tensor(out=cmpS, in0=incl.unsqueeze(1).to_broadcast([128, NST, 32]),
                                  in1=s128.unsqueeze(2).to_broadcast([128, NST, 32]), op=ALU.is_le))
    dv2(lambda h: h.tensor_reduce(out=eS, in_=cmpS, axis=AX.X, op=ALU.add))
    dv2(lambda h: h.tensor_scalar(out=idxf, in0=eS, scalar1=128.0, scalar2=piota, op0=ALU.mult, op1=ALU.add))
    S.add("dve", lambda h: h.tensor_copy(out=idxW, in_=idxf), reads=RR2, writes=["idxW"])

    S.phase = 'G_p4d_scatter'
    zt = SC.get(BF16, [D])
    h2t = [SC.get(BF16, [D]) for _ in range(2)]
    S.add("pool", lambda h: h.memset(zt, 0.0), writes=["zt"])
    for st in range(NST * SUB):
        dma("sp", HS[st * 128:(st + 1) * 128, :], zt, "hsz", reads=["zt"], writes=["HS"])
    for tt in range(NTT):
        b2 = tt % 2
        dma("sp", h2t[b2], H2S[tt * 128:(tt + 1) * 128, :], f"h2l{b2}", reads=[("H2S", tt)], writes=[f"h2t{b2}"])
        for k_, sl in enumerate((slot1_i, slot2_i)):
            S.add("pool", lambda h, sl=sl, tt=tt, b2=b2: h.indirect_dma_start(
                out=HS, out_offset=bass.IndirectOffsetOnAxis(ap=sl[:, tt:tt + 1], axis=0),
                in_=h2t[b2], in_offset=None, bounds_check=bc_reg(h, NST * TS - 1), oob_is_err=False),
                reads=[f"h2t{b2}", "slot1", "slot2", "HS"], writes=[("HSs", tt, k_)], dma=f"hss{b2}{k_}")

    S.fence()
    S.phase = 'G_p5_moe'
    SC.reset()
    ring = [sview(P_BYTES + r * 16384, BF16, [8192]) for r in range(3)] + \
           [sview(OB_OFF + r * 16384, BF16, [8192]) for r in range(4)]
    NR_ = len(ring)
    hsb = [SC.get(BF16, [SUB, D]) for _ in range(2)]
    hsT = [SC.get(BF16, [KC, TS]) for _ in range(2)]
    aT = [SC.get(BF16, [4, TS]) for _ in range(2)]
    sg = [SC.get(F32, [TS]) for _ in range(2)]
    ysb = [SC.get(F32, [D]) for _ in range(2)]
    ring_i = [0]

    def ring_next():
        r = ring_i[0] % NR_
        ring_i[0] += 1
        return r

    gu_i = [0]
    y_i = [0]
    ys_i = [0]
    n_tiles = dbg.get("_ntiles", NST) if dbg else NST
    hs_all = ["HS"] + [("HSs", tt, k_) for tt in range(NTT) for k_ in range(2)]

    def load_hs(st):
        for sub in range(SUB):
            r0 = (st * SUB + sub) * 128
            dma("sp", hsb[st % 2][:, sub, :], HS[r0:r0 + 128, :], f"hsl{st % 2}{sub}",
                reads=hs_all, writes=[f"hsb{st % 2}{sub}"])

    load_hs(0)
    for st in range(n_tiles):
        b2 = st % 2
        rg, ru, rd = ring_next(), ring_next(), ring_next()
        for (r_, wsrc) in ((rg, w_gate), (ru, w_up), (rd, w_down)):
            S.add("pool", lambda h, r_=r_, wsrc=wsrc, st=st: h.indirect_dma_start(
                out=ring[r_], out_offset=None, in_=wsrc,
                in_offset=bass.IndirectOffsetOnAxis(ap=idxW[:, st:st + 1], axis=0),
                bounds_check=bc_reg(h, N_EXP * 128 - 1), oob_is_err=False),
                reads=["idxW"], writes=[f"ring{r_}"], dma=f"ring{r_}")
        Wg = ring[rg].rearrange("p (a b) -> p a b", a=KC)
        Wup = ring[ru].rearrange("p (a b) -> p a b", a=KC)
        Wd = ring[rd].rearrange("p (a b) -> p a b", a=4)
        if st + 1 < n_tiles:
            load_hs(st + 1)
        for sub in range(SUB):
            for hb in range(2):
                pb = bank_bf(6 + hb)
                for k8 in range(8):
                    kc = hb * 8 + k8
                    S.add("pe", lambda h, pb=pb, k8=k8, kc=kc, b2=b2, sub=sub: h.transpose(
                        out=pb[:, k8 * 128:(k8 + 1) * 128], in_=hsb[b2][:, sub, kc * 128:(kc + 1) * 128], identity=ident_b),
                        reads=[f"hsb{b2}{sub}", "cst_b"], writes=[f"tp{hb}"])
                dst = hsT[b2][:, hb * 8:(hb + 1) * 8, sub * 128:(sub + 1) * 128]
                src = pb.rearrange("p (a b) -> p a b", a=8)
                if hb == 0:
                    S.add("act", lambda h, dst=dst, src=src: h.activation(out=dst, in_=src, func=AF.Copy),
                          reads=[f"tp{hb}"], writes=[f"hsT{b2}"])
                else:
                    S.add("dve", lambda h, dst=dst, src=src: h.tensor_copy(out=dst, in_=src),
                          reads=[f"tp{hb}"], writes=[f"hsT{b2}"])
        for mb in range(4):
            gi = gu_i[0] % 2
            gu_i[0] += 1
            G, U = bank(gi, 0, TS), bank(2 + gi, 0, TS)
            for kc in range(KC):
                S.add("pe", lambda h, kc=kc, G=G, Wg=Wg, mb=mb, b2=b2: h.matmul(
                    G, lhsT=Wg[:, kc, mb * 128:(mb + 1) * 128], rhs=hsT[b2][:, kc, :],
                    start=(kc == 0), stop=(kc == KC - 1)),
                    reads=[f"ring{rg}", f"hsT{b2}"], writes=[f"G{gi}"])
            for kc in range(KC):
                S.add("pe", lambda h, kc=kc, U=U, Wup=Wup, mb=mb, b2=b2: h.matmul(
                    U, lhsT=Wup[:, kc, mb * 128:(mb + 1) * 128], rhs=hsT[b2][:, kc, :],
                    start=(kc == 0), stop=(kc == KC - 1)),
                    reads=[f"ring{ru}", f"hsT{b2}"], writes=[f"U{gi}"])
            S.add("act", lambda h, gi=gi, G=G: h.activation(out=sg[gi], in_=G, func=AF.Silu),
                  reads=[f"G{gi}"], writes=[f"sg{gi}"])
            S.add("dve", lambda h, gi=gi, U=U, mb=mb, b2=b2: h.tensor_tensor(
                out=aT[b2][:, mb, :], in0=sg[gi], in1=U, op=ALU.mult),
                reads=[f"sg{gi}", f"U{gi}"], writes=[f"aT{b2}"])
        for sub in range(SUB):
            yb = ys_i[0] % 2
            ys_i[0] += 1
            for nb in range(4):
                yi = y_i[0] % 2
                y_i[0] += 1
                Y = bank(4 + yi)
                for kc in range(4):
                    S.add("pe", lambda h, kc=kc, Y=Y, Wd=Wd, nb=nb, b2=b2, sub=sub: h.matmul(
                        Y, lhsT=aT[b2][:, kc, sub * 128:(sub + 1) * 128], rhs=Wd[:, kc, nb * 512:(nb + 1) * 512],
                        start=(kc == 0), stop=(kc == 3)),
                        reads=[f"ring{rd}", f"aT{b2}"], writes=[f"acc{yi}"])
                if nb % 2 == 0:
                    S.add("act", lambda h, Y=Y, nb=nb, yb=yb: h.activation(
                        out=ysb[yb][:, nb * 512:(nb + 1) * 512], in_=Y, func=AF.Copy),
                        reads=[f"acc{yi}"], writes=[f"ysb{yb}"])
                else:
                    S.add("dve", lambda h, Y=Y, nb=nb, yb=yb: h.tensor_copy(out=ysb[yb][:, nb * 512:(nb + 1) * 512], in_=Y),
                          reads=[f"acc{yi}"], writes=[f"ysb{yb}"])
            r0 = (st * SUB + sub) * 128
            dma("sp", YS[r0:r0 + 128, :], ysb[yb], f"yst{yb}", reads=[f"ysb{yb}"], writes=[("YS", st, sub)])

    S.fence()
    S.phase = 'G_p6_final'
    SC.reset()
    junk = SC.get(BF16, [D])
    gfrow = SC.get(F32, [D])
    shfrow = SC.get(F32, [D])
    gt2row = SC.get(F32, [D])
    ot = [SC.get(F32, [D]) for _ in range(2)]
    G1 = [sview(P_BYTES + (2 * i) * 8192, F32, [D]) for i in range(2)]
    G2 = [sview(P_BYTES + (2 * i + 1) * 8192, F32, [D]) for i in range(2)]
    scfrow = ot[1]
    dma("sp", gfrow, bcast(g_fin), "gfr", writes=["gfrow"])
    dma("sp", shfrow, bcast(modrow[:, V_SHF * D:(V_SHF + 1) * D]), "shfr",
        reads=["modrow0", "modrow1"], writes=["shfrow"])
    dma("sp", scfrow, bcast(modrow[:, V_SCF * D:(V_SCF + 1) * D]), "scfr",
        reads=["modrow0", "modrow1"], writes=["ot1"])
    dma("sp", gt2row, bcast(modrow[:, V_GT2 * D:(V_GT2 + 1) * D]), "gt2r",
        reads=["modrow0", "modrow1"], writes=["gt2row"])
    S.add("dve", lambda h: h.scalar_tensor_tensor(out=gfrow, in0=scfrow, scalar=1.0, in1=gfrow,
                                                   op0=ALU.add, op1=ALU.mult),
          reads=["gfrow", "ot1"], writes=["gfrow"])
    ys_all = [("YS", st, sub) for st in range(n_tiles) for sub in range(SUB)]
    for tt in range(NTT):
        b2 = tt % 2
        xb_i = tt % 4
        xb = X1[:, xb_i, :]
        xres = [f"xb{xb_i}"]
        dma("sp", xb, X1S[tt * 128:(tt + 1) * 128, :], f"x1l{xb_i}", reads=[("X1S", tt)], writes=xres)
        g1, g2 = G1[b2], G2[b2]
        for (Gb, sl, nm) in ((g1, slot1_i, f"G1{b2}"), (g2, slot2_i, f"G2{b2}")):
            S.add("pool", lambda h, Gb=Gb, sl=sl, tt=tt: h.indirect_dma_start(
                out=Gb, out_offset=None, in_=YS,
                in_offset=bass.IndirectOffsetOnAxis(ap=sl[:, tt:tt + 1], axis=0),
                bounds_check=bc_reg(h, NST * TS - 1), oob_is_err=False),
                reads=ys_all + ["slot1", "slot2"], writes=[nm], dma=nm)
        S.add("dve", lambda h, tt=tt, g1=g1: h.tensor_scalar(out=g1, in0=g1, scalar1=w1p[:, tt:tt + 1], scalar2=None, op0=ALU.mult),
              reads=[f"G1{b2}", "w1p"], writes=[f"G1{b2}"])
        S.add("dve", lambda h, tt=tt, g1=g1, g2=g2: h.scalar_tensor_tensor(out=g1, in0=g2, scalar=w2p[:, tt:tt + 1], in1=g1,
                                                                          op0=ALU.mult, op1=ALU.add),
              reads=[f"G1{b2}", f"G2{b2}", "w2p"], writes=[f"G1{b2}"])
        S.add("pool", lambda h, g1=g1: h.tensor_tensor(out=g1, in0=g1, in1=gt2row, op=ALU.mult),
              reads=[f"G1{b2}", "gt2row"], writes=[f"G1{b2}"])
        S.add("dve", lambda h, xb=xb, g1=g1: h.tensor_tensor(out=xb, in0=xb, in1=g1, op=ALU.add),
              reads=[f"G1{b2}"] + xres, writes=xres)
        ss = stat[:, b2:b2 + 1]
        rs = stat[:, 2 + b2:3 + b2]
        S.add("act", lambda h, xb=xb, ss=ss: h.activation(out=junk, in_=xb, func=AF.Square, accum_out=ss),
              reads=xres, writes=["junk", f"ss{b2}"])
        rms_rstd(None, ss, rs, D, [f"ss{b2}"], [f"rs{b2}"])
        S.add("dve", lambda h, xb=xb, rs=rs, b2=b2: h.scalar_tensor_tensor(
            out=ot[b2], in0=xb, scalar=rs, in1=gfrow, op0=ALU.mult, op1=ALU.mult),
            reads=xres + [f"rs{b2}", "gfrow"], writes=[f"ot{b2}"])
        S.add("pool", lambda h, b2=b2: h.tensor_tensor(out=ot[b2], in0=ot[b2], in1=shfrow, op=ALU.add),
              reads=[f"ot{b2}", "shfrow"], writes=[f"ot{b2}"])
        r = tt * 128
        dma("sp", y_out[r:r + 128, :], ot[b2], f"yo{b2}", reads=[f"ot{b2}"], writes=["yout"])

    finals = ["yo0", "yo1"] + (["dbg"] if dbg and "dbg" in S.dma_cnt else [])
    S.emit(nc, es, final_waits=finals)
    es.close()
    return nc


def _col(v):
    return np.ascontiguousarray(np.asarray(v, np.float32).reshape(KC, 128).T)


def prepare_shared(inp):
    f = lambda a: np.asarray(a, dtype=np.float32)
    sh = {}
    sh["w_mod"] = np.ascontiguousarray(np.concatenate([f(inp["w_ada"])[0], f(inp["w_ada_final"])], axis=1))
    sh["b_mod"] = np.ascontiguousarray(np.concatenate([f(inp["b_ada"])[0], f(inp["b_ada_final"])])[None, :])
    g_out = np.concatenate([f(inp["g_out_a"])[0], f(inp["g_out_b"])[0]])
    sh["gcols"] = np.ascontiguousarray(np.stack([_col(f(inp["g_mix"])[0]), _col(f(inp["g_ffn"])[0]), _col(g_out)], axis=1))
    sh["g_fin"] = f(inp["g_final"])[None, :].copy()
    w_in = f(inp["w_in"])[0]
    units = []
    for a in range(8):
        g = a // 2
        q = w_in[:, a * 128:(a + 1) * 128]
        k = w_in[:, 1024 + g * 64:1024 + (g + 1) * 64]
        v = w_in[:, 1280 + g * 64:1280 + (g + 1) * 64]
        units.append(np.concatenate([q, k, k, v, v], axis=1))
    for bp in range(8):
        units.append(np.concatenate([w_in[:, 1536 + bp * 128:1536 + (bp + 1) * 128],
                                     w_in[:, 2560 + bp * 128:2560 + (bp + 1) * 128],
                                     w_in[:, 3584 + bp * 128:3584 + (bp + 1) * 128]], axis=1))
    wu = np.stack(units)
    sh["w_in_u"] = np.ascontiguousarray(wu.reshape(16, KC, 128, 384).transpose(0, 2, 1, 3))
    sh["w_out"] = np.ascontiguousarray(f(inp["w_out"])[0])
    sh["w_rt"] = np.ascontiguousarray(np.concatenate([f(inp["w_router_group"])[0], f(inp["w_router_expert"])[0]], axis=1))
    sh["b_rt"] = np.concatenate([f(inp["b_router_group"])[0], f(inp["b_router_expert"])[0]])[None, :].copy()
    sh["w_gate"] = np.ascontiguousarray(f(inp["w_gate"])[0].reshape(N_EXP, KC, 128, DE).transpose(0, 2, 1, 3)).reshape(N_EXP * 128, KC * DE)
    sh["w_up"] = np.ascontiguousarray(f(inp["w_up"])[0].reshape(N_EXP, KC, 128, DE).transpose(0, 2, 1, 3)).reshape(N_EXP * 128, KC * DE)
    sh["w_down"] = np.ascontiguousarray(f(inp["w_down"])[0].reshape(N_EXP, 4, 128, D).transpose(0, 2, 1, 3)).reshape(N_EXP * 128, 4 * D)
    sh["g_ffn_row"] = f(inp["g_ffn"])[0][None, :].copy()
    c2 = np.zeros((128, 192), np.float32)
    c2[:, 0:128] = (np.arange(128)[:, None] < np.arange(128)[None, :]).astype(np.float32)
    c2[:, 128] = np.arange(128)
    c2[:, 129:137] = float(TS) * np.arange(8)[None, :]
    c2[:, 137:137 + NST] = float(TS) * np.arange(NST)[None, :]
    sh["cst2"] = c2
    rb = f(inp["rel_bias_b"])[0]
    ki = np.arange(128)[:, None]
    qi = np.arange(128)[None, :]
    idx0 = np.clip(128 + qi - ki, -128, 128) + 128
    idx1 = np.clip(qi - ki, -128, 128) + 128
    tab = np.stack([rb[:, idx0], rb[:, idx1]], axis=1)
    sh["tabB"] = np.ascontiguousarray(tab.transpose(2, 0, 1, 3))
    sh["constB"] = np.ascontiguousarray(np.broadcast_to(rb[:, 256][None, :], (128, 16)))
    sh["sinks"] = np.ascontiguousarray(np.broadcast_to(f(inp["sinks_a"])[0][None, :], (128, 16)))
    cst = np.zeros((128, 6, 128), np.float32)
    cst[:, 0, :] = np.eye(128, dtype=np.float32)
    cst[:64, 1, 64:] = NEG
    cst[64:, 2, :64] = NEG
    cst[:, 3, :] = 128 + qi - ki
    cst[:, 4, :] = np.abs(qi - ki)
    sh["cst"] = cst
    return sh


def prepare_core(inp, core):
    b, hf = core // 2, core % 2
    x = np.asarray(inp["x"], dtype=np.float32)
    xh = np.zeros((HALO + NB_TOK_CORE, D), np.float32)
    lo = hf * NB_TOK_CORE
    xh[HALO:] = x[b, lo:lo + NB_TOK_CORE]
    if hf > 0:
        xh[:HALO] = x[b, lo - HALO:lo]
    d = {"xh": xh, "c_col": _col(np.asarray(inp["c"], np.float32)[b]),
         "hm": np.full((128, 1), 1.0 if hf > 0 else 0.0, np.float32)}
    return d


_NC_CACHE = {}


def kernel(**inputs):
    if "nc" not in _NC_CACHE:
        _NC_CACHE["nc"] = build_program()
    nc = _NC_CACHE["nc"]
    sh = prepare_shared(inputs)
    in_maps = []
    for core in range(8):
        d = dict(sh)
        d.update(prepare_core(inputs, core))
        in_maps.append(d)
    res = run_bass_kernel_spmd(nc, in_maps, core_ids=list(range(8)))
    B, S_, _ = np.asarray(inputs["x"]).shape
    out = np.empty((B, S_, D), np.float32)
    for core in range(8):
        b, hf = core // 2, core % 2
        out[b, hf * NB_TOK_CORE:(hf + 1) * NB_TOK_CORE] = res.results[core]["y"]
    return out
```

```python
import types
import numpy as np
from contextlib import ExitStack
import concourse.bass as bass
import concourse.mybir as mybir
from concourse.bass_utils import run_bass_kernel_spmd

F32 = mybir.dt.float32
BF16 = mybir.dt.bfloat16
AF = mybir.ActivationFunctionType
ALU = mybir.AluOpType
AX = mybir.AxisListType

ENGS = ("pe", "act", "dve", "pool", "sp")
NEG = -30000.0
EPS = 1e-6


def _freeze(fn):
    if fn.__closure__ is None:
        return fn
    cells = []
    for c in fn.__closure__:
        try:
            cells.append(types.CellType(c.cell_contents))
        except ValueError:
            cells.append(c)
    return types.FunctionType(fn.__code__, fn.__globals__, fn.__name__, fn.__defaults__, tuple(cells))


class Sched:
    def __init__(self):
        self.ops = []
        self.last_w = {}
        self.readers = {}
        self.dma_cnt = {}
        self.dma_last = {}
        self.eng_ops = {e: [] for e in ENGS}
        self.fence_set = set()
        self.fence_passed = {e: True for e in ENGS}
        self.profile_scopes = False
        self.profile_engs = ('pe',)
        self.phase = 'setup'

    def fence(self):
        s = set()
        for e in ENGS:
            if self.eng_ops[e]:
                s.add(self.eng_ops[e][-1])
        for k, i in self.dma_last.items():
            s.add(i)
        self.fence_set = s
        self.fence_passed = {e: False for e in ENGS}

    def add(self, eng, fn, reads=(), writes=(), dma=None):
        i = len(self.ops)
        deps = set()
        for r in reads:
            w = self.last_w.get(r)
            if w is not None:
                deps.add(w)
        for r in writes:
            w = self.last_w.get(r)
            if w is not None:
                deps.add(w)
            for rd in self.readers.get(r, ()):
                deps.add(rd)
        if not self.fence_passed[eng]:
            deps |= self.fence_set
            self.fence_passed[eng] = True
        if dma is not None and eng == "pool" and dma in self.dma_last:
            deps.add(self.dma_last[dma])
        op = dict(eng=eng, fn=_freeze(fn), deps=deps, dma=dma, idx=i, phase=getattr(self, 'phase', None))
        if dma is not None:
            self.dma_cnt[dma] = self.dma_cnt.get(dma, 0) + 16
            op["cnt"] = self.dma_cnt[dma]
            self.dma_last[dma] = i
        self.eng_ops[eng].append(i)
        op["ord"] = len(self.eng_ops[eng])
        self.ops.append(op)
        for r in reads:
            self.readers.setdefault(r, []).append(i)
        for r in writes:
            self.last_w[r] = i
            self.readers[r] = []
        return i

    def plan(self):
        ops = self.ops
        clock = {e: {} for e in ENGS}
        opclock = [None] * len(ops)
        waits = [None] * len(ops)
        signal = [False] * len(ops)

        def key_of(j):
            o = ops[j]
            if o["dma"] is not None:
                return ("dma", o["dma"]), o["cnt"]
            return o["eng"], o["ord"]

        for i, o in enumerate(ops):
            e = o["eng"]
            ck = clock[e]
            w = []
            for j in sorted(o["deps"]):
                pj = ops[j]
                if pj["dma"] is None and pj["eng"] == e == "pe":
                    continue
                k, v = key_of(j)
                if ck.get(k, 0) >= v:
                    continue
                w.append(j)
                signal[j] = True
                ck[k] = v
                for kk, vv in opclock[j].items():
                    if ck.get(kk, 0) < vv:
                        ck[kk] = vv
            waits[i] = w
            oc = dict(ck)
            if o["dma"] is not None:
                oc[("dma", o["dma"])] = o["cnt"]
            else:
                oc[e] = max(oc.get(e, 0), o["ord"])
            opclock[i] = oc
        semval = [0] * len(ops)
        for e in ENGS:
            c = 0
            for i in self.eng_ops[e]:
                if ops[i]["dma"] is None and signal[i]:
                    c += 1
                    semval[i] = c
        self.waits, self.signal, self.semval = waits, signal, semval

    def emit(self, nc, es, final_waits=()):
        self.plan()
        ops, waits, signal, semval = self.ops, self.waits, self.signal, self.semval
        eng_sems = {e: es.enter_context(nc.semaphore(f"s_{e}")) for e in ENGS}
        dma_sems = {k: es.enter_context(nc.semaphore(f"d_{k}")) for k in self.dma_cnt}
        block = es.enter_context(nc.Block())
        handles = {"pe": "tensor", "act": "scalar", "dve": "vector",
                   "pool": "gpsimd", "sp": "sync"}

        def run_engine(e, h):
            cur_scope = [None, None]
            for i in self.eng_ops[e]:
                o = ops[i]
                if self.profile_scopes and e in self.profile_engs and o["phase"] != cur_scope[0]:
                    if cur_scope[1] is not None:
                        cur_scope[1].__exit__(None, None, None)
                    cur_scope[0] = o["phase"]
                    cur_scope[1] = nc.named_scope(str(o["phase"]))
                    cur_scope[1].__enter__()
                for j in waits[i]:
                    pj = ops[j]
                    if pj["dma"] is not None:
                        h.wait_ge(dma_sems[pj["dma"]], pj["cnt"])
                    else:
                        h.wait_ge(eng_sems[pj["eng"]], semval[j])
                ins = o["fn"](h)
                if o["dma"] is not None:
                    ins.then_inc(dma_sems[o["dma"]], 16)
                elif signal[i]:
                    ins.then_inc(eng_sems[e], 1)
            if cur_scope[1] is not None:
                cur_scope[1].__exit__(None, None, None)
            if e == "sp":
                for k in final_waits:
                    h.wait_ge(dma_sems[k], self.dma_cnt[k])

        for e in ENGS:
            if not self.eng_ops[e] and not (e == "sp" and final_waits):
                continue
            getattr(block, handles[e])(lambda h, e=e: run_engine(e, h))


D = 2048
KC = 16
NB_TOK_CORE = 2048
HALF = 1024
HALO = 512
WIN = HALF + HALO
NWT = WIN // 128
NOT = HALF // 128
N_EXP = 32
DE = 512
NMODBLK = 32
SUB = 2
TS = SUB * 128
NTT = 16
NST = (2 * NB_TOK_CORE + 32 * (TS - 1)) // TS
I32 = mybir.dt.int32
SLOPES = [2.0 ** (-8.0 * (h + 1) / 16.0) for h in range(16)]
V_SH1, V_SC1, V_GT1, V_SH2, V_SC2, V_GT2, V_SHF, V_SCF = range(8)


def build_program(n_experts=N_EXP, dbg=None, profile_scopes=False):
    nc = bass.Bass("TRN2", target_bir_lowering=False)
    S = Sched()
    S.profile_scopes = profile_scopes

    def dram_in(name, shape, dt=F32):
        return nc.dram_tensor(name, list(shape), dt, kind="ExternalInput").ap()

    xh = dram_in("xh", [HALO + NB_TOK_CORE, D])
    c_col = dram_in("c_col", [128, KC])
    w_mod = dram_in("w_mod", [D, NMODBLK * 512])
    b_mod = dram_in("b_mod", [1, NMODBLK * 512])
    gcols = dram_in("gcols", [128, 3, KC])
    g_fin = dram_in("g_fin", [1, D])
    w_in_u = dram_in("w_in_u", [16, 128, KC, 384])
    w_out = dram_in("w_out", [D, D])
    w_rt = dram_in("w_rt", [D, 36])
    b_rt = dram_in("b_rt", [1, 36])
    w_gate = dram_in("w_gate", [N_EXP * 128, KC * DE])
    w_up = dram_in("w_up", [N_EXP * 128, KC * DE])
    w_down = dram_in("w_down", [N_EXP * 128, 4 * D])
    g_ffn_row = dram_in("g_ffn_row", [1, D])
    cst2_d = dram_in("cst2", [128, 192])
    tabB_d = dram_in("tabB", [128, 16, 2, 128])
    constB_d = dram_in("constB", [128, 16])
    sinks_d = dram_in("sinks", [128, 16])
    hm_d = dram_in("hm", [128, 1])
    cst_d = dram_in("cst", [128, 6, 128])
    y_out = nc.dram_tensor("y", [NB_TOK_CORE, D], F32, kind="ExternalOutput").ap()
    modrow = nc.dram_tensor("modrow", [1, NMODBLK * 512], F32, kind="Internal").ap()
    HS = nc.dram_tensor("HS", [NST * TS, D], BF16, kind="Internal").ap()
    YS = nc.dram_tensor("YS", [NST * TS, D], F32, kind="Internal").ap()
    H2S = nc.dram_tensor("H2S", [NB_TOK_CORE, D], BF16, kind="Internal").ap()
    X1S = nc.dram_tensor("X1S", [NB_TOK_CORE, D], F32, kind="Internal").ap()
    dbg_out = {}
    if dbg:
        for k, shp in dbg.items():
            if k.startswith('_'):
                continue
            dbg_out[k] = nc.dram_tensor("dbg_" + k, list(shp), F32, kind="ExternalOutput").ap()

    es = ExitStack()
    ARENA_F32 = 51200
    arena = es.enter_context(nc.sbuf_tensor("arena", [128, ARENA_F32], F32))
    psum = es.enter_context(nc.psum_tensor("psum", [128, 4096], F32))

    def sview(off, dt, shape):
        n = int(np.prod(shape))
        nbytes = n * (2 if dt == BF16 else 4)
        assert off % 4 == 0 and nbytes % 4 == 0
        assert off + nbytes <= ARENA_F32 * 4, (off, nbytes)
        a = arena[:, off // 4:(off + nbytes) // 4]
        if dt != F32:
            a = a.bitcast(dt)
        if len(shape) >= 2:
            names = [f"d{i}" for i in range(len(shape))]
            pat = "p (" + " ".join(names) + ") -> p " + " ".join(names)
            a = a.rearrange(pat, **{n: int(v) for n, v in zip(names[:-1], shape[:-1])})
        return a

    class Alloc:
        def __init__(self, base, limit):
            self.base, self.off, self.limit = base, base, limit

        def reset(self):
            self.off = self.base

        def get(self, dt, shape):
            n = int(np.prod(shape)) * (2 if dt == BF16 else 4)
            n = (n + 63) // 64 * 64
            v = sview(self.off, dt, shape)
            self.off += n
            assert self.off <= self.limit, (self.off, self.limit)
            return v

    P_BYTES = 24576
    FT_BYTES = KC * WIN * 2
    OB_BYTES = 65536
    PA = Alloc(0, P_BYTES)
    FT = sview(P_BYTES, BF16, [KC, WIN])
    OB_OFF = P_BYTES + FT_BYTES
    O_sb = sview(OB_OFF, BF16, [NOT, D])
    X1 = sview(OB_OFF, F32, [NOT, D])
    SC = Alloc(OB_OFF + OB_BYTES, ARENA_F32 * 4)

    def bank(b, lo=0, hi=512):
        return psum[:, b * 512 + lo:b * 512 + hi]

    def bank_bf(b):
        return psum[:, b * 512:(b + 1) * 512].bitcast(BF16)

    colall = PA.get(F32, [NMODBLK * 4])
    gc = PA.get(F32, [3, KC])
    gm1 = PA.get(F32, [KC])
    gm2 = PA.get(F32, [KC])
    ident_f = PA.get(F32, [128])
    cst_b = PA.get(BF16, [3, 128])
    relA = PA.get(F32, [2, 128])
    maskA = PA.get(F32, [2, 128])
    tabB = PA.get(BF16, [16, 2, 128])
    constB = PA.get(F32, [16])
    esink = PA.get(F32, [16])
    hm = PA.get(F32, [1])
    brow = PA.get(F32, [36])
    wr = PA.get(BF16, [KC, 36])
    comb = PA.get(F32, [NOT, 32])
    rsP = PA.get(F32, [NTT])
    w1p = PA.get(F32, [NTT])
    w2p = PA.get(F32, [NTT])
    slot1_i = PA.get(I32, [NTT])
    slot2_i = PA.get(I32, [NTT])
    oh1_all = PA.get(F32, [NTT, 32])
    oh2_all = PA.get(F32, [NTT, 32])
    idxW = PA.get(I32, [NST])
    cst2 = PA.get(F32, [64])
    ones_b = PA.get(BF16, [128])
    tri_b = PA.get(BF16, [128])
    piota = cst2[:, 0:1]
    thr8 = cst2[:, 1:9]
    s128 = cst2[:, 9:9 + NST]
    stat = PA.get(F32, [64])
    ident_b = cst_b[:, 0, :]
    maskF_b = cst_b[:, 1, :]

    _bc_regs = {}

    def bc_reg(h, val):
        if val not in _bc_regs:
            r = h.alloc_register(f"bc{val}")
            h.reg_mov(r, val)
            _bc_regs[val] = r
        return _bc_regs[val]

    def bcast(ap_row):
        return ap_row.to_broadcast([128, ap_row.shape[1]])

    def dma(eng, out, in_, key, reads=(), writes=()):
        return S.add(eng, lambda h: h.dma_start(out=out, in_=in_), reads=reads, writes=writes, dma=key)

    SC.reset()
    cst_f = SC.get(F32, [6, 128])
    ccol = SC.get(F32, [KC])
    cact = SC.get(F32, [KC])
    carep = SC.get(BF16, [KC, 128])
    sink_f = SC.get(F32, [16])
    dma("sp", cst_f, cst_d, "c0", writes=["cst_f"])
    dma("pool", tabB, tabB_d, "c1", writes=["tabB"])
    dma("sp", ccol, c_col, "c2", writes=["ccol"])
    dma("sp", gc, gcols, "c3", writes=["gc"])
    dma("sp", constB, constB_d, "c4", writes=["constB"])
    dma("sp", sink_f, sinks_d, "c5", writes=["sink_f"])
    dma("sp", hm, hm_d, "c6", writes=["hm"])
    dma("sp", brow, bcast(b_rt), "c7", writes=["brow"])
    dma("pool", wr, w_rt.rearrange("(kc p) n -> p kc n", p=128), "c8", writes=["wr"])
    dma("pool", tri_b, cst2_d[:, 0:128], "c9", writes=["tri_b"])
    dma("sp", cst2[:, 0:56], cst2_d[:, 128:184], "c10", writes=["cst2"])
    S.add("pool", lambda h: h.memset(ones_b, 1.0), writes=["ones_b"])

    S.add("dve", lambda h: h.tensor_copy(out=ident_f, in_=cst_f[:, 0, :]), reads=["cst_f"], writes=["ident_f"])
    S.add("dve", lambda h: h.tensor_copy(out=cst_b, in_=cst_f[:, 0:3, :]), reads=["cst_f"], writes=["cst_b"])
    S.add("dve", lambda h: h.tensor_copy(out=relA, in_=cst_f[:, 3:5, :]), reads=["cst_f"], writes=["relA"])
    S.add("dve", lambda h: h.tensor_copy(out=maskA, in_=cst_f[:, 1:3, :]), reads=["cst_f"], writes=["maskA"])
    S.add("dve", lambda h: h.tensor_tensor(out=tabB[:, :, 1, :], in0=tabB[:, :, 1, :],
                                            in1=cst_b[:, 2, :].unsqueeze(1).to_broadcast([128, 16, 128]),
                                            op=ALU.add), reads=["cst_b", "tabB"], writes=["tabB"])
    S.add("act", lambda h: h.activation(out=esink, in_=sink_f, func=AF.Exp), reads=["sink_f"], writes=["esink"])
    S.add("act", lambda h: h.activation(out=cact, in_=ccol, func=AF.Silu), reads=["ccol"], writes=["cact"])
    S.add("dve", lambda h: h.tensor_copy(out=carep, in_=cact.unsqueeze(2).to_broadcast([128, KC, 128])),
          reads=["cact"], writes=["carep"])

    S.phase = 'p0_mod'
    wblk = [SC.get(BF16, [KC, 512]) for _ in range(2)]
    rowt = [SC.get(F32, [512]) for _ in range(2)]
    biasb = [SC.get(F32, [512]) for _ in range(2)]
    diag = [SC.get(F32, [4, 128]) for _ in range(2)]
    w_mod_v = w_mod.rearrange("(kc p) n -> p kc n", p=128)
    for j in range(NMODBLK):
        wb = wblk[j % 2]
        rb_, bb, dg = rowt[j % 2], biasb[j % 2], diag[j % 2]
        acc = bank(4 + j % 2)
        dma("pool", wb, w_mod_v[:, :, j * 512:(j + 1) * 512], f"wm{j % 2}", writes=[f"wblk{j % 2}"])
        dma("sp", bb, bcast(b_mod[:, j * 512:(j + 1) * 512]), f"bm{j % 2}",
            writes=[f"biasb{j % 2}"])
        for kc in range(KC):
            S.add("pe", lambda h, kc=kc, wb=wb, acc=acc: h.matmul(acc, lhsT=carep[:, kc, :], rhs=wb[:, kc, :],
                                                                   start=(kc == 0), stop=(kc == KC - 1)),
                  reads=["carep", f"wblk{j % 2}"], writes=[f"acc{j % 2}"])
        S.add("dve", lambda h, rb_=rb_, bb=bb, acc=acc: h.tensor_tensor(out=rb_, in0=acc, in1=bb, op=ALU.add),
              reads=[f"acc{j % 2}", f"biasb{j % 2}"], writes=[f"rowt{j % 2}"])
        dma("sp", modrow[:, j * 512:(j + 1) * 512], rb_[0:1, :], f"mrow{j % 2}", reads=[f"rowt{j % 2}"], writes=[f"modrow{j % 2}"])
        S.add("dve", lambda h, rb_=rb_, dg=dg: h.tensor_tensor(
            out=dg, in0=rb_.rearrange("p (a b) -> p a b", a=4),
            in1=ident_f.unsqueeze(1).to_broadcast([128, 4, 128]), op=ALU.mult),
            reads=[f"rowt{j % 2}", "ident_f"], writes=[f"diag{j % 2}"])
        S.add("dve", lambda h, dg=dg, j=j: h.tensor_reduce(out=colall[:, j * 4:(j + 1) * 4], in_=dg,
                                                            axis=AX.X, op=ALU.add),
              reads=[f"diag{j % 2}"], writes=["colall"])

    def colv(v):
        return colall[:, v * KC:(v + 1) * KC]

    for (gm, gi, vs) in ((gm1, 0, V_SC1), (gm2, 1, V_SC2)):
        S.add("dve", lambda h, gm=gm, gi=gi, vs=vs: h.scalar_tensor_tensor(
            out=gm, in0=colv(vs), scalar=1.0, in1=gc[:, gi, :], op0=ALU.add, op1=ALU.mult),
            reads=["colall", "gc"], writes=["gm"])

    def dbg_dump(name, src_ap, dst_ap, reads):
        if dbg and name in dbg:
            dma("sp", dst_ap, src_ap, "dbg", reads=reads, writes=["dbgout"])

    if dbg and "colall" in dbg:
        dbg_dump("colall", colall, dbg_out["colall"], ["colall"])

    def rms_rstd(eng_h, ss, rstd, n, reads, writes):
        S.add("dve", lambda h: h.tensor_scalar(out=rstd, in0=ss, scalar1=1.0 / n, scalar2=EPS,
                                                op0=ALU.mult, op1=ALU.add), reads=reads, writes=writes)
        S.add("act", lambda h: h.activation(out=rstd, in_=rstd, func=AF.Ln), reads=writes, writes=writes)
        S.add("act", lambda h: h.activation(out=rstd, in_=rstd, func=AF.Exp, scale=-0.5), reads=writes, writes=writes)

    def transposes_to_FT(src_bf, src_res, lt, scale_col, bias_col, tag):
        for hb in range(2):
            pb = bank_bf(6 + hb)
            for k8 in range(8):
                kc = hb * 8 + k8
                S.add("pe", lambda h, pb=pb, k8=k8, kc=kc: h.transpose(
                    out=pb[:, k8 * 128:(k8 + 1) * 128], in_=src_bf[:, kc * 128:(kc + 1) * 128], identity=ident_b),
                    reads=[src_res, "cst_b"], writes=[f"tp{hb}"])
            for k8 in range(8):
                kc = hb * 8 + k8
                dst = FT[:, kc, lt * 128:(lt + 1) * 128]
                src = pb[:, k8 * 128:(k8 + 1) * 128]
                if k8 % 2 == 0:
                    if bias_col is not None:
                        S.add("act", lambda h, dst=dst, src=src, kc=kc: h.activation(
                            out=dst, in_=src, func=AF.Identity, scale=scale_col[:, kc:kc + 1],
                            bias=bias_col[:, kc:kc + 1]),
                            reads=[f"tp{hb}", "gm", "colall", "gc"], writes=[("FT", lt)])
                    else:
                        S.add("act", lambda h, dst=dst, src=src, kc=kc: h.activation(
                            out=dst, in_=src, func=AF.Copy, scale=scale_col[:, kc:kc + 1]),
                            reads=[f"tp{hb}", "gm", "colall", "gc"], writes=[("FT", lt)])
                else:
                    if bias_col is not None:
                        S.add("dve", lambda h, dst=dst, src=src, kc=kc: h.tensor_scalar(
                            out=dst, in0=src, scalar1=scale_col[:, kc:kc + 1], scalar2=bias_col[:, kc:kc + 1],
                            op0=ALU.mult, op1=ALU.add),
                            reads=[f"tp{hb}", "gm", "colall", "gc"], writes=[("FT", lt)])
                    else:
                        S.add("dve", lambda h, dst=dst, src=src, kc=kc: h.tensor_scalar(
                            out=dst, in0=src, scalar1=scale_col[:, kc:kc + 1], scalar2=None, op0=ALU.mult),
                            reads=[f"tp{hb}", "gm", "colall", "gc"], writes=[("FT", lt)])

    for H in range(2):
        row0 = H * HALF
        S.fence()
        S.phase = f'H{H}_p1_norm1'
        SC.reset()
        xt = [SC.get(F32, [D]) for _ in range(2)]
        xnb = [SC.get(BF16, [D]) for _ in range(2)]
        junk = SC.get(BF16, [D])
        xt = xt + [SC.get(F32, [D])]
        ss12 = stat[:, 16:16 + NWT]
        rs12 = stat[:, 32:32 + NWT]
        for lt in range(NWT):
            b3 = lt % 3
            dma("sp", xt[b3], xh[row0 + lt * 128: row0 + (lt + 1) * 128, :], f"xt{b3}", writes=[f"xt{b3}"])
            S.add("act", lambda h, b3=b3, lt=lt: h.activation(out=junk, in_=xt[b3], func=AF.Square,
                                                              accum_out=ss12[:, lt:lt + 1]),
                  reads=[f"xt{b3}"], writes=["junk", "ss12"])
        rms_rstd(None, ss12, rs12, D, ["ss12"], ["rs12"])

        def p1_norm(lt):
            b3, b2 = lt % 3, lt % 2
            dma("sp", xt[b3], xh[row0 + lt * 128: row0 + (lt + 1) * 128, :], f"xt{b3}", writes=[f"xt{b3}"])
            S.add("act", lambda h, b3=b3, b2=b2, lt=lt: h.activation(out=xnb[b2], in_=xt[b3], func=AF.Copy,
                                                                     scale=rs12[:, lt:lt + 1]),
                  reads=[f"xt{b3}", "rs12"], writes=[f"xnb{b2}"])

        p1_norm(0)
        for lt in range(NWT):
            if lt + 1 < NWT:
                p1_norm(lt + 1)
            transposes_to_FT(xnb[lt % 2], f"xnb{lt % 2}", lt, gm1, colv(V_SH1), "h")
        if dbg and "hT" in dbg and H == dbg.get("_H", 1):
            tmpf = SC.get(F32, [KC, 128])
            for lt in range(NWT):
                S.add("dve", lambda h, lt=lt: h.tensor_copy(out=tmpf, in_=FT[:, :, lt * 128:(lt + 1) * 128]),
                      reads=[("FT", lt)], writes=["tmpf"])
                dma("sp", dbg_out["hT"][lt], tmpf, "dbg", reads=["tmpf"], writes=["dbgout"])

        S.fence()
        S.phase = f'H{H}_p2_attn'
        SC.reset()
        Wu = [SC.get(BF16, [KC, 384]) for _ in range(2)]
        QT = SC.get(BF16, [HALF])
        KT = SC.get(BF16, [WIN])
        VA = SC.get(BF16, [NWT, 2, 65])
        tabA = [SC.get(BF16, [2, 2, 2, 128]) for _ in range(2)]
        tA_f = SC.get(F32, [2, 128])
        tA_h = SC.get(F32, [2, 128])
        PT = [SC.get(BF16, [5, 128]) for _ in range(2)]
        rec = [SC.get(F32, [2]) for _ in range(2)]
        S.add("pool", lambda h: h.memset(VA[:, :, :, 64:65], 1.0), writes=["VA"])
        if H == 0:
            S.add("pool", lambda h: h.tensor_copy(out=VA[:, 0:4, :, 64:65],
                                                  in_=hm.unsqueeze(1).unsqueeze(1).to_broadcast([128, 4, 2, 1])),
                  reads=["hm"], writes=["VA"])
        for b2 in range(2):
            S.add("pool", lambda h, b2=b2: h.memset(PT[b2], 0.0), writes=[f"PT{b2}"])

        acc_i = [0]

        def next_acc():
            a = acc_i[0] % 2
            acc_i[0] += 1
            return a

        for u in range(16):
            grpA = u < 8
            wu = Wu[u % 2]
            dma("pool", wu, w_in_u[u], f"wu{u % 2}", writes=[f"wu{u % 2}"])
            wres = f"wu{u % 2}"
            for tb in range(HALF // 512):
                a = next_acc()
                for kc in range(KC):
                    S.add("pe", lambda h, a=a, kc=kc, tb=tb, wu=wu: h.matmul(
                        bank(4 + a), lhsT=wu[:, kc, 0:128], rhs=FT[:, kc, HALO + tb * 512: HALO + (tb + 1) * 512],
                        start=(kc == 0), stop=(kc == KC - 1)),
                        reads=[wres] + [("FT", 4 + tb * 4 + i) for i in range(4)], writes=[f"acc{a}"])
                S.add("dve", lambda h, a=a, tb=tb: h.tensor_scalar(
                    out=QT[:, tb * 512:(tb + 1) * 512], in0=bank(4 + a), scalar1=0.125, scalar2=None, op0=ALU.mult),
                    reads=[f"acc{a}"], writes=["QT"])
            do_kv = (not grpA) or (u % 2 == 0)
            if do_kv:
                for tb in range(WIN // 512):
                    a = next_acc()
                    for kc in range(KC):
                        S.add("pe", lambda h, a=a, kc=kc, tb=tb, wu=wu: h.matmul(
                            bank(4 + a), lhsT=wu[:, kc, 128:256], rhs=FT[:, kc, tb * 512:(tb + 1) * 512],
                            start=(kc == 0), stop=(kc == KC - 1)),
                            reads=[wres] + [("FT", tb * 4 + i) for i in range(4)], writes=[f"acc{a}"])
                    S.add("dve", lambda h, a=a, tb=tb: h.tensor_copy(out=KT[:, tb * 512:(tb + 1) * 512], in_=bank(4 + a)),
                          reads=[f"acc{a}"], writes=["KT"])
                for lt in range(NWT):
                    a = next_acc()
                    for kc in range(KC):
                        S.add("pe", lambda h, a=a, kc=kc, lt=lt, wu=wu: h.matmul(
                            bank(4 + a, 0, 128), lhsT=FT[:, kc, lt * 128:(lt + 1) * 128], rhs=wu[:, kc, 256:384],
                            start=(kc == 0), stop=(kc == KC - 1)),
                            reads=[wres, ("FT", lt)], writes=[f"acc{a}"])
                    src = bank(4 + a, 0, 128).rearrange("p (a b) -> p a b", a=2)
                    if H == 0 and lt < 4:
                        S.add("dve", lambda h, lt=lt, src=src: h.tensor_scalar(
                            out=VA[:, lt, :, 0:64], in0=src, scalar1=hm[:, 0:1], scalar2=None, op0=ALU.mult),
                            reads=[f"acc{a}", "hm"], writes=["VA"])
                    else:
                        S.add("dve", lambda h, lt=lt, src=src: h.tensor_copy(out=VA[:, lt, :, 0:64], in_=src),
                              reads=[f"acc{a}"], writes=["VA"])
            if grpA:
                tb_ = tabA[u % 2]
                for j in range(2):
                    hd = 2 * u + j
                    S.add("pool", lambda h, hd=hd: h.tensor_scalar(
                        out=tA_f, in0=relA, scalar1=-SLOPES[hd], scalar2=None, op0=ALU.mult),
                        reads=["relA", "tA_h"], writes=["tA_f"])
                    S.add("pool", lambda h: h.tensor_tensor(out=tA_f, in0=tA_f, in1=maskA, op=ALU.add),
                          reads=["tA_f", "maskA"], writes=["tA_f"])
                    S.add("pool", lambda h, j=j, tb_=tb_: h.tensor_copy(out=tb_[:, j, 0, :, :], in_=tA_f),
                          reads=["tA_f"], writes=[f"tabA{u % 2}"])
                    S.add("pool", lambda h, j=j, tb_=tb_: h.tensor_copy(out=tA_h, in_=tb_[:, j, 0, :, :]),
                          reads=[f"tabA{u % 2}"], writes=["tA_h"])
                    S.add("pool", lambda h: h.tensor_tensor(out=tA_h, in0=tA_f, in1=tA_h, op=ALU.subtract),
                          reads=["tA_f", "tA_h"], writes=["tA_h"])
                    S.add("pool", lambda h, j=j, tb_=tb_: h.tensor_copy(out=tb_[:, j, 1, :, :], in_=tA_h),
                          reads=["tA_h"], writes=[f"tabA{u % 2}"])
            items = [(qt, j) for qt in range(NOT) for j in range(2)]

            def emit_qk(n):
                qt, j = items[n]
                sb = n % 2
                hd = (2 * u + j) if grpA else (2 * (u - 8) + j)
                pl, ph = j * 64, (j + 1) * 64
                q_ap = QT[pl:ph, qt * 128:(qt + 1) * 128]
                s_main = bank(sb, 0, 384)
                s_last = bank(2 + sb, 0, 256)
                blocks = range(3, 5) if grpA else range(5)
                for i in blocks:
                    lt = qt + i
                    dst = s_main[:, i * 128:(i + 1) * 128] if i < 3 else s_last[:, (i - 3) * 128:(i - 2) * 128]
                    sres = f"S{sb}" if i < 3 else f"S2{sb}"
                    has_tab = grpA or i in (0, 3, 4)
                    S.add("pe", lambda h, dst=dst, lt=lt, pl=pl, ph=ph, q_ap=q_ap, has_tab=has_tab: h.matmul(
                        dst, lhsT=KT[pl:ph, lt * 128:(lt + 1) * 128], rhs=q_ap, start=True, stop=not has_tab),
                        reads=["KT", "QT"], writes=[sres])
                    if grpA:
                        tb_ = tabA[u % 2]
                        for hl in range(2):
                            S.add("pe", lambda h, dst=dst, j=j, hl=hl, i=i, tb_=tb_: h.matmul(
                                dst, lhsT=ident_b, rhs=tb_[:, j, hl, i - 3, :], start=False, stop=(hl == 1)),
                                reads=["cst_b", f"tabA{u % 2}"], writes=[sres])
                    elif i == 0:
                        S.add("pe", lambda h, dst=dst: h.matmul(dst, lhsT=ident_b, rhs=maskF_b, start=False, stop=True),
                              reads=["cst_b"], writes=[sres])
                    elif i >= 3:
                        S.add("pe", lambda h, dst=dst, hd=hd, i=i: h.matmul(
                            dst, lhsT=ident_b, rhs=tabB[:, hd, i - 3, :], start=False, stop=True),
                            reads=["cst_b", "tabB"], writes=[sres])
                pt = PT[sb]
                if not grpA:
                    S.add("act", lambda h, pt=pt, s_main=s_main, hd=hd: h.activation(
                        out=pt[:, 0:3, :], in_=s_main.rearrange("p (a b) -> p a b", a=3), func=AF.Exp,
                        bias=constB[:, hd:hd + 1]),
                        reads=[f"S{sb}", "constB"], writes=[f"PT{sb}"])
                S.add("act", lambda h, pt=pt, s_last=s_last: h.activation(
                    out=pt[:, 3:5, :], in_=s_last.rearrange("p (a b) -> p a b", a=2), func=AF.Exp),
                    reads=[f"S2{sb}"], writes=[f"PT{sb}"])

            def emit_pv(n):
                qt, j = items[n]
                sb = n % 2
                ob = qt % 2
                o_ps = bank(6 + ob, 0, 130).rearrange("p (a b) -> p a b", a=2)
                blocks = list(range(3, 5)) if grpA else list(range(5))
                for bi, i in enumerate(blocks):
                    lt = qt + i
                    S.add("pe", lambda h, i=i, lt=lt, j=j, sb=sb, o_ps=o_ps, bi=bi, nb_=len(blocks): h.matmul(
                        o_ps[:, j, :], lhsT=PT[sb][:, i, :], rhs=VA[:, lt, 0 if grpA else j, :],
                        start=(bi == 0), stop=(bi == nb_ - 1)),
                        reads=[f"PT{sb}", "VA"], writes=[f"OP{ob}"])
                if j == 1:
                    rc = rec[ob]
                    den = o_ps[:, :, 64:65]
                    if grpA:
                        S.add("dve", lambda h, rc=rc, den=den: h.tensor_tensor(
                            out=rc.unsqueeze(2), in0=den, in1=esink[:, 2 * u:2 * u + 2].unsqueeze(2), op=ALU.add),
                            reads=[f"OP{ob}", "esink"], writes=[f"rec{ob}"])
                        S.add("dve", lambda h, rc=rc: h.reciprocal(out=rc, in_=rc),
                              reads=[f"rec{ob}"], writes=[f"rec{ob}"])
                    else:
                        S.add("dve", lambda h, rc=rc, den=den: h.reciprocal(out=rc.unsqueeze(2), in_=den),
                              reads=[f"OP{ob}"], writes=[f"rec{ob}"])
                    S.add("dve", lambda h, rc=rc, o_ps=o_ps, qt=qt: h.tensor_tensor(
                        out=O_sb[:, qt, u * 128:(u + 1) * 128].rearrange("p (a b) -> p a b", a=2),
                        in0=o_ps[:, :, 0:64], in1=rc.unsqueeze(2).to_broadcast([128, 2, 64]), op=ALU.mult),
                        reads=[f"OP{ob}", f"rec{ob}"], writes=[("O", qt, u)])

            for n in range(len(items) + 1):
                if n < len(items):
                    emit_qk(n)
                if n >= 1:
                    emit_pv(n - 1)

        if dbg and "O" in dbg and H == dbg.get("_H", 1):
            tmpo = SC.get(F32, [D])
            for t in range(NOT):
                S.add("dve", lambda h, t=t: h.tensor_copy(out=tmpo, in_=O_sb[:, t, :]),
                      reads=[("O", t, u) for u in range(16)], writes=["tmpo"])
                dma("sp", dbg_out["O"][t], tmpo, "dbg", reads=["tmpo"], writes=["dbgout"])

        S.fence()
        S.phase = f'H{H}_p3a_onorm'
        SC.reset()
        junk = SC.get(BF16, [D])
        onb = [SC.get(BF16, [D]) for _ in range(2)]
        ss16 = stat[:, 16:16 + 2 * NOT]
        rs16 = stat[:, 32:32 + 2 * NOT]
        for t in range(NOT):
            ores = [("O", t, u) for u in range(16)]
            for g in range(2):
                S.add("act", lambda h, t=t, g=g: h.activation(
                    out=junk[:, 0:1024], in_=O_sb[:, t, g * 1024:(g + 1) * 1024], func=AF.Square,
                    accum_out=ss16[:, 2 * t + g:2 * t + g + 1]), reads=ores, writes=["junk", "ss16"])
        rms_rstd(None, ss16, rs16, 1024, ["ss16"], ["rs16"])

        def p3_norm(t):
            b2 = t % 2
            ores = [("O", t, u) for u in range(16)]
            for g in range(2):
                S.add("act", lambda h, t=t, g=g, b2=b2: h.activation(
                    out=onb[b2][:, g * 1024:(g + 1) * 1024], in_=O_sb[:, t, g * 1024:(g + 1) * 1024],
                    func=AF.Copy, scale=rs16[:, 2 * t + g:2 * t + g + 1]), reads=ores + ["rs16"], writes=[f"onb{b2}"])

        p3_norm(0)
        for t in range(NOT):
            if t + 1 < NOT:
                p3_norm(t + 1)
            transposes_to_FT(onb[t % 2], f"onb{t % 2}", 4 + t, gc[:, 2, :], None, "o")

        S.fence()
        S.phase = f'H{H}_p3b_outproj'
        SC.reset()
        Wo = [SC.get(BF16, [KC, 512]) for _ in range(2)]
        gt1row = SC.get(F32, [D])
        tmpx = [SC.get(F32, [512]) for _ in range(2)]
        dma("sp", gt1row, bcast(modrow[:, V_GT1 * D:(V_GT1 + 1) * D]), "gt1r",
            reads=["modrow0", "modrow1"], writes=["gt1row"])
        for t in range(NOT):
            r = HALO + row0 + t * 128
            dma("sp", X1[:, t, :], xh[r:r + 128, :], f"x1_{t}", writes=[("X1", t, nb) for nb in range(4)])
        w_out_v = w_out.rearrange("(kc p) n -> p kc n", p=128)
        for nb in range(4):
            wo = Wo[nb % 2]
            dma("pool", wo, w_out_v[:, :, nb * 512:(nb + 1) * 512], f"wo{nb % 2}", writes=[f"wo{nb % 2}"])
            for t in range(NOT):
                a = next_acc()
                for kc in range(KC):
                    S.add("pe", lambda h, a=a, kc=kc, t=t, wo=wo: h.matmul(
                        bank(4 + a), lhsT=FT[:, kc, (4 + t) * 128:(5 + t) * 128], rhs=wo[:, kc, :],
                        start=(kc == 0), stop=(kc == KC - 1)),
                        reads=[f"wo{nb % 2}", ("FT", 4 + t)], writes=[f"acc{a}"])
                tx = tmpx[a]
                S.add("dve", lambda h, a=a, tx=tx, nb=nb: h.tensor_tensor(
                    out=tx, in0=bank(4 + a), in1=gt1row[:, nb * 512:(nb + 1) * 512], op=ALU.mult),
                    reads=[f"acc{a}", "gt1row"], writes=[f"tmpx{a}"])
                S.add("dve", lambda h, tx=tx, t=t, nb=nb: h.tensor_tensor(
                    out=X1[:, t, nb * 512:(nb + 1) * 512], in0=X1[:, t, nb * 512:(nb + 1) * 512], in1=tx, op=ALU.add),
                    reads=[f"tmpx{a}", ("X1", t, nb)], writes=[("X1", t, nb)])
        if dbg and "X1" in dbg and H == dbg.get("_H", 1):
            for t in range(NOT):
                dma("sp", dbg_out["X1"][t], X1[:, t, :], "dbg", reads=[("X1", t, nb) for nb in range(4)],
                    writes=["dbgout"])

        S.fence()
        S.phase = f'H{H}_p4_route'
        SC.reset()
        junk = SC.get(BF16, [D])
        xnb = [SC.get(BF16, [D]) for _ in range(2)]
        L = SC.get(F32, [NOT, 36])
        ss8 = stat[:, 16:16 + NOT]
        rs8 = rsP[:, H * NOT:(H + 1) * NOT]
        for t in range(NOT):
            xres = [("X1", t, nb) for nb in range(4)]
            S.add("act", lambda h, t=t: h.activation(out=junk, in_=X1[:, t, :], func=AF.Square, accum_out=ss8[:, t:t + 1]),
                  reads=xres, writes=["junk", "ss8"])
        rms_rstd(None, ss8, rs8, D, ["ss8"], [f"rsP{H * NOT + t}" for t in range(NOT)])

        def p4_norm(t):
            b2 = t % 2
            xres = [("X1", t, nb) for nb in range(4)]
            S.add("act", lambda h, t=t, b2=b2: h.activation(out=xnb[b2], in_=X1[:, t, :], func=AF.Copy,
                                                            scale=rsP[:, H * NOT + t:H * NOT + t + 1]),
                  reads=xres + [f"rsP{H * NOT + t}"], writes=[f"xnb{b2}"])

        p4_norm(0)
        for t in range(NOT):
            if t + 1 < NOT:
                p4_norm(t + 1)
            transposes_to_FT(xnb[t % 2], f"xnb{t % 2}", 4 + t, gm2, colv(V_SH2), "h2")
            a = next_acc()
            for kc in range(KC):
                S.add("pe", lambda h, a=a, kc=kc, t=t: h.matmul(
                    bank(4 + a, 0, 36), lhsT=FT[:, kc, (4 + t) * 128:(5 + t) * 128], rhs=wr[:, kc, :],
                    start=(kc == 0), stop=(kc == KC - 1)),
                    reads=["wr", ("FT", 4 + t)], writes=[f"acc{a}"])
            S.add("dve", lambda h, a=a, t=t: h.tensor_tensor(out=L[:, t, :], in0=bank(4 + a, 0, 36), in1=brow, op=ALU.add),
                  reads=[f"acc{a}", "brow"], writes=["L"])
        gl = L[:, :, 0:4]
        el = L[:, :, 4:36]
        gmax = SC.get(F32, [NOT]); gsum = SC.get(F32, [NOT]); pg = SC.get(F32, [NOT])
        gt_ = SC.get(F32, [NOT, 4]); ohg = SC.get(F32, [NOT, 4]); pen = SC.get(F32, [NOT, 4])
        elm = SC.get(F32, [NOT, 32]); oh1 = SC.get(F32, [NOT, 32]); oh2 = SC.get(F32, [NOT, 32])
        elm2 = SC.get(F32, [NOT, 32])
        m1 = SC.get(F32, [NOT]); m2 = SC.get(F32, [NOT]); e2 = SC.get(F32, [NOT]); w1 = SC.get(F32, [NOT])
        w2 = SC.get(F32, [NOT])
        RR = ["L", "rt"]

        def dv(fn):
            S.add("dve", fn, reads=RR, writes=["rt"])

        dv(lambda h: h.tensor_reduce(out=gmax, in_=gl, axis=AX.X, op=ALU.max))
        dv(lambda h: h.tensor_tensor(out=gt_, in0=gl, in1=gmax.unsqueeze(2).to_broadcast([128, NOT, 4]), op=ALU.subtract))
        S.add("act", lambda h: h.activation(out=gt_, in_=gt_, func=AF.Exp), reads=RR, writes=["rt"])
        dv(lambda h: h.tensor_reduce(out=gsum, in_=gt_, axis=AX.X, op=ALU.add))
        dv(lambda h: h.reciprocal(out=pg, in_=gsum))
        dv(lambda h: h.tensor_tensor(out=ohg, in0=gl, in1=gmax.unsqueeze(2).to_broadcast([128, NOT, 4]), op=ALU.is_ge))
        dv(lambda h: h.tensor_scalar(out=pen, in0=ohg, scalar1=-1.0, scalar2=1e9, op0=ALU.add, op1=ALU.mult))
        dv(lambda h: h.tensor_tensor(
            out=elm.rearrange("p t (g j) -> p t g j", g=4), in0=el.rearrange("p t (g j) -> p t g j", g=4),
            in1=pen.unsqueeze(3).to_broadcast([128, NOT, 4, 8]), op=ALU.add))
        dv(lambda h: h.tensor_reduce(out=m1, in_=elm, axis=AX.X, op=ALU.max))
        dv(lambda h: h.tensor_tensor(out=oh1, in0=elm, in1=m1.unsqueeze(2).to_broadcast([128, NOT, 32]), op=ALU.is_ge))
        dv(lambda h: h.scalar_tensor_tensor(out=elm2, in0=oh1, scalar=-1e9, in1=elm, op0=ALU.mult, op1=ALU.add))
        dv(lambda h: h.tensor_reduce(out=m2, in_=elm2, axis=AX.X, op=ALU.max))
        dv(lambda h: h.tensor_tensor(out=oh2, in0=elm2, in1=m2.unsqueeze(2).to_broadcast([128, NOT, 32]), op=ALU.is_ge))
        dv(lambda h: h.tensor_tensor(out=e2, in0=m2, in1=m1, op=ALU.subtract))
        S.add("act", lambda h: h.activation(out=e2, in_=e2, func=AF.Exp), reads=RR, writes=["rt"])
        dv(lambda h: h.tensor_scalar(out=w1, in0=e2, scalar1=1.0, scalar2=None, op0=ALU.add))
        dv(lambda h: h.reciprocal(out=w1, in_=w1))
        dv(lambda h: h.tensor_tensor(out=w1, in0=w1, in1=pg, op=ALU.mult))
        dv(lambda h: h.tensor_tensor(out=w2, in0=w1, in1=e2, op=ALU.mult))
        dv(lambda h: h.tensor_tensor(out=elm, in0=oh1, in1=w1.unsqueeze(2).to_broadcast([128, NOT, 32]), op=ALU.mult))
        dv(lambda h: h.tensor_tensor(out=elm2, in0=oh2, in1=w2.unsqueeze(2).to_broadcast([128, NOT, 32]), op=ALU.mult))
        S.add("dve", lambda h: h.tensor_tensor(out=comb, in0=elm, in1=elm2, op=ALU.add), reads=RR, writes=["comb"])
        if dbg and "comb" in dbg and H == dbg.get("_H", 1):
            dma("sp", dbg_out["comb"], comb, "dbg", reads=["comb"], writes=["dbgout"])
        hs_ = slice(H * NOT, (H + 1) * NOT)
        S.add("dve", lambda h: h.tensor_copy(out=w1p[:, hs_], in_=w1), reads=RR, writes=["w1p"])
        S.add("dve", lambda h: h.tensor_copy(out=w2p[:, hs_], in_=w2), reads=RR, writes=["w2p"])
        S.add("dve", lambda h: h.tensor_copy(out=oh1_all[:, hs_, :], in_=oh1), reads=RR, writes=["ohall"])
        S.add("dve", lambda h: h.tensor_copy(out=oh2_all[:, hs_, :], in_=oh2), reads=RR, writes=["ohall"])

        S.phase = f'H{H}_p4c_spill'
        gm2row = SC.get(F32, [D]); sh2row = SC.get(F32, [D])
        h2f = SC.get(F32, [D])
        gfr = h2f
        h2tok = xnb
        dma("sp", gm2row, bcast(modrow[:, V_SC2 * D:(V_SC2 + 1) * D]), "r0", reads=["modrow0", "modrow1"], writes=["gm2row"])
        dma("sp", sh2row, bcast(modrow[:, V_SH2 * D:(V_SH2 + 1) * D]), "r1", reads=["modrow0", "modrow1"], writes=["sh2row"])
        dma("sp", gfr, bcast(g_ffn_row), "r2", writes=["h2f"])
        S.add("dve", lambda h: h.scalar_tensor_tensor(out=gm2row, in0=gm2row, scalar=1.0, in1=gfr, op0=ALU.add, op1=ALU.mult),
              reads=["gm2row", "h2f"], writes=["gm2row"])
        for t in range(NOT):
            b2 = t % 2
            tt = H * NOT + t
            xres = [("X1", t, nb) for nb in range(4)]
            dma("sp", X1S[tt * 128:(tt + 1) * 128, :], X1[:, t, :], f"x1s{b2}", reads=xres, writes=[("X1S", tt)])
            S.add("dve", lambda h, t=t, tt=tt: h.scalar_tensor_tensor(out=h2f, in0=X1[:, t, :], scalar=rsP[:, tt:tt + 1], in1=gm2row,
                                                                      op0=ALU.mult, op1=ALU.mult),
                  reads=xres + [f"rsP{tt}", "gm2row"], writes=["h2f"])
            S.add("dve", lambda h, b2=b2: h.tensor_tensor(out=h2tok[b2], in0=h2f, in1=sh2row, op=ALU.add),
                  reads=["h2f", "sh2row"], writes=[f"xnb{b2}"])
            dma("sp", H2S[tt * 128:(tt + 1) * 128, :], h2tok[b2], f"h2s{b2}", reads=[f"xnb{b2}"], writes=[("H2S", tt)])

    S.fence()
    S.phase = 'G_p4b_slots'
    SC.reset()
    ohA = SC.get(F32, [NTT, 32]); ohAb = SC.get(BF16, [NTT, 32])
    cntS = SC.get(F32, [32]); rk = SC.get(F32, [NTT, 32])
    cmp8 = SC.get(F32, [32, 8]); nt_ = SC.get(F32, [32]); padded = SC.get(F32, [32])
    cs = [SC.get(F32, [32]) for _ in range(2)]
    off = SC.get(F32, [32]); sbt = SC.get(F32, [NTT, 32]); tmp3 = SC.get(F32, [NTT, 32])
    s1f = SC.get(F32, [NTT]); s2f = SC.get(F32, [NTT])
    cmpS = SC.get(F32, [NST, 32]); eS = SC.get(F32, [NST]); idxf = SC.get(F32, [NST])
    RR2 = ["rt", "cst2", "ohall"]

    def dv2(fn):
        S.add("dve", fn, reads=RR2, writes=["rt"])

    dv2(lambda h: h.tensor_tensor(out=ohA, in0=oh1_all, in1=oh2_all, op=ALU.add))
    dv2(lambda h: h.tensor_copy(out=ohAb, in_=ohA))
    CNT = bank(4, 0, 32)
    RK = bank(5, 0, NTT * 32).rearrange("p (t e) -> p t e", t=NTT)
    for t in range(NTT):
        S.add("pe", lambda h, t=t: h.matmul(CNT, lhsT=ones_b, rhs=ohAb[:, t, :], start=(t == 0), stop=(t == NTT - 1)),
              reads=["rt", "ones_b"], writes=["acc0"])
    for t in range(NTT):
        for tp in range(t):
            S.add("pe", lambda h, t=t, tp=tp: h.matmul(RK[:, t, :], lhsT=ones_b, rhs=ohAb[:, tp, :],
                                                       start=(tp == 0), stop=False),
                  reads=["rt", "ones_b"], writes=["acc1"])
        S.add("pe", lambda h, t=t: h.matmul(RK[:, t, :], lhsT=tri_b, rhs=ohAb[:, t, :], start=(t == 0), stop=True),
              reads=["rt", "tri_b"], writes=["acc1"])
    S.add("dve", lambda h: h.tensor_copy(out=cntS, in_=CNT), reads=["acc0"], writes=["rt"])
    S.add("dve", lambda h: h.tensor_copy(out=rk, in_=RK), reads=["acc1"], writes=["rt"])
    dv2(lambda h: h.tensor_tensor(out=cmp8, in0=cntS.unsqueeze(2).to_broadcast([128, 32, 8]),
                                  in1=thr8.unsqueeze(1).to_broadcast([128, 32, 8]), op=ALU.is_gt))
    dv2(lambda h: h.tensor_reduce(out=nt_, in_=cmp8, axis=AX.X, op=ALU.add))
    dv2(lambda h: h.tensor_scalar(out=padded, in0=nt_, scalar1=float(TS), scalar2=None, op0=ALU.mult))
    dv2(lambda h: h.tensor_copy(out=cs[0], in_=padded))
    cur = 0
    for sh in (1, 2, 4, 8, 16):
        a_, b_ = cs[cur], cs[1 - cur]
        dv2(lambda h, a_=a_, b_=b_: h.tensor_copy(out=b_, in_=a_))
        dv2(lambda h, a_=a_, b_=b_, sh=sh: h.tensor_tensor(out=b_[:, sh:32], in0=a_[:, sh:32], in1=a_[:, 0:32 - sh], op=ALU.add))
        cur = 1 - cur
    incl = cs[cur]
    dv2(lambda h: h.tensor_tensor(out=off, in0=incl, in1=padded, op=ALU.subtract))
    dv2(lambda h: h.tensor_tensor(out=sbt, in0=rk, in1=off.unsqueeze(1).to_broadcast([128, NTT, 32]), op=ALU.add))
    dv2(lambda h: h.tensor_tensor(out=tmp3, in0=sbt, in1=oh1_all, op=ALU.mult))
    dv2(lambda h: h.tensor_reduce(out=s1f, in_=tmp3, axis=AX.X, op=ALU.add))
    dv2(lambda h: h.tensor_tensor(out=tmp3, in0=sbt, in1=oh2_all, op=ALU.mult))
    dv2(lambda h: h.tensor_reduce(out=s2f, in_=tmp3, axis=AX.X, op=ALU.add))
    S.add("dve", lambda h: h.tensor_copy(out=slot1_i, in_=s1f), reads=RR2, writes=["slot1"])
    S.add("dve", lambda h: h.tensor_copy(out=slot2_i, in_=s2f), reads=RR2, writes=["slot2"])
    dv2(lambda h: h.tensor_tensor(out=cmpS, in0=incl.unsqueeze(1).to_broadcast([128, NST, 32]),
                                  in1=s128.unsqueeze(2).to_broadcast([128, NST, 32]), op=ALU.is_le))
    dv2(lambda h: h.tensor_reduce(out=eS, in_=cmpS, axis=AX.X, op=ALU.add))
    dv2(lambda h: h.tensor_scalar(out=idxf, in0=eS, scalar1=128.0, scalar2=piota, op0=ALU.mult, op1=ALU.add))
    S.add("dve", lambda h: h.tensor_copy(out=idxW, in_=idxf), reads=RR2, writes=["idxW"])

    S.phase = 'G_p4d_scatter'
    zt = SC.get(BF16, [D])
    h2t = [SC.get(BF16, [D]) for _ in range(2)]
    S.add("pool", lambda h: h.memset(zt, 0.0), writes=["zt"])
    for st in range(NST * SUB):
        dma("sp", HS[st * 128:(st + 1) * 128, :], zt, "hsz", reads=["zt"], writes=["HS"])
    for tt in range(NTT):
        b2 = tt % 2
        dma("sp", h2t[b2], H2S[tt * 128:(tt + 1) * 128, :], f"h2l{b2}", reads=[("H2S", tt)], writes=[f"h2t{b2}"])
        for k_, sl in enumerate((slot1_i, slot2_i)):
            S.add("pool", lambda h, sl=sl, tt=tt, b2=b2: h.indirect_dma_start(
                out=HS, out_offset=bass.IndirectOffsetOnAxis(ap=sl[:, tt:tt + 1], axis=0),
                in_=h2t[b2], in_offset=None, bounds_check=bc_reg(h, NST * TS - 1), oob_is_err=False),
                reads=[f"h2t{b2}", "slot1", "slot2", "HS"], writes=[("HSs", tt, k_)], dma=f"hss{b2}{k_}")

    S.fence()
    S.phase = 'G_p5_moe'
    SC.reset()
    ring = [sview(P_BYTES + r * 16384, BF16, [8192]) for r in range(3)] + \
           [sview(OB_OFF + r * 16384, BF16, [8192]) for r in range(4)]
    NR_ = len(ring)
    hsb = [SC.get(BF16, [SUB, D]) for _ in range(2)]
    hsT = [SC.get(BF16, [KC, TS]) for _ in range(2)]
    aT = [SC.get(BF16, [4, TS]) for _ in range(2)]
    sg = [SC.get(F32, [TS]) for _ in range(2)]
    ysb = [SC.get(F32, [D]) for _ in range(2)]
    for r_ in range(NR_):
        S.add("dve", lambda h, r_=r_: h.memset(ring[r_], 0.0), writes=[f"ring{r_}"])
    ring_i = [0]

    def ring_next():
        r = ring_i[0] % NR_
        ring_i[0] += 1
        return r

    gu_i = [0]
    y_i = [0]
    ys_i = [0]
    n_tiles = dbg.get("_ntiles", NST) if dbg else NST
    hs_all = ["HS"] + [("HSs", tt, k_) for tt in range(NTT) for k_ in range(2)]

    def load_hs(st):
        for sub in range(SUB):
            r0 = (st * SUB + sub) * 128
            dma("sp", hsb[st % 2][:, sub, :], HS[r0:r0 + 128, :], f"hsl{st % 2}{sub}",
                reads=hs_all, writes=[f"hsb{st % 2}{sub}"])

    load_hs(0)
    for st in range(n_tiles):
        b2 = st % 2
        rg, ru, rd = ring_next(), ring_next(), ring_next()
        for (r_, wsrc) in ((rg, w_gate), (ru, w_up), (rd, w_down)):
            S.add("pool", lambda h, r_=r_, wsrc=wsrc, st=st: h.indirect_dma_start(
                out=ring[r_], out_offset=None, in_=wsrc,
                in_offset=bass.IndirectOffsetOnAxis(ap=idxW[:, st:st + 1], axis=0),
                bounds_check=bc_reg(h, N_EXP * 128 - 1), oob_is_err=False),
                reads=["idxW"], writes=[f"ring{r_}"], dma=f"ring{r_}")
        Wg = ring[rg].rearrange("p (a b) -> p a b", a=KC)
        Wup = ring[ru].rearrange("p (a b) -> p a b", a=KC)
        Wd = ring[rd].rearrange("p (a b) -> p a b", a=4)
        if st + 1 < n_tiles:
            load_hs(st + 1)
        for sub in range(SUB):
            for hb in range(2):
                pb = bank_bf(6 + hb)
                for k8 in range(8):
                    kc = hb * 8 + k8
                    S.add("pe", lambda h, pb=pb, k8=k8, kc=kc, b2=b2, sub=sub: h.transpose(
                        out=pb[:, k8 * 128:(k8 + 1) * 128], in_=hsb[b2][:, sub, kc * 128:(kc + 1) * 128], identity=ident_b),
                        reads=[f"hsb{b2}{sub}", "cst_b"], writes=[f"tp{hb}"])
                dst = hsT[b2][:, hb * 8:(hb + 1) * 8, sub * 128:(sub + 1) * 128]
                src = pb.rearrange("p (a b) -> p a b", a=8)
                if hb == 0:
                    S.add("act", lambda h, dst=dst, src=src: h.activation(out=dst, in_=src, func=AF.Copy),
                          reads=[f"tp{hb}"], writes=[f"hsT{b2}"])
                else:
                    S.add("dve", lambda h, dst=dst, src=src: h.tensor_copy(out=dst, in_=src),
                          reads=[f"tp{hb}"], writes=[f"hsT{b2}"])
        for mb in range(4):
            gi = gu_i[0] % 2
            gu_i[0] += 1
            G, U = bank(gi, 0, TS), bank(2 + gi, 0, TS)
            for kc in range(KC):
                S.add("pe", lambda h, kc=kc, G=G, Wg=Wg, mb=mb, b2=b2: h.matmul(
                    G, lhsT=Wg[:, kc, mb * 128:(mb + 1) * 128], rhs=hsT[b2][:, kc, :],
                    start=(kc == 0), stop=(kc == KC - 1)),
                    reads=[f"ring{rg}", f"hsT{b2}"], writes=[f"G{gi}"])
            for kc in range(KC):
                S.add("pe", lambda h, kc=kc, U=U, Wup=Wup, mb=mb, b2=b2: h.matmul(
                    U, lhsT=Wup[:, kc, mb * 128:(mb + 1) * 128], rhs=hsT[b2][:, kc, :],
                    start=(kc == 0), stop=(kc == KC - 1)),
                    reads=[f"ring{ru}", f"hsT{b2}"], writes=[f"U{gi}"])
            S.add("act", lambda h, gi=gi, G=G: h.activation(out=sg[gi], in_=G, func=AF.Silu),
                  reads=[f"G{gi}"], writes=[f"sg{gi}"])
            S.add("dve", lambda h, gi=gi, U=U, mb=mb, b2=b2: h.tensor_tensor(
                out=aT[b2][:, mb, :], in0=sg[gi], in1=U, op=ALU.mult),
                reads=[f"sg{gi}", f"U{gi}"], writes=[f"aT{b2}"])
        for sub in range(SUB):
            yb = ys_i[0] % 2
            ys_i[0] += 1
            for nb in range(4):
                yi = y_i[0] % 2
                y_i[0] += 1
                Y = bank(4 + yi)
                for kc in range(4):
                    S.add("pe", lambda h, kc=kc, Y=Y, Wd=Wd, nb=nb, b2=b2, sub=sub: h.matmul(
                        Y, lhsT=aT[b2][:, kc, sub * 128:(sub + 1) * 128], rhs=Wd[:, kc, nb * 512:(nb + 1) * 512],
                        start=(kc == 0), stop=(kc == 3)),
                        reads=[f"ring{rd}", f"aT{b2}"], writes=[f"acc{yi}"])
                if nb % 2 == 0:
                    S.add("act", lambda h, Y=Y, nb=nb, yb=yb: h.activation(
                        out=ysb[yb][:, nb * 512:(nb + 1) * 512], in_=Y, func=AF.Copy),
                        reads=[f"acc{yi}"], writes=[f"ysb{yb}"])
                else:
                    S.add("dve", lambda h, Y=Y, nb=nb, yb=yb: h.tensor_copy(out=ysb[yb][:, nb * 512:(nb + 1) * 512], in_=Y),
                          reads=[f"acc{yi}"], writes=[f"ysb{yb}"])
            r0 = (st * SUB + sub) * 128
            dma("sp", YS[r0:r0 + 128, :], ysb[yb], f"yst{yb}", reads=[f"ysb{yb}"], writes=[("YS", st, sub)])

    S.fence()
    S.phase = 'G_p6_final'
    SC.reset()
    junk = SC.get(BF16, [D])
    gfrow = SC.get(F32, [D])
    shfrow = SC.get(F32, [D])
    gt2row = SC.get(F32, [D])
    ot = [SC.get(F32, [D]) for _ in range(2)]
    G1 = [sview(P_BYTES + (2 * i) * 8192, F32, [D]) for i in range(2)]
    G2 = [sview(P_BYTES + (2 * i + 1) * 8192, F32, [D]) for i in range(2)]
    scfrow = ot[1]
    dma("sp", gfrow, bcast(g_fin), "gfr", writes=["gfrow"])
    dma("sp", shfrow, bcast(modrow[:, V_SHF * D:(V_SHF + 1) * D]), "shfr",
        reads=["modrow0", "modrow1"], writes=["shfrow"])
    dma("sp", scfrow, bcast(modrow[:, V_SCF * D:(V_SCF + 1) * D]), "scfr",
        reads=["modrow0", "modrow1"], writes=["ot1"])
    dma("sp", gt2row, bcast(modrow[:, V_GT2 * D:(V_GT2 + 1) * D]), "gt2r",
        reads=["modrow0", "modrow1"], writes=["gt2row"])
    S.add("dve", lambda h: h.scalar_tensor_tensor(out=gfrow, in0=scfrow, scalar=1.0, in1=gfrow,
                                                   op0=ALU.add, op1=ALU.mult),
          reads=["gfrow", "ot1"], writes=["gfrow"])
    ys_all = [("YS", st, sub) for st in range(n_tiles) for sub in range(SUB)]
    def p6_a(tt):
        b2 = tt % 2
        xb_i = tt % 4
        xb = X1[:, xb_i, :]
        xres = [f"xb{xb_i}"]
        dma("sp", xb, X1S[tt * 128:(tt + 1) * 128, :], f"x1l{xb_i}", reads=[("X1S", tt)], writes=xres)
        g1, g2 = G1[b2], G2[b2]
        for (Gb, sl, nm) in ((g1, slot1_i, f"G1{b2}"), (g2, slot2_i, f"G2{b2}")):
            S.add("pool", lambda h, Gb=Gb, sl=sl, tt=tt: h.indirect_dma_start(
                out=Gb, out_offset=None, in_=YS,
                in_offset=bass.IndirectOffsetOnAxis(ap=sl[:, tt:tt + 1], axis=0),
                bounds_check=bc_reg(h, NST * TS - 1), oob_is_err=False),
                reads=ys_all + ["slot1", "slot2"], writes=[nm], dma=nm)
        S.add("dve", lambda h, tt=tt, g1=g1: h.tensor_scalar(out=g1, in0=g1, scalar1=w1p[:, tt:tt + 1], scalar2=None, op0=ALU.mult),
              reads=[f"G1{b2}", "w1p"], writes=[f"G1{b2}"])
        S.add("dve", lambda h, tt=tt, g1=g1, g2=g2: h.scalar_tensor_tensor(out=g1, in0=g2, scalar=w2p[:, tt:tt + 1], in1=g1,
                                                                          op0=ALU.mult, op1=ALU.add),
              reads=[f"G1{b2}", f"G2{b2}", "w2p"], writes=[f"G1{b2}"])
        S.add("pool", lambda h, g1=g1: h.tensor_tensor(out=g1, in0=g1, in1=gt2row, op=ALU.mult),
              reads=[f"G1{b2}", "gt2row"], writes=[f"G1{b2}"])
        S.add("dve", lambda h, xb=xb, g1=g1: h.tensor_tensor(out=xb, in0=xb, in1=g1, op=ALU.add),
              reads=[f"G1{b2}"] + xres, writes=xres)
        ss = stat[:, b2:b2 + 1]
        S.add("act", lambda h, xb=xb, ss=ss: h.activation(out=junk, in_=xb, func=AF.Square, accum_out=ss),
              reads=xres, writes=["junk", f"ss{b2}"])

    def p6_b(tt):
        b2 = tt % 2
        xb_i = tt % 4
        xb = X1[:, xb_i, :]
        xres = [f"xb{xb_i}"]
        ss = stat[:, b2:b2 + 1]
        rs = stat[:, 2 + b2:3 + b2]
        rms_rstd(None, ss, rs, D, [f"ss{b2}"], [f"rs{b2}"])
        S.add("dve", lambda h, xb=xb, rs=rs, b2=b2: h.scalar_tensor_tensor(
            out=ot[b2], in0=xb, scalar=rs, in1=gfrow, op0=ALU.mult, op1=ALU.mult),
            reads=xres + [f"rs{b2}", "gfrow"], writes=[f"ot{b2}"])
        S.add("pool", lambda h, b2=b2: h.tensor_tensor(out=ot[b2], in0=ot[b2], in1=shfrow, op=ALU.add),
              reads=[f"ot{b2}", "shfrow"], writes=[f"ot{b2}"])
        r = tt * 128
        dma("sp", y_out[r:r + 128, :], ot[b2], f"yo{b2}", reads=[f"ot{b2}"], writes=["yout"])

    p6_a(0)
    for tt in range(NTT):
        if tt + 1 < NTT:
            p6_a(tt + 1)
        p6_b(tt)

    finals = ["yo0", "yo1"] + (["dbg"] if dbg and "dbg" in S.dma_cnt else [])
    S.emit(nc, es, final_waits=finals)
    es.close()
    return nc


def _col(v):
    return np.ascontiguousarray(np.asarray(v, np.float32).reshape(KC, 128).T)


def prepare_shared(inp):
    f = lambda a: np.asarray(a, dtype=np.float32)
    sh = {}
    sh["w_mod"] = np.ascontiguousarray(np.concatenate([f(inp["w_ada"])[0], f(inp["w_ada_final"])], axis=1))
    sh["b_mod"] = np.ascontiguousarray(np.concatenate([f(inp["b_ada"])[0], f(inp["b_ada_final"])])[None, :])
    g_out = np.concatenate([f(inp["g_out_a"])[0], f(inp["g_out_b"])[0]])
    sh["gcols"] = np.ascontiguousarray(np.stack([_col(f(inp["g_mix"])[0]), _col(f(inp["g_ffn"])[0]), _col(g_out)], axis=1))
    sh["g_fin"] = f(inp["g_final"])[None, :].copy()
    w_in = f(inp["w_in"])[0]
    units = []
    for a in range(8):
        g = a // 2
        q = w_in[:, a * 128:(a + 1) * 128]
        k = w_in[:, 1024 + g * 64:1024 + (g + 1) * 64]
        v = w_in[:, 1280 + g * 64:1280 + (g + 1) * 64]
        units.append(np.concatenate([q, k, k, v, v], axis=1))
    for bp in range(8):
        units.append(np.concatenate([w_in[:, 1536 + bp * 128:1536 + (bp + 1) * 128],
                                     w_in[:, 2560 + bp * 128:2560 + (bp + 1) * 128],
                                     w_in[:, 3584 + bp * 128:3584 + (bp + 1) * 128]], axis=1))
    wu = np.stack(units)
    sh["w_in_u"] = np.ascontiguousarray(wu.reshape(16, KC, 128, 384).transpose(0, 2, 1, 3))
    sh["w_out"] = np.ascontiguousarray(f(inp["w_out"])[0])
    sh["w_rt"] = np.ascontiguousarray(np.concatenate([f(inp["w_router_group"])[0], f(inp["w_router_expert"])[0]], axis=1))
    sh["b_rt"] = np.concatenate([f(inp["b_router_group"])[0], f(inp["b_router_expert"])[0]])[None, :].copy()
    sh["w_gate"] = np.ascontiguousarray(f(inp["w_gate"])[0].reshape(N_EXP, KC, 128, DE).transpose(0, 2, 1, 3)).reshape(N_EXP * 128, KC * DE)
    sh["w_up"] = np.ascontiguousarray(f(inp["w_up"])[0].reshape(N_EXP, KC, 128, DE).transpose(0, 2, 1, 3)).reshape(N_EXP * 128, KC * DE)
    sh["w_down"] = np.ascontiguousarray(f(inp["w_down"])[0].reshape(N_EXP, 4, 128, D).transpose(0, 2, 1, 3)).reshape(N_EXP * 128, 4 * D)
    sh["g_ffn_row"] = f(inp["g_ffn"])[0][None, :].copy()
    c2 = np.zeros((128, 192), np.float32)
    c2[:, 0:128] = (np.arange(128)[:, None] < np.arange(128)[None, :]).astype(np.float32)
    c2[:, 128] = np.arange(128)
    c2[:, 129:137] = float(TS) * np.arange(8)[None, :]
    c2[:, 137:137 + NST] = float(TS) * np.arange(NST)[None, :]
    sh["cst2"] = c2
    rb = f(inp["rel_bias_b"])[0]
    ki = np.arange(128)[:, None]
    qi = np.arange(128)[None, :]
    idx0 = np.clip(128 + qi - ki, -128, 128) + 128
    idx1 = np.clip(qi - ki, -128, 128) + 128
    tab = np.stack([rb[:, idx0], rb[:, idx1]], axis=1)
    sh["tabB"] = np.ascontiguousarray(tab.transpose(2, 0, 1, 3))
    sh["constB"] = np.ascontiguousarray(np.broadcast_to(rb[:, 256][None, :], (128, 16)))
    sh["sinks"] = np.ascontiguousarray(np.broadcast_to(f(inp["sinks_a"])[0][None, :], (128, 16)))
    cst = np.zeros((128, 6, 128), np.float32)
    cst[:, 0, :] = np.eye(128, dtype=np.float32)
    cst[:64, 1, 64:] = NEG
    cst[64:, 2, :64] = NEG
    cst[:, 3, :] = 128 + qi - ki
    cst[:, 4, :] = np.abs(qi - ki)
    sh["cst"] = cst
    return sh


def prepare_core(inp, core):
    b, hf = core // 2, core % 2
    x = np.asarray(inp["x"], dtype=np.float32)
    xh = np.zeros((HALO + NB_TOK_CORE, D), np.float32)
    lo = hf * NB_TOK_CORE
    xh[HALO:] = x[b, lo:lo + NB_TOK_CORE]
    if hf > 0:
        xh[:HALO] = x[b, lo - HALO:lo]
    d = {"xh": xh, "c_col": _col(np.asarray(inp["c"], np.float32)[b]),
         "hm": np.full((128, 1), 1.0 if hf > 0 else 0.0, np.float32)}
    return d


_NC_CACHE = {}


def kernel(**inputs):
    if "nc" not in _NC_CACHE:
        _NC_CACHE["nc"] = build_program()
    nc = _NC_CACHE["nc"]
    sh = prepare_shared(inputs)
    in_maps = []
    for core in range(8):
        d = dict(sh)
        d.update(prepare_core(inputs, core))
        in_maps.append(d)
    res = run_bass_kernel_spmd(nc, in_maps, core_ids=list(range(8)))
    B, S_, _ = np.asarray(inputs["x"]).shape
    out = np.empty((B, S_, D), np.float32)
    for core in range(8):
        b, hf = core // 2, core % 2
        out[b, hf * NB_TOK_CORE:(hf + 1) * NB_TOK_CORE] = res.results[core]["y"]
    return out
```

```python
import types
import numpy as np
from contextlib import ExitStack
import concourse.bass as bass
import concourse.mybir as mybir
from concourse.bass_utils import run_bass_kernel_spmd

F32 = mybir.dt.float32
BF16 = mybir.dt.bfloat16
AF = mybir.ActivationFunctionType
ALU = mybir.AluOpType
AX = mybir.AxisListType

ENGS = ("pe", "act", "dve", "pool", "sp")
NEG = -30000.0
EPS = 1e-6


def _freeze(fn):
    if fn.__closure__ is None:
        return fn
    cells = []
    for c in fn.__closure__:
        try:
            cells.append(types.CellType(c.cell_contents))
        except ValueError:
            cells.append(c)
    return types.FunctionType(fn.__code__, fn.__globals__, fn.__name__, fn.__defaults__, tuple(cells))


class Sched:
    def __init__(self):
        self.ops = []
        self.last_w = {}
        self.readers = {}
        self.dma_cnt = {}
        self.dma_last = {}
        self.eng_ops = {e: [] for e in ENGS}
        self.fence_set = set()
        self.fence_passed = {e: True for e in ENGS}
        self.profile_scopes = False
        self.profile_engs = ('pe',)
        self.phase = 'setup'

    def fence(self):
        s = set()
        for e in ENGS:
            if self.eng_ops[e]:
                s.add(self.eng_ops[e][-1])
        for k, i in self.dma_last.items():
            s.add(i)
        self.fence_set = s
        self.fence_passed = {e: False for e in ENGS}

    def add(self, eng, fn, reads=(), writes=(), dma=None):
        i = len(self.ops)
        deps = set()
        for r in reads:
            w = self.last_w.get(r)
            if w is not None:
                deps.add(w)
        for r in writes:
            w = self.last_w.get(r)
            if w is not None:
                deps.add(w)
            for rd in self.readers.get(r, ()):
                deps.add(rd)
        if not self.fence_passed[eng]:
            deps |= self.fence_set
            self.fence_passed[eng] = True
        if dma is not None and eng == "pool" and dma in self.dma_last:
            deps.add(self.dma_last[dma])
        op = dict(eng=eng, fn=_freeze(fn), deps=deps, dma=dma, idx=i, phase=getattr(self, 'phase', None))
        if dma is not None:
            self.dma_cnt[dma] = self.dma_cnt.get(dma, 0) + 16
            op["cnt"] = self.dma_cnt[dma]
            self.dma_last[dma] = i
        self.eng_ops[eng].append(i)
        op["ord"] = len(self.eng_ops[eng])
        self.ops.append(op)
        for r in reads:
            self.readers.setdefault(r, []).append(i)
        for r in writes:
            self.last_w[r] = i
            self.readers[r] = []
        return i

    def plan(self):
        ops = self.ops
        clock = {e: {} for e in ENGS}
        opclock = [None] * len(ops)
        waits = [None] * len(ops)
        signal = [False] * len(ops)

        def key_of(j):
            o = ops[j]
            if o["dma"] is not None:
                return ("dma", o["dma"]), o["cnt"]
            return o["eng"], o["ord"]

        for i, o in enumerate(ops):
            e = o["eng"]
            ck = clock[e]
            w = []
            for j in sorted(o["deps"]):
                pj = ops[j]
                if pj["dma"] is None and pj["eng"] == e == "pe":
                    continue
                k, v = key_of(j)
                if ck.get(k, 0) >= v:
                    continue
                w.append(j)
                signal[j] = True
                ck[k] = v
                for kk, vv in opclock[j].items():
                    if ck.get(kk, 0) < vv:
                        ck[kk] = vv
            waits[i] = w
            oc = dict(ck)
            if o["dma"] is not None:
                oc[("dma", o["dma"])] = o["cnt"]
            else:
                oc[e] = max(oc.get(e, 0), o["ord"])
            opclock[i] = oc
        semval = [0] * len(ops)
        for e in ENGS:
            c = 0
            for i in self.eng_ops[e]:
                if ops[i]["dma"] is None and signal[i]:
                    c += 1
                    semval[i] = c
        self.waits, self.signal, self.semval = waits, signal, semval

    def emit(self, nc, es, final_waits=()):
        self.plan()
        ops, waits, signal, semval = self.ops, self.waits, self.signal, self.semval
        eng_sems = {e: es.enter_context(nc.semaphore(f"s_{e}")) for e in ENGS}
        dma_sems = {k: es.enter_context(nc.semaphore(f"d_{k}")) for k in self.dma_cnt}
        block = es.enter_context(nc.Block())
        handles = {"pe": "tensor", "act": "scalar", "dve": "vector",
                   "pool": "gpsimd", "sp": "sync"}

        def run_engine(e, h):
            cur_scope = [None, None]
            for i in self.eng_ops[e]:
                o = ops[i]
                if self.profile_scopes and e in self.profile_engs and o["phase"] != cur_scope[0]:
                    if cur_scope[1] is not None:
                        cur_scope[1].__exit__(None, None, None)
                    cur_scope[0] = o["phase"]
                    cur_scope[1] = nc.named_scope(str(o["phase"]))
                    cur_scope[1].__enter__()
                for j in waits[i]:
                    pj = ops[j]
                    if pj["dma"] is not None:
                        h.wait_ge(dma_sems[pj["dma"]], pj["cnt"])
                    else:
                        h.wait_ge(eng_sems[pj["eng"]], semval[j])
                ins = o["fn"](h)
                if o["dma"] is not None:
                    ins.then_inc(dma_sems[o["dma"]], 16)
                elif signal[i]:
                    ins.then_inc(eng_sems[e], 1)
            if cur_scope[1] is not None:
                cur_scope[1].__exit__(None, None, None)
            if e == "sp":
                for k in final_waits:
                    h.wait_ge(dma_sems[k], self.dma_cnt[k])

        for e in ENGS:
            if not self.eng_ops[e] and not (e == "sp" and final_waits):
                continue
            getattr(block, handles[e])(lambda h, e=e: run_engine(e, h))


D = 2048
KC = 16
NB_TOK_CORE = 2048
HALF = 1024
HALO = 512
WIN = HALF + HALO
NWT = WIN // 128
NOT = HALF // 128
N_EXP = 32
DE = 512
NMODBLK = 32
SUB = 2
TS = SUB * 128
NTT = 16
NST = (2 * NB_TOK_CORE + 32 * (TS - 1)) // TS
I32 = mybir.dt.int32
SLOPES = [2.0 ** (-8.0 * (h + 1) / 16.0) for h in range(16)]
V_SH1, V_SC1, V_GT1, V_SH2, V_SC2, V_GT2, V_SHF, V_SCF = range(8)


def build_program(n_experts=N_EXP, dbg=None, profile_scopes=False):
    nc = bass.Bass("TRN2", target_bir_lowering=False)
    S = Sched()
    S.profile_scopes = profile_scopes

    def dram_in(name, shape, dt=F32):
        return nc.dram_tensor(name, list(shape), dt, kind="ExternalInput").ap()

    xh = dram_in("xh", [HALO + NB_TOK_CORE, D])
    c_col = dram_in("c_col", [128, KC])
    w_mod = dram_in("w_mod", [D, NMODBLK * 512])
    b_mod = dram_in("b_mod", [1, NMODBLK * 512])
    gcols = dram_in("gcols", [128, 3, KC])
    g_fin = dram_in("g_fin", [1, D])
    w_in_u = dram_in("w_in_u", [16, 128, KC, 384])
    w_out = dram_in("w_out", [D, D])
    w_rt = dram_in("w_rt", [D, 36])
    b_rt = dram_in("b_rt", [1, 36])
    w_gate = dram_in("w_gate", [N_EXP * 128, KC * DE])
    w_up = dram_in("w_up", [N_EXP * 128, KC * DE])
    w_down = dram_in("w_down", [N_EXP * 128, 4 * D])
    g_ffn_row = dram_in("g_ffn_row", [1, D])
    cst2_d = dram_in("cst2", [128, 192])
    tabB_d = dram_in("tabB", [128, 16, 2, 128])
    constB_d = dram_in("constB", [128, 16])
    sinks_d = dram_in("sinks", [128, 16])
    hm_d = dram_in("hm", [128, 1])
    cst_d = dram_in("cst", [128, 6, 128])
    y_out = nc.dram_tensor("y", [NB_TOK_CORE, D], F32, kind="ExternalOutput").ap()
    modrow = nc.dram_tensor("modrow", [1, NMODBLK * 512], F32, kind="Internal").ap()
    HS = nc.dram_tensor("HS", [NST * TS, D], BF16, kind="Internal").ap()
    YS = nc.dram_tensor("YS", [NST * TS, D], F32, kind="Internal").ap()
    H2S = nc.dram_tensor("H2S", [NB_TOK_CORE, D], BF16, kind="Internal").ap()
    X1S = nc.dram_tensor("X1S", [NB_TOK_CORE, D], F32, kind="Internal").ap()
    dbg_out = {}
    if dbg:
        for k, shp in dbg.items():
            if k.startswith('_'):
                continue
            dbg_out[k] = nc.dram_tensor("dbg_" + k, list(shp), F32, kind="ExternalOutput").ap()

    es = ExitStack()
    ARENA_F32 = 51200
    arena = es.enter_context(nc.sbuf_tensor("arena", [128, ARENA_F32], F32))
    psum = es.enter_context(nc.psum_tensor("psum", [128, 4096], F32))

    def sview(off, dt, shape):
        n = int(np.prod(shape))
        nbytes = n * (2 if dt == BF16 else 4)
        assert off % 4 == 0 and nbytes % 4 == 0
        assert off + nbytes <= ARENA_F32 * 4, (off, nbytes)
        a = arena[:, off // 4:(off + nbytes) // 4]
        if dt != F32:
            a = a.bitcast(dt)
        if len(shape) >= 2:
            names = [f"d{i}" for i in range(len(shape))]
            pat = "p (" + " ".join(names) + ") -> p " + " ".join(names)
            a = a.rearrange(pat, **{n: int(v) for n, v in zip(names[:-1], shape[:-1])})
        return a

    class Alloc:
        def __init__(self, base, limit):
            self.base, self.off, self.limit = base, base, limit

        def reset(self):
            self.off = self.base

        def get(self, dt, shape):
            n = int(np.prod(shape)) * (2 if dt == BF16 else 4)
            n = (n + 63) // 64 * 64
            v = sview(self.off, dt, shape)
            self.off += n
            assert self.off <= self.limit, (self.off, self.limit)
            return v

    P_BYTES = 24576
    FT_BYTES = KC * WIN * 2
    OB_BYTES = 65536
    PA = Alloc(0, P_BYTES)
    FT = sview(P_BYTES, BF16, [KC, WIN])
    OB_OFF = P_BYTES + FT_BYTES
    O_sb = sview(OB_OFF, BF16, [NOT, D])
    X1 = sview(OB_OFF, F32, [NOT, D])
    SC = Alloc(OB_OFF + OB_BYTES, ARENA_F32 * 4)

    def bank(b, lo=0, hi=512):
        return psum[:, b * 512 + lo:b * 512 + hi]

    def bank_bf(b):
        return psum[:, b * 512:(b + 1) * 512].bitcast(BF16)

    colall = PA.get(F32, [NMODBLK * 4])
    gc = PA.get(F32, [3, KC])
    gm1 = PA.get(F32, [KC])
    gm2 = PA.get(F32, [KC])
    ident_f = PA.get(F32, [128])
    cst_b = PA.get(BF16, [3, 128])
    relA = PA.get(F32, [2, 128])
    maskA = PA.get(F32, [2, 128])
    tabB = PA.get(BF16, [16, 2, 128])
    constB = PA.get(F32, [16])
    esink = PA.get(F32, [16])
    hm = PA.get(F32, [1])
    brow = PA.get(F32, [36])
    wr = PA.get(BF16, [KC, 36])
    comb = PA.get(F32, [NOT, 32])
    rsP = PA.get(F32, [NTT])
    w1p = PA.get(F32, [NTT])
    w2p = PA.get(F32, [NTT])
    slot1_i = PA.get(I32, [NTT])
    slot2_i = PA.get(I32, [NTT])
    oh1_all = PA.get(F32, [NTT, 32])
    oh2_all = PA.get(F32, [NTT, 32])
    idxW = PA.get(I32, [NST])
    cst2 = PA.get(F32, [64])
    ones_b = PA.get(BF16, [128])
    tri_b = PA.get(BF16, [128])
    piota = cst2[:, 0:1]
    thr8 = cst2[:, 1:9]
    s128 = cst2[:, 9:9 + NST]
    stat = PA.get(F32, [64])
    ident_b = cst_b[:, 0, :]
    maskF_b = cst_b[:, 1, :]

    _bc_regs = {}

    def bc_reg(h, val):
        if val not in _bc_regs:
            r = h.alloc_register(f"bc{val}")
            h.reg_mov(r, val)
            _bc_regs[val] = r
        return _bc_regs[val]

    def bcast(ap_row):
        return ap_row.to_broadcast([128, ap_row.shape[1]])

    def dma(eng, out, in_, key, reads=(), writes=()):
        return S.add(eng, lambda h: h.dma_start(out=out, in_=in_), reads=reads, writes=writes, dma=key)

    SC.reset()
    cst_f = SC.get(F32, [6, 128])
    ccol = SC.get(F32, [KC])
    cact = SC.get(F32, [KC])
    carep = SC.get(BF16, [KC, 128])
    sink_f = SC.get(F32, [16])
    dma("sp", cst_f, cst_d, "c0", writes=["cst_f"])
    dma("pool", tabB, tabB_d, "c1", writes=["tabB"])
    dma("sp", ccol, c_col, "c2", writes=["ccol"])
    dma("sp", gc, gcols, "c3", writes=["gc"])
    dma("sp", constB, constB_d, "c4", writes=["constB"])
    dma("sp", sink_f, sinks_d, "c5", writes=["sink_f"])
    dma("sp", hm, hm_d, "c6", writes=["hm"])
    dma("sp", brow, bcast(b_rt), "c7", writes=["brow"])
    dma("pool", wr, w_rt.rearrange("(kc p) n -> p kc n", p=128), "c8", writes=["wr"])
    dma("pool", tri_b, cst2_d[:, 0:128], "c9", writes=["tri_b"])
    dma("sp", cst2[:, 0:56], cst2_d[:, 128:184], "c10", writes=["cst2"])
    S.add("pool", lambda h: h.memset(ones_b, 1.0), writes=["ones_b"])

    S.add("dve", lambda h: h.tensor_copy(out=ident_f, in_=cst_f[:, 0, :]), reads=["cst_f"], writes=["ident_f"])
    S.add("dve", lambda h: h.tensor_copy(out=cst_b, in_=cst_f[:, 0:3, :]), reads=["cst_f"], writes=["cst_b"])
    S.add("dve", lambda h: h.tensor_copy(out=relA, in_=cst_f[:, 3:5, :]), reads=["cst_f"], writes=["relA"])
    S.add("dve", lambda h: h.tensor_copy(out=maskA, in_=cst_f[:, 1:3, :]), reads=["cst_f"], writes=["maskA"])
    S.add("dve", lambda h: h.tensor_tensor(out=tabB[:, :, 1, :], in0=tabB[:, :, 1, :],
                                            in1=cst_b[:, 2, :].unsqueeze(1).to_broadcast([128, 16, 128]),
                                            op=ALU.add), reads=["cst_b", "tabB"], writes=["tabB"])
    S.add("act", lambda h: h.activation(out=esink, in_=sink_f, func=AF.Exp), reads=["sink_f"], writes=["esink"])
    S.add("act", lambda h: h.activation(out=cact, in_=ccol, func=AF.Silu), reads=["ccol"], writes=["cact"])
    S.add("dve", lambda h: h.tensor_copy(out=carep, in_=cact.unsqueeze(2).to_broadcast([128, KC, 128])),
          reads=["cact"], writes=["carep"])

    S.phase = 'p0_mod'
    wblk = [SC.get(BF16, [KC, 512]) for _ in range(2)]
    rowt = [SC.get(F32, [512]) for _ in range(2)]
    biasb = [SC.get(F32, [512]) for _ in range(2)]
    diag = [SC.get(F32, [4, 128]) for _ in range(2)]
    w_mod_v = w_mod.rearrange("(kc p) n -> p kc n", p=128)
    for j in range(NMODBLK):
        wb = wblk[j % 2]
        rb_, bb, dg = rowt[j % 2], biasb[j % 2], diag[j % 2]
        acc = bank(4 + j % 2)
        dma("pool", wb, w_mod_v[:, :, j * 512:(j + 1) * 512], f"wm{j % 2}", writes=[f"wblk{j % 2}"])
        dma("sp", bb, bcast(b_mod[:, j * 512:(j + 1) * 512]), f"bm{j % 2}",
            writes=[f"biasb{j % 2}"])
        for kc in range(KC):
            S.add("pe", lambda h, kc=kc, wb=wb, acc=acc: h.matmul(acc, lhsT=carep[:, kc, :], rhs=wb[:, kc, :],
                                                                   start=(kc == 0), stop=(kc == KC - 1)),
                  reads=["carep", f"wblk{j % 2}"], writes=[f"acc{j % 2}"])
        S.add("dve", lambda h, rb_=rb_, bb=bb, acc=acc: h.tensor_tensor(out=rb_, in0=acc, in1=bb, op=ALU.add),
              reads=[f"acc{j % 2}", f"biasb{j % 2}"], writes=[f"rowt{j % 2}"])
        dma("sp", modrow[:, j * 512:(j + 1) * 512], rb_[0:1, :], f"mrow{j % 2}", reads=[f"rowt{j % 2}"], writes=[f"modrow{j % 2}"])
        S.add("dve", lambda h, rb_=rb_, dg=dg: h.tensor_tensor(
            out=dg, in0=rb_.rearrange("p (a b) -> p a b", a=4),
            in1=ident_f.unsqueeze(1).to_broadcast([128, 4, 128]), op=ALU.mult),
            reads=[f"rowt{j % 2}", "ident_f"], writes=[f"diag{j % 2}"])
        S.add("dve", lambda h, dg=dg, j=j: h.tensor_reduce(out=colall[:, j * 4:(j + 1) * 4], in_=dg,
                                                            axis=AX.X, op=ALU.add),
              reads=[f"diag{j % 2}"], writes=["colall"])

    def colv(v):
        return colall[:, v * KC:(v + 1) * KC]

    for (gm, gi, vs) in ((gm1, 0, V_SC1), (gm2, 1, V_SC2)):
        S.add("dve", lambda h, gm=gm, gi=gi, vs=vs: h.scalar_tensor_tensor(
            out=gm, in0=colv(vs), scalar=1.0, in1=gc[:, gi, :], op0=ALU.add, op1=ALU.mult),
            reads=["colall", "gc"], writes=["gm"])

    def dbg_dump(name, src_ap, dst_ap, reads):
        if dbg and name in dbg:
            dma("sp", dst_ap, src_ap, "dbg", reads=reads, writes=["dbgout"])

    if dbg and "colall" in dbg:
        dbg_dump("colall", colall, dbg_out["colall"], ["colall"])

    def rms_rstd(eng_h, ss, rstd, n, reads, writes):
        S.add("dve", lambda h: h.tensor_scalar(out=rstd, in0=ss, scalar1=1.0 / n, scalar2=EPS,
                                                op0=ALU.mult, op1=ALU.add), reads=reads, writes=writes)
        S.add("act", lambda h: h.activation(out=rstd, in_=rstd, func=AF.Ln), reads=writes, writes=writes)
        S.add("act", lambda h: h.activation(out=rstd, in_=rstd, func=AF.Exp, scale=-0.5), reads=writes, writes=writes)

    def transposes_to_FT(src_bf, src_res, lt, scale_col, bias_col, tag):
        for hb in range(2):
            pb = bank_bf(6 + hb)
            for k8 in range(8):
                kc = hb * 8 + k8
                S.add("pe", lambda h, pb=pb, k8=k8, kc=kc: h.transpose(
                    out=pb[:, k8 * 128:(k8 + 1) * 128], in_=src_bf[:, kc * 128:(kc + 1) * 128], identity=ident_b),
                    reads=[src_res, "cst_b"], writes=[f"tp{hb}"])
            for k8 in range(8):
                kc = hb * 8 + k8
                dst = FT[:, kc, lt * 128:(lt + 1) * 128]
                src = pb[:, k8 * 128:(k8 + 1) * 128]
                if k8 % 2 == 0:
                    if bias_col is not None:
                        S.add("act", lambda h, dst=dst, src=src, kc=kc: h.activation(
                            out=dst, in_=src, func=AF.Identity, scale=scale_col[:, kc:kc + 1],
                            bias=bias_col[:, kc:kc + 1]),
                            reads=[f"tp{hb}", "gm", "colall", "gc"], writes=[("FT", lt)])
                    else:
                        S.add("act", lambda h, dst=dst, src=src, kc=kc: h.activation(
                            out=dst, in_=src, func=AF.Copy, scale=scale_col[:, kc:kc + 1]),
                            reads=[f"tp{hb}", "gm", "colall", "gc"], writes=[("FT", lt)])
                else:
                    if bias_col is not None:
                        S.add("dve", lambda h, dst=dst, src=src, kc=kc: h.tensor_scalar(
                            out=dst, in0=src, scalar1=scale_col[:, kc:kc + 1], scalar2=bias_col[:, kc:kc + 1],
                            op0=ALU.mult, op1=ALU.add),
                            reads=[f"tp{hb}", "gm", "colall", "gc"], writes=[("FT", lt)])
                    else:
                        S.add("dve", lambda h, dst=dst, src=src, kc=kc: h.tensor_scalar(
                            out=dst, in0=src, scalar1=scale_col[:, kc:kc + 1], scalar2=None, op0=ALU.mult),
                            reads=[f"tp{hb}", "gm", "colall", "gc"], writes=[("FT", lt)])

    for H in range(2):
        row0 = H * HALF
        S.fence()
        S.phase = f'H{H}_p1_norm1'
        SC.reset()
        xt = [SC.get(F32, [D]) for _ in range(2)]
        xnb = [SC.get(BF16, [D]) for _ in range(2)]
        junk = SC.get(BF16, [D])
        xt = xt + [SC.get(F32, [D])]
        ss12 = stat[:, 16:16 + NWT]
        rs12 = stat[:, 32:32 + NWT]
        for lt in range(NWT):
            b3 = lt % 3
            dma("sp", xt[b3], xh[row0 + lt * 128: row0 + (lt + 1) * 128, :], f"xt{b3}", writes=[f"xt{b3}"])
            S.add("act", lambda h, b3=b3, lt=lt: h.activation(out=junk, in_=xt[b3], func=AF.Square,
                                                              accum_out=ss12[:, lt:lt + 1]),
                  reads=[f"xt{b3}"], writes=["junk", "ss12"])
        rms_rstd(None, ss12, rs12, D, ["ss12"], ["rs12"])

        def p1_norm(lt):
            b3, b2 = lt % 3, lt % 2
            dma("sp", xt[b3], xh[row0 + lt * 128: row0 + (lt + 1) * 128, :], f"xt{b3}", writes=[f"xt{b3}"])
            S.add("act", lambda h, b3=b3, b2=b2, lt=lt: h.activation(out=xnb[b2], in_=xt[b3], func=AF.Copy,
                                                                     scale=rs12[:, lt:lt + 1]),
                  reads=[f"xt{b3}", "rs12"], writes=[f"xnb{b2}"])

        p1_norm(0)
        for lt in range(NWT):
            if lt + 1 < NWT:
                p1_norm(lt + 1)
            transposes_to_FT(xnb[lt % 2], f"xnb{lt % 2}", lt, gm1, colv(V_SH1), "h")
        if dbg and "hT" in dbg and H == dbg.get("_H", 1):
            tmpf = SC.get(F32, [KC, 128])
            for lt in range(NWT):
                S.add("dve", lambda h, lt=lt: h.tensor_copy(out=tmpf, in_=FT[:, :, lt * 128:(lt + 1) * 128]),
                      reads=[("FT", lt)], writes=["tmpf"])
                dma("sp", dbg_out["hT"][lt], tmpf, "dbg", reads=["tmpf"], writes=["dbgout"])

        S.fence()
        S.phase = f'H{H}_p2_attn'
        SC.reset()
        Wu = [SC.get(BF16, [KC, 384]) for _ in range(2)]
        QT = SC.get(BF16, [HALF])
        KT = SC.get(BF16, [WIN])
        VA = SC.get(BF16, [NWT, 2, 65])
        tabA = [SC.get(BF16, [2, 2, 2, 128]) for _ in range(2)]
        tA_f = SC.get(F32, [2, 128])
        tA_h = SC.get(F32, [2, 128])
        PT = [SC.get(BF16, [5, 128]) for _ in range(2)]
        rec = [SC.get(F32, [2]) for _ in range(2)]
        S.add("pool", lambda h: h.memset(VA[:, :, :, 64:65], 1.0), writes=["VA"])
        if H == 0:
            S.add("pool", lambda h: h.tensor_copy(out=VA[:, 0:4, :, 64:65],
                                                  in_=hm.unsqueeze(1).unsqueeze(1).to_broadcast([128, 4, 2, 1])),
                  reads=["hm"], writes=["VA"])
        for b2 in range(2):
            S.add("pool", lambda h, b2=b2: h.memset(PT[b2], 0.0), writes=[f"PT{b2}"])

        acc_i = [0]

        def next_acc():
            a = acc_i[0] % 2
            acc_i[0] += 1
            return a

        for u in range(16):
            grpA = u < 8
            wu = Wu[u % 2]
            dma("pool", wu, w_in_u[u], f"wu{u % 2}", writes=[f"wu{u % 2}"])
            wres = f"wu{u % 2}"
            for tb in range(HALF // 512):
                a = next_acc()
                for kc in range(KC):
                    S.add("pe", lambda h, a=a, kc=kc, tb=tb, wu=wu: h.matmul(
                        bank(4 + a), lhsT=wu[:, kc, 0:128], rhs=FT[:, kc, HALO + tb * 512: HALO + (tb + 1) * 512],
                        start=(kc == 0), stop=(kc == KC - 1)),
                        reads=[wres] + [("FT", 4 + tb * 4 + i) for i in range(4)], writes=[f"acc{a}"])
                S.add("dve", lambda h, a=a, tb=tb: h.tensor_scalar(
                    out=QT[:, tb * 512:(tb + 1) * 512], in0=bank(4 + a), scalar1=0.125, scalar2=None, op0=ALU.mult),
                    reads=[f"acc{a}"], writes=["QT"])
            do_kv = (not grpA) or (u % 2 == 0)
            if do_kv:
                for tb in range(WIN // 512):
                    a = next_acc()
                    for kc in range(KC):
                        S.add("pe", lambda h, a=a, kc=kc, tb=tb, wu=wu: h.matmul(
                            bank(4 + a), lhsT=wu[:, kc, 128:256], rhs=FT[:, kc, tb * 512:(tb + 1) * 512],
                            start=(kc == 0), stop=(kc == KC - 1)),
                            reads=[wres] + [("FT", tb * 4 + i) for i in range(4)], writes=[f"acc{a}"])
                    S.add("dve", lambda h, a=a, tb=tb: h.tensor_copy(out=KT[:, tb * 512:(tb + 1) * 512], in_=bank(4 + a)),
                          reads=[f"acc{a}"], writes=["KT"])
                for lt in range(NWT):
                    a = next_acc()
                    for kc in range(KC):
                        S.add("pe", lambda h, a=a, kc=kc, lt=lt, wu=wu: h.matmul(
                            bank(4 + a, 0, 128), lhsT=FT[:, kc, lt * 128:(lt + 1) * 128], rhs=wu[:, kc, 256:384],
                            start=(kc == 0), stop=(kc == KC - 1)),
                            reads=[wres, ("FT", lt)], writes=[f"acc{a}"])
                    src = bank(4 + a, 0, 128).rearrange("p (a b) -> p a b", a=2)
                    if H == 0 and lt < 4:
                        S.add("dve", lambda h, lt=lt, src=src: h.tensor_scalar(
                            out=VA[:, lt, :, 0:64], in0=src, scalar1=hm[:, 0:1], scalar2=None, op0=ALU.mult),
                            reads=[f"acc{a}", "hm"], writes=["VA"])
                    else:
                        S.add("dve", lambda h, lt=lt, src=src: h.tensor_copy(out=VA[:, lt, :, 0:64], in_=src),
                              reads=[f"acc{a}"], writes=["VA"])
            if grpA:
                tb_ = tabA[u % 2]
                for j in range(2):
                    hd = 2 * u + j
                    S.add("pool", lambda h, hd=hd: h.tensor_scalar(
                        out=tA_f, in0=relA, scalar1=-SLOPES[hd], scalar2=None, op0=ALU.mult),
                        reads=["relA", "tA_h"], writes=["tA_f"])
                    S.add("pool", lambda h: h.tensor_tensor(out=tA_f, in0=tA_f, in1=maskA, op=ALU.add),
                          reads=["tA_f", "maskA"], writes=["tA_f"])
                    S.add("pool", lambda h, j=j, tb_=tb_: h.tensor_copy(out=tb_[:, j, 0, :, :], in_=tA_f),
                          reads=["tA_f"], writes=[f"tabA{u % 2}"])
                    S.add("pool", lambda h, j=j, tb_=tb_: h.tensor_copy(out=tA_h, in_=tb_[:, j, 0, :, :]),
                          reads=[f"tabA{u % 2}"], writes=["tA_h"])
                    S.add("pool", lambda h: h.tensor_tensor(out=tA_h, in0=tA_f, in1=tA_h, op=ALU.subtract),
                          reads=["tA_f", "tA_h"], writes=["tA_h"])
                    S.add("pool", lambda h, j=j, tb_=tb_: h.tensor_copy(out=tb_[:, j, 1, :, :], in_=tA_h),
                          reads=["tA_h"], writes=[f"tabA{u % 2}"])
            items = [(qt, j) for qt in range(NOT) for j in range(2)]

            def emit_qk(n):
                qt, j = items[n]
                sb = n % 2
                hd = (2 * u + j) if grpA else (2 * (u - 8) + j)
                pl, ph = j * 64, (j + 1) * 64
                q_ap = QT[pl:ph, qt * 128:(qt + 1) * 128]
                s_main = bank(sb, 0, 384)
                s_last = bank(2 + sb, 0, 256)
                blocks = range(3, 5) if grpA else range(5)
                for i in blocks:
                    lt = qt + i
                    dst = s_main[:, i * 128:(i + 1) * 128] if i < 3 else s_last[:, (i - 3) * 128:(i - 2) * 128]
                    sres = f"S{sb}" if i < 3 else f"S2{sb}"
                    has_tab = grpA or i in (0, 3, 4)
                    S.add("pe", lambda h, dst=dst, lt=lt, pl=pl, ph=ph, q_ap=q_ap, has_tab=has_tab: h.matmul(
                        dst, lhsT=KT[pl:ph, lt * 128:(lt + 1) * 128], rhs=q_ap, start=True, stop=not has_tab),
                        reads=["KT", "QT"], writes=[sres])
                    if grpA:
                        tb_ = tabA[u % 2]
                        for hl in range(2):
                            S.add("pe", lambda h, dst=dst, j=j, hl=hl, i=i, tb_=tb_: h.matmul(
                                dst, lhsT=ident_b, rhs=tb_[:, j, hl, i - 3, :], start=False, stop=(hl == 1)),
                                reads=["cst_b", f"tabA{u % 2}"], writes=[sres])
                    elif i == 0:
                        S.add("pe", lambda h, dst=dst: h.matmul(dst, lhsT=ident_b, rhs=maskF_b, start=False, stop=True),
                              reads=["cst_b"], writes=[sres])
                    elif i >= 3:
                        S.add("pe", lambda h, dst=dst, hd=hd, i=i: h.matmul(
                            dst, lhsT=ident_b, rhs=tabB[:, hd, i - 3, :], start=False, stop=True),
                            reads=["cst_b", "tabB"], writes=[sres])
                pt = PT[sb]
                if not grpA:
                    S.add("act", lambda h, pt=pt, s_main=s_main, hd=hd: h.activation(
                        out=pt[:, 0:3, :], in_=s_main.rearrange("p (a b) -> p a b", a=3), func=AF.Exp,
                        bias=constB[:, hd:hd + 1]),
                        reads=[f"S{sb}", "constB"], writes=[f"PT{sb}"])
                S.add("act", lambda h, pt=pt, s_last=s_last: h.activation(
                    out=pt[:, 3:5, :], in_=s_last.rearrange("p (a b) -> p a b", a=2), func=AF.Exp),
                    reads=[f"S2{sb}"], writes=[f"PT{sb}"])

            def emit_pv(n):
                qt, j = items[n]
                sb = n % 2
                ob = qt % 2
                o_ps = bank(6 + ob, 0, 130).rearrange("p (a b) -> p a b", a=2)
                blocks = list(range(3, 5)) if grpA else list(range(5))
                for bi, i in enumerate(blocks):
                    lt = qt + i
                    S.add("pe", lambda h, i=i, lt=lt, j=j, sb=sb, o_ps=o_ps, bi=bi, nb_=len(blocks): h.matmul(
                        o_ps[:, j, :], lhsT=PT[sb][:, i, :], rhs=VA[:, lt, 0 if grpA else j, :],
                        start=(bi == 0), stop=(bi == nb_ - 1)),
                        reads=[f"PT{sb}", "VA"], writes=[f"OP{ob}"])
                if j == 1:
                    rc = rec[ob]
                    den = o_ps[:, :, 64:65]
                    if grpA:
                        S.add("dve", lambda h, rc=rc, den=den: h.tensor_tensor(
                            out=rc.unsqueeze(2), in0=den, in1=esink[:, 2 * u:2 * u + 2].unsqueeze(2), op=ALU.add),
                            reads=[f"OP{ob}", "esink"], writes=[f"rec{ob}"])
                        S.add("dve", lambda h, rc=rc: h.reciprocal(out=rc, in_=rc),
                              reads=[f"rec{ob}"], writes=[f"rec{ob}"])
                    else:
                        S.add("dve", lambda h, rc=rc, den=den: h.reciprocal(out=rc.unsqueeze(2), in_=den),
                              reads=[f"OP{ob}"], writes=[f"rec{ob}"])
                    S.add("dve", lambda h, rc=rc, o_ps=o_ps, qt=qt: h.tensor_tensor(
                        out=O_sb[:, qt, u * 128:(u + 1) * 128].rearrange("p (a b) -> p a b", a=2),
                        in0=o_ps[:, :, 0:64], in1=rc.unsqueeze(2).to_broadcast([128, 2, 64]), op=ALU.mult),
                        reads=[f"OP{ob}", f"rec{ob}"], writes=[("O", qt, u)])

            for n in range(len(items) + 1):
                if n < len(items):
                    emit_qk(n)
                if n >= 1:
                    emit_pv(n - 1)

        if dbg and "O" in dbg and H == dbg.get("_H", 1):
            tmpo = SC.get(F32, [D])
            for t in range(NOT):
                S.add("dve", lambda h, t=t: h.tensor_copy(out=tmpo, in_=O_sb[:, t, :]),
                      reads=[("O", t, u) for u in range(16)], writes=["tmpo"])
                dma("sp", dbg_out["O"][t], tmpo, "dbg", reads=["tmpo"], writes=["dbgout"])

        S.fence()
        S.phase = f'H{H}_p3a_onorm'
        SC.reset()
        junk = SC.get(BF16, [D])
        onb = [SC.get(BF16, [D]) for _ in range(2)]
        ss16 = stat[:, 16:16 + 2 * NOT]
        rs16 = stat[:, 32:32 + 2 * NOT]
        for t in range(NOT):
            ores = [("O", t, u) for u in range(16)]
            for g in range(2):
                S.add("act", lambda h, t=t, g=g: h.activation(
                    out=junk[:, 0:1024], in_=O_sb[:, t, g * 1024:(g + 1) * 1024], func=AF.Square,
                    accum_out=ss16[:, 2 * t + g:2 * t + g + 1]), reads=ores, writes=["junk", "ss16"])
        rms_rstd(None, ss16, rs16, 1024, ["ss16"], ["rs16"])

        def p3_norm(t):
            b2 = t % 2
            ores = [("O", t, u) for u in range(16)]
            for g in range(2):
                S.add("act", lambda h, t=t, g=g, b2=b2: h.activation(
                    out=onb[b2][:, g * 1024:(g + 1) * 1024], in_=O_sb[:, t, g * 1024:(g + 1) * 1024],
                    func=AF.Copy, scale=rs16[:, 2 * t + g:2 * t + g + 1]), reads=ores + ["rs16"], writes=[f"onb{b2}"])

        p3_norm(0)
        for t in range(NOT):
            if t + 1 < NOT:
                p3_norm(t + 1)
            transposes_to_FT(onb[t % 2], f"onb{t % 2}", 4 + t, gc[:, 2, :], None, "o")

        S.fence()
        S.phase = f'H{H}_p3b_outproj'
        SC.reset()
        Wo = [SC.get(BF16, [KC, 512]) for _ in range(2)]
        gt1row = SC.get(F32, [D])
        tmpx = [SC.get(F32, [512]) for _ in range(2)]
        dma("sp", gt1row, bcast(modrow[:, V_GT1 * D:(V_GT1 + 1) * D]), "gt1r",
            reads=["modrow0", "modrow1"], writes=["gt1row"])
        for t in range(NOT):
            r = HALO + row0 + t * 128
            dma("sp", X1[:, t, :], xh[r:r + 128, :], f"x1_{t}", writes=[("X1", t, nb) for nb in range(4)])
        w_out_v = w_out.rearrange("(kc p) n -> p kc n", p=128)
        for nb in range(4):
            wo = Wo[nb % 2]
            dma("pool", wo, w_out_v[:, :, nb * 512:(nb + 1) * 512], f"wo{nb % 2}", writes=[f"wo{nb % 2}"])
            for t in range(NOT):
                a = next_acc()
                for kc in range(KC):
                    S.add("pe", lambda h, a=a, kc=kc, t=t, wo=wo: h.matmul(
                        bank(4 + a), lhsT=FT[:, kc, (4 + t) * 128:(5 + t) * 128], rhs=wo[:, kc, :],
                        start=(kc == 0), stop=(kc == KC - 1)),
                        reads=[f"wo{nb % 2}", ("FT", 4 + t)], writes=[f"acc{a}"])
                tx = tmpx[a]
                S.add("dve", lambda h, a=a, tx=tx, nb=nb: h.tensor_tensor(
                    out=tx, in0=bank(4 + a), in1=gt1row[:, nb * 512:(nb + 1) * 512], op=ALU.mult),
                    reads=[f"acc{a}", "gt1row"], writes=[f"tmpx{a}"])
                S.add("dve", lambda h, tx=tx, t=t, nb=nb: h.tensor_tensor(
                    out=X1[:, t, nb * 512:(nb + 1) * 512], in0=X1[:, t, nb * 512:(nb + 1) * 512], in1=tx, op=ALU.add),
                    reads=[f"tmpx{a}", ("X1", t, nb)], writes=[("X1", t, nb)])
        if dbg and "X1" in dbg and H == dbg.get("_H", 1):
            for t in range(NOT):
                dma("sp", dbg_out["X1"][t], X1[:, t, :], "dbg", reads=[("X1", t, nb) for nb in range(4)],
                    writes=["dbgout"])

        S.fence()
        S.phase = f'H{H}_p4_route'
        SC.reset()
        junk = SC.get(BF16, [D])
        xnb = [SC.get(BF16, [D]) for _ in range(2)]
        L = SC.get(F32, [NOT, 36])
        ss8 = stat[:, 16:16 + NOT]
        rs8 = rsP[:, H * NOT:(H + 1) * NOT]
        for t in range(NOT):
            xres = [("X1", t, nb) for nb in range(4)]
            S.add("act", lambda h, t=t: h.activation(out=junk, in_=X1[:, t, :], func=AF.Square, accum_out=ss8[:, t:t + 1]),
                  reads=xres, writes=["junk", "ss8"])
        rms_rstd(None, ss8, rs8, D, ["ss8"], [f"rsP{H * NOT + t}" for t in range(NOT)])

        def p4_norm(t):
            b2 = t % 2
            xres = [("X1", t, nb) for nb in range(4)]
            S.add("act", lambda h, t=t, b2=b2: h.activation(out=xnb[b2], in_=X1[:, t, :], func=AF.Copy,
                                                            scale=rsP[:, H * NOT + t:H * NOT + t + 1]),
                  reads=xres + [f"rsP{H * NOT + t}"], writes=[f"xnb{b2}"])

        p4_norm(0)
        for t in range(NOT):
            if t + 1 < NOT:
                p4_norm(t + 1)
            transposes_to_FT(xnb[t % 2], f"xnb{t % 2}", 4 + t, gm2, colv(V_SH2), "h2")
            a = next_acc()
            for kc in range(KC):
                S.add("pe", lambda h, a=a, kc=kc, t=t: h.matmul(
                    bank(4 + a, 0, 36), lhsT=FT[:, kc, (4 + t) * 128:(5 + t) * 128], rhs=wr[:, kc, :],
                    start=(kc == 0), stop=(kc == KC - 1)),
                    reads=["wr", ("FT", 4 + t)], writes=[f"acc{a}"])
            S.add("dve", lambda h, a=a, t=t: h.tensor_tensor(out=L[:, t, :], in0=bank(4 + a, 0, 36), in1=brow, op=ALU.add),
                  reads=[f"acc{a}", "brow"], writes=["L"])
        gl = L[:, :, 0:4]
        el = L[:, :, 4:36]
        gmax = SC.get(F32, [NOT]); gsum = SC.get(F32, [NOT]); pg = SC.get(F32, [NOT])
        gt_ = SC.get(F32, [NOT, 4]); ohg = SC.get(F32, [NOT, 4]); pen = SC.get(F32, [NOT, 4])
        elm = SC.get(F32, [NOT, 32]); oh1 = SC.get(F32, [NOT, 32]); oh2 = SC.get(F32, [NOT, 32])
        elm2 = SC.get(F32, [NOT, 32])
        m1 = SC.get(F32, [NOT]); m2 = SC.get(F32, [NOT]); e2 = SC.get(F32, [NOT]); w1 = SC.get(F32, [NOT])
        w2 = SC.get(F32, [NOT])
        RR = ["L", "rt"]

        def dv(fn):
            S.add("dve", fn, reads=RR, writes=["rt"])

        dv(lambda h: h.tensor_reduce(out=gmax, in_=gl, axis=AX.X, op=ALU.max))
        dv(lambda h: h.tensor_tensor(out=gt_, in0=gl, in1=gmax.unsqueeze(2).to_broadcast([128, NOT, 4]), op=ALU.subtract))
        S.add("act", lambda h: h.activation(out=gt_, in_=gt_, func=AF.Exp), reads=RR, writes=["rt"])
        dv(lambda h: h.tensor_reduce(out=gsum, in_=gt_, axis=AX.X, op=ALU.add))
        dv(lambda h: h.reciprocal(out=pg, in_=gsum))
        dv(lambda h: h.tensor_tensor(out=ohg, in0=gl, in1=gmax.unsqueeze(2).to_broadcast([128, NOT, 4]), op=ALU.is_ge))
        dv(lambda h: h.tensor_scalar(out=pen, in0=ohg, scalar1=-1.0, scalar2=1e9, op0=ALU.add, op1=ALU.mult))
        dv(lambda h: h.tensor_tensor(
            out=elm.rearrange("p t (g j) -> p t g j", g=4), in0=el.rearrange("p t (g j) -> p t g j", g=4),
            in1=pen.unsqueeze(3).to_broadcast([128, NOT, 4, 8]), op=ALU.add))
        dv(lambda h: h.tensor_reduce(out=m1, in_=elm, axis=AX.X, op=ALU.max))
        dv(lambda h: h.tensor_tensor(out=oh1, in0=elm, in1=m1.unsqueeze(2).to_broadcast([128, NOT, 32]), op=ALU.is_ge))
        dv(lambda h: h.scalar_tensor_tensor(out=elm2, in0=oh1, scalar=-1e9, in1=elm, op0=ALU.mult, op1=ALU.add))
        dv(lambda h: h.tensor_reduce(out=m2, in_=elm2, axis=AX.X, op=ALU.max))
        dv(lambda h: h.tensor_tensor(out=oh2, in0=elm2, in1=m2.unsqueeze(2).to_broadcast([128, NOT, 32]), op=ALU.is_ge))
        dv(lambda h: h.tensor_tensor(out=e2, in0=m2, in1=m1, op=ALU.subtract))
        S.add("act", lambda h: h.activation(out=e2, in_=e2, func=AF.Exp), reads=RR, writes=["rt"])
        dv(lambda h: h.tensor_scalar(out=w1, in0=e2, scalar1=1.0, scalar2=None, op0=ALU.add))
        dv(lambda h: h.reciprocal(out=w1, in_=w1))
        dv(lambda h: h.tensor_tensor(out=w1, in0=w1, in1=pg, op=ALU.mult))
        dv(lambda h: h.tensor_tensor(out=w2, in0=w1, in1=e2, op=ALU.mult))
        dv(lambda h: h.tensor_tensor(out=elm, in0=oh1, in1=w1.unsqueeze(2).to_broadcast([128, NOT, 32]), op=ALU.mult))
        dv(lambda h: h.tensor_tensor(out=elm2, in0=oh2, in1=w2.unsqueeze(2).to_broadcast([128, NOT, 32]), op=ALU.mult))
        S.add("dve", lambda h: h.tensor_tensor(out=comb, in0=elm, in1=elm2, op=ALU.add), reads=RR, writes=["comb"])
        if dbg and "comb" in dbg and H == dbg.get("_H", 1):
            dma("sp", dbg_out["comb"], comb, "dbg", reads=["comb"], writes=["dbgout"])
        hs_ = slice(H * NOT, (H + 1) * NOT)
        S.add("dve", lambda h: h.tensor_copy(out=w1p[:, hs_], in_=w1), reads=RR, writes=["w1p"])
        S.add("dve", lambda h: h.tensor_copy(out=w2p[:, hs_], in_=w2), reads=RR, writes=["w2p"])
        S.add("dve", lambda h: h.tensor_copy(out=oh1_all[:, hs_, :], in_=oh1), reads=RR, writes=["ohall"])
        S.add("dve", lambda h: h.tensor_copy(out=oh2_all[:, hs_, :], in_=oh2), reads=RR, writes=["ohall"])

        S.phase = f'H{H}_p4c_spill'
        gm2row = SC.get(F32, [D]); sh2row = SC.get(F32, [D])
        h2f = SC.get(F32, [D])
        gfr = h2f
        h2tok = xnb
        dma("sp", gm2row, bcast(modrow[:, V_SC2 * D:(V_SC2 + 1) * D]), "r0", reads=["modrow0", "modrow1"], writes=["gm2row"])
        dma("sp", sh2row, bcast(modrow[:, V_SH2 * D:(V_SH2 + 1) * D]), "r1", reads=["modrow0", "modrow1"], writes=["sh2row"])
        dma("sp", gfr, bcast(g_ffn_row), "r2", writes=["h2f"])
        S.add("dve", lambda h: h.scalar_tensor_tensor(out=gm2row, in0=gm2row, scalar=1.0, in1=gfr, op0=ALU.add, op1=ALU.mult),
              reads=["gm2row", "h2f"], writes=["gm2row"])
        for t in range(NOT):
            b2 = t % 2
            tt = H * NOT + t
            xres = [("X1", t, nb) for nb in range(4)]
            dma("sp", X1S[tt * 128:(tt + 1) * 128, :], X1[:, t, :], f"x1s{b2}", reads=xres, writes=[("X1S", tt)])
            S.add("dve", lambda h, t=t, tt=tt: h.scalar_tensor_tensor(out=h2f, in0=X1[:, t, :], scalar=rsP[:, tt:tt + 1], in1=gm2row,
                                                                      op0=ALU.mult, op1=ALU.mult),
                  reads=xres + [f"rsP{tt}", "gm2row"], writes=["h2f"])
            S.add("dve", lambda h, b2=b2: h.tensor_tensor(out=h2tok[b2], in0=h2f, in1=sh2row, op=ALU.add),
                  reads=["h2f", "sh2row"], writes=[f"xnb{b2}"])
            dma("sp", H2S[tt * 128:(tt + 1) * 128, :], h2tok[b2], f"h2s{b2}", reads=[f"xnb{b2}"], writes=[("H2S", tt)])

    S.fence()
    S.phase = 'G_p4b_slots'
    SC.reset()
    ohA = SC.get(F32, [NTT, 32]); ohAb = SC.get(BF16, [NTT, 32])
    cntS = SC.get(F32, [32]); rk = SC.get(F32, [NTT, 32])
    cmp8 = SC.get(F32, [32, 8]); nt_ = SC.get(F32, [32]); padded = SC.get(F32, [32])
    cs = [SC.get(F32, [32]) for _ in range(2)]
    off = SC.get(F32, [32]); sbt = SC.get(F32, [NTT, 32]); tmp3 = SC.get(F32, [NTT, 32])
    s1f = SC.get(F32, [NTT]); s2f = SC.get(F32, [NTT])
    cmpS = SC.get(F32, [NST, 32]); eS = SC.get(F32, [NST]); idxf = SC.get(F32, [NST])
    RR2 = ["rt", "cst2", "ohall"]

    def dv2(fn):
        S.add("dve", fn, reads=RR2, writes=["rt"])

    dv2(lambda h: h.tensor_tensor(out=ohA, in0=oh1_all, in1=oh2_all, op=ALU.add))
    dv2(lambda h: h.tensor_copy(out=ohAb, in_=ohA))
    CNT = bank(4, 0, 32)
    RK = bank(5, 0, NTT * 32).rearrange("p (t e) -> p t e", t=NTT)
    for t in range(NTT):
        S.add("pe", lambda h, t=t: h.matmul(CNT, lhsT=ones_b, rhs=ohAb[:, t, :], start=(t == 0), stop=(t == NTT - 1)),
              reads=["rt", "ones_b"], writes=["acc0"])
    for t in range(NTT):
        for tp in range(t):
            S.add("pe", lambda h, t=t, tp=tp: h.matmul(RK[:, t, :], lhsT=ones_b, rhs=ohAb[:, tp, :],
                                                       start=(tp == 0), stop=False),
                  reads=["rt", "ones_b"], writes=["acc1"])
        S.add("pe", lambda h, t=t: h.matmul(RK[:, t, :], lhsT=tri_b, rhs=ohAb[:, t, :], start=(t == 0), stop=True),
              reads=["rt", "tri_b"], writes=["acc1"])
    S.add("dve", lambda h: h.tensor_copy(out=cntS, in_=CNT), reads=["acc0"], writes=["rt"])
    S.add("dve", lambda h: h.tensor_copy(out=rk, in_=RK), reads=["acc1"], writes=["rt"])
    dv2(lambda h: h.tensor_tensor(out=cmp8, in0=cntS.unsqueeze(2).to_broadcast([128, 32, 8]),
                                  in1=thr8.unsqueeze(1).to_broadcast([128, 32, 8]), op=ALU.is_gt))
    dv2(lambda h: h.tensor_reduce(out=nt_, in_=cmp8, axis=AX.X, op=ALU.add))
    dv2(lambda h: h.tensor_scalar(out=padded, in0=nt_, scalar1=float(TS), scalar2=None, op0=ALU.mult))
    dv2(lambda h: h.tensor_copy(out=cs[0], in_=padded))
    cur = 0
    for sh in (1, 2, 4, 8, 16):
        a_, b_ = cs[cur], cs[1 - cur]
        dv2(lambda h, a_=a_, b_=b_: h.tensor_copy(out=b_, in_=a_))
        dv2(lambda h, a_=a_, b_=b_, sh=sh: h.tensor_tensor(out=b_[:, sh:32], in0=a_[:, sh:32], in1=a_[:, 0:32 - sh], op=ALU.add))
        cur = 1 - cur
    incl = cs[cur]
    dv2(lambda h: h.tensor_tensor(out=off, in0=incl, in1=padded, op=ALU.subtract))
    dv2(lambda h: h.tensor_tensor(out=sbt, in0=rk, in1=off.unsqueeze(1).to_broadcast([128, NTT, 32]), op=ALU.add))
    dv2(lambda h: h.tensor_tensor(out=tmp3, in0=sbt, in1=oh1_all, op=ALU.mult))
    dv2(lambda h: h.tensor_reduce(out=s1f, in_=tmp3, axis=AX.X, op=ALU.add))
    dv2(lambda h: h.tensor_tensor(out=tmp3, in0=sbt, in1=oh2_all, op=ALU.mult))
    dv2(lambda h: h.tensor_reduce(out=s2f, in_=tmp3, axis=AX.X, op=ALU.add))
    S.add("dve", lambda h: h.tensor_copy(out=slot1_i, in_=s1f), reads=RR2, writes=["slot1"])
    S.add("dve", lambda h: h.tensor_copy(out=slot2_i, in_=s2f), reads=RR2, writes=["slot2"])
    dv2(lambda h: h.tensor_tensor(out=cmpS, in0=incl.unsqueeze(1).to_broadcast([128, NST, 32]),
                                  in1=s128.unsqueeze(2).to_broadcast([128, NST, 32]), op=ALU.is_le))
    dv2(lambda h: h.tensor_reduce(out=eS, in_=cmpS, axis=AX.X, op=ALU.add))
    dv2(lambda h: h.tensor_scalar(out=idxf, in0=eS, scalar1=128.0, scalar2=piota, op0=ALU.mult, op1=ALU.add))
    S.add("dve", lambda h: h.tensor_copy(out=idxW, in_=idxf), reads=RR2, writes=["idxW"])

    S.phase = 'G_p4d_scatter'
    zt = SC.get(BF16, [D])
    h2t = [SC.get(BF16, [D]) for _ in range(2)]
    S.add("pool", lambda h: h.memset(zt, 0.0), writes=["zt"])
    for st in range(NST * SUB):
        dma("sp", HS[st * 128:(st + 1) * 128, :], zt, "hsz", reads=["zt"], writes=["HS"])
    for tt in range(NTT):
        b2 = tt % 2
        dma("sp", h2t[b2], H2S[tt * 128:(tt + 1) * 128, :], f"h2l{b2}", reads=[("H2S", tt)], writes=[f"h2t{b2}"])
        for k_, sl in enumerate((slot1_i, slot2_i)):
            S.add("pool", lambda h, sl=sl, tt=tt, b2=b2: h.indirect_dma_start(
                out=HS, out_offset=bass.IndirectOffsetOnAxis(ap=sl[:, tt:tt + 1], axis=0),
                in_=h2t[b2], in_offset=None, bounds_check=bc_reg(h, NST * TS - 1), oob_is_err=False),
                reads=[f"h2t{b2}", "slot1", "slot2", "HS"], writes=[("HSs", tt, k_)], dma=f"hss{b2}{k_}")

    S.fence()
    S.phase = 'G_p5_moe'
    SC.reset()
    ring = [sview(P_BYTES + r * 16384, BF16, [8192]) for r in range(3)] + \
           [sview(OB_OFF + r * 16384, BF16, [8192]) for r in range(4)]
    NR_ = len(ring)
    hsb = [SC.get(BF16, [SUB, D]) for _ in range(2)]
    hsT = [SC.get(BF16, [KC, TS]) for _ in range(2)]
    aT = [SC.get(BF16, [4, TS]) for _ in range(2)]
    sg = [SC.get(F32, [TS]) for _ in range(2)]
    ysb = [SC.get(F32, [D]) for _ in range(2)]
    for r_ in range(NR_):
        S.add("dve", lambda h, r_=r_: h.memset(ring[r_], 0.0), writes=[f"ring{r_}"])
    ring_i = [0]

    def ring_next():
        r = ring_i[0] % NR_
        ring_i[0] += 1
        return r

    gu_i = [0]
    y_i = [0]
    ys_i = [0]
    n_tiles = dbg.get("_ntiles", NST) if dbg else NST
    hs_all = ["HS"] + [("HSs", tt, k_) for tt in range(NTT) for k_ in range(2)]

    def load_hs(st):
        for sub in range(SUB):
            r0 = (st * SUB + sub) * 128
            dma("sp", hsb[st % 2][:, sub, :], HS[r0:r0 + 128, :], f"hsl{st % 2}{sub}",
                reads=hs_all, writes=[f"hsb{st % 2}{sub}"])

    load_hs(0)
    for st in range(n_tiles):
        b2 = st % 2
        rg, ru, rd = ring_next(), ring_next(), ring_next()
        for (r_, wsrc) in ((rg, w_gate), (ru, w_up), (rd, w_down)):
            S.add("pool", lambda h, r_=r_, wsrc=wsrc, st=st: h.indirect_dma_start(
                out=ring[r_], out_offset=None, in_=wsrc,
                in_offset=bass.IndirectOffsetOnAxis(ap=idxW[:, st:st + 1], axis=0),
                bounds_check=bc_reg(h, N_EXP * 128 - 1), oob_is_err=False),
                reads=["idxW"], writes=[f"ring{r_}"], dma=f"ring{r_}")
        Wg = ring[rg].rearrange("p (a b) -> p a b", a=KC)
        Wup = ring[ru].rearrange("p (a b) -> p a b", a=KC)
        Wd = ring[rd].rearrange("p (a b) -> p a b", a=4)
        if st + 1 < n_tiles:
            load_hs(st + 1)
        for sub in range(SUB):
            for hb in range(2):
                pb = bank_bf(6 + hb)
                for k8 in range(8):
                    kc = hb * 8 + k8
                    S.add("pe", lambda h, pb=pb, k8=k8, kc=kc, b2=b2, sub=sub: h.transpose(
                        out=pb[:, k8 * 128:(k8 + 1) * 128], in_=hsb[b2][:, sub, kc * 128:(kc + 1) * 128], identity=ident_b),
                        reads=[f"hsb{b2}{sub}", "cst_b"], writes=[f"tp{hb}"])
                dst = hsT[b2][:, hb * 8:(hb + 1) * 8, sub * 128:(sub + 1) * 128]
                src = pb.rearrange("p (a b) -> p a b", a=8)
                if hb == 0:
                    S.add("act", lambda h, dst=dst, src=src: h.activation(out=dst, in_=src, func=AF.Copy),
                          reads=[f"tp{hb}"], writes=[f"hsT{b2}"])
                else:
                    S.add("dve", lambda h, dst=dst, src=src: h.tensor_copy(out=dst, in_=src),
                          reads=[f"tp{hb}"], writes=[f"hsT{b2}"])
        for mb in range(4):
            gi = gu_i[0] % 2
            gu_i[0] += 1
            G, U = bank(gi, 0, TS), bank(2 + gi, 0, TS)
            for kc in range(KC):
                S.add("pe", lambda h, kc=kc, G=G, Wg=Wg, mb=mb, b2=b2: h.matmul(
                    G, lhsT=Wg[:, kc, mb * 128:(mb + 1) * 128], rhs=hsT[b2][:, kc, :],
                    start=(kc == 0), stop=(kc == KC - 1)),
                    reads=[f"ring{rg}", f"hsT{b2}"], writes=[f"G{gi}"])
            for kc in range(KC):
                S.add("pe", lambda h, kc=kc, U=U, Wup=Wup, mb=mb, b2=b2: h.matmul(
                    U, lhsT=Wup[:, kc, mb * 128:(mb + 1) * 128], rhs=hsT[b2][:, kc, :],
                    start=(kc == 0), stop=(kc == KC - 1)),
                    reads=[f"ring{ru}", f"hsT{b2}"], writes=[f"U{gi}"])
            S.add("act", lambda h, gi=gi, G=G: h.activation(out=sg[gi], in_=G, func=AF.Silu),
                  reads=[f"G{gi}"], writes=[f"sg{gi}"])
            S.add("dve", lambda h, gi=gi, U=U, mb=mb, b2=b2: h.tensor_tensor(
                out=aT[b2][:, mb, :], in0=sg[gi], in1=U, op=ALU.mult),
                reads=[f"sg{gi}", f"U{gi}"], writes=[f"aT{b2}"])
        for sub in range(SUB):
            yb = ys_i[0] % 2
            ys_i[0] += 1
            for nb in range(4):
                yi = y_i[0] % 2
                y_i[0] += 1
                Y = bank(4 + yi)
                for kc in range(4):
                    S.add("pe", lambda h, kc=kc, Y=Y, Wd=Wd, nb=nb, b2=b2, sub=sub: h.matmul(
                        Y, lhsT=aT[b2][:, kc, sub * 128:(sub + 1) * 128], rhs=Wd[:, kc, nb * 512:(nb + 1) * 512],
                        start=(kc == 0), stop=(kc == 3)),
                        reads=[f"ring{rd}", f"aT{b2}"], writes=[f"acc{yi}"])
                if nb % 2 == 0:
                    S.add("act", lambda h, Y=Y, nb=nb, yb=yb: h.activation(
                        out=ysb[yb][:, nb * 512:(nb + 1) * 512], in_=Y, func=AF.Copy),
                        reads=[f"acc{yi}"], writes=[f"ysb{yb}"])
                else:
                    S.add("dve", lambda h, Y=Y, nb=nb, yb=yb: h.tensor_copy(out=ysb[yb][:, nb * 512:(nb + 1) * 512], in_=Y),
                          reads=[f"acc{yi}"], writes=[f"ysb{yb}"])
            r0 = (st * SUB + sub) * 128
            dma("sp", YS[r0:r0 + 128, :], ysb[yb], f"yst{yb}", reads=[f"ysb{yb}"], writes=[("YS", st, sub)])

    S.fence()
    S.phase = 'G_p6_final'
    SC.reset()
    junk = SC.get(BF16, [D])
    gfrow = SC.get(F32, [D])
    shfrow = SC.get(F32, [D])
    gt2row = SC.get(F32, [D])
    ot = [SC.get(F32, [D]) for _ in range(2)]
    G1 = [sview(P_BYTES + (2 * i) * 8192, F32, [D]) for i in range(2)]
    G2 = [sview(P_BYTES + (2 * i + 1) * 8192, F32, [D]) for i in range(2)]
    scfrow = ot[1]
    dma("sp", gfrow, bcast(g_fin), "gfr", writes=["gfrow"])
    dma("sp", shfrow, bcast(modrow[:, V_SHF * D:(V_SHF + 1) * D]), "shfr",
        reads=["modrow0", "modrow1"], writes=["shfrow"])
    dma("sp", scfrow, bcast(modrow[:, V_SCF * D:(V_SCF + 1) * D]), "scfr",
        reads=["modrow0", "modrow1"], writes=["ot1"])
    dma("sp", gt2row, bcast(modrow[:, V_GT2 * D:(V_GT2 + 1) * D]), "gt2r",
        reads=["modrow0", "modrow1"], writes=["gt2row"])
    S.add("dve", lambda h: h.scalar_tensor_tensor(out=gfrow, in0=scfrow, scalar=1.0, in1=gfrow,
                                                   op0=ALU.add, op1=ALU.mult),
          reads=["gfrow", "ot1"], writes=["gfrow"])
    ys_all = [("YS", st, sub) for st in range(n_tiles) for sub in range(SUB)]
    def p6_a(tt):
        b2 = tt % 2
        xb_i = tt % 4
        xb = X1[:, xb_i, :]
        xres = [f"xb{xb_i}"]
        dma("sp", xb, X1S[tt * 128:(tt + 1) * 128, :], f"x1l{xb_i}", reads=[("X1S", tt)], writes=xres)
        g1, g2 = G1[b2], G2[b2]
        for (Gb, sl, nm) in ((g1, slot1_i, f"G1{b2}"), (g2, slot2_i, f"G2{b2}")):
            S.add("pool", lambda h, Gb=Gb, sl=sl, tt=tt: h.indirect_dma_start(
                out=Gb, out_offset=None, in_=YS,
                in_offset=bass.IndirectOffsetOnAxis(ap=sl[:, tt:tt + 1], axis=0),
                bounds_check=bc_reg(h, NST * TS - 1), oob_is_err=False),
                reads=ys_all + ["slot1", "slot2"], writes=[nm], dma=nm)
        S.add("dve", lambda h, tt=tt, g1=g1: h.tensor_scalar(out=g1, in0=g1, scalar1=w1p[:, tt:tt + 1], scalar2=None, op0=ALU.mult),
              reads=[f"G1{b2}", "w1p"], writes=[f"G1{b2}"])
        S.add("dve", lambda h, tt=tt, g1=g1, g2=g2: h.scalar_tensor_tensor(out=g1, in0=g2, scalar=w2p[:, tt:tt + 1], in1=g1,
                                                                          op0=ALU.mult, op1=ALU.add),
              reads=[f"G1{b2}", f"G2{b2}", "w2p"], writes=[f"G1{b2}"])
        S.add("pool", lambda h, g1=g1: h.tensor_tensor(out=g1, in0=g1, in1=gt2row, op=ALU.mult),
              reads=[f"G1{b2}", "gt2row"], writes=[f"G1{b2}"])
        S.add("dve", lambda h, xb=xb, g1=g1: h.tensor_tensor(out=xb, in0=xb, in1=g1, op=ALU.add),
              reads=[f"G1{b2}"] + xres, writes=xres)
        ss = stat[:, b2:b2 + 1]
        S.add("act", lambda h, xb=xb, ss=ss: h.activation(out=junk, in_=xb, func=AF.Square, accum_out=ss),
              reads=xres, writes=["junk", f"ss{b2}"])

    def p6_b(tt):
        b2 = tt % 2
        xb_i = tt % 4
        xb = X1[:, xb_i, :]
        xres = [f"xb{xb_i}"]
        ss = stat[:, b2:b2 + 1]
        rs = stat[:, 2 + b2:3 + b2]
        rms_rstd(None, ss, rs, D, [f"ss{b2}"], [f"rs{b2}"])
        S.add("dve", lambda h, xb=xb, rs=rs, b2=b2: h.scalar_tensor_tensor(
            out=ot[b2], in0=xb, scalar=rs, in1=gfrow, op0=ALU.mult, op1=ALU.mult),
            reads=xres + [f"rs{b2}", "gfrow"], writes=[f"ot{b2}"])
        S.add("dve", lambda h, b2=b2: h.tensor_tensor(out=ot[b2], in0=ot[b2], in1=shfrow, op=ALU.add),
              reads=[f"ot{b2}", "shfrow"], writes=[f"ot{b2}"])
        r = tt * 128
        dma("sp", y_out[r:r + 128, :], ot[b2], f"yo{b2}", reads=[f"ot{b2}"], writes=["yout"])

    p6_a(0)
    for tt in range(NTT):
        if tt + 1 < NTT:
            p6_a(tt + 1)
        p6_b(tt)

    finals = ["yo0", "yo1"] + (["dbg"] if dbg and "dbg" in S.dma_cnt else [])
    S.emit(nc, es, final_waits=finals)
    es.close()
    return nc


def _col(v):
    return np.ascontiguousarray(np.asarray(v, np.float32).reshape(KC, 128).T)


def prepare_shared(inp):
    f = lambda a: np.asarray(a, dtype=np.float32)
    sh = {}
    sh["w_mod"] = np.ascontiguousarray(np.concatenate([f(inp["w_ada"])[0], f(inp["w_ada_final"])], axis=1))
    sh["b_mod"] = np.ascontiguousarray(np.concatenate([f(inp["b_ada"])[0], f(inp["b_ada_final"])])[None, :])
    g_out = np.concatenate([f(inp["g_out_a"])[0], f(inp["g_out_b"])[0]])
    sh["gcols"] = np.ascontiguousarray(np.stack([_col(f(inp["g_mix"])[0]), _col(f(inp["g_ffn"])[0]), _col(g_out)], axis=1))
    sh["g_fin"] = f(inp["g_final"])[None, :].copy()
    w_in = f(inp["w_in"])[0]
    units = []
    for a in range(8):
        g = a // 2
        q = w_in[:, a * 128:(a + 1) * 128]
        k = w_in[:, 1024 + g * 64:1024 + (g + 1) * 64]
        v = w_in[:, 1280 + g * 64:1280 + (g + 1) * 64]
        units.append(np.concatenate([q, k, k, v, v], axis=1))
    for bp in range(8):
        units.append(np.concatenate([w_in[:, 1536 + bp * 128:1536 + (bp + 1) * 128],
                                     w_in[:, 2560 + bp * 128:2560 + (bp + 1) * 128],
                                     w_in[:, 3584 + bp * 128:3584 + (bp + 1) * 128]], axis=1))
    wu = np.stack(units)
    sh["w_in_u"] = np.ascontiguousarray(wu.reshape(16, KC, 128, 384).transpose(0, 2, 1, 3))
    sh["w_out"] = np.ascontiguousarray(f(inp["w_out"])[0])
    sh["w_rt"] = np.ascontiguousarray(np.concatenate([f(inp["w_router_group"])[0], f(inp["w_router_expert"])[0]], axis=1))
    sh["b_rt"] = np.concatenate([f(inp["b_router_group"])[0], f(inp["b_router_expert"])[0]])[None, :].copy()
    sh["w_gate"] = np.ascontiguousarray(f(inp["w_gate"])[0].reshape(N_EXP, KC, 128, DE).transpose(0, 2, 1, 3)).reshape(N_EXP * 128, KC * DE)
    sh["w_up"] = np.ascontiguousarray(f(inp["w_up"])[0].reshape(N_EXP, KC, 128, DE).transpose(0, 2, 1, 3)).reshape(N_EXP * 128, KC * DE)
    sh["w_down"] = np.ascontiguousarray(f(inp["w_down"])[0].reshape(N_EXP, 4, 128, D).transpose(0, 2, 1, 3)).reshape(N_EXP * 128, 4 * D)
    sh["g_ffn_row"] = f(inp["g_ffn"])[0][None, :].copy()
    c2 = np.zeros((128, 192), np.float32)
    c2[:, 0:128] = (np.arange(128)[:, None] < np.arange(128)[None, :]).astype(np.float32)
    c2[:, 128] = np.arange(128)
    c2[:, 129:137] = float(TS) * np.arange(8)[None, :]
    c2[:, 137:137 + NST] = float(TS) * np.arange(NST)[None, :]
    sh["cst2"] = c2
    rb = f(inp["rel_bias_b"])[0]
    ki = np.arange(128)[:, None]
    qi = np.arange(128)[None, :]
    idx0 = np.clip(128 + qi - ki, -128, 128) + 128
    idx1 = np.clip(qi - ki, -128, 128) + 128
    tab = np.stack([rb[:, idx0], rb[:, idx1]], axis=1)
    sh["tabB"] = np.ascontiguousarray(tab.transpose(2, 0, 1, 3))
    sh["constB"] = np.ascontiguousarray(np.broadcast_to(rb[:, 256][None, :], (128, 16)))
    sh["sinks"] = np.ascontiguousarray(np.broadcast_to(f(inp["sinks_a"])[0][None, :], (128, 16)))
    cst = np.zeros((128, 6, 128), np.float32)
    cst[:, 0, :] = np.eye(128, dtype=np.float32)
    cst[:64, 1, 64:] = NEG
    cst[64:, 2, :64] = NEG
    cst[:, 3, :] = 128 + qi - ki
    cst[:, 4, :] = np.abs(qi - ki)
    sh["cst"] = cst
    return sh


def prepare_core(inp, core):
    b, hf = core // 2, core % 2
    x = np.asarray(inp["x"], dtype=np.float32)
    xh = np.zeros((HALO + NB_TOK_CORE, D), np.float32)
    lo = hf * NB_TOK_CORE
    xh[HALO:] = x[b, lo:lo + NB_TOK_CORE]
    if hf > 0:
        xh[:HALO] = x[b, lo - HALO:lo]
    d = {"xh": xh, "c_col": _col(np.asarray(inp["c"], np.float32)[b]),
         "hm": np.full((128, 1), 1.0 if hf > 0 else 0.0, np.float32)}
    return d


_NC_CACHE = {}


def kernel(**inputs):
    if "nc" not in _NC_CACHE:
        _NC_CACHE["nc"] = build_program()
    nc = _NC_CACHE["nc"]
    sh = prepare_shared(inputs)
    in_maps = []
    for core in range(8):
        d = dict(sh)
        d.update(prepare_core(inputs, core))
        in_maps.append(d)
    res = run_bass_kernel_spmd(nc, in_maps, core_ids=list(range(8)))
    B, S_, _ = np.asarray(inputs["x"]).shape
    out = np.empty((B, S_, D), np.float32)
    for core in range(8):
        b, hf = core // 2, core % 2
        out[b, hf * NB_TOK_CORE:(hf + 1) * NB_TOK_CORE] = res.results[core]["y"]
    return out
```

```python
import types
import numpy as np
from contextlib import ExitStack
import concourse.bass as bass
import concourse.mybir as mybir
from concourse.bass_utils import run_bass_kernel_spmd

F32 = mybir.dt.float32
BF16 = mybir.dt.bfloat16
AF = mybir.ActivationFunctionType
ALU = mybir.AluOpType
AX = mybir.AxisListType

ENGS = ("pe", "act", "dve", "pool", "sp")
NEG = -30000.0
EPS = 1e-6


def _freeze(fn):
    if fn.__closure__ is None:
        return fn
    cells = []
    for c in fn.__closure__:
        try:
            cells.append(types.CellType(c.cell_contents))
        except ValueError:
            cells.append(c)
    return types.FunctionType(fn.__code__, fn.__globals__, fn.__name__, fn.__defaults__, tuple(cells))


class Sched:
    def __init__(self):
        self.ops = []
        self.last_w = {}
        self.readers = {}
        self.dma_cnt = {}
        self.dma_last = {}
        self.eng_ops = {e: [] for e in ENGS}
        self.fence_set = set()
        self.fence_passed = {e: True for e in ENGS}
        self.profile_scopes = False
        self.profile_engs = ('pe',)
        self.phase = 'setup'

    def fence(self):
        s = set()
        for e in ENGS:
            if self.eng_ops[e]:
                s.add(self.eng_ops[e][-1])
        for k, i in self.dma_last.items():
            s.add(i)
        self.fence_set = s
        self.fence_passed = {e: False for e in ENGS}

    def add(self, eng, fn, reads=(), writes=(), dma=None):
        i = len(self.ops)
        deps = set()
        for r in reads:
            w = self.last_w.get(r)
            if w is not None:
                deps.add(w)
        for r in writes:
            w = self.last_w.get(r)
            if w is not None:
                deps.add(w)
            for rd in self.readers.get(r, ()):
                deps.add(rd)
        if not self.fence_passed[eng]:
            deps |= self.fence_set
            self.fence_passed[eng] = True
        if dma is not None and eng == "pool" and dma in self.dma_last:
            deps.add(self.dma_last[dma])
        op = dict(eng=eng, fn=_freeze(fn), deps=deps, dma=dma, idx=i, phase=getattr(self, 'phase', None))
        if dma is not None:
            self.dma_cnt[dma] = self.dma_cnt.get(dma, 0) + 16
            op["cnt"] = self.dma_cnt[dma]
            self.dma_last[dma] = i
        self.eng_ops[eng].append(i)
        op["ord"] = len(self.eng_ops[eng])
        self.ops.append(op)
        for r in reads:
            self.readers.setdefault(r, []).append(i)
        for r in writes:
            self.last_w[r] = i
            self.readers[r] = []
        return i

    def plan(self):
        ops = self.ops
        clock = {e: {} for e in ENGS}
        opclock = [None] * len(ops)
        waits = [None] * len(ops)
        signal = [False] * len(ops)

        def key_of(j):
            o = ops[j]
            if o["dma"] is not None:
                return ("dma", o["dma"]), o["cnt"]
            return o["eng"], o["ord"]

        for i, o in enumerate(ops):
            e = o["eng"]
            ck = clock[e]
            w = []
            for j in sorted(o["deps"]):
                pj = ops[j]
                if pj["dma"] is None and pj["eng"] == e == "pe":
                    continue
                k, v = key_of(j)
                if ck.get(k, 0) >= v:
                    continue
                w.append(j)
                signal[j] = True
                ck[k] = v
                for kk, vv in opclock[j].items():
                    if ck.get(kk, 0) < vv:
                        ck[kk] = vv
            waits[i] = w
            oc = dict(ck)
            if o["dma"] is not None:
                oc[("dma", o["dma"])] = o["cnt"]
            else:
                oc[e] = max(oc.get(e, 0), o["ord"])
            opclock[i] = oc
        semval = [0] * len(ops)
        for e in ENGS:
            c = 0
            for i in self.eng_ops[e]:
                if ops[i]["dma"] is None and signal[i]:
                    c += 1
                    semval[i] = c
        self.waits, self.signal, self.semval = waits, signal, semval

    def emit(self, nc, es, final_waits=()):
        self.plan()
        ops, waits, signal, semval = self.ops, self.waits, self.signal, self.semval
        eng_sems = {e: es.enter_context(nc.semaphore(f"s_{e}")) for e in ENGS}
        dma_sems = {k: es.enter_context(nc.semaphore(f"d_{k}")) for k in self.dma_cnt}
        block = es.enter_context(nc.Block())
        handles = {"pe": "tensor", "act": "scalar", "dve": "vector",
                   "pool": "gpsimd", "sp": "sync"}

        def run_engine(e, h):
            cur_scope = [None, None]
            for i in self.eng_ops[e]:
                o = ops[i]
                if self.profile_scopes and e in self.profile_engs and o["phase"] != cur_scope[0]:
                    if cur_scope[1] is not None:
                        cur_scope[1].__exit__(None, None, None)
                    cur_scope[0] = o["phase"]
                    cur_scope[1] = nc.named_scope(str(o["phase"]))
                    cur_scope[1].__enter__()
                for j in waits[i]:
                    pj = ops[j]
                    if pj["dma"] is not None:
                        h.wait_ge(dma_sems[pj["dma"]], pj["cnt"])
                    else:
                        h.wait_ge(eng_sems[pj["eng"]], semval[j])
                ins = o["fn"](h)
                if o["dma"] is not None:
                    ins.then_inc(dma_sems[o["dma"]], 16)
                elif signal[i]:
                    ins.then_inc(eng_sems[e], 1)
            if cur_scope[1] is not None:
                cur_scope[1].__exit__(None, None, None)
            if e == "sp":
                for k in final_waits:
                    h.wait_ge(dma_sems[k], self.dma_cnt[k])

        for e in ENGS:
            if not self.eng_ops[e] and not (e == "sp" and final_waits):
                continue
            getattr(block, handles[e])(lambda h, e=e: run_engine(e, h))


D = 2048
KC = 16
NB_TOK_CORE = 2048
HALF = 1024
HALO = 512
WIN = HALF + HALO
NWT = WIN // 128
NOT = HALF // 128
N_EXP = 32
DE = 512
NMODBLK = 32
SUB = 2
TS = SUB * 128
NTT = 16
NST = (2 * NB_TOK_CORE + 32 * (TS - 1)) // TS
I32 = mybir.dt.int32
SLOPES = [2.0 ** (-8.0 * (h + 1) / 16.0) for h in range(16)]
V_SH1, V_SC1, V_GT1, V_SH2, V_SC2, V_GT2, V_SHF, V_SCF = range(8)


def build_program(n_experts=N_EXP, dbg=None, profile_scopes=False):
    nc = bass.Bass("TRN2", target_bir_lowering=False)
    S = Sched()
    S.profile_scopes = profile_scopes

    def dram_in(name, shape, dt=F32):
        return nc.dram_tensor(name, list(shape), dt, kind="ExternalInput").ap()

    xh = dram_in("xh", [HALO + NB_TOK_CORE, D])
    c_col = dram_in("c_col", [128, KC])
    w_mod = dram_in("w_mod", [D, NMODBLK * 512])
    b_mod = dram_in("b_mod", [1, NMODBLK * 512])
    gcols = dram_in("gcols", [128, 3, KC])
    g_fin = dram_in("g_fin", [1, D])
    w_in_u = dram_in("w_in_u", [16, 128, KC, 384])
    w_out = dram_in("w_out", [D, D])
    w_rt = dram_in("w_rt", [D, 36])
    b_rt = dram_in("b_rt", [1, 36])
    w_gate = dram_in("w_gate", [N_EXP * 128, KC * DE])
    w_up = dram_in("w_up", [N_EXP * 128, KC * DE])
    w_down = dram_in("w_down", [N_EXP * 128, 4 * D])
    g_ffn_row = dram_in("g_ffn_row", [1, D])
    cst2_d = dram_in("cst2", [128, 192])
    tabB_d = dram_in("tabB", [128, 16, 2, 128])
    constB_d = dram_in("constB", [128, 16])
    sinks_d = dram_in("sinks", [128, 16])
    hm_d = dram_in("hm", [128, 1])
    cst_d = dram_in("cst", [128, 6, 128])
    y_out = nc.dram_tensor("y", [NB_TOK_CORE, D], F32, kind="ExternalOutput").ap()
    modrow = nc.dram_tensor("modrow", [1, NMODBLK * 512], F32, kind="Internal").ap()
    HS = nc.dram_tensor("HS", [NST * TS, D], BF16, kind="Internal").ap()
    YS = nc.dram_tensor("YS", [NST * TS, D], F32, kind="Internal").ap()
    H2S = nc.dram_tensor("H2S", [NB_TOK_CORE, D], BF16, kind="Internal").ap()
    X1S = nc.dram_tensor("X1S", [NB_TOK_CORE, D], F32, kind="Internal").ap()
    dbg_out = {}
    if dbg:
        for k, shp in dbg.items():
            if k.startswith('_'):
                continue
            dbg_out[k] = nc.dram_tensor("dbg_" + k, list(shp), F32, kind="ExternalOutput").ap()

    es = ExitStack()
    ARENA_F32 = 51200
    arena = es.enter_context(nc.sbuf_tensor("arena", [128, ARENA_F32], F32))
    psum = es.enter_context(nc.psum_tensor("psum", [128, 4096], F32))

    def sview(off, dt, shape):
        n = int(np.prod(shape))
        nbytes = n * (2 if dt == BF16 else 4)
        assert off % 4 == 0 and nbytes % 4 == 0
        assert off + nbytes <= ARENA_F32 * 4, (off, nbytes)
        a = arena[:, off // 4:(off + nbytes) // 4]
        if dt != F32:
            a = a.bitcast(dt)
        if len(shape) >= 2:
            names = [f"d{i}" for i in range(len(shape))]
            pat = "p (" + " ".join(names) + ") -> p " + " ".join(names)
            a = a.rearrange(pat, **{n: int(v) for n, v in zip(names[:-1], shape[:-1])})
        return a

    class Alloc:
        def __init__(self, base, limit):
            self.base, self.off, self.limit = base, base, limit

        def reset(self):
            self.off = self.base

        def get(self, dt, shape):
            n = int(np.prod(shape)) * (2 if dt == BF16 else 4)
            n = (n + 63) // 64 * 64
            v = sview(self.off, dt, shape)
            self.off += n
            assert self.off <= self.limit, (self.off, self.limit)
            return v

    P_BYTES = 24576
    FT_BYTES = KC * WIN * 2
    OB_BYTES = 65536
    PA = Alloc(0, P_BYTES)
    FT = sview(P_BYTES, BF16, [KC, WIN])
    OB_OFF = P_BYTES + FT_BYTES
    O_sb = sview(OB_OFF, BF16, [NOT, D])
    X1 = sview(OB_OFF, F32, [NOT, D])
    SC = Alloc(OB_OFF + OB_BYTES, ARENA_F32 * 4)

    def bank(b, lo=0, hi=512):
        return psum[:, b * 512 + lo:b * 512 + hi]

    def bank_bf(b):
        return psum[:, b * 512:(b + 1) * 512].bitcast(BF16)

    colall = PA.get(F32, [NMODBLK * 4])
    gc = PA.get(F32, [3, KC])
    gm1 = PA.get(F32, [KC])
    gm2 = PA.get(F32, [KC])
    ident_f = PA.get(F32, [128])
    cst_b = PA.get(BF16, [3, 128])
    relA = PA.get(F32, [2, 128])
    maskA = PA.get(F32, [2, 128])
    tabB = PA.get(BF16, [16, 2, 128])
    constB = PA.get(F32, [16])
    esink = PA.get(F32, [16])
    hm = PA.get(F32, [1])
    brow = PA.get(F32, [36])
    wr = PA.get(BF16, [KC, 36])
    comb = PA.get(F32, [NOT, 32])
    rsP = PA.get(F32, [NTT])
    w1p = PA.get(F32, [NTT])
    w2p = PA.get(F32, [NTT])
    slot1_i = PA.get(I32, [NTT])
    slot2_i = PA.get(I32, [NTT])
    oh1_all = PA.get(F32, [NTT, 32])
    oh2_all = PA.get(F32, [NTT, 32])
    idxW = PA.get(I32, [NST])
    cst2 = PA.get(F32, [64])
    ones_b = PA.get(BF16, [128])
    tri_b = PA.get(BF16, [128])
    piota = cst2[:, 0:1]
    thr8 = cst2[:, 1:9]
    s128 = cst2[:, 9:9 + NST]
    stat = PA.get(F32, [64])
    ident_b = cst_b[:, 0, :]
    maskF_b = cst_b[:, 1, :]

    _bc_regs = {}

    def bc_reg(h, val):
        if val not in _bc_regs:
            r = h.alloc_register(f"bc{val}")
            h.reg_mov(r, val)
            _bc_regs[val] = r
        return _bc_regs[val]

    def bcast(ap_row):
        return ap_row.to_broadcast([128, ap_row.shape[1]])

    def dma(eng, out, in_, key, reads=(), writes=()):
        return S.add(eng, lambda h: h.dma_start(out=out, in_=in_), reads=reads, writes=writes, dma=key)

    SC.reset()
    cst_f = SC.get(F32, [6, 128])
    ccol = SC.get(F32, [KC])
    cact = SC.get(F32, [KC])
    carep = SC.get(BF16, [KC, 128])
    sink_f = SC.get(F32, [16])
    dma("sp", cst_f, cst_d, "c0", writes=["cst_f"])
    dma("pool", tabB, tabB_d, "c1", writes=["tabB"])
    dma("sp", ccol, c_col, "c2", writes=["ccol"])
    dma("sp", gc, gcols, "c3", writes=["gc"])
    dma("sp", constB, constB_d, "c4", writes=["constB"])
    dma("sp", sink_f, sinks_d, "c5", writes=["sink_f"])
    dma("sp", hm, hm_d, "c6", writes=["hm"])
    dma("sp", brow, bcast(b_rt), "c7", writes=["brow"])
    dma("pool", wr, w_rt.rearrange("(kc p) n -> p kc n", p=128), "c8", writes=["wr"])
    dma("pool", tri_b, cst2_d[:, 0:128], "c9", writes=["tri_b"])
    dma("sp", cst2[:, 0:56], cst2_d[:, 128:184], "c10", writes=["cst2"])
    S.add("pool", lambda h: h.memset(ones_b, 1.0), writes=["ones_b"])

    S.add("dve", lambda h: h.tensor_copy(out=ident_f, in_=cst_f[:, 0, :]), reads=["cst_f"], writes=["ident_f"])
    S.add("dve", lambda h: h.tensor_copy(out=cst_b, in_=cst_f[:, 0:3, :]), reads=["cst_f"], writes=["cst_b"])
    S.add("dve", lambda h: h.tensor_copy(out=relA, in_=cst_f[:, 3:5, :]), reads=["cst_f"], writes=["relA"])
    S.add("dve", lambda h: h.tensor_copy(out=maskA, in_=cst_f[:, 1:3, :]), reads=["cst_f"], writes=["maskA"])
    S.add("dve", lambda h: h.tensor_tensor(out=tabB[:, :, 1, :], in0=tabB[:, :, 1, :],
                                            in1=cst_b[:, 2, :].unsqueeze(1).to_broadcast([128, 16, 128]),
                                            op=ALU.add), reads=["cst_b", "tabB"], writes=["tabB"])
    S.add("act", lambda h: h.activation(out=esink, in_=sink_f, func=AF.Exp), reads=["sink_f"], writes=["esink"])
    S.add("act", lambda h: h.activation(out=cact, in_=ccol, func=AF.Silu), reads=["ccol"], writes=["cact"])
    S.add("dve", lambda h: h.tensor_copy(out=carep, in_=cact.unsqueeze(2).to_broadcast([128, KC, 128])),
          reads=["cact"], writes=["carep"])

    S.phase = 'p0_mod'
    wblk = [SC.get(BF16, [KC, 512]) for _ in range(2)]
    rowt = [SC.get(F32, [512]) for _ in range(2)]
    biasb = [SC.get(F32, [512]) for _ in range(2)]
    diag = [SC.get(F32, [4, 128]) for _ in range(2)]
    w_mod_v = w_mod.rearrange("(kc p) n -> p kc n", p=128)
    for j in range(NMODBLK):
        wb = wblk[j % 2]
        rb_, bb, dg = rowt[j % 2], biasb[j % 2], diag[j % 2]
        acc = bank(4 + j % 2)
        dma("pool", wb, w_mod_v[:, :, j * 512:(j + 1) * 512], f"wm{j % 2}", writes=[f"wblk{j % 2}"])
        dma("sp", bb, bcast(b_mod[:, j * 512:(j + 1) * 512]), f"bm{j % 2}",
            writes=[f"biasb{j % 2}"])
        for kc in range(KC):
            S.add("pe", lambda h, kc=kc, wb=wb, acc=acc: h.matmul(acc, lhsT=carep[:, kc, :], rhs=wb[:, kc, :],
                                                                   start=(kc == 0), stop=(kc == KC - 1)),
                  reads=["carep", f"wblk{j % 2}"], writes=[f"acc{j % 2}"])
        S.add("dve", lambda h, rb_=rb_, bb=bb, acc=acc: h.tensor_tensor(out=rb_, in0=acc, in1=bb, op=ALU.add),
              reads=[f"acc{j % 2}", f"biasb{j % 2}"], writes=[f"rowt{j % 2}"])
        dma("sp", modrow[:, j * 512:(j + 1) * 512], rb_[0:1, :], f"mrow{j % 2}", reads=[f"rowt{j % 2}"], writes=[f"modrow{j % 2}"])
        S.add("dve", lambda h, rb_=rb_, dg=dg: h.tensor_tensor(
            out=dg, in0=rb_.rearrange("p (a b) -> p a b", a=4),
            in1=ident_f.unsqueeze(1).to_broadcast([128, 4, 128]), op=ALU.mult),
            reads=[f"rowt{j % 2}", "ident_f"], writes=[f"diag{j % 2}"])
        S.add("dve", lambda h, dg=dg, j=j: h.tensor_reduce(out=colall[:, j * 4:(j + 1) * 4], in_=dg,
                                                            axis=AX.X, op=ALU.add),
              reads=[f"diag{j % 2}"], writes=["colall"])

    def colv(v):
        return colall[:, v * KC:(v + 1) * KC]

    for (gm, gi, vs) in ((gm1, 0, V_SC1), (gm2, 1, V_SC2)):
        S.add("dve", lambda h, gm=gm, gi=gi, vs=vs: h.scalar_tensor_tensor(
            out=gm, in0=colv(vs), scalar=1.0, in1=gc[:, gi, :], op0=ALU.add, op1=ALU.mult),
            reads=["colall", "gc"], writes=["gm"])

    def dbg_dump(name, src_ap, dst_ap, reads):
        if dbg and name in dbg:
            dma("sp", dst_ap, src_ap, "dbg", reads=reads, writes=["dbgout"])

    if dbg and "colall" in dbg:
        dbg_dump("colall", colall, dbg_out["colall"], ["colall"])

    def rms_rstd(eng_h, ss, rstd, n, reads, writes):
        S.add("dve", lambda h: h.tensor_scalar(out=rstd, in0=ss, scalar1=1.0 / n, scalar2=EPS,
                                                op0=ALU.mult, op1=ALU.add), reads=reads, writes=writes)
        S.add("act", lambda h: h.activation(out=rstd, in_=rstd, func=AF.Ln), reads=writes, writes=writes)
        S.add("act", lambda h: h.activation(out=rstd, in_=rstd, func=AF.Exp, scale=-0.5), reads=writes, writes=writes)

    def transposes_to_FT(src_bf, src_res, lt, scale_col, bias_col, tag):
        for hb in range(2):
            pb = bank_bf(6 + hb)
            for k8 in range(8):
                kc = hb * 8 + k8
                S.add("pe", lambda h, pb=pb, k8=k8, kc=kc: h.transpose(
                    out=pb[:, k8 * 128:(k8 + 1) * 128], in_=src_bf[:, kc * 128:(kc + 1) * 128], identity=ident_b),
                    reads=[src_res, "cst_b"], writes=[f"tp{hb}"])
            for k8 in range(8):
                kc = hb * 8 + k8
                dst = FT[:, kc, lt * 128:(lt + 1) * 128]
                src = pb[:, k8 * 128:(k8 + 1) * 128]
                if k8 % 2 == 0:
                    if bias_col is not None:
                        S.add("act", lambda h, dst=dst, src=src, kc=kc: h.activation(
                            out=dst, in_=src, func=AF.Identity, scale=scale_col[:, kc:kc + 1],
                            bias=bias_col[:, kc:kc + 1]),
                            reads=[f"tp{hb}", "gm", "colall", "gc"], writes=[("FT", lt)])
                    else:
                        S.add("act", lambda h, dst=dst, src=src, kc=kc: h.activation(
                            out=dst, in_=src, func=AF.Copy, scale=scale_col[:, kc:kc + 1]),
                            reads=[f"tp{hb}", "gm", "colall", "gc"], writes=[("FT", lt)])
                else:
                    if bias_col is not None:
                        S.add("dve", lambda h, dst=dst, src=src, kc=kc: h.tensor_scalar(
                            out=dst, in0=src, scalar1=scale_col[:, kc:kc + 1], scalar2=bias_col[:, kc:kc + 1],
                            op0=ALU.mult, op1=ALU.add),
                            reads=[f"tp{hb}", "gm", "colall", "gc"], writes=[("FT", lt)])
                    else:
                        S.add("dve", lambda h, dst=dst, src=src, kc=kc: h.tensor_scalar(
                            out=dst, in0=src, scalar1=scale_col[:, kc:kc + 1], scalar2=None, op0=ALU.mult),
                            reads=[f"tp{hb}", "gm", "colall", "gc"], writes=[("FT", lt)])

    for H in range(2):
        row0 = H * HALF
        S.fence()
        S.phase = f'H{H}_p1_norm1'
        SC.reset()
        xt = [SC.get(F32, [D]) for _ in range(2)]
        xnb = [SC.get(BF16, [D]) for _ in range(2)]
        junk = SC.get(BF16, [D])
        xt = xt + [SC.get(F32, [D])]
        ss12 = stat[:, 16:16 + NWT]
        rs12 = stat[:, 32:32 + NWT]
        for lt in range(NWT):
            b3 = lt % 3
            dma("sp", xt[b3], xh[row0 + lt * 128: row0 + (lt + 1) * 128, :], f"xt{b3}", writes=[f"xt{b3}"])
            S.add("act", lambda h, b3=b3, lt=lt: h.activation(out=junk, in_=xt[b3], func=AF.Square,
                                                              accum_out=ss12[:, lt:lt + 1]),
                  reads=[f"xt{b3}"], writes=["junk", "ss12"])
        rms_rstd(None, ss12, rs12, D, ["ss12"], ["rs12"])

        def p1_norm(lt):
            b3, b2 = lt % 3, lt % 2
            dma("sp", xt[b3], xh[row0 + lt * 128: row0 + (lt + 1) * 128, :], f"xt{b3}", writes=[f"xt{b3}"])
            S.add("act", lambda h, b3=b3, b2=b2, lt=lt: h.activation(out=xnb[b2], in_=xt[b3], func=AF.Copy,
                                                                     scale=rs12[:, lt:lt + 1]),
                  reads=[f"xt{b3}", "rs12"], writes=[f"xnb{b2}"])

        p1_norm(0)
        for lt in range(NWT):
            if lt + 1 < NWT:
                p1_norm(lt + 1)
            transposes_to_FT(xnb[lt % 2], f"xnb{lt % 2}", lt, gm1, colv(V_SH1), "h")
        if dbg and "hT" in dbg and H == dbg.get("_H", 1):
            tmpf = SC.get(F32, [KC, 128])
            for lt in range(NWT):
                S.add("dve", lambda h, lt=lt: h.tensor_copy(out=tmpf, in_=FT[:, :, lt * 128:(lt + 1) * 128]),
                      reads=[("FT", lt)], writes=["tmpf"])
                dma("sp", dbg_out["hT"][lt], tmpf, "dbg", reads=["tmpf"], writes=["dbgout"])

        S.fence()
        S.phase = f'H{H}_p2_attn'
        SC.reset()
        Wu = [SC.get(BF16, [KC, 384]) for _ in range(2)]
        QT = SC.get(BF16, [HALF])
        KT = SC.get(BF16, [WIN])
        VA = SC.get(BF16, [NWT, 2, 65])
        tabA = [SC.get(BF16, [2, 2, 2, 128]) for _ in range(2)]
        tA_f = SC.get(F32, [2, 128])
        tA_h = SC.get(F32, [2, 128])
        PT = [SC.get(BF16, [5, 128]) for _ in range(2)]
        rec = [SC.get(F32, [2]) for _ in range(2)]
        S.add("pool", lambda h: h.memset(VA[:, :, :, 64:65], 1.0), writes=["VA"])
        if H == 0:
            S.add("pool", lambda h: h.tensor_copy(out=VA[:, 0:4, :, 64:65],
                                                  in_=hm.unsqueeze(1).unsqueeze(1).to_broadcast([128, 4, 2, 1])),
                  reads=["hm"], writes=["VA"])
        for b2 in range(2):
            S.add("pool", lambda h, b2=b2: h.memset(PT[b2], 0.0), writes=[f"PT{b2}"])
        if H == 1:
            zt = SC.get(BF16, [D])
            S.add("pool", lambda h: h.memset(zt, 0.0), writes=["zt"])
            for st in range(NST * SUB):
                dma("sp", HS[st * 128:(st + 1) * 128, :], zt, "hsz", reads=["zt"], writes=["HS"])

        acc_i = [0]

        def next_acc():
            a = acc_i[0] % 2
            acc_i[0] += 1
            return a

        for u in range(16):
            grpA = u < 8
            wu = Wu[u % 2]
            dma("pool", wu, w_in_u[u], f"wu{u % 2}", writes=[f"wu{u % 2}"])
            wres = f"wu{u % 2}"
            for tb in range(HALF // 512):
                a = next_acc()
                for kc in range(KC):
                    S.add("pe", lambda h, a=a, kc=kc, tb=tb, wu=wu: h.matmul(
                        bank(4 + a), lhsT=wu[:, kc, 0:128], rhs=FT[:, kc, HALO + tb * 512: HALO + (tb + 1) * 512],
                        start=(kc == 0), stop=(kc == KC - 1)),
                        reads=[wres] + [("FT", 4 + tb * 4 + i) for i in range(4)], writes=[f"acc{a}"])
                S.add("dve", lambda h, a=a, tb=tb: h.tensor_scalar(
                    out=QT[:, tb * 512:(tb + 1) * 512], in0=bank(4 + a), scalar1=0.125, scalar2=None, op0=ALU.mult),
                    reads=[f"acc{a}"], writes=["QT"])
            do_kv = (not grpA) or (u % 2 == 0)
            if do_kv:
                for tb in range(WIN // 512):
                    a = next_acc()
                    for kc in range(KC):
                        S.add("pe", lambda h, a=a, kc=kc, tb=tb, wu=wu: h.matmul(
                            bank(4 + a), lhsT=wu[:, kc, 128:256], rhs=FT[:, kc, tb * 512:(tb + 1) * 512],
                            start=(kc == 0), stop=(kc == KC - 1)),
                            reads=[wres] + [("FT", tb * 4 + i) for i in range(4)], writes=[f"acc{a}"])
                    S.add("dve", lambda h, a=a, tb=tb: h.tensor_copy(out=KT[:, tb * 512:(tb + 1) * 512], in_=bank(4 + a)),
                          reads=[f"acc{a}"], writes=["KT"])
                for lt in range(NWT):
                    a = next_acc()
                    for kc in range(KC):
                        S.add("pe", lambda h, a=a, kc=kc, lt=lt, wu=wu: h.matmul(
                            bank(4 + a, 0, 128), lhsT=FT[:, kc, lt * 128:(lt + 1) * 128], rhs=wu[:, kc, 256:384],
                            start=(kc == 0), stop=(kc == KC - 1)),
                            reads=[wres, ("FT", lt)], writes=[f"acc{a}"])
                    src = bank(4 + a, 0, 128).rearrange("p (a b) -> p a b", a=2)
                    if H == 0 and lt < 4:
                        S.add("dve", lambda h, lt=lt, src=src: h.tensor_scalar(
                            out=VA[:, lt, :, 0:64], in0=src, scalar1=hm[:, 0:1], scalar2=None, op0=ALU.mult),
                            reads=[f"acc{a}", "hm"], writes=["VA"])
                    else:
                        S.add("dve", lambda h, lt=lt, src=src: h.tensor_copy(out=VA[:, lt, :, 0:64], in_=src),
                              reads=[f"acc{a}"], writes=["VA"])
            if grpA:
                tb_ = tabA[u % 2]
                for j in range(2):
                    hd = 2 * u + j
                    S.add("pool", lambda h, hd=hd: h.tensor_scalar(
                        out=tA_f, in0=relA, scalar1=-SLOPES[hd], scalar2=None, op0=ALU.mult),
                        reads=["relA", "tA_h"], writes=["tA_f"])
                    S.add("pool", lambda h: h.tensor_tensor(out=tA_f, in0=tA_f, in1=maskA, op=ALU.add),
                          reads=["tA_f", "maskA"], writes=["tA_f"])
                    S.add("pool", lambda h, j=j, tb_=tb_: h.tensor_copy(out=tb_[:, j, 0, :, :], in_=tA_f),
                          reads=["tA_f"], writes=[f"tabA{u % 2}"])
                    S.add("pool", lambda h, j=j, tb_=tb_: h.tensor_copy(out=tA_h, in_=tb_[:, j, 0, :, :]),
                          reads=[f"tabA{u % 2}"], writes=["tA_h"])
                    S.add("pool", lambda h: h.tensor_tensor(out=tA_h, in0=tA_f, in1=tA_h, op=ALU.subtract),
                          reads=["tA_f", "tA_h"], writes=["tA_h"])
                    S.add("pool", lambda h, j=j, tb_=tb_: h.tensor_copy(out=tb_[:, j, 1, :, :], in_=tA_h),
                          reads=["tA_h"], writes=[f"tabA{u % 2}"])
            items = [(qt, j) for qt in range(NOT) for j in range(2)]

            def emit_qk(n):
                qt, j = items[n]
                sb = n % 2
                hd = (2 * u + j) if grpA else (2 * (u - 8) + j)
                pl, ph = j * 64, (j + 1) * 64
                q_ap = QT[pl:ph, qt * 128:(qt + 1) * 128]
                s_main = bank(sb, 0, 384)
                s_last = bank(2 + sb, 0, 256)
                blocks = range(3, 5) if grpA else range(5)
                for i in blocks:
                    lt = qt + i
                    dst = s_main[:, i * 128:(i + 1) * 128] if i < 3 else s_last[:, (i - 3) * 128:(i - 2) * 128]
                    sres = f"S{sb}" if i < 3 else f"S2{sb}"
                    has_tab = grpA or i in (0, 3, 4)
                    S.add("pe", lambda h, dst=dst, lt=lt, pl=pl, ph=ph, q_ap=q_ap, has_tab=has_tab: h.matmul(
                        dst, lhsT=KT[pl:ph, lt * 128:(lt + 1) * 128], rhs=q_ap, start=True, stop=not has_tab),
                        reads=["KT", "QT"], writes=[sres])
                    if grpA:
                        tb_ = tabA[u % 2]
                        for hl in range(2):
                            S.add("pe", lambda h, dst=dst, j=j, hl=hl, i=i, tb_=tb_: h.matmul(
                                dst, lhsT=ident_b, rhs=tb_[:, j, hl, i - 3, :], start=False, stop=(hl == 1)),
                                reads=["cst_b", f"tabA{u % 2}"], writes=[sres])
                    elif i == 0:
                        S.add("pe", lambda h, dst=dst: h.matmul(dst, lhsT=ident_b, rhs=maskF_b, start=False, stop=True),
                              reads=["cst_b"], writes=[sres])
                    elif i >= 3:
                        S.add("pe", lambda h, dst=dst, hd=hd, i=i: h.matmul(
                            dst, lhsT=ident_b, rhs=tabB[:, hd, i - 3, :], start=False, stop=True),
                            reads=["cst_b", "tabB"], writes=[sres])
                pt = PT[sb]
                if not grpA:
                    S.add("act", lambda h, pt=pt, s_main=s_main, hd=hd: h.activation(
                        out=pt[:, 0:3, :], in_=s_main.rearrange("p (a b) -> p a b", a=3), func=AF.Exp,
                        bias=constB[:, hd:hd + 1]),
                        reads=[f"S{sb}", "constB"], writes=[f"PT{sb}"])
                S.add("act", lambda h, pt=pt, s_last=s_last: h.activation(
                    out=pt[:, 3:5, :], in_=s_last.rearrange("p (a b) -> p a b", a=2), func=AF.Exp),
                    reads=[f"S2{sb}"], writes=[f"PT{sb}"])

            def emit_pv(n):
                qt, j = items[n]
                sb = n % 2
                ob = qt % 2
                o_ps = bank(6 + ob, 0, 130).rearrange("p (a b) -> p a b", a=2)
                blocks = list(range(3, 5)) if grpA else list(range(5))
                for bi, i in enumerate(blocks):
                    lt = qt + i
                    S.add("pe", lambda h, i=i, lt=lt, j=j, sb=sb, o_ps=o_ps, bi=bi, nb_=len(blocks): h.matmul(
                        o_ps[:, j, :], lhsT=PT[sb][:, i, :], rhs=VA[:, lt, 0 if grpA else j, :],
                        start=(bi == 0), stop=(bi == nb_ - 1)),
                        reads=[f"PT{sb}", "VA"], writes=[f"OP{ob}"])
                if j == 1:
                    rc = rec[ob]
                    den = o_ps[:, :, 64:65]
                    if grpA:
                        S.add("dve", lambda h, rc=rc, den=den: h.tensor_tensor(
                            out=rc.unsqueeze(2), in0=den, in1=esink[:, 2 * u:2 * u + 2].unsqueeze(2), op=ALU.add),
                            reads=[f"OP{ob}", "esink"], writes=[f"rec{ob}"])
                        S.add("dve", lambda h, rc=rc: h.reciprocal(out=rc, in_=rc),
                              reads=[f"rec{ob}"], writes=[f"rec{ob}"])
                    else:
                        S.add("dve", lambda h, rc=rc, den=den: h.reciprocal(out=rc.unsqueeze(2), in_=den),
                              reads=[f"OP{ob}"], writes=[f"rec{ob}"])
                    S.add("dve", lambda h, rc=rc, o_ps=o_ps, qt=qt: h.tensor_tensor(
                        out=O_sb[:, qt, u * 128:(u + 1) * 128].rearrange("p (a b) -> p a b", a=2),
                        in0=o_ps[:, :, 0:64], in1=rc.unsqueeze(2).to_broadcast([128, 2, 64]), op=ALU.mult),
                        reads=[f"OP{ob}", f"rec{ob}"], writes=[("O", qt, u)])

            for n in range(len(items) + 1):
                if n < len(items):
                    emit_qk(n)
                if n >= 1:
                    emit_pv(n - 1)

        if dbg and "O" in dbg and H == dbg.get("_H", 1):
            tmpo = SC.get(F32, [D])
            for t in range(NOT):
                S.add("dve", lambda h, t=t: h.tensor_copy(out=tmpo, in_=O_sb[:, t, :]),
                      reads=[("O", t, u) for u in range(16)], writes=["tmpo"])
                dma("sp", dbg_out["O"][t], tmpo, "dbg", reads=["tmpo"], writes=["dbgout"])

        S.fence()
        S.phase = f'H{H}_p3a_onorm'
        SC.reset()
        junk = SC.get(BF16, [D])
        onb = [SC.get(BF16, [D]) for _ in range(2)]
        ss16 = stat[:, 16:16 + 2 * NOT]
        rs16 = stat[:, 32:32 + 2 * NOT]
        for t in range(NOT):
            ores = [("O", t, u) for u in range(16)]
            for g in range(2):
                S.add("act", lambda h, t=t, g=g: h.activation(
                    out=junk[:, 0:1024], in_=O_sb[:, t, g * 1024:(g + 1) * 1024], func=AF.Square,
                    accum_out=ss16[:, 2 * t + g:2 * t + g + 1]), reads=ores, writes=["junk", "ss16"])
        rms_rstd(None, ss16, rs16, 1024, ["ss16"], ["rs16"])

        def p3_norm(t):
            b2 = t % 2
            ores = [("O", t, u) for u in range(16)]
            for g in range(2):
                S.add("act", lambda h, t=t, g=g, b2=b2: h.activation(
                    out=onb[b2][:, g * 1024:(g + 1) * 1024], in_=O_sb[:, t, g * 1024:(g + 1) * 1024],
                    func=AF.Copy, scale=rs16[:, 2 * t + g:2 * t + g + 1]), reads=ores + ["rs16"], writes=[f"onb{b2}"])

        p3_norm(0)
        for t in range(NOT):
            if t + 1 < NOT:
                p3_norm(t + 1)
            transposes_to_FT(onb[t % 2], f"onb{t % 2}", 4 + t, gc[:, 2, :], None, "o")

        S.fence()
        S.phase = f'H{H}_p3b_outproj'
        SC.reset()
        Wo = [SC.get(BF16, [KC, 512]) for _ in range(2)]
        gt1row = SC.get(F32, [D])
        tmpx = [SC.get(F32, [512]) for _ in range(2)]
        dma("sp", gt1row, bcast(modrow[:, V_GT1 * D:(V_GT1 + 1) * D]), "gt1r",
            reads=["modrow0", "modrow1"], writes=["gt1row"])
        for t in range(NOT):
            r = HALO + row0 + t * 128
            dma("sp", X1[:, t, :], xh[r:r + 128, :], f"x1_{t}", writes=[("X1", t, nb) for nb in range(4)])
        w_out_v = w_out.rearrange("(kc p) n -> p kc n", p=128)
        for nb in range(4):
            wo = Wo[nb % 2]
            dma("pool", wo, w_out_v[:, :, nb * 512:(nb + 1) * 512], f"wo{nb % 2}", writes=[f"wo{nb % 2}"])
            for t in range(NOT):
                a = next_acc()
                for kc in range(KC):
                    S.add("pe", lambda h, a=a, kc=kc, t=t, wo=wo: h.matmul(
                        bank(4 + a), lhsT=FT[:, kc, (4 + t) * 128:(5 + t) * 128], rhs=wo[:, kc, :],
                        start=(kc == 0), stop=(kc == KC - 1)),
                        reads=[f"wo{nb % 2}", ("FT", 4 + t)], writes=[f"acc{a}"])
                tx = tmpx[a]
                S.add("dve", lambda h, a=a, tx=tx, nb=nb: h.tensor_tensor(
                    out=tx, in0=bank(4 + a), in1=gt1row[:, nb * 512:(nb + 1) * 512], op=ALU.mult),
                    reads=[f"acc{a}", "gt1row"], writes=[f"tmpx{a}"])
                S.add("dve", lambda h, tx=tx, t=t, nb=nb: h.tensor_tensor(
                    out=X1[:, t, nb * 512:(nb + 1) * 512], in0=X1[:, t, nb * 512:(nb + 1) * 512], in1=tx, op=ALU.add),
                    reads=[f"tmpx{a}", ("X1", t, nb)], writes=[("X1", t, nb)])
        if dbg and "X1" in dbg and H == dbg.get("_H", 1):
            for t in range(NOT):
                dma("sp", dbg_out["X1"][t], X1[:, t, :], "dbg", reads=[("X1", t, nb) for nb in range(4)],
                    writes=["dbgout"])

        S.fence()
        S.phase = f'H{H}_p4_route'
        SC.reset()
        junk = SC.get(BF16, [D])
        xnb = [SC.get(BF16, [D]) for _ in range(2)]
        L = SC.get(F32, [NOT, 36])
        ss8 = stat[:, 16:16 + NOT]
        rs8 = rsP[:, H * NOT:(H + 1) * NOT]
        for t in range(NOT):
            xres = [("X1", t, nb) for nb in range(4)]
            S.add("act", lambda h, t=t: h.activation(out=junk, in_=X1[:, t, :], func=AF.Square, accum_out=ss8[:, t:t + 1]),
                  reads=xres, writes=["junk", "ss8"])
        rms_rstd(None, ss8, rs8, D, ["ss8"], [f"rsP{H * NOT + t}" for t in range(NOT)])

        def p4_norm(t):
            b2 = t % 2
            xres = [("X1", t, nb) for nb in range(4)]
            S.add("act", lambda h, t=t, b2=b2: h.activation(out=xnb[b2], in_=X1[:, t, :], func=AF.Copy,
                                                            scale=rsP[:, H * NOT + t:H * NOT + t + 1]),
                  reads=xres + [f"rsP{H * NOT + t}"], writes=[f"xnb{b2}"])

        p4_norm(0)
        for t in range(NOT):
            if t + 1 < NOT:
                p4_norm(t + 1)
            transposes_to_FT(xnb[t % 2], f"xnb{t % 2}", 4 + t, gm2, colv(V_SH2), "h2")
            a = next_acc()
            for kc in range(KC):
                S.add("pe", lambda h, a=a, kc=kc, t=t: h.matmul(
                    bank(4 + a, 0, 36), lhsT=FT[:, kc, (4 + t) * 128:(5 + t) * 128], rhs=wr[:, kc, :],
                    start=(kc == 0), stop=(kc == KC - 1)),
                    reads=["wr", ("FT", 4 + t)], writes=[f"acc{a}"])
            S.add("dve", lambda h, a=a, t=t: h.tensor_tensor(out=L[:, t, :], in0=bank(4 + a, 0, 36), in1=brow, op=ALU.add),
                  reads=[f"acc{a}", "brow"], writes=["L"])
        gl = L[:, :, 0:4]
        el = L[:, :, 4:36]
        gmax = SC.get(F32, [NOT]); gsum = SC.get(F32, [NOT]); pg = SC.get(F32, [NOT])
        gt_ = SC.get(F32, [NOT, 4]); ohg = SC.get(F32, [NOT, 4]); pen = SC.get(F32, [NOT, 4])
        elm = SC.get(F32, [NOT, 32]); oh1 = SC.get(F32, [NOT, 32]); oh2 = SC.get(F32, [NOT, 32])
        elm2 = SC.get(F32, [NOT, 32])
        m1 = SC.get(F32, [NOT]); m2 = SC.get(F32, [NOT]); e2 = SC.get(F32, [NOT]); w1 = SC.get(F32, [NOT])
        w2 = SC.get(F32, [NOT])
        RR = ["L", "rt"]

        def dv(fn):
            S.add("dve", fn, reads=RR, writes=["rt"])

        dv(lambda h: h.tensor_reduce(out=gmax, in_=gl, axis=AX.X, op=ALU.max))
        dv(lambda h: h.tensor_tensor(out=gt_, in0=gl, in1=gmax.unsqueeze(2).to_broadcast([128, NOT, 4]), op=ALU.subtract))
        S.add("act", lambda h: h.activation(out=gt_, in_=gt_, func=AF.Exp), reads=RR, writes=["rt"])
        dv(lambda h: h.tensor_reduce(out=gsum, in_=gt_, axis=AX.X, op=ALU.add))
        dv(lambda h: h.reciprocal(out=pg, in_=gsum))
        dv(lambda h: h.tensor_tensor(out=ohg, in0=gl, in1=gmax.unsqueeze(2).to_broadcast([128, NOT, 4]), op=ALU.is_ge))
        dv(lambda h: h.tensor_scalar(out=pen, in0=ohg, scalar1=-1.0, scalar2=1e9, op0=ALU.add, op1=ALU.mult))
        dv(lambda h: h.tensor_tensor(
            out=elm.rearrange("p t (g j) -> p t g j", g=4), in0=el.rearrange("p t (g j) -> p t g j", g=4),
            in1=pen.unsqueeze(3).to_broadcast([128, NOT, 4, 8]), op=ALU.add))
        dv(lambda h: h.tensor_reduce(out=m1, in_=elm, axis=AX.X, op=ALU.max))
        dv(lambda h: h.tensor_tensor(out=oh1, in0=elm, in1=m1.unsqueeze(2).to_broadcast([128, NOT, 32]), op=ALU.is_ge))
        dv(lambda h: h.scalar_tensor_tensor(out=elm2, in0=oh1, scalar=-1e9, in1=elm, op0=ALU.mult, op1=ALU.add))
        dv(lambda h: h.tensor_reduce(out=m2, in_=elm2, axis=AX.X, op=ALU.max))
        dv(lambda h: h.tensor_tensor(out=oh2, in0=elm2, in1=m2.unsqueeze(2).to_broadcast([128, NOT, 32]), op=ALU.is_ge))
        dv(lambda h: h.tensor_tensor(out=e2, in0=m2, in1=m1, op=ALU.subtract))
        S.add("act", lambda h: h.activation(out=e2, in_=e2, func=AF.Exp), reads=RR, writes=["rt"])
        dv(lambda h: h.tensor_scalar(out=w1, in0=e2, scalar1=1.0, scalar2=None, op0=ALU.add))
        dv(lambda h: h.reciprocal(out=w1, in_=w1))
        dv(lambda h: h.tensor_tensor(out=w1, in0=w1, in1=pg, op=ALU.mult))
        dv(lambda h: h.tensor_tensor(out=w2, in0=w1, in1=e2, op=ALU.mult))
        dv(lambda h: h.tensor_tensor(out=elm, in0=oh1, in1=w1.unsqueeze(2).to_broadcast([128, NOT, 32]), op=ALU.mult))
        dv(lambda h: h.tensor_tensor(out=elm2, in0=oh2, in1=w2.unsqueeze(2).to_broadcast([128, NOT, 32]), op=ALU.mult))
        S.add("dve", lambda h: h.tensor_tensor(out=comb, in0=elm, in1=elm2, op=ALU.add), reads=RR, writes=["comb"])
        if dbg and "comb" in dbg and H == dbg.get("_H", 1):
            dma("sp", dbg_out["comb"], comb, "dbg", reads=["comb"], writes=["dbgout"])
        hs_ = slice(H * NOT, (H + 1) * NOT)
        S.add("dve", lambda h: h.tensor_copy(out=w1p[:, hs_], in_=w1), reads=RR, writes=["w1p"])
        S.add("dve", lambda h: h.tensor_copy(out=w2p[:, hs_], in_=w2), reads=RR, writes=["w2p"])
        S.add("dve", lambda h: h.tensor_copy(out=oh1_all[:, hs_, :], in_=oh1), reads=RR, writes=["ohall"])
        S.add("dve", lambda h: h.tensor_copy(out=oh2_all[:, hs_, :], in_=oh2), reads=RR, writes=["ohall"])

        S.phase = f'H{H}_p4c_spill'
        gm2row = SC.get(F32, [D]); sh2row = SC.get(F32, [D])
        h2f = SC.get(F32, [D])
        gfr = h2f
        h2tok = xnb
        dma("sp", gm2row, bcast(modrow[:, V_SC2 * D:(V_SC2 + 1) * D]), "r0", reads=["modrow0", "modrow1"], writes=["gm2row"])
        dma("sp", sh2row, bcast(modrow[:, V_SH2 * D:(V_SH2 + 1) * D]), "r1", reads=["modrow0", "modrow1"], writes=["sh2row"])
        dma("sp", gfr, bcast(g_ffn_row), "r2", writes=["h2f"])
        S.add("dve", lambda h: h.scalar_tensor_tensor(out=gm2row, in0=gm2row, scalar=1.0, in1=gfr, op0=ALU.add, op1=ALU.mult),
              reads=["gm2row", "h2f"], writes=["gm2row"])
        for t in range(NOT):
            b2 = t % 2
            tt = H * NOT + t
            xres = [("X1", t, nb) for nb in range(4)]
            dma("sp", X1S[tt * 128:(tt + 1) * 128, :], X1[:, t, :], f"x1s{b2}", reads=xres, writes=[("X1S", tt)])
            S.add("dve", lambda h, t=t, tt=tt: h.scalar_tensor_tensor(out=h2f, in0=X1[:, t, :], scalar=rsP[:, tt:tt + 1], in1=gm2row,
                                                                      op0=ALU.mult, op1=ALU.mult),
                  reads=xres + [f"rsP{tt}", "gm2row"], writes=["h2f"])
            S.add("dve", lambda h, b2=b2: h.tensor_tensor(out=h2tok[b2], in0=h2f, in1=sh2row, op=ALU.add),
                  reads=["h2f", "sh2row"], writes=[f"xnb{b2}"])
            dma("sp", H2S[tt * 128:(tt + 1) * 128, :], h2tok[b2], f"h2s{b2}", reads=[f"xnb{b2}"], writes=[("H2S", tt)])

    S.fence()
    S.phase = 'G_p4b_slots'
    SC.reset()
    ohA = SC.get(F32, [NTT, 32]); ohAb = SC.get(BF16, [NTT, 32])
    cntS = SC.get(F32, [32]); rk = SC.get(F32, [NTT, 32])
    cmp8 = SC.get(F32, [32, 8]); nt_ = SC.get(F32, [32]); padded = SC.get(F32, [32])
    cs = [SC.get(F32, [32]) for _ in range(2)]
    off = SC.get(F32, [32]); sbt = SC.get(F32, [NTT, 32]); tmp3 = SC.get(F32, [NTT, 32])
    s1f = SC.get(F32, [NTT]); s2f = SC.get(F32, [NTT])
    cmpS = SC.get(F32, [NST, 32]); eS = SC.get(F32, [NST]); idxf = SC.get(F32, [NST])
    RR2 = ["rt", "cst2", "ohall"]

    def dv2(fn):
        S.add("dve", fn, reads=RR2, writes=["rt"])

    dv2(lambda h: h.tensor_tensor(out=ohA, in0=oh1_all, in1=oh2_all, op=ALU.add))
    dv2(lambda h: h.tensor_copy(out=ohAb, in_=ohA))
    CNT = bank(4, 0, 32)
    RK = bank(5, 0, NTT * 32).rearrange("p (t e) -> p t e", t=NTT)
    for t in range(NTT):
        S.add("pe", lambda h, t=t: h.matmul(CNT, lhsT=ones_b, rhs=ohAb[:, t, :], start=(t == 0), stop=(t == NTT - 1)),
              reads=["rt", "ones_b"], writes=["acc0"])
    for t in range(NTT):
        for tp in range(t):
            S.add("pe", lambda h, t=t, tp=tp: h.matmul(RK[:, t, :], lhsT=ones_b, rhs=ohAb[:, tp, :],
                                                       start=(tp == 0), stop=False),
                  reads=["rt", "ones_b"], writes=["acc1"])
        S.add("pe", lambda h, t=t: h.matmul(RK[:, t, :], lhsT=tri_b, rhs=ohAb[:, t, :], start=(t == 0), stop=True),
              reads=["rt", "tri_b"], writes=["acc1"])
    S.add("dve", lambda h: h.tensor_copy(out=cntS, in_=CNT), reads=["acc0"], writes=["rt"])
    S.add("dve", lambda h: h.tensor_copy(out=rk, in_=RK), reads=["acc1"], writes=["rt"])
    dv2(lambda h: h.tensor_tensor(out=cmp8, in0=cntS.unsqueeze(2).to_broadcast([128, 32, 8]),
                                  in1=thr8.unsqueeze(1).to_broadcast([128, 32, 8]), op=ALU.is_gt))
    dv2(lambda h: h.tensor_reduce(out=nt_, in_=cmp8, axis=AX.X, op=ALU.add))
    dv2(lambda h: h.tensor_scalar(out=padded, in0=nt_, scalar1=float(TS), scalar2=None, op0=ALU.mult))
    dv2(lambda h: h.tensor_copy(out=cs[0], in_=padded))
    cur = 0
    for sh in (1, 2, 4, 8, 16):
        a_, b_ = cs[cur], cs[1 - cur]
        dv2(lambda h, a_=a_, b_=b_: h.tensor_copy(out=b_, in_=a_))
        dv2(lambda h, a_=a_, b_=b_, sh=sh: h.tensor_tensor(out=b_[:, sh:32], in0=a_[:, sh:32], in1=a_[:, 0:32 - sh], op=ALU.add))
        cur = 1 - cur
    incl = cs[cur]
    dv2(lambda h: h.tensor_tensor(out=off, in0=incl, in1=padded, op=ALU.subtract))
    dv2(lambda h: h.tensor_tensor(out=sbt, in0=rk, in1=off.unsqueeze(1).to_broadcast([128, NTT, 32]), op=ALU.add))
    dv2(lambda h: h.tensor_tensor(out=tmp3, in0=sbt, in1=oh1_all, op=ALU.mult))
    dv2(lambda h: h.tensor_reduce(out=s1f, in_=tmp3, axis=AX.X, op=ALU.add))
    dv2(lambda h: h.tensor_tensor(out=tmp3, in0=sbt, in1=oh2_all, op=ALU.mult))
    dv2(lambda h: h.tensor_reduce(out=s2f, in_=tmp3, axis=AX.X, op=ALU.add))
    S.add("dve", lambda h: h.tensor_copy(out=slot1_i, in_=s1f), reads=RR2, writes=["slot1"])
    S.add("dve", lambda h: h.tensor_copy(out=slot2_i, in_=s2f), reads=RR2, writes=["slot2"])
    dv2(lambda h: h.tensor_tensor(out=cmpS, in0=incl.unsqueeze(1).to_broadcast([128, NST, 32]),
                                  in1=s128.unsqueeze(2).to_broadcast([128, NST, 32]), op=ALU.is_le))
    dv2(lambda h: h.tensor_reduce(out=eS, in_=cmpS, axis=AX.X, op=ALU.add))
    dv2(lambda h: h.tensor_scalar(out=idxf, in0=eS, scalar1=128.0, scalar2=piota, op0=ALU.mult, op1=ALU.add))
    S.add("dve", lambda h: h.tensor_copy(out=idxW, in_=idxf), reads=RR2, writes=["idxW"])

    S.phase = 'G_p4d_scatter'
    h2t = [SC.get(BF16, [D]) for _ in range(2)]
    for tt in range(NTT):
        b2 = tt % 2
        dma("sp", h2t[b2], H2S[tt * 128:(tt + 1) * 128, :], f"h2l{b2}", reads=[("H2S", tt)], writes=[f"h2t{b2}"])
        for k_, sl in enumerate((slot1_i, slot2_i)):
            S.add("pool", lambda h, sl=sl, tt=tt, b2=b2: h.indirect_dma_start(
                out=HS, out_offset=bass.IndirectOffsetOnAxis(ap=sl[:, tt:tt + 1], axis=0),
                in_=h2t[b2], in_offset=None, bounds_check=bc_reg(h, NST * TS - 1), oob_is_err=False),
                reads=[f"h2t{b2}", "slot1", "slot2", "HS"], writes=[("HSs", tt, k_)], dma=f"hss{b2}{k_}")

    S.fence()
    S.phase = 'G_p5_moe'
    SC.reset()
    ring = [sview(P_BYTES + r * 16384, BF16, [8192]) for r in range(3)] + \
           [sview(OB_OFF + r * 16384, BF16, [8192]) for r in range(4)]
    NR_ = len(ring)
    hsb = [SC.get(BF16, [SUB, D]) for _ in range(2)]
    hsT = [SC.get(BF16, [KC, TS]) for _ in range(2)]
    aT = [SC.get(BF16, [4, TS]) for _ in range(2)]
    sg = [SC.get(F32, [TS]) for _ in range(2)]
    ysb = [SC.get(F32, [D]) for _ in range(2)]
    for r_ in range(NR_):
        S.add("dve", lambda h, r_=r_: h.memset(ring[r_], 0.0), writes=[f"ring{r_}"])
    ring_i = [0]

    def ring_next():
        r = ring_i[0] % NR_
        ring_i[0] += 1
        return r

    gu_i = [0]
    y_i = [0]
    ys_i = [0]
    n_tiles = dbg.get("_ntiles", NST) if dbg else NST
    hs_all = ["HS"] + [("HSs", tt, k_) for tt in range(NTT) for k_ in range(2)]

    def load_hs(st):
        for sub in range(SUB):
            r0 = (st * SUB + sub) * 128
            dma("sp", hsb[st % 2][:, sub, :], HS[r0:r0 + 128, :], f"hsl{st % 2}{sub}",
                reads=hs_all, writes=[f"hsb{st % 2}{sub}"])

    load_hs(0)
    for st in range(n_tiles):
        b2 = st % 2
        rg, ru, rd = ring_next(), ring_next(), ring_next()
        for (r_, wsrc) in ((rg, w_gate), (ru, w_up), (rd, w_down)):
            S.add("pool", lambda h, r_=r_, wsrc=wsrc, st=st: h.indirect_dma_start(
                out=ring[r_], out_offset=None, in_=wsrc,
                in_offset=bass.IndirectOffsetOnAxis(ap=idxW[:, st:st + 1], axis=0),
                bounds_check=bc_reg(h, N_EXP * 128 - 1), oob_is_err=False),
                reads=["idxW"], writes=[f"ring{r_}"], dma=f"ring{r_}")
        Wg = ring[rg].rearrange("p (a b) -> p a b", a=KC)
        Wup = ring[ru].rearrange("p (a b) -> p a b", a=KC)
        Wd = ring[rd].rearrange("p (a b) -> p a b", a=4)
        if st + 1 < n_tiles:
            load_hs(st + 1)
        for sub in range(SUB):
            for hb in range(2):
                pb = bank_bf(6 + hb)
                for k8 in range(8):
                    kc = hb * 8 + k8
                    S.add("pe", lambda h, pb=pb, k8=k8, kc=kc, b2=b2, sub=sub: h.transpose(
                        out=pb[:, k8 * 128:(k8 + 1) * 128], in_=hsb[b2][:, sub, kc * 128:(kc + 1) * 128], identity=ident_b),
                        reads=[f"hsb{b2}{sub}", "cst_b"], writes=[f"tp{hb}"])
                dst = hsT[b2][:, hb * 8:(hb + 1) * 8, sub * 128:(sub + 1) * 128]
                src = pb.rearrange("p (a b) -> p a b", a=8)
                if hb == 0:
                    S.add("act", lambda h, dst=dst, src=src: h.activation(out=dst, in_=src, func=AF.Copy),
                          reads=[f"tp{hb}"], writes=[f"hsT{b2}"])
                else:
                    S.add("dve", lambda h, dst=dst, src=src: h.tensor_copy(out=dst, in_=src),
                          reads=[f"tp{hb}"], writes=[f"hsT{b2}"])
        for mb in range(4):
            gi = gu_i[0] % 2
            gu_i[0] += 1
            G, U = bank(gi, 0, TS), bank(2 + gi, 0, TS)
            for kc in range(KC):
                S.add("pe", lambda h, kc=kc, G=G, Wg=Wg, mb=mb, b2=b2: h.matmul(
                    G, lhsT=Wg[:, kc, mb * 128:(mb + 1) * 128], rhs=hsT[b2][:, kc, :],
                    start=(kc == 0), stop=(kc == KC - 1)),
                    reads=[f"ring{rg}", f"hsT{b2}"], writes=[f"G{gi}"])
            for kc in range(KC):
                S.add("pe", lambda h, kc=kc, U=U, Wup=Wup, mb=mb, b2=b2: h.matmul(
                    U, lhsT=Wup[:, kc, mb * 128:(mb + 1) * 128], rhs=hsT[b2][:, kc, :],
                    start=(kc == 0), stop=(kc == KC - 1)),
                    reads=[f"ring{ru}", f"hsT{b2}"], writes=[f"U{gi}"])
            S.add("act", lambda h, gi=gi, G=G: h.activation(out=sg[gi], in_=G, func=AF.Silu),
                  reads=[f"G{gi}"], writes=[f"sg{gi}"])
            S.add("dve", lambda h, gi=gi, U=U, mb=mb, b2=b2: h.tensor_tensor(
                out=aT[b2][:, mb, :], in0=sg[gi], in1=U, op=ALU.mult),
                reads=[f"sg{gi}", f"U{gi}"], writes=[f"aT{b2}"])
        for sub in range(SUB):
            yb = ys_i[0] % 2
            ys_i[0] += 1
            for nb in range(4):
                yi = y_i[0] % 2
                y_i[0] += 1
                Y = bank(4 + yi)
                for kc in range(4):
                    S.add("pe", lambda h, kc=kc, Y=Y, Wd=Wd, nb=nb, b2=b2, sub=sub: h.matmul(
                        Y, lhsT=aT[b2][:, kc, sub * 128:(sub + 1) * 128], rhs=Wd[:, kc, nb * 512:(nb + 1) * 512],
                        start=(kc == 0), stop=(kc == 3)),
                        reads=[f"ring{rd}", f"aT{b2}"], writes=[f"acc{yi}"])
                if nb % 2 == 0:
                    S.add("act", lambda h, Y=Y, nb=nb, yb=yb: h.activation(
                        out=ysb[yb][:, nb * 512:(nb + 1) * 512], in_=Y, func=AF.Copy),
                        reads=[f"acc{yi}"], writes=[f"ysb{yb}"])
                else:
                    S.add("dve", lambda h, Y=Y, nb=nb, yb=yb: h.tensor_copy(out=ysb[yb][:, nb * 512:(nb + 1) * 512], in_=Y),
                          reads=[f"acc{yi}"], writes=[f"ysb{yb}"])
            r0 = (st * SUB + sub) * 128
            dma("sp", YS[r0:r0 + 128, :], ysb[yb], f"yst{yb}", reads=[f"ysb{yb}"], writes=[("YS", st, sub)])

    S.fence()
    S.phase = 'G_p6_final'
    SC.reset()
    junk = SC.get(BF16, [D])
    gfrow = SC.get(F32, [D])
    shfrow = SC.get(F32, [D])
    gt2row = SC.get(F32, [D])
    ot = [SC.get(F32, [D]) for _ in range(2)]
    G1 = [sview(P_BYTES + (2 * i) * 8192, F32, [D]) for i in range(2)]
    G2 = [sview(P_BYTES + (2 * i + 1) * 8192, F32, [D]) for i in range(2)]
    scfrow = ot[1]
    dma("sp", gfrow, bcast(g_fin), "gfr", writes=["gfrow"])
    dma("sp", shfrow, bcast(modrow[:, V_SHF * D:(V_SHF + 1) * D]), "shfr",
        reads=["modrow0", "modrow1"], writes=["shfrow"])
    dma("sp", scfrow, bcast(modrow[:, V_SCF * D:(V_SCF + 1) * D]), "scfr",
        reads=["modrow0", "modrow1"], writes=["ot1"])
    dma("sp", gt2row, bcast(modrow[:, V_GT2 * D:(V_GT2 + 1) * D]), "gt2r",
        reads=["modrow0", "modrow1"], writes=["gt2row"])
    S.add("dve", lambda h: h.scalar_tensor_tensor(out=gfrow, in0=scfrow, scalar=1.0, in1=gfrow,
                                                   op0=ALU.add, op1=ALU.mult),
          reads=["gfrow", "ot1"], writes=["gfrow"])
    ys_all = [("YS", st, sub) for st in range(n_tiles) for sub in range(SUB)]
    def p6_a(tt):
        b2 = tt % 2
        xb_i = tt % 4
        xb = X1[:, xb_i, :]
        xres = [f"xb{xb_i}"]
        dma("sp", xb, X1S[tt * 128:(tt + 1) * 128, :], f"x1l{xb_i}", reads=[("X1S", tt)], writes=xres)
        g1, g2 = G1[b2], G2[b2]
        for (Gb, sl, nm) in ((g1, slot1_i, f"G1{b2}"), (g2, slot2_i, f"G2{b2}")):
            S.add("pool", lambda h, Gb=Gb, sl=sl, tt=tt: h.indirect_dma_start(
                out=Gb, out_offset=None, in_=YS,
                in_offset=bass.IndirectOffsetOnAxis(ap=sl[:, tt:tt + 1], axis=0),
                bounds_check=bc_reg(h, NST * TS - 1), oob_is_err=False),
                reads=ys_all + ["slot1", "slot2"], writes=[nm], dma=nm)
        S.add("dve", lambda h, tt=tt, g1=g1: h.tensor_scalar(out=g1, in0=g1, scalar1=w1p[:, tt:tt + 1], scalar2=None, op0=ALU.mult),
              reads=[f"G1{b2}", "w1p"], writes=[f"G1{b2}"])
        S.add("dve", lambda h, tt=tt, g1=g1, g2=g2: h.scalar_tensor_tensor(out=g1, in0=g2, scalar=w2p[:, tt:tt + 1], in1=g1,
                                                                          op0=ALU.mult, op1=ALU.add),
              reads=[f"G1{b2}", f"G2{b2}", "w2p"], writes=[f"G1{b2}"])
        S.add("pool", lambda h, g1=g1: h.tensor_tensor(out=g1, in0=g1, in1=gt2row, op=ALU.mult),
              reads=[f"G1{b2}", "gt2row"], writes=[f"G1{b2}"])
        S.add("dve", lambda h, xb=xb, g1=g1: h.tensor_tensor(out=xb, in0=xb, in1=g1, op=ALU.add),
              reads=[f"G1{b2}"] + xres, writes=xres)
        ss = stat[:, b2:b2 + 1]
        S.add("act", lambda h, xb=xb, ss=ss: h.activation(out=junk, in_=xb, func=AF.Square, accum_out=ss),
              reads=xres, writes=["junk", f"ss{b2}"])

    def p6_b(tt):
        b2 = tt % 2
        xb_i = tt % 4
        xb = X1[:, xb_i, :]
        xres = [f"xb{xb_i}"]
        ss = stat[:, b2:b2 + 1]
        rs = stat[:, 2 + b2:3 + b2]
        rms_rstd(None, ss, rs, D, [f"ss{b2}"], [f"rs{b2}"])
        S.add("dve", lambda h, xb=xb, rs=rs, b2=b2: h.scalar_tensor_tensor(
            out=ot[b2], in0=xb, scalar=rs, in1=gfrow, op0=ALU.mult, op1=ALU.mult),
            reads=xres + [f"rs{b2}", "gfrow"], writes=[f"ot{b2}"])
        S.add("dve", lambda h, b2=b2: h.tensor_tensor(out=ot[b2], in0=ot[b2], in1=shfrow, op=ALU.add),
              reads=[f"ot{b2}", "shfrow"], writes=[f"ot{b2}"])
        r = tt * 128
        dma("sp", y_out[r:r + 128, :], ot[b2], f"yo{b2}", reads=[f"ot{b2}"], writes=["yout"])

    p6_a(0)
    for tt in range(NTT):
        if tt + 1 < NTT:
            p6_a(tt + 1)
        p6_b(tt)

    finals = ["yo0", "yo1"] + (["dbg"] if dbg and "dbg" in S.dma_cnt else [])
    S.emit(nc, es, final_waits=finals)
    es.close()
    return nc


def _col(v):
    return np.ascontiguousarray(np.asarray(v, np.float32).reshape(KC, 128).T)


def prepare_shared(inp):
    f = lambda a: np.asarray(a, dtype=np.float32)
    sh = {}
    sh["w_mod"] = np.ascontiguousarray(np.concatenate([f(inp["w_ada"])[0], f(inp["w_ada_final"])], axis=1))
    sh["b_mod"] = np.ascontiguousarray(np.concatenate([f(inp["b_ada"])[0], f(inp["b_ada_final"])])[None, :])
    g_out = np.concatenate([f(inp["g_out_a"])[0], f(inp["g_out_b"])[0]])
    sh["gcols"] = np.ascontiguousarray(np.stack([_col(f(inp["g_mix"])[0]), _col(f(inp["g_ffn"])[0]), _col(g_out)], axis=1))
    sh["g_fin"] = f(inp["g_final"])[None, :].copy()
    w_in = f(inp["w_in"])[0]
    units = []
    for a in range(8):
        g = a // 2
        q = w_in[:, a * 128:(a + 1) * 128]
        k = w_in[:, 1024 + g * 64:1024 + (g + 1) * 64]
        v = w_in[:, 1280 + g * 64:1280 + (g + 1) * 64]
        units.append(np.concatenate([q, k, k, v, v], axis=1))
    for bp in range(8):
        units.append(np.concatenate([w_in[:, 1536 + bp * 128:1536 + (bp + 1) * 128],
                                     w_in[:, 2560 + bp * 128:2560 + (bp + 1) * 128],
                                     w_in[:, 3584 + bp * 128:3584 + (bp + 1) * 128]], axis=1))
    wu = np.stack(units)
    sh["w_in_u"] = np.ascontiguousarray(wu.reshape(16, KC, 128, 384).transpose(0, 2, 1, 3))
    sh["w_out"] = np.ascontiguousarray(f(inp["w_out"])[0])
    sh["w_rt"] = np.ascontiguousarray(np.concatenate([f(inp["w_router_group"])[0], f(inp["w_router_expert"])[0]], axis=1))
    sh["b_rt"] = np.concatenate([f(inp["b_router_group"])[0], f(inp["b_router_expert"])[0]])[None, :].copy()
    sh["w_gate"] = np.ascontiguousarray(f(inp["w_gate"])[0].reshape(N_EXP, KC, 128, DE).transpose(0, 2, 1, 3)).reshape(N_EXP * 128, KC * DE)
    sh["w_up"] = np.ascontiguousarray(f(inp["w_up"])[0].reshape(N_EXP, KC, 128, DE).transpose(0, 2, 1, 3)).reshape(N_EXP * 128, KC * DE)
    sh["w_down"] = np.ascontiguousarray(f(inp["w_down"])[0].reshape(N_EXP, 4, 128, D).transpose(0, 2, 1, 3)).reshape(N_EXP * 128, 4 * D)
    sh["g_ffn_row"] = f(inp["g_ffn"])[0][None, :].copy()
    c2 = np.zeros((128, 192), np.float32)
    c2[:, 0:128] = (np.arange(128)[:, None] < np.arange(128)[None, :]).astype(np.float32)
    c2[:, 128] = np.arange(128)
    c2[:, 129:137] = float(TS) * np.arange(8)[None, :]
    c2[:, 137:137 + NST] = float(TS) * np.arange(NST)[None, :]
    sh["cst2"] = c2
    rb = f(inp["rel_bias_b"])[0]
    ki = np.arange(128)[:, None]
    qi = np.arange(128)[None, :]
    idx0 = np.clip(128 + qi - ki, -128, 128) + 128
    idx1 = np.clip(qi - ki, -128, 128) + 128
    tab = np.stack([rb[:, idx0], rb[:, idx1]], axis=1)
    sh["tabB"] = np.ascontiguousarray(tab.transpose(2, 0, 1, 3))
    sh["constB"] = np.ascontiguousarray(np.broadcast_to(rb[:, 256][None, :], (128, 16)))
    sh["sinks"] = np.ascontiguousarray(np.broadcast_to(f(inp["sinks_a"])[0][None, :], (128, 16)))
    cst = np.zeros((128, 6, 128), np.float32)
    cst[:, 0, :] = np.eye(128, dtype=np.float32)
    cst[:64, 1, 64:] = NEG
    cst[64:, 2, :64] = NEG
    cst[:, 3, :] = 128 + qi - ki
    cst[:, 4, :] = np.abs(qi - ki)
    sh["cst"] = cst
    return sh


def prepare_core(inp, core):
    b, hf = core // 2, core % 2
    x = np.asarray(inp["x"], dtype=np.float32)
    xh = np.zeros((HALO + NB_TOK_CORE, D), np.float32)
    lo = hf * NB_TOK_CORE
    xh[HALO:] = x[b, lo:lo + NB_TOK_CORE]
    if hf > 0:
        xh[:HALO] = x[b, lo - HALO:lo]
    d = {"xh": xh, "c_col": _col(np.asarray(inp["c"], np.float32)[b]),
         "hm": np.full((128, 1), 1.0 if hf > 0 else 0.0, np.float32)}
    return d


_NC_CACHE = {}


def kernel(**inputs):
    if "nc" not in _NC_CACHE:
        _NC_CACHE["nc"] = build_program()
    nc = _NC_CACHE["nc"]
    sh = prepare_shared(inputs)
    in_maps = []
    for core in range(8):
        d = dict(sh)
        d.update(prepare_core(inputs, core))
        in_maps.append(d)
    res = run_bass_kernel_spmd(nc, in_maps, core_ids=list(range(8)))
    B, S_, _ = np.asarray(inputs["x"]).shape
    out = np.empty((B, S_, D), np.float32)
    for core in range(8):
        b, hf = core // 2, core % 2
        out[b, hf * NB_TOK_CORE:(hf + 1) * NB_TOK_CORE] = res.results[core]["y"]
    return out
```
